# Optimizing a Trainium2 kernel written in Bass

```python
import jax, jax.numpy as jnp
from jax import lax
import numpy as np


D_MODEL = 1024
BATCH = 16
SEQ = 4096
DEPTH = 1

CTX_LEN = 256
GRID_W = 64
N_RET_HEADS = 4
RET_QK_DIM = 256
RET_V_DIM = 512
RET_CHUNK = 128
N_MLA_HEADS = 8
MLA_Q_RANK = 384
MLA_KV_RANK = 256
MLA_QK_NOPE = 128
MLA_QK_ROPE = 64
MLA_V_DIM = 128
Q_BLOCK = 128
N_EXPERTS = 32
TOP_K = 4
D_FF_EXPERT = D_MODEL
SWIGLU_LIMIT = 7.0
SWIGLU_ALPHA = 1.702
MOE_BLOCK = 128
ROPE_BASE = 10000.0
EPS = 1e-6
IN_COLS = (2 * N_RET_HEADS * RET_QK_DIM + 2 * N_RET_HEADS * RET_V_DIM
           + MLA_Q_RANK + MLA_KV_RANK + MLA_QK_ROPE + 2 * D_MODEL)

kernel_name = 'hybrid_retention_mla_moe_dit_block'


def rms_norm(t, g):
    tf = t.astype(jnp.float32)
    n = tf * lax.rsqrt(jnp.mean(tf * tf, axis=-1, keepdims=True) + EPS)
    return n.astype(t.dtype) * g


def head_rms(t):
    tf = t.astype(jnp.float32)
    return (tf * lax.rsqrt(jnp.mean(tf * tf, axis=-1, keepdims=True) + EPS)).astype(t.dtype)


def ada_mod(cvec, w, b):
    m = jax.nn.silu(cvec) @ w + b
    return jnp.split(m[:, None, :], 6, axis=-1)


def modulate(t, shift, scale):
    return t * (1.0 + scale) + shift


def rope_1d(t, pos):
    half = t.shape[-1] // 2
    freqs = ROPE_BASE ** (-jnp.arange(half, dtype=jnp.float32) / half)
    ang = pos.astype(jnp.float32)[:, None] * freqs[None, :]
    cos = jnp.cos(ang)[None, :, None, :].astype(t.dtype)
    sin = jnp.sin(ang)[None, :, None, :].astype(t.dtype)
    t1, t2 = t[..., :half], t[..., half:]
    return jnp.concatenate([t1 * cos - t2 * sin, t1 * sin + t2 * cos], axis=-1)


def rope_2d(t, rows, cols):
    half = t.shape[-1] // 2
    return jnp.concatenate([rope_1d(t[..., :half], rows), rope_1d(t[..., half:], cols)], axis=-1)


def split_proj(z):
    sizes = (N_RET_HEADS * RET_QK_DIM, N_RET_HEADS * RET_QK_DIM, N_RET_HEADS * RET_V_DIM,
             N_RET_HEADS * RET_V_DIM, MLA_Q_RANK, MLA_KV_RANK, MLA_QK_ROPE, D_MODEL, D_MODEL)
    idx, acc = [], 0
    for s in sizes[:-1]:
        acc += s
        idx.append(acc)
    return jnp.split(z, idx, axis=-1)


def retention_qkv(rq, rk, rv, rope_fn):
    B, L, _ = rq.shape
    q = rq.reshape(B, L, N_RET_HEADS, RET_QK_DIM)
    k = rk.reshape(B, L, N_RET_HEADS, RET_QK_DIM)
    v = rv.reshape(B, L, N_RET_HEADS, RET_V_DIM)
    if rope_fn is not None:
        q, k = rope_fn(q), rope_fn(k)
    return q, k * (RET_QK_DIM ** -0.5), v


def retention_state(k, v, lg):
    L = k.shape[1]
    w = jnp.exp(lg[:, None] * (L - 1 - jnp.arange(L, dtype=jnp.float32))[None, :]).astype(v.dtype)
    return jnp.einsum('blhd,blhe,hl->bhde', k, v, w)


def retention_chunked(q, k, v, lg, init_state, include_diag):
    B, L, H, dk = q.shape
    dv = v.shape[-1]
    nc = L // RET_CHUNK
    idx = jnp.arange(RET_CHUNK, dtype=jnp.float32)
    diff = idx[:, None] - idx[None, :]
    mask = (diff >= 0) if include_diag else (diff > 0)
    intra = jnp.where(mask[None], jnp.exp(lg[:, None, None] * jnp.maximum(diff, 0.0)[None]), 0.0).astype(v.dtype)
    q_dec = jnp.exp(lg[:, None] * (idx + 1.0)[None, :]).astype(v.dtype)
    k_dec = jnp.exp(lg[:, None] * (RET_CHUNK - 1.0 - idx)[None, :]).astype(v.dtype)
    chunk_dec = jnp.exp(lg * RET_CHUNK).astype(v.dtype)

    def to_chunks(t):
        return t.reshape(B, nc, RET_CHUNK, H, t.shape[-1]).transpose(1, 0, 3, 2, 4)

    def step(state, blk):
        qc, kc, vc = blk
        s = jnp.einsum('bhid,bhjd->bhij', qc, kc) * intra[None]
        o = (jnp.einsum('bhij,bhje->bhie', s, vc)
             + jnp.einsum('bhid,bhde->bhie', qc * q_dec[None, :, :, None], state))
        state = (state * chunk_dec[None, :, None, None]
                 + jnp.einsum('bhjd,bhje->bhde', kc * k_dec[None, :, :, None], vc))
        return state, o

    _, o = lax.scan(step, init_state, (to_chunks(q), to_chunks(k), to_chunks(v)))
    return o.transpose(1, 0, 3, 2, 4).reshape(B, L, H, dv)


def bidir_retention(q, k, v, lg_f, lg_b, s_f, s_b):
    fwd = retention_chunked(q, k, v, lg_f, s_f, True)
    bwd = retention_chunked(jnp.flip(q, 1), jnp.flip(k, 1), jnp.flip(v, 1), lg_b, s_b, False)
    return fwd + jnp.flip(bwd, 1)


def mla_qkv(cq, ckv, kr, q_norm_g, w_uq, kv_norm_g, w_ukv, rope_fn):
    B, L, _ = cq.shape
    q = (rms_norm(cq, q_norm_g) @ w_uq).reshape(B, L, N_MLA_HEADS, MLA_QK_NOPE + MLA_QK_ROPE)
    q_nope, q_rope = q[..., :MLA_QK_NOPE], q[..., MLA_QK_NOPE:]
    kv = (rms_norm(ckv, kv_norm_g) @ w_ukv).reshape(B, L, N_MLA_HEADS, MLA_QK_NOPE + MLA_V_DIM)
    k_nope, v = kv[..., :MLA_QK_NOPE], kv[..., MLA_QK_NOPE:]
    k_rope = kr[:, :, None, :]
    if rope_fn is not None:
        q_rope, k_rope = rope_fn(q_rope), rope_fn(k_rope)
    return q_nope, q_rope, k_nope, k_rope[:, :, 0, :], v


def mla_attention(q_nope, q_rope, k_nope, k_rope, v):
    B, Lq, H, _ = q_nope.shape
    nb = Lq // Q_BLOCK
    scale = (MLA_QK_NOPE + MLA_QK_ROPE) ** -0.5

    def blocks(t):
        return t.reshape(B, nb, Q_BLOCK, H, t.shape[-1]).swapaxes(0, 1)

    def one_block(qb):
        qn, qr = qb
        s = (jnp.einsum('bqhd,bkhd->bhqk', qn, k_nope)
             + jnp.einsum('bqhd,bkd->bhqk', qr, k_rope)) * scale
        p = jax.nn.softmax(s.astype(jnp.float32), axis=-1).astype(v.dtype)
        return jnp.einsum('bhqk,bkhd->bqhd', p, v)

    o = lax.map(one_block, (blocks(q_nope), blocks(q_rope)))
    return o.swapaxes(0, 1).reshape(B, Lq, H, MLA_V_DIM)


def merge_branches(ret_o, ret_gate, att_o, g_ret, g_mla, w_branch_ret, w_branch_mla, w_out):
    B, L = ret_o.shape[:2]
    r = head_rms(ret_o).reshape(B, L, N_RET_HEADS * RET_V_DIM) * jax.nn.silu(ret_gate)
    m = att_o.reshape(B, L, N_MLA_HEADS * MLA_V_DIM)
    merged = jax.nn.sigmoid(g_ret) * (r @ w_branch_ret) + jax.nn.sigmoid(g_mla) * (m @ w_branch_mla)
    return merged @ w_out


def token_mixers(h_lat, h_ctx, rope_fn, need_ctx, w_in, ret_decay_fwd, ret_decay_bwd, mla_q_norm_g,
                 mla_w_uq, mla_kv_norm_g, mla_w_ukv, w_branch_ret, w_branch_mla, w_out):
    zl = split_proj(h_lat @ w_in)
    zc = split_proj(h_ctx @ w_in)
    lg_f = jax.nn.log_sigmoid(ret_decay_fwd.astype(jnp.float32))
    lg_b = jax.nn.log_sigmoid(ret_decay_bwd.astype(jnp.float32))
    ql, kl, vl = retention_qkv(zl[0], zl[1], zl[2], rope_fn)
    qc, kc, vc = retention_qkv(zc[0], zc[1], zc[2], None)
    s_f = retention_state(kc, vc, lg_f)
    s_b = retention_state(jnp.flip(kc, 1), jnp.flip(vc, 1), lg_b)
    ret_lat = bidir_retention(ql, kl, vl, lg_f, lg_b, s_f, s_b)
    ml = mla_qkv(zl[4], zl[5], zl[6], mla_q_norm_g, mla_w_uq, mla_kv_norm_g, mla_w_ukv, rope_fn)
    mc = mla_qkv(zc[4], zc[5], zc[6], mla_q_norm_g, mla_w_uq, mla_kv_norm_g, mla_w_ukv, None)
    att_lat = mla_attention(ml[0], ml[1], jnp.concatenate([mc[2], ml[2]], axis=1),
                            jnp.concatenate([mc[3], ml[3]], axis=1), jnp.concatenate([mc[4], ml[4]], axis=1))
    y_lat = merge_branches(ret_lat, zl[3], att_lat, zl[7], zl[8], w_branch_ret, w_branch_mla, w_out)
    y_ctx = None
    if need_ctx:
        zeros = jnp.zeros_like(s_f)
        ret_ctx = bidir_retention(qc, kc, vc, lg_f, lg_b, zeros, zeros)
        att_ctx = mla_attention(mc[0], mc[1], mc[2], mc[3], mc[4])
        y_ctx = merge_branches(ret_ctx, zc[3], att_ctx, zc[7], zc[8], w_branch_ret, w_branch_mla, w_out)
    return y_lat, y_ctx


def moe(h, router_w, router_b, w_gu, b_gu, w_down, b_down):
    B, L, D = h.shape
    tok = h.reshape(-1, D)
    n_tok = tok.shape[0]
    logits = tok @ router_w + router_b
    top_val, top_idx = lax.top_k(logits, TOP_K)
    gates = jax.nn.softmax(top_val.astype(jnp.float32), axis=-1)
    nk = n_tok * TOP_K
    flat_e = top_idx.reshape(-1)
    flat_tok = jnp.arange(nk, dtype=jnp.int32) // TOP_K
    order = jnp.argsort(flat_e)
    sorted_e = flat_e[order]
    sorted_tok = flat_tok[order]
    sorted_gate = gates.reshape(-1)[order]
    counts = jnp.zeros((N_EXPERTS,), jnp.int32).at[flat_e].add(1)
    padded = (counts + MOE_BLOCK - 1) // MOE_BLOCK * MOE_BLOCK
    start = jnp.cumsum(counts) - counts
    pad_end = jnp.cumsum(padded)
    pad_start = pad_end - padded
    dest = pad_start[sorted_e] + (jnp.arange(nk, dtype=jnp.int32) - start[sorted_e])
    n_rows = nk + N_EXPERTS * MOE_BLOCK
    n_blocks = n_rows // MOE_BLOCK
    xs = jnp.zeros((n_rows, D), tok.dtype).at[dest].set(tok[sorted_tok])
    block_e = jnp.minimum(jnp.searchsorted(pad_end, jnp.arange(n_blocks, dtype=jnp.int32) * MOE_BLOCK,
                                           side='right'), N_EXPERTS - 1)

    def expert_block(args):
        xb, e = args
        gu = xb @ w_gu[e] + b_gu[e]
        gate, up = gu[:, :D_FF_EXPERT], gu[:, D_FF_EXPERT:]
        gate = jnp.minimum(gate, SWIGLU_LIMIT)
        up = jnp.clip(up, -SWIGLU_LIMIT, SWIGLU_LIMIT)
        glu = gate * jax.nn.sigmoid(SWIGLU_ALPHA * gate)
        return ((up + 1.0) * glu) @ w_down[e] + b_down[e]

    ys = lax.map(expert_block, (xs.reshape(n_blocks, MOE_BLOCK, D), block_e)).reshape(n_rows, D)
    out = jnp.zeros_like(tok).at[sorted_tok].add(ys[dest] * sorted_gate[:, None].astype(ys.dtype))
    return out.reshape(B, L, D)


def setup_inputs(seed: int = 0) -> dict:
    key = jax.random.key(seed)
    ks = jax.random.split(key, 25)
    f32 = jnp.float32

    def nrm(k, shape, scale):
        return jax.random.normal(k, shape, f32) * scale

    ret_base = jnp.log(jnp.exp2(5.0 + jnp.arange(N_RET_HEADS, dtype=f32)) - 1.0)
    ret_v_w = N_RET_HEADS * RET_V_DIM
    mla_v_w = N_MLA_HEADS * MLA_V_DIM
    return {
        'x': nrm(ks[0], (BATCH, SEQ, D_MODEL), 1.0),
        'c': nrm(ks[1], (BATCH, D_MODEL), 1.0),
        'ctx': nrm(ks[2], (BATCH, CTX_LEN, D_MODEL), 1.0),
        'c_ctx': nrm(ks[3], (D_MODEL,), 1.0),
        'norm1_g': 1.0 + nrm(ks[4], (DEPTH, D_MODEL), 0.02),
        'norm2_g': 1.0 + nrm(ks[5], (DEPTH, D_MODEL), 0.02),
        'ada_w': nrm(ks[6], (DEPTH, D_MODEL, 6 * D_MODEL), 0.5 * D_MODEL ** -0.5),
        'ada_b': nrm(ks[7], (DEPTH, 6 * D_MODEL), 0.01),
        'w_in': nrm(ks[8], (DEPTH, D_MODEL, IN_COLS), D_MODEL ** -0.5),
        'ret_decay_fwd': ret_base[None, :] + nrm(ks[9], (DEPTH, N_RET_HEADS), 0.1),
        'ret_decay_bwd': ret_base[None, :] + nrm(ks[10], (DEPTH, N_RET_HEADS), 0.1),
        'mla_q_norm_g': 1.0 + nrm(ks[11], (DEPTH, MLA_Q_RANK), 0.02),
        'mla_w_uq': nrm(ks[12], (DEPTH, MLA_Q_RANK, N_MLA_HEADS * (MLA_QK_NOPE + MLA_QK_ROPE)), MLA_Q_RANK ** -0.5),
        'mla_kv_norm_g': 1.0 + nrm(ks[13], (DEPTH, MLA_KV_RANK), 0.02),
        'mla_w_ukv': nrm(ks[14], (DEPTH, MLA_KV_RANK, N_MLA_HEADS * (MLA_QK_NOPE + MLA_V_DIM)), MLA_KV_RANK ** -0.5),
        'w_branch_ret': nrm(ks[15], (DEPTH, ret_v_w, D_MODEL), ret_v_w ** -0.5),
        'w_branch_mla': nrm(ks[16], (DEPTH, mla_v_w, D_MODEL), mla_v_w ** -0.5),
        'w_out': nrm(ks[17], (DEPTH, D_MODEL, D_MODEL), D_MODEL ** -0.5),
        'router_w': nrm(ks[18], (DEPTH, D_MODEL, N_EXPERTS), D_MODEL ** -0.5),
        'router_b': nrm(ks[19], (DEPTH, N_EXPERTS), 0.01),
        'exp_w_gu': nrm(ks[20], (DEPTH, N_EXPERTS, D_MODEL, 2 * D_FF_EXPERT), D_MODEL ** -0.5),
        'exp_b_gu': nrm(ks[21], (DEPTH, N_EXPERTS, 2 * D_FF_EXPERT), 0.01),
        'exp_w_down': nrm(ks[22], (DEPTH, N_EXPERTS, D_FF_EXPERT, D_MODEL), D_FF_EXPERT ** -0.5),
        'exp_b_down': nrm(ks[23], (DEPTH, N_EXPERTS, D_MODEL), 0.01),
        'final_norm_g': 1.0 + nrm(ks[24], (D_MODEL,), 0.02),
    }


def reference(x, c, ctx, c_ctx, norm1_g, norm2_g, ada_w, ada_b, w_in, ret_decay_fwd, ret_decay_bwd,
              mla_q_norm_g, mla_w_uq, mla_kv_norm_g, mla_w_ukv, w_branch_ret, w_branch_mla, w_out,
              router_w, router_b, exp_w_gu, exp_b_gu, exp_w_down, exp_b_down, final_norm_g):
    L = x.shape[1]
    n_grid_rows = L // GRID_W
    rows = jnp.repeat(jnp.arange(n_grid_rows, dtype=jnp.int32), GRID_W)
    cols = jnp.tile(jnp.arange(GRID_W, dtype=jnp.int32), n_grid_rows)
    rope_fn = lambda t: rope_2d(t, rows, cols)
    for layer in range(DEPTH):
        need_ctx = layer < DEPTH - 1
        m_lat = ada_mod(c, ada_w[layer], ada_b[layer])
        m_ctx = ada_mod(c_ctx[None, :], ada_w[layer], ada_b[layer])
        h_lat = modulate(rms_norm(x, norm1_g[layer]), m_lat[0], m_lat[1])
        h_ctx = modulate(rms_norm(ctx, norm1_g[layer]), m_ctx[0], m_ctx[1])
        y_lat, y_ctx = token_mixers(h_lat, h_ctx, rope_fn, need_ctx, w_in[layer], ret_decay_fwd[layer],
                                    ret_decay_bwd[layer], mla_q_norm_g[layer], mla_w_uq[layer],
                                    mla_kv_norm_g[layer], mla_w_ukv[layer], w_branch_ret[layer],
                                    w_branch_mla[layer], w_out[layer])
        x = x + m_lat[2] * y_lat
        h2 = modulate(rms_norm(x, norm2_g[layer]), m_lat[3], m_lat[4])
        x = x + m_lat[5] * moe(h2, router_w[layer], router_b[layer], exp_w_gu[layer], exp_b_gu[layer],
                               exp_w_down[layer], exp_b_down[layer])
        if need_ctx:
            ctx = ctx + m_ctx[2] * y_ctx
            h2c = modulate(rms_norm(ctx, norm2_g[layer]), m_ctx[3], m_ctx[4])
            ctx = ctx + m_ctx[5] * moe(h2c, router_w[layer], router_b[layer], exp_w_gu[layer],
                                       exp_b_gu[layer], exp_w_down[layer], exp_b_down[layer])
    return rms_norm(x, final_norm_g)
```

```python
import contextlib
import numpy as np
import concourse.bass as bass
import concourse.mybir as mybir
from concourse.bass_utils import run_bass_kernel_spmd

F32 = mybir.dt.float32
BF16 = mybir.dt.bfloat16
AF = mybir.ActivationFunctionType
ALU = mybir.AluOpType

NB = 2
L = 4096
CT = 256
LT = L + CT
NT = LT // 128
D = 1024
EPS = 1e-6
NS_DMA = 16
ERA = 16000


class Tk:
    __slots__ = ("w", "r", "name")

    def __init__(self, name=""):
        self.w = []
        self.r = []
        self.name = name


class Sched:
    def __init__(self, nc, stack):
        self.nc = nc
        self.stack = stack
        self.engs = {"pe": nc.tensor, "act": nc.scalar, "dve": nc.vector, "pool": nc.gpsimd, "sp": nc.sync}
        self.sems = {}
        self.seq = {e: 0 for e in self.engs}
        self.sig = {e: [] for e in self.engs}
        self.cnt = {e: 0 for e in self.engs}
        self.waited = {e: {} for e in self.engs}
        self.waited_d = {e: {} for e in self.engs}
        self.ring = [stack.enter_context(nc.semaphore(f"dq{i}")) for i in range(NS_DMA)]
        self.dma_n = 0
        self.n_ops = 0

    def _sem(self, eng, era):
        k = (eng, era)
        if k not in self.sems:
            self.sems[k] = self.stack.enter_context(self.nc.semaphore(f"s_{eng}_{era}"))
        return self.sems[k]

    def _wait(self, eng, tk):
        e = self.engs[eng]
        if tk[0] == "d":
            _, ring, val = tk
            if self.waited_d[eng].get(ring, 0) >= val:
                return
            e.wait_ge(self.ring[ring], val)
            self.waited_d[eng][ring] = val
            return
        _, peng, seq = tk
        lst = self.sig[peng]
        lo, hi = 0, len(lst)
        while lo < hi:
            mid = (lo + hi) // 2
            if lst[mid][0] >= seq:
                hi = mid
            else:
                lo = mid + 1
        if lo >= len(lst):
            raise RuntimeError(f"no signalling op after seq {seq} on {peng}")
        count = lst[lo][1]
        if self.waited[eng].get(peng, 0) >= count:
            return
        era, val = (count - 1) // ERA, (count - 1) % ERA + 1
        e.wait_ge(self._sem(peng, era), val)
        self.waited[eng][peng] = count

    def op(self, eng, fn, reads=(), writes=(), signal=True):
        deps = []
        for t in reads:
            deps.extend(t.w)
        for t in writes:
            for tk in t.w:
                if tk[0] == "d" or tk[1] != eng:
                    deps.append(tk)
            for tk in t.r:
                if tk[0] == "d" or tk[1] != eng:
                    deps.append(tk)
        for tk in deps:
            self._wait(eng, tk)
        ins = fn(self.engs[eng])
        self.seq[eng] += 1
        seq = self.seq[eng]
        if signal:
            self.cnt[eng] += 1
            c = self.cnt[eng]
            era, val = (c - 1) // ERA, (c - 1) % ERA + 1
            ins.then_inc(self._sem(eng, era), 1)
            self.sig[eng].append((seq, c))
        tk = ("c", eng, seq)
        for t in reads:
            t.r = [x for x in t.r if not (x[0] == "c" and x[1] == eng)]
            t.r.append(tk)
        for t in writes:
            t.w = [tk]
            t.r = []
        self.n_ops += 1
        return ins

    def dma(self, out, in_, reads=(), writes=()):
        eng = "sp"
        deps = []
        for t in reads:
            deps.extend(t.w)
        for t in writes:
            deps.extend(t.w)
            deps.extend(t.r)
        n = self.dma_n
        ring = n % NS_DMA
        val = 16 * (n // NS_DMA + 1)
        if n >= NS_DMA:
            deps.append(("d", ring, val - 16))
        for tk in deps:
            self._wait(eng, tk)
        self.engs[eng].dma_start(out=out, in_=in_).then_inc(self.ring[ring], 16)
        self.dma_n += 1
        tk = ("d", ring, val)
        for t in reads:
            t.r.append(tk)
        for t in writes:
            t.w = [tk]
            t.r = []
        self.n_ops += 1
        return tk

    def dma_ind(self, fn, reads=(), writes=()):
        eng = "pool"
        deps = []
        for t in reads:
            deps.extend(t.w)
        for t in writes:
            deps.extend(t.w)
            deps.extend(t.r)
        n = self.dma_n
        ring = n % NS_DMA
        val = 16 * (n // NS_DMA + 1)
        if n >= NS_DMA:
            deps.append(("d", ring, val - 16))
        for tk in deps:
            self._wait(eng, tk)
        fn(self.engs[eng]).then_inc(self.ring[ring], 16)
        self.dma_n += 1
        tk = ("d", ring, val)
        for t in reads:
            t.r.append(tk)
        for t in writes:
            t.w = [tk]
            t.r = []
        self.n_ops += 1
        return tk

    def barrier(self):
        tks = []
        for e in self.engs:
            if self.sig[e]:
                tks.append(("c", e, self.sig[e][-1][0]))
        n = self.dma_n
        for k in range(max(0, n - NS_DMA), n):
            tks.append(("d", k % NS_DMA, 16 * (k // NS_DMA + 1)))
        for e in self.engs:
            for tk in tks:
                if tk[0] == "c" and tk[1] == e:
                    continue
                self._wait(e, tk)

    def final_wait(self):
        n = self.dma_n
        for k in range(max(0, n - NS_DMA), n):
            self._wait("sp", ("d", k % NS_DMA, 16 * (k // NS_DMA + 1)))


def rope_tables():
    pos = np.arange(L)
    rows = (pos // 64).astype(np.float32)
    cols = (pos % 64).astype(np.float32)

    def tab(dr):
        half = dr // 2
        hh = half // 2
        freqs = (10000.0 ** (-np.arange(hh, dtype=np.float32) / hh)).astype(np.float32)
        C = np.ones((LT, dr), np.float32)
        S = np.zeros((LT, dr), np.float32)
        for part, p in enumerate((rows, cols)):
            ang = (p[:, None] * freqs[None, :]).astype(np.float32)
            c, s = np.cos(ang).astype(np.float32), np.sin(ang).astype(np.float32)
            o = part * half
            C[CT:, o:o + hh] = c
            C[CT:, o + hh:o + half] = c
            S[CT:, o:o + hh] = -s
            S[CT:, o + hh:o + half] = s
        return C, S

    RC, RS = tab(256)
    MC, MS = tab(64)
    return RC, RS, np.ascontiguousarray(MC.T), np.ascontiguousarray(MS.T)


def const_tables():
    j = np.arange(128, dtype=np.float32)[:, None]
    i = np.arange(128, dtype=np.float32)[None, :]
    c = {}
    c["ident"] = np.eye(128, dtype=np.float32)
    c["ones"] = np.ones((128, 128), np.float32)
    c["d1"] = np.maximum(i - j, 0.0) + 0 * j
    c["mf"] = (i >= j).astype(np.float32)
    c["d2"] = np.maximum(j - i, 0.0)
    c["mb"] = (i < j).astype(np.float32)
    c["ip1"] = (i + 1.0) + 0 * j
    c["rev"] = (128.0 - i) + 0 * j
    col = np.zeros((128, 128), np.float32)
    col[:, 0] = 127.0 - np.arange(128)
    col[:, 1] = np.arange(128)
    col[:, 2] = 128.0
    c["col"] = col
    c["lt"] = (i > j).astype(np.float32)
    c["iota"] = i + 0 * j
    rowb = np.zeros((128, 128), np.float32)
    for kc in range(8):
        rowb[:, kc] = kc * 128 + np.arange(128)
    c["rowb"] = rowb
    names = ["ident", "ones", "d1", "mf", "d2", "mb", "ip1", "rev", "col", "lt", "iota", "rowb"]
    return np.stack([c[n].astype(np.float32) for n in names], axis=1), names


def build(dbg=()):
    nc = bass.Bass("TRN2", target_bir_lowering=False)
    try:
        nc.allow_low_precision("bf16 matmul operands with fp32 accumulation")
    except Exception:
        pass
    try:
        nc.allow_non_contiguous_dma("strided weight/activation tiles")
    except Exception:
        pass

    def din(name, shape, dt=F32):
        return nc.dram_tensor(name, list(shape), dt, kind="ExternalInput").ap()

    def dscr(name, shape, dt):
        kind = "ExternalOutput" if name in dbg else "Internal"
        return nc.dram_tensor(name, list(shape), dt, kind=kind).ap()

    x_d = din("x", [NB, L, D])
    ctx_d = din("ctx", [NB, CT, D])
    cv_d = din("cvec", [3, D])
    n1_d = din("norm1_g", [1, D])
    n2_d = din("norm2_g", [1, D])
    adaw_d = din("ada_w", [1, D, 6 * D])
    adab_d = din("ada_b", [1, 6 * D])
    win_d = din("w_in", [1, D, 8896])
    rdf_d = din("ret_decay_fwd", [1, 4])
    rdb_d = din("ret_decay_bwd", [1, 4])
    qg_d = din("mla_q_norm_g", [1, 384])
    wuq_d = din("mla_w_uq", [1, 384, 1536])
    kvg_d = din("mla_kv_norm_g", [1, 256])
    wukv_d = din("mla_w_ukv", [1, 256, 2048])
    wbr_d = din("w_branch_ret", [1, 2048, D])
    wbm_d = din("w_branch_mla", [1, D, D])
    wo_d = din("w_out", [1, D, D])
    rw_d = din("router_w", [1, D, 32])
    rb_d = din("router_b", [1, 32])
    egu_d = din("exp_w_gu", [1, 32, D, 2 * D])
    ebgu_d = din("exp_b_gu", [1, 32, 2 * D])
    edn_d = din("exp_w_down", [1, 32, D, D])
    ebd_d = din("exp_b_down", [1, 32, D])
    fg_d = din("final_norm_g", [D])
    cst_d = din("consts", [128, 12, 128])
    rc_d = din("rope_rc", [LT, 256])
    rs_d = din("rope_rs", [LT, 256])
    mc_d = din("rope_mc", [64, LT])
    ms_d = din("rope_ms", [64, LT])
    out_d = nc.dram_tensor("out", [NB, L, D], F32, kind="ExternalOutput").ap()

    hT_d = dscr("hT_s", [NT, 128, 8, 128], BF16)
    att_d = dscr("att_s", [8, 128, L], BF16)
    rT_d = dscr("rT_s", [4, 4, 128, L], BF16)
    of_d = dscr("of_s", [32, 128, 512], F32)
    x1_d = dscr("x1_s", [NB * 32, 128, D], F32)
    NROWS = NB * L * 4 + 32 * 512
    NBLK = NROWS // 512
    h2_d = dscr("h2_s", [NB * 32, 128, D], BF16)
    xs_d = dscr("xs_s", [NROWS, D], BF16)
    ys_d = dscr("ys_s", [NROWS, D], F32)
    bias_d = dscr("bias_s", [32, 3072], BF16)
    hT_t = [Tk() for _ in range(NT)]
    att_t = [Tk() for _ in range(8)]
    rT_t = [Tk() for _ in range(8)]
    of_t = [Tk() for _ in range(32)]
    x1_t = [Tk() for _ in range(NB * 32)]
    out_t = Tk()

    with contextlib.ExitStack() as gstack:
        S = Sched(nc, gstack)

        uid = [0]

        def sb(stack, name, shape, dt):
            uid[0] += 1
            return stack.enter_context(nc.sbuf_tensor(f"{name}_u{uid[0]}", list(shape), dt))

        def ps(stack, name, shape, dt=F32):
            uid[0] += 1
            return stack.enter_context(nc.psum_tensor(f"{name}_u{uid[0]}", list(shape), dt))

        cst = sb(gstack, "cst", [128, 12, 128], F32)
        cst_t = Tk()
        S.dma(cst[:], cst_d[:, :, :], writes=[cst_t])
        identf = cst[:, 0, :]
        onesf = cst[:, 1, :]
        identb = sb(gstack, "identb", [128, 128], BF16)
        onesb = sb(gstack, "onesb", [128, 128], BF16)
        cb_t = Tk()
        S.op("dve", lambda e: e.tensor_copy(out=identb[:], in_=identf), reads=[cst_t], writes=[cb_t])
        S.op("dve", lambda e: e.tensor_copy(out=onesb[:], in_=onesf), reads=[cst_t], writes=[cb_t])

        mT = sb(gstack, "mT", [128, 48, 3], F32)
        mT_t = Tk()
        gs1 = sb(gstack, "gs1", [128, 3, 8], F32)
        gs2 = sb(gstack, "gs2", [128, 3, 8], F32)
        gs_t = Tk()
        mix_stack = contextlib.ExitStack()
        G2 = sb(gstack, "G2", [128, NB, D], F32)
        FG = sb(gstack, "FG", [128, D], F32)
        G_t = Tk()
        gq = sb(gstack, "gq", [128, 8], F32)
        gq_t = Tk()
        KD = sb(gstack, "KD", [128, 4, 8], F32)
        G1 = sb(mix_stack, "G1", [128, NB, D], F32)
        MTf = sb(mix_stack, "MTf", [128, 4, 128], F32)
        MTb = sb(mix_stack, "MTb", [128, 4, 128], F32)
        QDf = sb(mix_stack, "QDf", [128, 4, 128], F32)
        QDb = sb(mix_stack, "QDb", [128, 4, 128], F32)
        dec_t = Tk()

        def featmajor_load(stack, pst, pst_t, dst_ap, src_rows_ap, nrows, tmpname):
            tmp = sb(stack, tmpname, [nrows, 128], F32)
            tt = Tk()
            S.dma(tmp[:], src_rows_ap, writes=[tt])
            S.op("pe", lambda e: e.transpose(out=pst[:, 0:nrows], in_=tmp[:], identity=cst[0:nrows, 0, 0:nrows]),
                 reads=[tt, cst_t], writes=[pst_t])
            return tmp

        with contextlib.ExitStack() as st:
            pA = ps(st, "pA0", [128, 512])
            pA_t = Tk()
            pB = ps(st, "pB0", [128, 2, 512])
            pB_t = Tk()
            cv = sb(st, "cv", [3, D], F32)
            cvs = sb(st, "cvs", [3, D], F32)
            cv_t = Tk()
            S.dma(cv[:], cv_d[:, :], writes=[cv_t])
            cvs_t = Tk()
            S.op("act", lambda e: e.activation(out=cvs[:], in_=cv[:], func=AF.Silu), reads=[cv_t], writes=[cvs_t])
            sT = sb(st, "sT", [128, 8, 3], F32)
            sT_t = Tk()
            for kc in range(8):
                S.op("pe", lambda e: e.transpose(out=pA[:, kc * 4:kc * 4 + 3], in_=cvs[:, kc * 128:(kc + 1) * 128],
                                                 identity=cst[0:3, 0, 0:3]), reads=[cvs_t, cst_t], writes=[pA_t])
            for kc in range(8):
                S.op("dve", lambda e: e.tensor_copy(out=sT[:, kc, :], in_=pA[:, kc * 4:kc * 4 + 3]),
                     reads=[pA_t], writes=[sT_t])
            abT = sb(st, "abT", [128, 48], F32)
            g12 = sb(st, "g12", [128, 16], F32)
            ab_t = Tk()
            featmajor_load(st, pA, pA_t, None, adab_d[0, :].rearrange("(r p) -> r p", p=128), 48, "t_ab")
            S.op("dve", lambda e: e.tensor_copy(out=abT[:], in_=pA[:, 0:48]), reads=[pA_t], writes=[ab_t])
            featmajor_load(st, pA, pA_t, None, n1_d[0, :].rearrange("(r p) -> r p", p=128), 8, "t_n1")
            S.op("dve", lambda e: e.tensor_copy(out=g12[:, 0:8], in_=pA[:, 0:8]), reads=[pA_t], writes=[ab_t])
            featmajor_load(st, pA, pA_t, None, n2_d[0, :].rearrange("(r p) -> r p", p=128), 8, "t_n2")
            S.op("dve", lambda e: e.tensor_copy(out=g12[:, 8:16], in_=pA[:, 0:8]), reads=[pA_t], writes=[ab_t])
            featmajor_load(st, pA, pA_t, None, qg_d[0, :].rearrange("(r p) -> r p", p=128), 3, "t_qg")
            S.op("dve", lambda e: e.tensor_copy(out=gq[:, 0:3], in_=pA[:, 0:3]), reads=[pA_t], writes=[gq_t])
            featmajor_load(st, pA, pA_t, None, kvg_d[0, :].rearrange("(r p) -> r p", p=128), 2, "t_kvg")
            S.op("dve", lambda e: e.tensor_copy(out=gq[:, 4:6], in_=pA[:, 0:2]), reads=[pA_t], writes=[gq_t])
            fgT = sb(st, "fgT", [128, 8], F32)
            fg_t = Tk()
            featmajor_load(st, pA, pA_t, None, fg_d.rearrange("(r p) -> r p", p=128), 8, "t_fg")
            S.op("dve", lambda e: e.tensor_copy(out=fgT[:], in_=pA[:, 0:8]), reads=[pA_t], writes=[fg_t])
            awv = adaw_d[0].rearrange("(kc p) n -> p kc n", p=128)
            aw = [sb(st, f"aw{i}", [128, 8, 512], F32) for i in range(2)]
            aw_t = [Tk(), Tk()]
            pm = ps(st, "pm", [128, 48, 4])
            pm_t = Tk()
            for blk in range(12):
                w = aw[blk % 2]
                wt = aw_t[blk % 2]
                S.dma(w[:], awv[:, :, blk * 512:(blk + 1) * 512], writes=[wt])
                for jj in range(4):
                    j = blk * 4 + jj
                    for kc in range(8):
                        S.op("pe", lambda e: e.matmul(out=pm[:, j, 0:3], lhsT=w[:, kc, jj * 128:(jj + 1) * 128],
                                                      rhs=sT[:, kc, :], start=(kc == 0), stop=(kc == 7)),
                             reads=[wt, sT_t], writes=[pm_t], signal=(kc == 7))
            for r in range(3):
                S.op("dve", lambda e: e.tensor_tensor(out=mT[:, :, r], in0=pm[:, :, r], in1=abT[:], op=ALU.add),
                     reads=[pm_t, ab_t], writes=[mT_t])
            for r in range(3):
                S.op("dve", lambda e: e.scalar_tensor_tensor(out=gs1[:, r, :], in0=mT[:, 8:16, r], scalar=1.0,
                                                             in1=g12[:, 0:8], op0=ALU.add, op1=ALU.mult),
                     reads=[mT_t, ab_t], writes=[gs_t])
                S.op("dve", lambda e: e.scalar_tensor_tensor(out=gs2[:, r, :], in0=mT[:, 32:40, r], scalar=1.0,
                                                             in1=g12[:, 8:16], op0=ALU.add, op1=ALU.mult),
                     reads=[mT_t, ab_t], writes=[gs_t])
            dg = sb(st, "dg", [128, 8, 128], F32)
            dg_t = Tk()

            def bcast_tile(dst_ap, vec_fn):
                for c in range(8):
                    S.op("dve", lambda e: e.tensor_scalar(out=dg[:, c, :], in0=identf, scalar1=vec_fn(c), scalar2=None,
                                                          op0=ALU.mult), reads=[cst_t, mT_t, fg_t], writes=[dg_t])
                for c in range(8):
                    S.op("pe", lambda e: e.matmul(out=pB[:, c // 4, (c % 4) * 128:(c % 4 + 1) * 128], lhsT=onesf,
                                                  rhs=dg[:, c, :], start=True, stop=True),
                         reads=[dg_t, cst_t], writes=[pB_t])
                S.op("act", lambda e: e.copy(out=dst_ap, in_=pB[:].rearrange("p a b -> p (a b)")),
                     reads=[pB_t], writes=[G_t])

            for b in range(NB):
                bcast_tile(G1[:, b, :], lambda c: mT[:, 16 + c, b:b + 1])
                bcast_tile(G2[:, b, :], lambda c: mT[:, 40 + c, b:b + 1])
            bcast_tile(FG[:], lambda c: fgT[:, c:c + 1])

            rd = sb(st, "rd", [1, 8], F32)
            rd_t = Tk()
            S.dma(rd[:, 0:4], rdf_d[:, :], writes=[rd_t])
            S.dma(rd[:, 4:8], rdb_d[:, :], writes=[rd_t])
            S.op("pe", lambda e: e.matmul(out=pA[:, 0:8], lhsT=cst[0:1, 1, :], rhs=rd[:], start=True, stop=True),
                 reads=[rd_t, cst_t], writes=[pA_t])
            lg = sb(st, "lg", [128, 8], F32)
            lg_t = Tk()
            S.op("act", lambda e: e.activation(out=lg[:], in_=pA[:, 0:8], func=AF.Exp, scale=-1.0),
                 reads=[pA_t], writes=[lg_t])
            S.op("act", lambda e: e.activation(out=lg[:], in_=lg[:], func=AF.Ln, bias=1.0, scale=1.0),
                 reads=[lg_t], writes=[lg_t])
            S.op("dve", lambda e: e.tensor_scalar(out=lg[:], in0=lg[:], scalar1=-1.0, scalar2=None, op0=ALU.mult),
                 reads=[lg_t], writes=[lg_t])
            tmpd = sb(st, "tmpd", [128, 128], F32)
            tmpd_t = Tk()
            for h in range(4):
                for (dst, dtab, mtab, col) in ((MTf, 2, 3, h), (MTb, 4, 5, 4 + h)):
                    S.op("act", lambda e: e.activation(out=tmpd[:], in_=cst[:, dtab, :], func=AF.Exp,
                                                       scale=lg[:, col:col + 1]),
                         reads=[cst_t, lg_t], writes=[tmpd_t])
                    S.op("dve", lambda e: e.tensor_tensor(out=dst[:, h, :], in0=tmpd[:], in1=cst[:, mtab, :],
                                                          op=ALU.mult), reads=[tmpd_t, cst_t], writes=[dec_t])
                S.op("act", lambda e: e.activation(out=QDf[:, h, :], in_=cst[:, 6, :], func=AF.Exp,
                                                   scale=lg[:, h:h + 1]), reads=[cst_t, lg_t], writes=[dec_t])
                S.op("act", lambda e: e.activation(out=QDb[:, h, :], in_=cst[:, 7, :], func=AF.Exp,
                                                   scale=lg[:, 4 + h:5 + h]), reads=[cst_t, lg_t], writes=[dec_t])
                S.op("act", lambda e: e.activation(out=KD[:, h, 0:1], in_=cst[:, 8, 0:1], func=AF.Exp,
                                                   scale=lg[:, h:h + 1]), reads=[cst_t, lg_t], writes=[dec_t])
                S.op("act", lambda e: e.activation(out=KD[:, h, 1:2], in_=cst[:, 8, 1:2], func=AF.Exp,
                                                   scale=lg[:, 4 + h:5 + h]), reads=[cst_t, lg_t], writes=[dec_t])
                S.op("act", lambda e: e.activation(out=KD[:, h, 2:3], in_=cst[:, 8, 2:3], func=AF.Exp,
                                                   scale=lg[:, h:h + 1]), reads=[cst_t, lg_t], writes=[dec_t])
                S.op("act", lambda e: e.activation(out=KD[:, h, 3:4], in_=cst[:, 8, 2:3], func=AF.Exp,
                                                   scale=lg[:, 4 + h:5 + h]), reads=[cst_t, lg_t], writes=[dec_t])
            S.barrier()

        def load_cast(stack_stage, dst, dst_t, src_ap, shape, stg, stg_t, eng, scale=None, dst_view=None):
            S.dma(stg, src_ap, writes=[stg_t])
            dv = dst if dst_view is None else dst_view
            if scale is None:
                S.op(eng, lambda e: e.tensor_copy(out=dv, in_=stg), reads=[stg_t], writes=[dst_t])
            else:
                S.op(eng, lambda e: e.tensor_scalar(out=dv, in0=stg, scalar1=scale, scalar2=None, op0=ALU.mult),
                     reads=[stg_t], writes=[dst_t])

        def norm_T(stack, pfx, src_tile, src_t, gs_ap, sh_ap, pT, pT_t, hTo, hTo_t, scr, ssb, tmp_t, xn, xn_t,
                   f32_out=None, f32_t=None):
            S.op("pool", lambda e: e.memset(ssb[:, 0:1], 0.0), writes=[tmp_t])
            S.op("act", lambda e: e.activation(out=scr[:], in_=src_tile, func=AF.Square, accum_out=ssb[:, 0:1]),
                 reads=[src_t, tmp_t], writes=[tmp_t])
            S.op("act", lambda e: e.activation(out=ssb[:, 1:2], in_=ssb[:, 0:1], func=AF.Sqrt, scale=1.0 / D, bias=EPS),
                 reads=[tmp_t], writes=[tmp_t])
            S.op("dve", lambda e: e.reciprocal(out=ssb[:, 2:3], in_=ssb[:, 1:2]), reads=[tmp_t], writes=[tmp_t])
            S.op("dve", lambda e: e.tensor_scalar(out=xn[:], in0=src_tile, scalar1=ssb[:, 2:3], scalar2=None,
                                                  op0=ALU.mult), reads=[src_t, tmp_t], writes=[xn_t])
            for c in range(8):
                S.op("pe", lambda e: e.transpose(out=pT[c // 4][:, (c % 4) * 128:(c % 4 + 1) * 128],
                                                 in_=xn[:, c * 128:(c + 1) * 128], identity=identf),
                     reads=[xn_t, cst_t], writes=[pT_t[c // 4]], signal=(c % 4 == 3))
            for c in range(8):
                src = pT[c // 4][:, (c % 4) * 128:(c % 4 + 1) * 128]
                if f32_out is not None:
                    S.op("dve", lambda e: e.tensor_scalar(out=f32_out[:, c, :], in0=src, scalar1=gs_ap[:, c:c + 1],
                                                          scalar2=sh_ap(c), op0=ALU.mult, op1=ALU.add),
                         reads=[pT_t[c // 4], gs_t, mT_t], writes=[f32_t])
                    S.op("pool", lambda e: e.tensor_copy(out=hTo[:, c, :], in_=f32_out[:, c, :]),
                         reads=[f32_t], writes=[hTo_t])
                else:
                    S.op("dve", lambda e: e.tensor_scalar(out=hTo[:, c, :], in0=src, scalar1=gs_ap[:, c:c + 1],
                                                          scalar2=sh_ap(c), op0=ALU.mult, op1=ALU.add),
                         reads=[pT_t[c // 4], gs_t, mT_t], writes=[hTo_t])

        winv = win_d[0].rearrange("(kc p) n -> p kc n", p=128)

        for b in range(NB):
            with contextlib.ExitStack() as st:
                xt = [sb(st, f"s0x{i}", [128, D], F32) for i in range(2)]
                xt_t = [Tk(), Tk()]
                xn = sb(st, "s0xn", [128, D], F32)
                xn_t = Tk()
                scr = sb(st, "s0scr", [128, D], BF16)
                ssb = sb(st, "s0ss", [128, 4], F32)
                tmp_t = Tk()
                hTo = [sb(st, f"s0h{i}", [128, 8, 128], BF16) for i in range(2)]
                hTo_t = [Tk(), Tk()]
                pT = [ps(st, f"s0p{i}", [128, 512]) for i in range(2)]
                pT_t = [Tk(), Tk()]

                def s0_load(t):
                    src = ctx_d[b, t * 128:(t + 1) * 128, :] if t < 2 else x_d[b, (t - 2) * 128:(t - 1) * 128, :]
                    S.dma(xt[t % 2][:], src, writes=[xt_t[t % 2]])

                s0_load(0)
                for t in range(NT):
                    if t + 1 < NT:
                        s0_load(t + 1)
                    r = 2 if t < 2 else b
                    norm_T(st, "s0", xt[t % 2][:], xt_t[t % 2], gs1[:, r, :], lambda c: mT[:, c, r:r + 1],
                           pT, pT_t, hTo[t % 2], hTo_t[t % 2], scr, ssb, tmp_t, xn, xn_t)
                    S.dma(hT_d[t], hTo[t % 2][:], reads=[hTo_t[t % 2]], writes=[hT_t[t]])
                S.barrier()

            with contextlib.ExitStack() as st:
                cqn = sb(st, "cqn", [128, 3, L], BF16)
                cqn_t = Tk()
                ckvn = sb(st, "ckvn", [128, 2, LT], BF16)
                ckvn_t = Tk()
                krT = sb(st, "krT", [64, LT], BF16)
                krT_t = Tk()
                with contextlib.ExitStack() as s1:
                    Wm = sb(s1, "Wm", [128, 8, 768], BF16)
                    Wm_t = Tk()
                    stg = sb(s1, "s1stg", [128, 8, 704], F32)
                    stg_t = Tk()
                    S.dma(stg[:], winv[:, :, 6144:6848], writes=[stg_t])
                    S.op("dve", lambda e: e.tensor_copy(out=Wm[:, :, 0:704], in_=stg[:]), reads=[stg_t], writes=[Wm_t])
                    for (dst, src) in ((704, 656), (720, 640), (736, 688), (752, 672)):
                        S.op("dve", lambda e: e.tensor_copy(out=Wm[:, :, dst:dst + 16], in_=stg[:, :, src:src + 16]),
                             reads=[stg_t], writes=[Wm_t])
                    hb = [sb(s1, f"s1h{i}", [128, 8, 512], BF16) for i in range(2)]
                    hb_t = [Tk(), Tk()]
                    tc_ = [sb(s1, f"s1tc{i}", [64, 2, 512], F32) for i in range(2)]
                    tc_t = [Tk(), Tk()]
                    pq = [ps(s1, f"s1pq{i}", [128, 512]) for i in range(3)]
                    pq_t = [Tk() for _ in range(3)]
                    pk = [ps(s1, f"s1pk{i}", [128, 512]) for i in range(2)]
                    pk_t = [Tk() for _ in range(2)]
                    pr = [ps(s1, f"s1pr{i}", [128, 512]) for i in range(2)]
                    pr_t = [Tk() for _ in range(2)]
                    pss = ps(s1, "s1pss", [128, 512])
                    pss_t = Tk()
                    xf = sb(s1, "s1xf", [128, 3, 512], F32)
                    xf_t = Tk()
                    sq = sb(s1, "s1sq", [128, 3, 512], F32)
                    sq_t = Tk()
                    rstd = sb(s1, "s1rstd", [128, 512], F32)
                    rstd_t = Tk()
                    r1 = sb(s1, "s1r1", [64, 512], F32)
                    r2 = sb(s1, "s1r2", [64, 512], F32)
                    r_t = Tk()

                    def blk_range(j):
                        return (0, 256) if j == 0 else (256 + (j - 1) * 512, 512)

                    def s1_load(j):
                        t0, n = blk_range(j)
                        for i in range(n // 128):
                            S.dma(hb[j % 2][:, :, i * 128:(i + 1) * 128], hT_d[t0 // 128 + i],
                                  reads=[hT_t[t0 // 128 + i]], writes=[hb_t[j % 2]])
                        S.dma(tc_[j % 2][:, 0, 0:n], mc_d[:, t0:t0 + n], writes=[tc_t[j % 2]])
                        S.dma(tc_[j % 2][:, 1, 0:n], ms_d[:, t0:t0 + n], writes=[tc_t[j % 2]])

                    def rms_T(pl, pl_t, nch, rank, gcol, dst, dst_t, dcol0, n):
                        for c in range(nch):
                            S.op("act", lambda e: e.copy(out=xf[:, c, 0:n], in_=pl[c][:, 0:n]),
                                 reads=[pl_t[c]], writes=[xf_t])
                            S.op("act", lambda e: e.activation(out=sq[:, c, 0:n], in_=pl[c][:, 0:n], func=AF.Square),
                                 reads=[pl_t[c]], writes=[sq_t])
                        for c in range(nch):
                            S.op("pe", lambda e: e.matmul(out=pss[:, 0:n], lhsT=onesf, rhs=sq[:, c, 0:n],
                                                          start=(c == 0), stop=(c == nch - 1)),
                                 reads=[sq_t, cst_t], writes=[pss_t], signal=(c == nch - 1))
                        S.op("act", lambda e: e.activation(out=rstd[:, 0:n], in_=pss[:, 0:n], func=AF.Sqrt,
                                                           scale=1.0 / rank, bias=EPS), reads=[pss_t], writes=[rstd_t])
                        S.op("dve", lambda e: e.reciprocal(out=rstd[:, 0:n], in_=rstd[:, 0:n]),
                             reads=[rstd_t], writes=[rstd_t])
                        for c in range(nch):
                            S.op("dve", lambda e: e.scalar_tensor_tensor(
                                out=dst[:, c, dcol0:dcol0 + n], in0=xf[:, c, 0:n], scalar=gq[:, gcol + c:gcol + c + 1],
                                in1=rstd[:, 0:n], op0=ALU.mult, op1=ALU.mult),
                                 reads=[xf_t, rstd_t, gq_t], writes=[dst_t])

                    s1_load(0)
                    for j in range(9):
                        if j + 1 < 9:
                            s1_load(j + 1)
                        t0, n = blk_range(j)
                        h_, h_t = hb[j % 2], hb_t[j % 2]
                        if j > 0:
                            for c in range(3):
                                for kc in range(8):
                                    S.op("pe", lambda e: e.matmul(out=pq[c][:, 0:n], lhsT=Wm[:, kc, c * 128:(c + 1) * 128],
                                                                  rhs=h_[:, kc, 0:n], start=(kc == 0), stop=(kc == 7)),
                                         reads=[Wm_t, h_t], writes=[pq_t[c]], signal=(kc == 7))
                        for c in range(2):
                            for kc in range(8):
                                S.op("pe", lambda e: e.matmul(out=pk[c][:, 0:n], lhsT=Wm[:, kc, 384 + c * 128:384 + (c + 1) * 128],
                                                              rhs=h_[:, kc, 0:n], start=(kc == 0), stop=(kc == 7)),
                                     reads=[Wm_t, h_t], writes=[pk_t[c]], signal=(kc == 7))
                        for c in range(2):
                            for kc in range(8):
                                S.op("pe", lambda e: e.matmul(out=pr[c][0:64, 0:n], lhsT=Wm[:, kc, 640 + c * 64:704 + c * 64],
                                                              rhs=h_[:, kc, 0:n], start=(kc == 0), stop=(kc == 7)),
                                     reads=[Wm_t, h_t], writes=[pr_t[c]], signal=(kc == 7))
                        if j > 0:
                            rms_T(pq, pq_t, 3, 384, 0, cqn, cqn_t, t0 - 256, n)
                        rms_T(pk, pk_t, 2, 256, 4, ckvn, ckvn_t, t0, n)
                        tcc, tcc_t = tc_[j % 2], tc_t[j % 2]
                        S.op("dve", lambda e: e.tensor_tensor(out=r1[:, 0:n], in0=pr[0][0:64, 0:n], in1=tcc[:, 0, 0:n],
                                                              op=ALU.mult), reads=[pr_t[0], tcc_t], writes=[r_t])
                        S.op("dve", lambda e: e.tensor_tensor(out=r2[:, 0:n], in0=pr[1][0:64, 0:n], in1=tcc[:, 1, 0:n],
                                                              op=ALU.mult), reads=[pr_t[1], tcc_t], writes=[r_t])
                        S.op("dve", lambda e: e.tensor_tensor(out=krT[:, t0:t0 + n], in0=r1[:, 0:n], in1=r2[:, 0:n],
                                                              op=ALU.add), reads=[r_t], writes=[krT_t])
                    S.barrier()

                with contextlib.ExitStack() as s2:
                    KT = sb(s2, "KT", [128, LT], BF16)
                    KT_t = Tk()
                    V = sb(s2, "V", [128, NT, 128], BF16)
                    V_t = Tk()
                    QT = sb(s2, "QT", [128, L], BF16)
                    QT_t = Tk()
                    QrT = sb(s2, "QrT", [64, L], BF16)
                    QrT_t = Tk()
                    Wq = sb(s2, "Wq", [128, 3, 256], BF16)
                    Wq_t = Tk()
                    Wkv = sb(s2, "Wkv", [128, 2, 256], BF16)
                    Wkv_t = Tk()
                    sq_ = sb(s2, "s2sq", [128, 3, 192], F32)
                    sq_t = Tk()
                    skv = sb(s2, "s2skv", [128, 2, 256], F32)
                    skv_t = Tk()
                    tq = [sb(s2, f"s2tq{i}", [64, 2, 512], F32) for i in range(2)]
                    tq_t = [Tk(), Tk()]
                    r1 = sb(s2, "s2r1", [64, 512], F32)
                    r2 = sb(s2, "s2r2", [64, 512], F32)
                    r_t = Tk()
                    PT = [sb(s2, f"PT{i}", [128, 512], BF16) for i in range(4)]
                    PT_t = [Tk() for _ in range(4)]
                    accA = [sb(s2, f"s2accA{i}", [128, 512], F32) for i in range(2)]
                    accB = [sb(s2, f"s2accB{i}", [128, 512], F32) for i in range(2)]
                    accA_t = [Tk(), Tk()]
                    accB_t = [Tk(), Tk()]
                    rs = sb(s2, "s2rs", [128, 512], F32)
                    rs_t = Tk()
                    ot = [sb(s2, f"s2ot{i}", [128, 512], BF16) for i in range(2)]
                    ot_t = [Tk(), Tk()]
                    pS = [ps(s2, f"pS{i}", [128, 512]) for i in range(3)]
                    pS_t = [Tk() for _ in range(3)]
                    pO = [ps(s2, f"pO{i}", [128, 512]) for i in range(2)]
                    pO_t = [Tk() for _ in range(2)]
                    pZ = [ps(s2, f"pZ{i}", [128, 512]) for i in range(2)]
                    pZ_t = [Tk() for _ in range(2)]
                    pX = ps(s2, "pX", [128, 512])
                    pX_t = Tk()
                    wuqv = wuq_d[0].rearrange("(kc p) n -> p kc n", p=128)
                    wukvv = wukv_d[0].rearrange("(kc p) n -> p kc n", p=128)
                    sc = 192.0 ** -0.5
                    cnt = 0
                    for h in range(8):
                        S.dma(sq_[:], wuqv[:, :, h * 192:(h + 1) * 192], writes=[sq_t])
                        S.op("pool", lambda e: e.tensor_copy(out=Wq[:, :, 0:192], in_=sq_[:]), reads=[sq_t], writes=[Wq_t])
                        for (dst, src) in ((192, 144), (208, 128), (224, 176), (240, 160)):
                            S.op("pool", lambda e: e.tensor_copy(out=Wq[:, :, dst:dst + 16], in_=sq_[:, :, src:src + 16]),
                                 reads=[sq_t], writes=[Wq_t])
                        S.dma(skv[:], wukvv[:, :, h * 256:(h + 1) * 256], writes=[skv_t])
                        S.op("pool", lambda e: e.tensor_copy(out=Wkv[:], in_=skv[:]), reads=[skv_t], writes=[Wkv_t])
                        for j in range(9):
                            t0, n = (0, 256) if j == 0 else (256 + (j - 1) * 512, 512)
                            for kc in range(2):
                                S.op("pe", lambda e: e.matmul(out=pX[:, 0:n], lhsT=Wkv[:, kc, 0:128], rhs=ckvn[:, kc, t0:t0 + n],
                                                              start=(kc == 0), stop=(kc == 1)),
                                     reads=[Wkv_t, ckvn_t], writes=[pX_t], signal=(kc == 1))
                            S.op("act" if j % 2 else "dve", (lambda e: e.copy(out=KT[:, t0:t0 + n], in_=pX[:, 0:n])) if j % 2
                                 else (lambda e: e.tensor_copy(out=KT[:, t0:t0 + n], in_=pX[:, 0:n])),
                                 reads=[pX_t], writes=[KT_t])
                        for g in range(9):
                            tiles = list(range(g * 4, min(g * 4 + 4, NT)))
                            for i, t in enumerate(tiles):
                                for kc in range(2):
                                    S.op("pe", lambda e: e.matmul(out=pX[:, i * 128:(i + 1) * 128],
                                                                  lhsT=ckvn[:, kc, t * 128:(t + 1) * 128], rhs=Wkv[:, kc, 128:256],
                                                                  start=(kc == 0), stop=(kc == 1)),
                                         reads=[Wkv_t, ckvn_t], writes=[pX_t], signal=(kc == 1 and i == len(tiles) - 1))
                            nn = len(tiles)
                            S.op("act" if g % 2 else "dve",
                                 (lambda e: e.copy(out=V[:, tiles[0]:tiles[0] + nn, :].rearrange("p a b -> p (a b)"),
                                                   in_=pX[:, 0:nn * 128])) if g % 2 else
                                 (lambda e: e.tensor_copy(out=V[:, tiles[0]:tiles[0] + nn, :].rearrange("p a b -> p (a b)"),
                                                          in_=pX[:, 0:nn * 128])),
                                 reads=[pX_t], writes=[V_t])
                        for j in range(8):
                            q0 = j * 512
                            S.dma(tq[j % 2][:, 0, :], mc_d[:, 256 + q0:256 + q0 + 512], writes=[tq_t[j % 2]])
                            S.dma(tq[j % 2][:, 1, :], ms_d[:, 256 + q0:256 + q0 + 512], writes=[tq_t[j % 2]])
                            for kc in range(3):
                                S.op("pe", lambda e: e.matmul(out=pX[:], lhsT=Wq[:, kc, 0:128], rhs=cqn[:, kc, q0:q0 + 512],
                                                              start=(kc == 0), stop=(kc == 2)),
                                     reads=[Wq_t, cqn_t], writes=[pX_t], signal=(kc == 2))
                            S.op("act", lambda e: e.copy(out=QT[:, q0:q0 + 512], in_=pX[:]), reads=[pX_t], writes=[QT_t])
                            for c in range(2):
                                for kc in range(3):
                                    S.op("pe", lambda e: e.matmul(out=pZ[c][0:64, :], lhsT=Wq[:, kc, 128 + c * 64:192 + c * 64],
                                                                  rhs=cqn[:, kc, q0:q0 + 512], start=(kc == 0), stop=(kc == 2)),
                                         reads=[Wq_t, cqn_t], writes=[pZ_t[c]], signal=(kc == 2))
                            tt_, tt_t = tq[j % 2], tq_t[j % 2]
                            S.op("dve", lambda e: e.tensor_tensor(out=r1[:], in0=pZ[0][0:64, :], in1=tt_[:, 0, :], op=ALU.mult),
                                 reads=[pZ_t[0], tt_t], writes=[r_t])
                            S.op("dve", lambda e: e.tensor_tensor(out=r2[:], in0=pZ[1][0:64, :], in1=tt_[:, 1, :], op=ALU.mult),
                                 reads=[pZ_t[1], tt_t], writes=[r_t])
                            S.op("dve", lambda e: e.tensor_tensor(out=QrT[:, q0:q0 + 512], in0=r1[:], in1=r2[:], op=ALU.add),
                                 reads=[r_t], writes=[QrT_t])
                        for qb in range(8):
                            q0 = qb * 512
                            po, po_t = pO[qb % 2], pO_t[qb % 2]
                            pz, pz_t = pZ[qb % 2], pZ_t[qb % 2]
                            def emit_st(kt, cn):
                                s_, s_t = pS[cn % 3], pS_t[cn % 3]
                                S.op("pe", lambda e: e.matmul(out=s_[:], lhsT=KT[:, kt * 128:(kt + 1) * 128], rhs=QT[:, q0:q0 + 512],
                                                              start=True, stop=False), reads=[KT_t, QT_t], writes=[s_t], signal=False)
                                S.op("pe", lambda e: e.matmul(out=s_[:], lhsT=krT[:, kt * 128:(kt + 1) * 128], rhs=QrT[:, q0:q0 + 512],
                                                              start=False, stop=True), reads=[krT_t, QrT_t], writes=[s_t])

                            emit_st(0, cnt)
                            for kt in range(NT):
                                s_, s_t = pS[cnt % 3], pS_t[cnt % 3]
                                p_, p_t = PT[cnt % 4], PT_t[cnt % 4]
                                if kt + 1 < NT:
                                    emit_st(kt + 1, cnt + 1)
                                cnt += 1
                                S.op("act", lambda e: e.activation(out=p_[:], in_=s_[:], func=AF.Exp, scale=sc),
                                     reads=[s_t], writes=[p_t])
                                S.op("pe", lambda e: e.matmul(out=po[:], lhsT=V[:, kt, :], rhs=p_[:], start=(kt == 0),
                                                              stop=(kt == NT - 1)), reads=[V_t, p_t], writes=[po_t])
                                aa, aa_t = accA[qb % 2], accA_t[qb % 2]
                                if kt == 0:
                                    S.op("dve", lambda e: e.tensor_copy(out=aa[:], in_=p_[:]), reads=[p_t], writes=[aa_t])
                                else:
                                    S.op("dve", lambda e: e.tensor_tensor(out=aa[:], in0=aa[:], in1=p_[:], op=ALU.add),
                                         reads=[p_t, aa_t], writes=[aa_t])
                            S.op("pe", lambda e: e.matmul(out=pz[:], lhsT=onesf, rhs=accA[qb % 2][:], start=True, stop=True),
                                 reads=[cst_t, accA_t[qb % 2]], writes=[pz_t])
                            S.op("dve", lambda e: e.reciprocal(out=rs[:], in_=pz[:]), reads=[pz_t], writes=[rs_t])
                            o_, o_t = ot[qb % 2], ot_t[qb % 2]
                            S.op("dve", lambda e: e.tensor_tensor(out=o_[:], in0=po[:], in1=rs[:], op=ALU.mult),
                                 reads=[po_t, rs_t], writes=[o_t])
                            S.dma(att_d[h, :, q0:q0 + 512], o_[:], reads=[o_t], writes=[att_t[qb]])
                    S.barrier()

            with contextlib.ExitStack() as st:
                Wqk = sb(st, "Wqk", [128, 8, 512], BF16)
                Wv = sb(st, "Wv", [128, 8, 512], BF16)
                Wg = sb(st, "Wg", [128, 8, 512], BF16)
                W_t = Tk()
                stg = sb(st, "s3stg", [128, 8, 512], F32)
                stg_t = Tk()
                hb = [sb(st, f"s3h{i}", [128, 8, 128], BF16) for i in range(3)]
                hb_t = [Tk() for _ in range(3)]
                tb = [sb(st, f"s3tb{i}", [128, 2, 256], F32) for i in range(3)]
                tb_t = [Tk() for _ in range(3)]
                ofb = [sb(st, f"s3of{i}", [128, 512], F32) for i in range(3)]
                ofb_t = [Tk() for _ in range(3)]
                Sf = sb(st, "Sf", [128, 2, 512], F32)
                Sb_ = sb(st, "Sb", [128, 2, 512], BF16)
                Sf_t = Tk()
                Sb_t = Tk()
                t1 = sb(st, "s3t1", [128, 512], F32)
                t2 = sb(st, "s3t2", [128, 512], F32)
                t12_t = Tk()
                qk_all = sb(st, "s3qkall", [128, NT, 512], BF16)
                qka_t = [Tk() for _ in range(NT)]
                V_all = sb(st, "s3Vall", [128, NT, 512], BF16)
                Va_t = [Tk() for _ in range(NT)]
                Kdb = [sb(st, f"s3Kd{i}", [128, 256], BF16) for i in range(2)]
                Kdb_t = [Tk(), Tk()]
                KTt = sb(st, "s3KT", [128, 2, 128], BF16)
                QTt = sb(st, "s3QT", [128, 2, 128], BF16)
                QdT = sb(st, "s3QdT", [128, 2, 128], BF16)
                tr_t = Tk()
                STm = sb(st, "s3STm", [128, 128], BF16)
                STm_t = Tk()
                osb = sb(st, "s3o", [128, 512], F32)
                osb_t = Tk()
                sg = sb(st, "s3sg", [128, 512], F32)
                sg_t = Tk()
                scr = sb(st, "s3scr", [128, 512], BF16)
                ssb = sb(st, "s3ss", [128, 4], F32)
                ss_t = Tk()
                rr = sb(st, "s3r", [128, 512], BF16)
                rr_t = Tk()
                rTs = [sb(st, f"s3rT{i}", [128, 4, 128], BF16) for i in range(2)]
                rTs_t = [Tk(), Tk()]
                pAl = [ps(st, f"s3pA{i}", [128, 512]) for i in range(2)]
                pAl_t = [Tk(), Tk()]
                pBl = [ps(st, f"s3pB{i}", [128, 512]) for i in range(2)]
                pBl_t = [Tk(), Tk()]
                pC = ps(st, "s3pC", [128, 512])
                pD = ps(st, "s3pD", [128, 2, 512], BF16)
                pE = ps(st, "s3pE", [128, 512])
                pF = ps(st, "s3pF", [128, 512])
                pC_t, pD_t, pD2_t, pE_t, pF_t = (Tk() for _ in range(5))
                pH = [pC, pE]
                pH_t = [pC_t, pE_t]
                for h in range(4):
                    for (dst, c0, scl) in ((Wqk[:, :, 0:256], h * 256, None), (Wqk[:, :, 256:512], 1024 + h * 256, 0.0625)):
                        S.dma(stg[:, :, 0:256], winv[:, :, c0:c0 + 256], writes=[stg_t])
                        if scl is None:
                            S.op("act", lambda e: e.copy(out=dst, in_=stg[:, :, 0:256]), reads=[stg_t], writes=[W_t])
                        else:
                            S.op("act", lambda e: e.mul(out=dst, in_=stg[:, :, 0:256], mul=scl), reads=[stg_t], writes=[W_t])
                    for (dst, c0) in ((Wv, 2048 + h * 512), (Wg, 4096 + h * 512)):
                        S.dma(stg[:], winv[:, :, c0:c0 + 512], writes=[stg_t])
                        S.op("act", lambda e: e.copy(out=dst[:], in_=stg[:]), reads=[stg_t], writes=[W_t])
                    for di, dirn in enumerate(("f", "b")):
                        order = list(range(NT)) if dirn == "f" else [1, 0] + list(range(NT - 1, 1, -1))
                        MT = MTf if dirn == "f" else MTb
                        QD = QDf if dirn == "f" else QDb
                        kdc = KD[:, h, di:di + 1]
                        cdc = KD[:, h, 2 + di:3 + di]
                        S.op("pool", lambda e: e.memset(Sf[:], 0.0), writes=[Sf_t])
                        S.op("pool", lambda e: e.memset(Sb_[:], 0.0), writes=[Sb_t])

                        def s3_load(idx):
                            t = order[idx]
                            S.dma(hb[idx % 3][:], hT_d[t], reads=[hT_t[t]], writes=[hb_t[idx % 3]])
                            if dirn == "f":
                                S.dma(tb[idx % 3][:, 0, :], rc_d[t * 128:(t + 1) * 128, :], writes=[tb_t[idx % 3]])
                                S.dma(tb[idx % 3][:, 1, :], rs_d[t * 128:(t + 1) * 128, :], writes=[tb_t[idx % 3]])
                            if dirn == "b" and t >= 2:
                                S.dma(ofb[idx % 3][:], of_d[t - 2], reads=[of_t[t - 2]], writes=[ofb_t[idx % 3]])

                        def s3_proj(idx):
                            if dirn == "b":
                                return
                            h_, h_t = hb[idx % 3], hb_t[idx % 3]
                            pA, pA_t = pAl[idx % 2], pAl_t[idx % 2]
                            pB, pB_t = pBl[idx % 2], pBl_t[idx % 2]
                            for kc in range(8):
                                S.op("pe", lambda e: e.matmul(out=pA[:], lhsT=h_[:, kc, :], rhs=Wqk[:, kc, :], start=(kc == 0),
                                                              stop=(kc == 7)), reads=[h_t, W_t], writes=[pA_t], signal=(kc == 7))
                            for kc in range(8):
                                S.op("pe", lambda e: e.matmul(out=pB[:], lhsT=h_[:, kc, :], rhs=Wv[:, kc, :], start=(kc == 0),
                                                              stop=(kc == 7)), reads=[h_t, W_t], writes=[pB_t], signal=(kc == 7))

                        def s3_A(idx):
                            t = order[idx]
                            tb_, tbt = tb[idx % 3], tb_t[idx % 3]
                            pA, pA_t = pAl[idx % 2], pAl_t[idx % 2]
                            pB, pB_t = pBl[idx % 2], pBl_t[idx % 2]
                            qk, qk_t = qk_all[:, t, :], qka_t[t]
                            Vb, Vb_t = V_all[:, t, :], Va_t[t]
                            Kd, Kd_t = Kdb[idx % 2], Kdb_t[idx % 2]
                            for half in (range(2) if dirn == "f" else ()):
                                o = half * 256
                                S.op("dve", lambda e: e.tensor_tensor(out=t1[:, o:o + 256], in0=pA[:, o:o + 256], in1=tb_[:, 0, :],
                                                                      op=ALU.mult), reads=[pA_t, tbt], writes=[t12_t])
                                for part in range(2):
                                    a = o + part * 128
                                    S.op("dve", lambda e: e.tensor_tensor(out=t2[:, a:a + 64], in0=pA[:, a + 64:a + 128],
                                                                          in1=tb_[:, 1, part * 128:part * 128 + 64], op=ALU.mult),
                                         reads=[pA_t, tbt], writes=[t12_t])
                                    S.op("dve", lambda e: e.tensor_tensor(out=t2[:, a + 64:a + 128], in0=pA[:, a:a + 64],
                                                                          in1=tb_[:, 1, part * 128 + 64:part * 128 + 128], op=ALU.mult),
                                         reads=[pA_t, tbt], writes=[t12_t])
                            if dirn == "f":
                                S.op("pool", lambda e: e.tensor_tensor(out=qk[:], in0=t1[:], in1=t2[:], op=ALU.add),
                                     reads=[t12_t], writes=[qk_t])
                                S.op("act", lambda e: e.copy(out=Vb[:], in_=pB[:]), reads=[pB_t], writes=[Vb_t])
                            S.op("pool", lambda e: e.tensor_scalar(out=Kd[:], in0=qk[:, 256:512], scalar1=kdc, scalar2=None, op0=ALU.mult),
                                 reads=[qk_t, dec_t], writes=[Kd_t])

                        s3_load(0)
                        s3_load(1)
                        s3_proj(0)
                        s3_A(0)
                        for idx in range(NT):
                            if idx + 2 < NT:
                                s3_load(idx + 2)
                            if idx + 1 < NT:
                                s3_proj(idx + 1)
                                s3_A(idx + 1)
                            t = order[idx]
                            lat = t >= 2
                            h_, h_t = hb[idx % 3], hb_t[idx % 3]
                            tb_, tbt = tb[idx % 3], tb_t[idx % 3]
                            pA, pA_t = pAl[idx % 2], pAl_t[idx % 2]
                            pB, pB_t = pBl[idx % 2], pBl_t[idx % 2]
                            qk, qk_t = qk_all[:, t, :], qka_t[t]
                            Vb, Vb_t = V_all[:, t, :], Va_t[t]
                            Kd, Kd_t = Kdb[idx % 2], Kdb_t[idx % 2]
                            if lat and dirn == "b":
                                for kc in range(8):
                                    S.op("pe", lambda e: e.matmul(out=pC[:], lhsT=h_[:, kc, :], rhs=Wg[:, kc, :], start=(kc == 0),
                                                                  stop=(kc == 7)), reads=[h_t, W_t], writes=[pC_t], signal=(kc == 7))
                                S.op("act", lambda e: e.activation(out=sg[:], in_=pC[:], func=AF.Silu), reads=[pC_t], writes=[sg_t])
                            if lat:
                                for c in range(4):
                                    S.op("pe", lambda e: e.transpose(out=pD[:, 0, c * 128:(c + 1) * 128], in_=qk[:, c * 128:(c + 1) * 128],
                                                                     identity=identb[:]), reads=[qk_t, cb_t], writes=[pD_t], signal=(c == 3))
                                S.op("act", lambda e: e.copy(out=KTt[:].rearrange("p a b -> p (a b)"), in_=pD[:, 0, 256:512]),
                                     reads=[pD_t], writes=[tr_t])
                                S.op("act", lambda e: e.copy(out=QTt[:].rearrange("p a b -> p (a b)"), in_=pD[:, 0, 0:256]),
                                     reads=[pD_t], writes=[tr_t])
                                for dc in range(2):
                                    S.op("dve", lambda e: e.tensor_tensor(out=QdT[:, dc, :], in0=pD[:, 0, dc * 128:(dc + 1) * 128],
                                                                          in1=QD[:, h, :], op=ALU.mult), reads=[pD_t, dec_t], writes=[tr_t])
                                for dc in range(2):
                                    S.op("pe", lambda e: e.matmul(out=pE[:, 0:128], lhsT=KTt[:, dc, :], rhs=QTt[:, dc, :], start=(dc == 0),
                                                                  stop=(dc == 1)), reads=[tr_t], writes=[pE_t], signal=(dc == 1))
                                S.op("dve", lambda e: e.tensor_tensor(out=STm[:], in0=pE[:, 0:128], in1=MT[:, h, :], op=ALU.mult),
                                     reads=[pE_t, dec_t], writes=[STm_t])
                                S.op("pe", lambda e: e.matmul(out=pF[:], lhsT=STm[:], rhs=Vb[:], start=True, stop=False),
                                     reads=[STm_t, Vb_t], writes=[pF_t], signal=False)
                                for dc in range(2):
                                    S.op("pe", lambda e: e.matmul(out=pF[:], lhsT=QdT[:, dc, :], rhs=Sb_[:, dc, :], start=False,
                                                                  stop=(dc == 1)), reads=[tr_t, Sb_t], writes=[pF_t], signal=(dc == 1))
                            for dc in range(2):
                                S.op("pe", lambda e: e.matmul(out=pH[dc][:], lhsT=Kd[:, dc * 128:(dc + 1) * 128], rhs=Vb[:], start=True,
                                                              stop=True), reads=[Kd_t, Vb_t], writes=[pH_t[dc]])
                            if lat:
                                if dirn == "f":
                                    S.op("act", lambda e: e.copy(out=osb[:], in_=pF[:]), reads=[pF_t], writes=[osb_t])
                                    S.dma(of_d[t - 2], osb[:], reads=[osb_t], writes=[of_t[t - 2]])
                                else:
                                    of_, of_tt = ofb[idx % 3], ofb_t[idx % 3]
                                    S.op("dve", lambda e: e.tensor_tensor(out=osb[:], in0=pF[:], in1=of_[:], op=ALU.add),
                                         reads=[pF_t, of_tt], writes=[osb_t])
                                    S.op("pool", lambda e: e.memset(ssb[:, 0:1], 0.0), writes=[ss_t])
                                    S.op("act", lambda e: e.activation(out=scr[:], in_=osb[:], func=AF.Square, accum_out=ssb[:, 0:1]),
                                         reads=[osb_t, ss_t], writes=[ss_t])
                                    S.op("act", lambda e: e.activation(out=ssb[:, 1:2], in_=ssb[:, 0:1], func=AF.Sqrt, scale=1.0 / 512,
                                                                       bias=EPS), reads=[ss_t], writes=[ss_t])
                                    S.op("dve", lambda e: e.reciprocal(out=ssb[:, 2:3], in_=ssb[:, 1:2]), reads=[ss_t], writes=[ss_t])
                                    S.op("dve", lambda e: e.scalar_tensor_tensor(out=rr[:], in0=osb[:], scalar=ssb[:, 2:3], in1=sg[:],
                                                                                 op0=ALU.mult, op1=ALU.mult),
                                         reads=[osb_t, ss_t, sg_t], writes=[rr_t])
                                    for c in range(4):
                                        S.op("pe", lambda e: e.transpose(out=pD[:, 1, c * 128:(c + 1) * 128], in_=rr[:, c * 128:(c + 1) * 128],
                                                                         identity=identb[:]), reads=[rr_t, cb_t], writes=[pD2_t], signal=(c == 3))
                                    rT_, rT_tt = rTs[idx % 2], rTs_t[idx % 2]
                                    S.op("act", lambda e: e.copy(out=rT_[:].rearrange("p a b -> p (a b)"), in_=pD[:, 1, :]),
                                         reads=[pD2_t], writes=[rT_tt])
                                    tk0 = (t - 2) * 128
                                    S.dma(rT_d[h, :, :, tk0:tk0 + 128].rearrange("c p t -> p c t"), rT_[:], reads=[rT_tt],
                                          writes=[rT_t[(t - 2) // 4]])
                            for dc in range(2):
                                S.op("dve", lambda e: e.scalar_tensor_tensor(out=Sb_[:, dc, :], in0=Sf[:, dc, :], scalar=cdc, in1=pH[dc][:],
                                                                             op0=ALU.mult, op1=ALU.add),
                                     reads=[Sf_t, pH_t[dc], dec_t], writes=[Sb_t])
                                S.op("dve", lambda e: e.scalar_tensor_tensor(out=Sf[:, dc, :], in0=Sf[:, dc, :], scalar=cdc, in1=pH[dc][:],
                                                                             op0=ALU.mult, op1=ALU.add),
                                     reads=[Sf_t, pH_t[dc], dec_t], writes=[Sf_t])
                S.barrier()

            with contextlib.ExitStack() as st:
                Wgr = sb(st, "Wgr", [128, 8, 1024], BF16)
                Wgm = sb(st, "Wgm", [128, 8, 1024], BF16)
                Wbr = sb(st, "Wbr", [128, 16, 1024], BF16)
                Wbm = sb(st, "Wbm", [128, 8, 1024], BF16)
                Wo = sb(st, "Wo", [128, 8, 1024], BF16)
                W_t = Tk()
                stg = [sb(st, f"s4stg{i}", [128, 8, 256], F32) for i in range(2)]
                stg_t = [Tk(), Tk()]
                wbrv = wbr_d[0].rearrange("(kc p) n -> p kc n", p=128)
                wbmv = wbm_d[0].rearrange("(kc p) n -> p kc n", p=128)
                wov = wo_d[0].rearrange("(kc p) n -> p kc n", p=128)
                k = 0
                jobs = []
                for cb in range(4):
                    cs = slice(cb * 256, (cb + 1) * 256)
                    jobs.append((Wgr[:, :, cs], winv[:, :, 6848 + cb * 256:6848 + (cb + 1) * 256]))
                    jobs.append((Wgm[:, :, cs], winv[:, :, 7872 + cb * 256:7872 + (cb + 1) * 256]))
                    jobs.append((Wbr[:, 0:8, cs], wbrv[:, 0:8, cs]))
                    jobs.append((Wbr[:, 8:16, cs], wbrv[:, 8:16, cs]))
                    jobs.append((Wbm[:, :, cs], wbmv[:, :, cs]))
                    jobs.append((Wo[:, :, cs], wov[:, :, cs]))
                for (dst, src) in jobs:
                    load_cast(st, dst, W_t, src, None, stg[k % 2][:], stg_t[k % 2], "pool" if k % 2 else "dve")
                    k += 1
                TK4 = 256
                hb = sb(st, "s4h", [128, 8, TK4], BF16)
                hb_t = Tk()
                rb = sb(st, "s4r", [128, 16, TK4], BF16)
                rb_t = Tk()
                ab = sb(st, "s4a", [128, 8, TK4], BF16)
                ab_t = Tk()
                mTb = sb(st, "s4m", [128, 8, TK4], BF16)
                mTb_t = Tk()
                s3_ = sb(st, "s4s3", [128, TK4], F32)
                s4_ = sb(st, "s4s4", [128, TK4], F32)
                sg_t = Tk()
                m1 = sb(st, "s4m1", [128, TK4], F32)
                m2 = sb(st, "s4m2", [128, TK4], F32)
                m_t = Tk()
                x_ = sb(st, "s4x", [128, D], F32)
                x_t = Tk()
                yt = sb(st, "s4y", [128, D], F32)
                yt_t = Tk()
                o_ = sb(st, "s4xo", [128, D], F32)
                o_t = Tk()
                P = [ps(st, f"s4p{i}", [128, 512]) for i in range(4)]
                P_t = [Tk() for _ in range(4)]
                PY = [ps(st, f"s4py{i}", [128, 512]) for i in range(2)]
                PY_t = [Tk() for _ in range(2)]
                for tbk in range(L // TK4):
                    q0 = tbk * TK4
                    for i in range(TK4 // 128):
                        tl = 2 + tbk * (TK4 // 128) + i
                        S.dma(hb[:, :, i * 128:(i + 1) * 128], hT_d[tl], reads=[hT_t[tl]], writes=[hb_t])
                    for hh in range(4):
                        S.dma(rb[:, hh * 4:(hh + 1) * 4, :], rT_d[hh, :, :, q0:q0 + TK4].rearrange("c p t -> p c t"),
                              reads=[rT_t[q0 // 512]], writes=[rb_t])
                    S.dma(ab[:], att_d[:, :, q0:q0 + TK4].rearrange("h p t -> p h t"), reads=[att_t[q0 // 512]], writes=[ab_t])
                    for fc in range(8):
                        fs = slice(fc * 128, (fc + 1) * 128)
                        for kc in range(16):
                            S.op("pe", lambda e: e.matmul(out=P[0][:, 0:TK4], lhsT=Wbr[:, kc, fs], rhs=rb[:, kc, :], start=(kc == 0), stop=(kc == 15)),
                                 reads=[W_t, rb_t], writes=[P_t[0]], signal=(kc == 15))
                        for (pi, Wx, src, src_t) in ((1, Wbm, ab, ab_t), (2, Wgr, hb, hb_t), (3, Wgm, hb, hb_t)):
                            for kc in range(8):
                                S.op("pe", lambda e: e.matmul(out=P[pi][:, 0:TK4], lhsT=Wx[:, kc, fs], rhs=src[:, kc, :], start=(kc == 0),
                                                              stop=(kc == 7)), reads=[W_t, src_t], writes=[P_t[pi]], signal=(kc == 7))
                        S.op("act", lambda e: e.activation(out=s3_[:], in_=P[2][:, 0:TK4], func=AF.Sigmoid), reads=[P_t[2]], writes=[sg_t])
                        S.op("act", lambda e: e.activation(out=s4_[:], in_=P[3][:, 0:TK4], func=AF.Sigmoid), reads=[P_t[3]], writes=[sg_t])
                        S.op("dve", lambda e: e.tensor_tensor(out=m1[:], in0=P[0][:, 0:TK4], in1=s3_[:], op=ALU.mult),
                             reads=[P_t[0], sg_t], writes=[m_t])
                        S.op("dve", lambda e: e.tensor_tensor(out=m2[:], in0=P[1][:, 0:TK4], in1=s4_[:], op=ALU.mult),
                             reads=[P_t[1], sg_t], writes=[m_t])
                        S.op("pool", lambda e: e.tensor_tensor(out=mTb[:, fc, :], in0=m1[:], in1=m2[:], op=ALU.add),
                             reads=[m_t], writes=[mTb_t])
                    for tt in range(TK4 // 128):
                        gt = tbk * (TK4 // 128) + tt
                        S.dma(x_[:], x_d[b, gt * 128:(gt + 1) * 128, :], writes=[x_t])
                        for cb in range(2):
                            for kc in range(8):
                                S.op("pe", lambda e: e.matmul(out=PY[cb][:], lhsT=mTb[:, kc, tt * 128:(tt + 1) * 128],
                                                              rhs=Wo[:, kc, cb * 512:(cb + 1) * 512], start=(kc == 0), stop=(kc == 7)),
                                     reads=[mTb_t, W_t], writes=[PY_t[cb]], signal=(kc == 7))
                            S.op("dve", lambda e: e.tensor_tensor(out=yt[:, cb * 512:(cb + 1) * 512], in0=PY[cb][:],
                                                                  in1=G1[:, b, cb * 512:(cb + 1) * 512], op=ALU.mult),
                                 reads=[PY_t[cb], G_t], writes=[yt_t])
                        S.op("pool", lambda e: e.tensor_tensor(out=o_[:], in0=yt[:], in1=x_[:], op=ALU.add),
                             reads=[yt_t, x_t], writes=[o_t])
                        S.dma(x1_d[b * 32 + gt], o_[:], reads=[o_t], writes=[x1_t[b * 32 + gt]])
                S.barrier()

        mix_stack.close()
        I32 = mybir.dt.int32
        NTI = NB * 32
        with contextlib.ExitStack() as st:
            posk = sb(st, "posk", [128, NTI * 4], F32)
            ekk = sb(st, "ekk", [128, NTI * 4], F32)
            g4 = sb(st, "g4", [128, NTI * 4], F32)
            pk_t = Tk()
            base = sb(st, "base", [128, 32], F32)
            base_t = Tk()
            desti = sb(st, "desti", [128, NTI * 4], I32)
            desti_t = Tk()
            widx = sb(st, "widx", [128, 8, NBLK], I32)
            bidx = sb(st, "bidx", [2, NBLK], I32)
            widx_t = Tk()
            iota32 = cst[:, 10, 0:32]
            h2d_t = [Tk() for _ in range(NTI)]
            with contextlib.ExitStack() as p1:
                GS2 = sb(p1, "GS2", [128, NB, D], F32)
                SH2 = sb(p1, "SH2", [128, NB, D], F32)
                GS_t = Tk()
                pB2 = [ps(p1, f"p1B{i}", [128, 512]) for i in range(2)]
                pB2_t = [Tk(), Tk()]
                pR = ps(p1, "p1R", [128, 512])
                pR_t = Tk()
                dg = sb(p1, "p1dg", [128, 8, 128], F32)
                dg_t = Tk()
                for bb in range(NB):
                    for (dst, vfn) in ((GS2, lambda c: gs2[:, bb, c:c + 1]), (SH2, lambda c: mT[:, 24 + c, bb:bb + 1])):
                        for c in range(8):
                            S.op("dve", lambda e: e.tensor_scalar(out=dg[:, c, :], in0=identf, scalar1=vfn(c), scalar2=None,
                                                                  op0=ALU.mult), reads=[cst_t, mT_t, gs_t], writes=[dg_t])
                        for c in range(8):
                            S.op("pe", lambda e: e.matmul(out=pB2[c // 4][:, (c % 4) * 128:(c % 4 + 1) * 128], lhsT=onesf,
                                                          rhs=dg[:, c, :], start=True, stop=True),
                                 reads=[dg_t, cst_t], writes=[pB2_t[c // 4]])
                        for hh in range(2):
                            S.op("act", lambda e: e.copy(out=dst[:, bb, hh * 512:(hh + 1) * 512], in_=pB2[hh][:]),
                                 reads=[pB2_t[hh]], writes=[GS_t])
                bfull = sb(p1, "p1bfull", [32, 3072], F32)
                bfb = sb(p1, "p1bfb", [32, 3072], BF16)
                bf_t = Tk()
                S.dma(bfull[:, 0:2048], ebgu_d[0], writes=[bf_t])
                S.dma(bfull[:, 2048:3072], ebd_d[0], writes=[bf_t])
                bfb_t = Tk()
                S.op("dve", lambda e: e.tensor_copy(out=bfb[:], in_=bfull[:]), reads=[bf_t], writes=[bfb_t])
                S.dma(bias_d[:, :], bfb[:], reads=[bfb_t])
                Wr = sb(p1, "Wr", [128, 8, 32], F32)
                Wr_t = Tk()
                S.dma(Wr[:], rw_d[0].rearrange("(kc p) n -> p kc n", p=128), writes=[Wr_t])
                rbs = sb(p1, "rbs", [1, 32], F32)
                rbs_t = Tk()
                S.dma(rbs[:], rb_d[:, :], writes=[rbs_t])
                xt = [sb(p1, f"p1x{i}", [128, D], F32) for i in range(2)]
                xt_t = [Tk(), Tk()]
                xn = sb(p1, "p1xn", [128, D], F32)
                xn_t = Tk()
                h2 = sb(p1, "p1h2", [128, D], F32)
                h2_t = Tk()
                h2bf = [sb(p1, f"p1h2b{i}", [128, D], BF16) for i in range(2)]
                h2bf_t = [Tk(), Tk()]
                h2f = sb(p1, "p1h2f", [128, 8, 128], F32)
                h2f_t = Tk()
                scr = sb(p1, "p1scr", [128, D], BF16)
                ssb = sb(p1, "p1ss", [128, 4], F32)
                tmp_t = Tk()
                lgt = sb(p1, "p1lg", [128, 32], F32)
                mk = sb(p1, "p1mk", [128, 32], F32)
                pos = sb(p1, "p1pos", [128, 32], F32)
                s32 = sb(p1, "p1s32", [128, 32], F32)
                m8 = sb(p1, "p1m8", [128, 8], F32)
                e4 = sb(p1, "p1e4", [128, 4], F32)
                sm = sb(p1, "p1sm", [128, 4], F32)
                rt_t = Tk()
                S.op("pool", lambda e: e.memset(base[:], 0.0), writes=[base_t])
                S.dma(xt[0][:], x1_d[0], reads=[x1_t[0]], writes=[xt_t[0]])
                for ti in range(NTI):
                    bb = ti // 32
                    if ti + 1 < NTI:
                        S.dma(xt[(ti + 1) % 2][:], x1_d[ti + 1], reads=[x1_t[ti + 1]], writes=[xt_t[(ti + 1) % 2]])
                    x_, x_t = xt[ti % 2], xt_t[ti % 2]
                    S.op("pool", lambda e: e.memset(ssb[:, 0:1], 0.0), writes=[tmp_t])
                    S.op("act", lambda e: e.activation(out=scr[:], in_=x_[:], func=AF.Square, accum_out=ssb[:, 0:1]),
                         reads=[x_t, tmp_t], writes=[tmp_t])
                    S.op("act", lambda e: e.activation(out=ssb[:, 1:2], in_=ssb[:, 0:1], func=AF.Sqrt, scale=1.0 / D, bias=EPS),
                         reads=[tmp_t], writes=[tmp_t])
                    S.op("dve", lambda e: e.reciprocal(out=ssb[:, 2:3], in_=ssb[:, 1:2]), reads=[tmp_t], writes=[tmp_t])
                    S.op("dve", lambda e: e.tensor_scalar(out=xn[:], in0=x_[:], scalar1=ssb[:, 2:3], scalar2=None, op0=ALU.mult),
                         reads=[x_t, tmp_t], writes=[xn_t])
                    S.op("dve", lambda e: e.tensor_tensor(out=xn[:], in0=xn[:], in1=GS2[:, bb, :], op=ALU.mult),
                         reads=[xn_t, GS_t], writes=[xn_t])
                    S.op("pool", lambda e: e.tensor_tensor(out=h2[:], in0=xn[:], in1=SH2[:, bb, :], op=ALU.add),
                         reads=[xn_t, GS_t], writes=[h2_t])
                    hb_, hb_t = h2bf[ti % 2], h2bf_t[ti % 2]
                    S.op("act", lambda e: e.copy(out=hb_[:], in_=h2[:]), reads=[h2_t], writes=[hb_t])
                    S.dma(h2_d[ti], hb_[:], reads=[hb_t], writes=[h2d_t[ti]])
                    for c in range(8):
                        S.op("pe", lambda e: e.transpose(out=pB2[c // 4][:, (c % 4) * 128:(c % 4 + 1) * 128],
                                                         in_=h2[:, c * 128:(c + 1) * 128], identity=identf),
                             reads=[h2_t, cst_t], writes=[pB2_t[c // 4]], signal=(c % 4 == 3))
                    S.op("dve", lambda e: e.tensor_copy(out=h2f[:, 0:4, :].rearrange("p a b -> p (a b)"), in_=pB2[0][:]),
                         reads=[pB2_t[0]], writes=[h2f_t])
                    S.op("act", lambda e: e.copy(out=h2f[:, 4:8, :].rearrange("p a b -> p (a b)"), in_=pB2[1][:]),
                         reads=[pB2_t[1]], writes=[h2f_t])
                    for kc in range(8):
                        S.op("pe", lambda e: e.matmul(out=pR[:, 0:32], lhsT=h2f[:, kc, :], rhs=Wr[:, kc, :], start=(kc == 0), stop=False),
                             reads=[h2f_t, Wr_t], writes=[pR_t], signal=False)
                    S.op("pe", lambda e: e.matmul(out=pR[:, 0:32], lhsT=cst[0:1, 1, :], rhs=rbs[:], start=False, stop=True),
                         reads=[cst_t, rbs_t], writes=[pR_t])
                    S.op("dve", lambda e: e.tensor_copy(out=lgt[:], in_=pR[:, 0:32]), reads=[pR_t], writes=[rt_t])
                    S.op("dve", lambda e: e.max(out=m8[:], in_=lgt[:]), reads=[rt_t], writes=[rt_t])
                    S.op("dve", lambda e: e.tensor_scalar(out=mk[:], in0=lgt[:], scalar1=m8[:, 3:4], scalar2=None, op0=ALU.is_ge),
                         reads=[rt_t], writes=[rt_t])
                    S.op("dve", lambda e: e.tensor_scalar(out=sm[:, 0:1], in0=m8[:, 0:1], scalar1=-1.0, scalar2=None, op0=ALU.mult),
                         reads=[rt_t], writes=[rt_t])
                    S.op("act", lambda e: e.activation(out=e4[:], in_=m8[:, 0:4], func=AF.Exp, bias=sm[:, 0:1], scale=1.0),
                         reads=[rt_t], writes=[rt_t])
                    S.op("dve", lambda e: e.reduce_sum(out=sm[:, 1:2], in_=e4[:], axis=mybir.AxisListType.X), reads=[rt_t], writes=[rt_t])
                    S.op("dve", lambda e: e.reciprocal(out=sm[:, 2:3], in_=sm[:, 1:2]), reads=[rt_t], writes=[rt_t])
                    S.op("dve", lambda e: e.tensor_scalar(out=g4[:, ti * 4:ti * 4 + 4], in0=e4[:], scalar1=sm[:, 2:3], scalar2=None, op0=ALU.mult),
                         reads=[rt_t], writes=[pk_t])
                    S.op("pe", lambda e: e.matmul(out=pR[:, 32:64], lhsT=cst[:, 9, :], rhs=mk[:], start=True, stop=True),
                         reads=[rt_t, cst_t], writes=[pR_t])
                    S.op("pe", lambda e: e.matmul(out=pR[:, 64:96], lhsT=onesf, rhs=mk[:], start=True, stop=True),
                         reads=[rt_t, cst_t], writes=[pR_t])
                    S.op("dve", lambda e: e.tensor_tensor(out=pos[:], in0=pR[:, 32:64], in1=base[:], op=ALU.add),
                         reads=[pR_t, base_t], writes=[rt_t])
                    for k in range(4):
                        S.op("dve", lambda e: e.scalar_tensor_tensor(out=s32[:], in0=lgt[:], scalar=m8[:, k:k + 1], in1=pos[:],
                                                                     op0=ALU.is_equal, op1=ALU.mult), reads=[rt_t], writes=[rt_t])
                        S.op("dve", lambda e: e.reduce_sum(out=posk[:, ti * 4 + k:ti * 4 + k + 1], in_=s32[:], axis=mybir.AxisListType.X),
                             reads=[rt_t], writes=[pk_t])
                        S.op("dve", lambda e: e.scalar_tensor_tensor(out=s32[:], in0=lgt[:], scalar=m8[:, k:k + 1], in1=iota32,
                                                                     op0=ALU.is_equal, op1=ALU.mult), reads=[rt_t, cst_t, pk_t], writes=[rt_t])
                        S.op("dve", lambda e: e.reduce_sum(out=ekk[:, ti * 4 + k:ti * 4 + k + 1], in_=s32[:], axis=mybir.AxisListType.X),
                             reads=[rt_t], writes=[pk_t])
                    S.op("dve", lambda e: e.tensor_tensor(out=base[:], in0=pR[:, 64:96], in1=base[:], op=ALU.add),
                         reads=[pR_t, base_t, rt_t], writes=[base_t])
                ci = sb(p1, "p2ci", [128, 32], I32)
                padded = sb(p1, "p2pad", [128, 32], F32)
                pst = sb(p1, "p2pst", [128, 32], F32)
                pend = sb(p1, "p2pend", [128, 32], F32)
                bst = sb(p1, "p2bst", [128, NBLK], F32)
                be = sb(p1, "p2be", [128, NBLK], F32)
                wf = sb(p1, "p2wf", [128, 8, NBLK], F32)
                df = sb(p1, "p2df", [128, NTI * 4], F32)
                p2_t = Tk()
                S.op("dve", lambda e: e.tensor_copy(out=ci[:], in_=base[:]), reads=[base_t], writes=[p2_t])
                S.op("dve", lambda e: e.tensor_scalar(out=ci[:], in0=ci[:], scalar1=511, scalar2=None, op0=ALU.add), reads=[p2_t], writes=[p2_t])
                S.op("dve", lambda e: e.tensor_scalar(out=ci[:], in0=ci[:], scalar1=-512, scalar2=None, op0=ALU.bitwise_and), reads=[p2_t], writes=[p2_t])
                S.op("dve", lambda e: e.tensor_copy(out=padded[:], in_=ci[:]), reads=[p2_t], writes=[p2_t])
                S.op("dve", lambda e: e.memset(pst[:], 0.0), reads=[p2_t], writes=[p2_t])
                for ee in range(1, 32):
                    S.op("dve", lambda e: e.tensor_tensor(out=pst[:, ee:ee + 1], in0=pst[:, ee - 1:ee], in1=padded[:, ee - 1:ee], op=ALU.add),
                         reads=[p2_t], writes=[p2_t])
                S.op("dve", lambda e: e.tensor_tensor(out=pend[:], in0=pst[:], in1=padded[:], op=ALU.add), reads=[p2_t], writes=[p2_t])
                S.op("dve", lambda e: e.tensor_scalar(out=bst[:], in0=cst[:, 10, 0:NBLK], scalar1=512.0, scalar2=None, op0=ALU.mult),
                     reads=[cst_t, p2_t], writes=[p2_t])
                S.op("dve", lambda e: e.memset(be[:], 0.0), reads=[p2_t], writes=[p2_t])
                for ee in range(32):
                    S.op("dve", lambda e: e.scalar_tensor_tensor(out=be[:], in0=bst[:], scalar=pend[:, ee:ee + 1], in1=be[:],
                                                                 op0=ALU.is_ge, op1=ALU.add), reads=[p2_t], writes=[p2_t])
                S.op("dve", lambda e: e.tensor_scalar(out=be[:], in0=be[:], scalar1=31.0, scalar2=None, op0=ALU.min), reads=[p2_t], writes=[p2_t])
                for kc in range(8):
                    S.op("dve", lambda e: e.tensor_scalar(out=wf[:, kc, :], in0=be[:], scalar1=1024.0, scalar2=cst[:, 11, kc:kc + 1],
                                                          op0=ALU.mult, op1=ALU.add), reads=[p2_t, cst_t], writes=[p2_t])
                S.op("dve", lambda e: e.tensor_copy(out=widx[:], in_=wf[:]), reads=[p2_t], writes=[widx_t])
                S.op("dve", lambda e: e.tensor_copy(out=bidx[:], in_=be[0:2, :]), reads=[p2_t], writes=[widx_t])
                for c in range(NTI * 4):
                    S.op("dve", lambda e: e.scalar_tensor_tensor(out=s32[:], in0=iota32, scalar=ekk[:, c:c + 1], in1=pst[:],
                                                                 op0=ALU.is_equal, op1=ALU.mult), reads=[p2_t, pk_t, cst_t, rt_t], writes=[rt_t])
                    S.op("dve", lambda e: e.reduce_sum(out=df[:, c:c + 1], in_=s32[:], axis=mybir.AxisListType.X), reads=[rt_t], writes=[p2_t])
                S.op("dve", lambda e: e.tensor_tensor(out=df[:], in0=df[:], in1=posk[:], op=ALU.add), reads=[p2_t, pk_t], writes=[p2_t])
                S.op("dve", lambda e: e.tensor_copy(out=desti[:], in_=df[:]), reads=[p2_t], writes=[desti_t])
                for ti in range(NTI):
                    hb_, hb_t = h2bf[ti % 2], h2bf_t[ti % 2]
                    S.dma(hb_[:], h2_d[ti], reads=[h2d_t[ti]], writes=[hb_t])
                    for k in range(4):
                        cidx = ti * 4 + k
                        S.dma_ind(lambda e: e.indirect_dma_start(
                            out=xs_d[:, :], out_offset=bass.IndirectOffsetOnAxis(ap=desti[:, cidx:cidx + 1], axis=0),
                            in_=hb_[:], in_offset=None),
                            reads=[hb_t, desti_t])
                S.barrier()

            with contextlib.ExitStack() as p4:
                egu2 = egu_d[0].rearrange("e k n -> (e k) n")
                edn2 = edn_d[0].rearrange("e k n -> (e k) n")
                Wgu = [sb(p4, f"Wgu{i}", [128, 8, 1024], BF16) for i in range(3)]
                Wgu_t = [Tk(), Tk(), Tk()]
                Wd = [sb(p4, f"Wd{i}", [128, 4, 1024], BF16) for i in range(2)]
                Wd_t = [Tk(), Tk()]
                stg = [sb(p4, f"p4stg{i}", [128, 2048], F32) for i in range(4)]
                stg_t = [Tk() for _ in range(4)]
                browb = [sb(p4, f"browb{i}", [2, 3072], BF16) for i in range(2)]
                browb_t = [Tk(), Tk()]
                xs = [sb(p4, f"p4xs{i}", [128, D], BF16) for i in range(4)]
                xs_t = [Tk() for _ in range(4)]
                xsT = sb(p4, "p4xsT", [128, 8, 512], BF16)
                xsT_t = Tk()
                aT = [sb(p4, f"aT{i}", [128, 4, 512], BF16) for i in range(2)]
                aT_t = [Tk(), Tk()]
                g1 = sb(p4, "p4g1", [128, 512], F32)
                u1 = sb(p4, "p4u1", [128, 512], F32)
                glu = sb(p4, "p4gl", [128, 512], F32)
                g1_t, u1_t, glu_t = Tk(), Tk(), Tk()
                ysb = sb(p4, "p4ys", [128, 4, D], F32)
                ysb_t = [[Tk(), Tk()] for _ in range(4)]
                pG = [ps(p4, f"p4G{i}", [128, 512]) for i in range(2)]
                pG_t = [Tk(), Tk()]
                pU = [ps(p4, f"p4U{i}", [128, 512]) for i in range(2)]
                pU_t = [Tk(), Tk()]
                pY = [ps(p4, f"p4Y{i}", [128, 512]) for i in range(2)]
                pY_t = [Tk(), Tk()]
                pX = [ps(p4, f"p4X{i}", [128, 2, 512], BF16) for i in range(2)]
                pX_t = [Tk(), Tk()]
                ISC = 1.0 / 1.702
                cnt = dict(sk=0, gk=0, yk=0)

                def w_load_gu_block(blk):
                    for kc in range(8):
                        si = cnt["sk"] % 4
                        cnt["sk"] += 1
                        S.dma_ind(lambda e: e.indirect_dma_start(
                            out=stg[si][:, :], out_offset=None, in_=egu2[:, :],
                            in_offset=bass.IndirectOffsetOnAxis(ap=widx[:, kc, blk:blk + 1], axis=0)),
                            reads=[widx_t], writes=[stg_t[si]])
                        for hf in range(2):
                            wi3 = (2 * blk + hf) % 3
                            S.op("act", lambda e: e.copy(out=Wgu[wi3][:, kc, :].rearrange("p (g n) -> p g n", g=2),
                                                         in_=stg[si][:].rearrange("p (g h n) -> p g h n", g=2, h=2)[:, :, hf, :]),
                                 reads=[stg_t[si]], writes=[Wgu_t[wi3]])

                def w_load_d(blk, hf, wi):
                    wd, wd_t = Wd[wi], Wd_t[wi]
                    for jq in range(2):
                        si = cnt["sk"] % 4
                        cnt["sk"] += 1
                        for i in range(2):
                            kc = hf * 4 + jq * 2 + i
                            S.dma_ind(lambda e: e.indirect_dma_start(
                                out=stg[si][:, i * 1024:(i + 1) * 1024], out_offset=None, in_=edn2[:, :],
                                in_offset=bass.IndirectOffsetOnAxis(ap=widx[:, kc, blk:blk + 1], axis=0)),
                                reads=[widx_t], writes=[stg_t[si]])
                        S.op("act", lambda e: e.mul(out=wd[:, jq * 2:(jq + 1) * 2, :], in_=stg[si][:].rearrange("p (a b) -> p a b", a=2), mul=ISC),
                             reads=[stg_t[si]], writes=[wd_t])

                def b_load(blk):
                    S.dma_ind(lambda e: e.indirect_dma_start(
                        out=browb[blk % 2][0:2, :], out_offset=None, in_=bias_d[:, :],
                        in_offset=bass.IndirectOffsetOnAxis(ap=bidx[0:2, blk:blk + 1], axis=0)),
                        reads=[widx_t], writes=[browb_t[blk % 2]])

                def x_dma(blk):
                    for tt in range(4):
                        r0 = blk * 512 + tt * 128
                        S.dma(xs[tt][:], xs_d[r0:r0 + 128, :], writes=[xs_t[tt]])

                def x_tr(blk):
                    for tt in range(4):
                        for kc in range(8):
                            S.op("pe", lambda e: e.transpose(out=pX[tt % 2][:, kc // 4, (kc % 4) * 128:(kc % 4 + 1) * 128],
                                                             in_=xs[tt][:, kc * 128:(kc + 1) * 128], identity=identb[:]),
                                 reads=[xs_t[tt], cb_t], writes=[pX_t[tt % 2]], signal=(kc == 7))
                        S.op("dve", lambda e: e.tensor_copy(out=xsT[:, 0:4, tt * 128:(tt + 1) * 128],
                                                            in_=pX[tt % 2][:, 0, :].rearrange("p (a b) -> p a b", a=4)),
                             reads=[pX_t[tt % 2]], writes=[xsT_t])
                        S.op("act", lambda e: e.copy(out=xsT[:, 4:8, tt * 128:(tt + 1) * 128],
                                                     in_=pX[tt % 2][:, 1, :].rearrange("p (a b) -> p a b", a=4)),
                             reads=[pX_t[tt % 2]], writes=[xsT_t])

                def gu_unit(blk, hf, wi, au):
                    wg, wg_t = Wgu[(2 * blk + hf) % 3], Wgu_t[(2 * blk + hf) % 3]
                    bb_, bb_t = browb[blk % 2], browb_t[blk % 2]
                    a_, a_t = aT[au], aT_t[au]
                    for j in range(4):
                        i2 = cnt["gk"] % 2
                        cnt["gk"] += 1
                        fcol = hf * 512 + j * 128
                        for kc in range(8):
                            S.op("pe", lambda e: e.matmul(out=pG[i2][:], lhsT=wg[:, kc, j * 128:(j + 1) * 128], rhs=xsT[:, kc, :],
                                                          start=(kc == 0), stop=False), reads=[wg_t, xsT_t], writes=[pG_t[i2]], signal=False)
                        S.op("pe", lambda e: e.matmul(out=pG[i2][:], lhsT=bb_[0:1, fcol:fcol + 128], rhs=cbones[0:1, :], start=False, stop=True),
                             reads=[bb_t, cb_t], writes=[pG_t[i2]])
                        for kc in range(8):
                            S.op("pe", lambda e: e.matmul(out=pU[i2][:], lhsT=wg[:, kc, 512 + j * 128:512 + (j + 1) * 128], rhs=xsT[:, kc, :],
                                                          start=(kc == 0), stop=False), reads=[wg_t, xsT_t], writes=[pU_t[i2]], signal=False)
                        S.op("pe", lambda e: e.matmul(out=pU[i2][:], lhsT=bb_[0:1, 1024 + fcol:1024 + fcol + 128], rhs=cbones[0:1, :],
                                                      start=False, stop=True), reads=[bb_t, cb_t], writes=[pU_t[i2]])
                        S.op("dve", lambda e: e.tensor_scalar(out=g1[:], in0=pG[i2][:], scalar1=7.0, scalar2=None, op0=ALU.min),
                             reads=[pG_t[i2]], writes=[g1_t])
                        S.op("act", lambda e: e.activation(out=glu[:], in_=g1[:], func=AF.Silu, scale=1.702), reads=[g1_t], writes=[glu_t])
                        S.op("dve", lambda e: e.tensor_scalar(out=u1[:], in0=pU[i2][:], scalar1=1.0, scalar2=8.0, op0=ALU.add, op1=ALU.min),
                             reads=[pU_t[i2]], writes=[u1_t])
                        S.op("dve", lambda e: e.scalar_tensor_tensor(out=a_[:, j, :], in0=u1[:], scalar=-6.0, in1=glu[:],
                                                                     op0=ALU.max, op1=ALU.mult), reads=[u1_t, glu_t], writes=[a_t])

                def dn_unit(blk, hf, wi, au):
                    wd, wd_t = Wd[wi], Wd_t[wi]
                    bb_, bb_t = browb[blk % 2], browb_t[blk % 2]
                    a_, a_t = aT[au], aT_t[au]
                    for tt in range(4):
                        for cb in range(2):
                            yi = cnt["yk"] % 2
                            cnt["yk"] += 1
                            for j in range(4):
                                S.op("pe", lambda e: e.matmul(out=pY[yi][:], lhsT=a_[:, j, tt * 128:(tt + 1) * 128],
                                                              rhs=wd[:, j, cb * 512:(cb + 1) * 512], start=(j == 0), stop=(j == 3 and hf == 1)),
                                     reads=[a_t, wd_t], writes=[pY_t[yi]], signal=(j == 3 and hf == 1))
                            yv = ysb[:, tt, cb * 512:(cb + 1) * 512]
                            if hf == 0:
                                S.op("pe", lambda e: e.matmul(out=pY[yi][:], lhsT=cbones[0:1, 0:128], rhs=bb_[0:1, 2048 + cb * 512:2048 + (cb + 1) * 512],
                                                              start=False, stop=True), reads=[bb_t, cb_t], writes=[pY_t[yi]])
                                S.op("act", lambda e: e.copy(out=yv, in_=pY[yi][:]), reads=[pY_t[yi]], writes=[ysb_t[tt][cb]])
                            else:
                                S.op("dve", lambda e: e.tensor_tensor(out=yv, in0=pY[yi][:], in1=yv, op=ALU.add),
                                     reads=[pY_t[yi], ysb_t[tt][cb]], writes=[ysb_t[tt][cb]])
                    if hf == 1:
                        S.dma(ys_d[blk * 512:(blk + 1) * 512, :].rearrange("(t p) c -> p t c", p=128), ysb[:],
                              reads=[ysb_t[tt][cb] for tt in range(4) for cb in range(2)])

                cbones = sb(p4, "cbones", [1, 512], BF16)
                S.op("dve", lambda e: e.memset(cbones[:], 1.0), reads=[cb_t], writes=[cb_t])
                units = [(blk, hf) for blk in range(NBLK) for hf in range(2)]
                NU = len(units)
                b_load(0)
                x_dma(0)
                w_load_gu_block(0)
                w_load_d(0, 0, 0)
                w_load_d(0, 1, 1)
                x_tr(0)
                gu_unit(0, 0, 0, 0)
                w_load_gu_block(1)
                b_load(1)
                x_dma(1)
                for ui, (blk, hf) in enumerate(units):
                    if ui + 1 < NU:
                        nb_, nh_ = units[ui + 1]
                        if nh_ == 0:
                            x_tr(nb_)
                        gu_unit(nb_, nh_, (ui + 1) % 2, (ui + 1) % 2)
                        if nh_ == 0 and nb_ + 1 < NBLK:
                            w_load_gu_block(nb_ + 1)
                        if nh_ == 0 and nb_ + 1 < NBLK:
                            b_load(nb_ + 1)
                            x_dma(nb_ + 1)
                    dn_unit(blk, hf, ui % 2, ui % 2)
                    if ui + 2 < NU:
                        b2, h2_ = units[ui + 2]
                        w_load_d(b2, h2_, (ui + 2) % 2)
                S.barrier()

            with contextlib.ExitStack() as p5:
                yk = [sb(p5, f"p5y{i}", [128, D], F32) for i in range(4)]
                yk_t = [Tk() for _ in range(4)]
                accm = sb(p5, "p5acc", [128, D], F32)
                acc_t = Tk()
                x_ = sb(p5, "p5x", [128, D], F32)
                x_t = Tk()
                scr = sb(p5, "p5scr", [128, D], BF16)
                ssb = sb(p5, "p5ss", [128, 4], F32)
                tmp_t = Tk()
                yo = sb(p5, "p5yo", [128, D], F32)
                yo_t = Tk()
                for ti in range(NTI):
                    bb = ti // 32
                    S.dma(x_[:], x1_d[ti], reads=[x1_t[ti]], writes=[x_t])
                    for k in range(4):
                        cidx = ti * 4 + k
                        S.dma_ind(lambda e: e.indirect_dma_start(
                            out=yk[k][:], out_offset=None, in_=ys_d[:, :],
                            in_offset=bass.IndirectOffsetOnAxis(ap=desti[:, cidx:cidx + 1], axis=0)),
                            reads=[desti_t], writes=[yk_t[k]])
                    S.op("dve", lambda e: e.tensor_scalar(out=accm[:], in0=yk[0][:], scalar1=g4[:, ti * 4:ti * 4 + 1], scalar2=None, op0=ALU.mult),
                         reads=[yk_t[0], pk_t], writes=[acc_t])
                    for k in range(1, 4):
                        S.op("dve", lambda e: e.scalar_tensor_tensor(out=accm[:], in0=yk[k][:], scalar=g4[:, ti * 4 + k:ti * 4 + k + 1], in1=accm[:],
                                                                     op0=ALU.mult, op1=ALU.add), reads=[yk_t[k], pk_t, acc_t], writes=[acc_t])
                    S.op("dve", lambda e: e.tensor_tensor(out=accm[:], in0=accm[:], in1=G2[:, bb, :], op=ALU.mult),
                         reads=[acc_t, G_t], writes=[acc_t])
                    S.op("dve", lambda e: e.tensor_tensor(out=accm[:], in0=accm[:], in1=x_[:], op=ALU.add), reads=[acc_t, x_t], writes=[acc_t])
                    S.op("dve", lambda e: e.memset(ssb[:, 0:1], 0.0), writes=[tmp_t])
                    S.op("act", lambda e: e.activation(out=scr[:], in_=accm[:], func=AF.Square, accum_out=ssb[:, 0:1]),
                         reads=[acc_t, tmp_t], writes=[tmp_t])
                    S.op("act", lambda e: e.activation(out=ssb[:, 1:2], in_=ssb[:, 0:1], func=AF.Sqrt, scale=1.0 / D, bias=EPS),
                         reads=[tmp_t], writes=[tmp_t])
                    S.op("dve", lambda e: e.reciprocal(out=ssb[:, 2:3], in_=ssb[:, 1:2]), reads=[tmp_t], writes=[tmp_t])
                    S.op("dve", lambda e: e.scalar_tensor_tensor(out=yo[:], in0=accm[:], scalar=ssb[:, 2:3], in1=FG[:], op0=ALU.mult, op1=ALU.mult),
                         reads=[acc_t, tmp_t, G_t], writes=[yo_t])
                    r0 = (ti % 32) * 128
                    S.dma(out_d[bb, r0:r0 + 128, :], yo[:], reads=[yo_t])
            S.final_wait()
        print("ops:", S.n_ops, "dmas:", S.dma_n, "sig:", S.cnt)
    return nc


_CACHE = {}


def make_in_maps(inputs, n_cores=8):
    RC, RS, MC, MS = rope_tables()
    cst, _ = const_tables()
    f = lambda a: np.ascontiguousarray(np.asarray(a, dtype=np.float32))
    shared = {k: f(inputs[k]) for k in ("norm1_g", "norm2_g", "ada_w", "ada_b", "w_in", "ret_decay_fwd", "ret_decay_bwd",
                                        "mla_q_norm_g", "mla_w_uq", "mla_kv_norm_g", "mla_w_ukv", "w_branch_ret",
                                        "w_branch_mla", "w_out", "router_w", "router_b", "exp_w_gu", "exp_b_gu",
                                        "exp_w_down", "exp_b_down", "final_norm_g")}
    shared.update(consts=cst, rope_rc=RC, rope_rs=RS, rope_mc=MC, rope_ms=MS)
    x, c, ctx, c_ctx = f(inputs["x"]), f(inputs["c"]), f(inputs["ctx"]), f(inputs["c_ctx"])
    maps = []
    for i in range(n_cores):
        m = dict(shared)
        m["x"] = x[i * NB:(i + 1) * NB]
        m["ctx"] = ctx[i * NB:(i + 1) * NB]
        m["cvec"] = np.ascontiguousarray(np.concatenate([c[i * NB:(i + 1) * NB], c_ctx[None, :]], axis=0))
        maps.append(m)
    return maps


def kernel(**inputs):
    if "nc" not in _CACHE:
        _CACHE["nc"] = build()
    nc = _CACHE["nc"]
    maps = make_in_maps(inputs)
    res = run_bass_kernel_spmd(nc, maps, core_ids=list(range(8)))
    return np.concatenate([r["out"] for r in res.results], axis=0).astype(np.float32)
```

```python
import contextlib
import numpy as np
import concourse.bass as bass
import concourse.mybir as mybir
from concourse.bass_utils import run_bass_kernel_spmd

F32 = mybir.dt.float32
BF16 = mybir.dt.bfloat16
AF = mybir.ActivationFunctionType
ALU = mybir.AluOpType

NB = 2
L = 4096
CT = 256
LT = L + CT
NT = LT // 128
D = 1024
EPS = 1e-6
NS_DMA = 16
ERA = 16000


class Tk:
    __slots__ = ("w", "r", "name")

    def __init__(self, name=""):
        self.w = []
        self.r = []
        self.name = name


class Sched:
    def __init__(self, nc, stack):
        self.nc = nc
        self.stack = stack
        self.engs = {"pe": nc.tensor, "act": nc.scalar, "dve": nc.vector, "pool": nc.gpsimd, "sp": nc.sync}
        self.sems = {}
        self.seq = {e: 0 for e in self.engs}
        self.sig = {e: [] for e in self.engs}
        self.cnt = {e: 0 for e in self.engs}
        self.waited = {e: {} for e in self.engs}
        self.waited_d = {e: {} for e in self.engs}
        self.ring = [stack.enter_context(nc.semaphore(f"dq{i}")) for i in range(NS_DMA)]
        self.dma_n = 0
        self.n_ops = 0

    def _sem(self, eng, era):
        k = (eng, era)
        if k not in self.sems:
            self.sems[k] = self.stack.enter_context(self.nc.semaphore(f"s_{eng}_{era}"))
        return self.sems[k]

    def _wait(self, eng, tk):
        e = self.engs[eng]
        if tk[0] == "d":
            _, ring, val = tk
            if self.waited_d[eng].get(ring, 0) >= val:
                return
            e.wait_ge(self.ring[ring], val)
            self.waited_d[eng][ring] = val
            return
        _, peng, seq = tk
        lst = self.sig[peng]
        lo, hi = 0, len(lst)
        while lo < hi:
            mid = (lo + hi) // 2
            if lst[mid][0] >= seq:
                hi = mid
            else:
                lo = mid + 1
        if lo >= len(lst):
            raise RuntimeError(f"no signalling op after seq {seq} on {peng}")
        count = lst[lo][1]
        if self.waited[eng].get(peng, 0) >= count:
            return
        era, val = (count - 1) // ERA, (count - 1) % ERA + 1
        e.wait_ge(self._sem(peng, era), val)
        self.waited[eng][peng] = count

    def op(self, eng, fn, reads=(), writes=(), signal=True):
        deps = []
        for t in reads:
            deps.extend(t.w)
        for t in writes:
            for tk in t.w:
                if tk[0] == "d" or tk[1] != eng:
                    deps.append(tk)
            for tk in t.r:
                if tk[0] == "d" or tk[1] != eng:
                    deps.append(tk)
        for tk in deps:
            self._wait(eng, tk)
        ins = fn(self.engs[eng])
        self.seq[eng] += 1
        seq = self.seq[eng]
        if signal:
            self.cnt[eng] += 1
            c = self.cnt[eng]
            era, val = (c - 1) // ERA, (c - 1) % ERA + 1
            ins.then_inc(self._sem(eng, era), 1)
            self.sig[eng].append((seq, c))
        tk = ("c", eng, seq)
        for t in reads:
            t.r = [x for x in t.r if not (x[0] == "c" and x[1] == eng)]
            t.r.append(tk)
        for t in writes:
            t.w = [tk]
            t.r = []
        self.n_ops += 1
        return ins

    def dma(self, out, in_, reads=(), writes=()):
        eng = "sp"
        deps = []
        for t in reads:
            deps.extend(t.w)
        for t in writes:
            deps.extend(t.w)
            deps.extend(t.r)
        n = self.dma_n
        ring = n % NS_DMA
        val = 16 * (n // NS_DMA + 1)
        if n >= NS_DMA:
            deps.append(("d", ring, val - 16))
        for tk in deps:
            self._wait(eng, tk)
        self.engs[eng].dma_start(out=out, in_=in_).then_inc(self.ring[ring], 16)
        self.dma_n += 1
        tk = ("d", ring, val)
        for t in reads:
            t.r.append(tk)
        for t in writes:
            t.w = [tk]
            t.r = []
        self.n_ops += 1
        return tk

    def dma_ind(self, fn, reads=(), writes=()):
        eng = "pool"
        deps = []
        for t in reads:
            deps.extend(t.w)
        for t in writes:
            deps.extend(t.w)
            deps.extend(t.r)
        n = self.dma_n
        ring = n % NS_DMA
        val = 16 * (n // NS_DMA + 1)
        if n >= NS_DMA:
            deps.append(("d", ring, val - 16))
        for tk in deps:
            self._wait(eng, tk)
        fn(self.engs[eng]).then_inc(self.ring[ring], 16)
        self.dma_n += 1
        tk = ("d", ring, val)
        for t in reads:
            t.r.append(tk)
        for t in writes:
            t.w = [tk]
            t.r = []
        self.n_ops += 1
        return tk

    def barrier(self):
        tks = []
        for e in self.engs:
            if self.sig[e]:
                tks.append(("c", e, self.sig[e][-1][0]))
        n = self.dma_n
        for k in range(max(0, n - NS_DMA), n):
            tks.append(("d", k % NS_DMA, 16 * (k // NS_DMA + 1)))
        for e in self.engs:
            for tk in tks:
                if tk[0] == "c" and tk[1] == e:
                    continue
                self._wait(e, tk)

    def final_wait(self):
        n = self.dma_n
        for k in range(max(0, n - NS_DMA), n):
            self._wait("sp", ("d", k % NS_DMA, 16 * (k // NS_DMA + 1)))


def rope_tables():
    pos = np.arange(L)
    rows = (pos // 64).astype(np.float32)
    cols = (pos % 64).astype(np.float32)

    def tab(dr):
        half = dr // 2
        hh = half // 2
        freqs = (10000.0 ** (-np.arange(hh, dtype=np.float32) / hh)).astype(np.float32)
        C = np.ones((LT, dr), np.float32)
        S = np.zeros((LT, dr), np.float32)
        for part, p in enumerate((rows, cols)):
            ang = (p[:, None] * freqs[None, :]).astype(np.float32)
            c, s = np.cos(ang).astype(np.float32), np.sin(ang).astype(np.float32)
            o = part * half
            C[CT:, o:o + hh] = c
            C[CT:, o + hh:o + half] = c
            S[CT:, o:o + hh] = -s
            S[CT:, o + hh:o + half] = s
        return C, S

    RC, RS = tab(256)
    MC, MS = tab(64)
    return RC, RS, np.ascontiguousarray(MC.T), np.ascontiguousarray(MS.T)


def const_tables():
    j = np.arange(128, dtype=np.float32)[:, None]
    i = np.arange(128, dtype=np.float32)[None, :]
    c = {}
    c["ident"] = np.eye(128, dtype=np.float32)
    c["ones"] = np.ones((128, 128), np.float32)
    c["d1"] = np.maximum(i - j, 0.0) + 0 * j
    c["mf"] = (i >= j).astype(np.float32)
    c["d2"] = np.maximum(j - i, 0.0)
    c["mb"] = (i < j).astype(np.float32)
    c["ip1"] = (i + 1.0) + 0 * j
    c["rev"] = (128.0 - i) + 0 * j
    col = np.zeros((128, 128), np.float32)
    col[:, 0] = 127.0 - np.arange(128)
    col[:, 1] = np.arange(128)
    col[:, 2] = 128.0
    c["col"] = col
    c["lt"] = (i > j).astype(np.float32)
    c["iota"] = i + 0 * j
    rowb = np.zeros((128, 128), np.float32)
    for kc in range(8):
        rowb[:, kc] = kc * 128 + np.arange(128)
    c["rowb"] = rowb
    names = ["ident", "ones", "d1", "mf", "d2", "mb", "ip1", "rev", "col", "lt", "iota", "rowb"]
    return np.stack([c[n].astype(np.float32) for n in names], axis=1), names


def build(dbg=()):
    nc = bass.Bass("TRN2", target_bir_lowering=False)
    try:
        nc.allow_low_precision("bf16 matmul operands with fp32 accumulation")
    except Exception:
        pass
    try:
        nc.allow_non_contiguous_dma("strided weight/activation tiles")
    except Exception:
        pass

    def din(name, shape, dt=F32):
        return nc.dram_tensor(name, list(shape), dt, kind="ExternalInput").ap()

    def dscr(name, shape, dt):
        kind = "ExternalOutput" if name in dbg else "Internal"
        return nc.dram_tensor(name, list(shape), dt, kind=kind).ap()

    x_d = din("x", [NB, L, D])
    ctx_d = din("ctx", [NB, CT, D])
    cv_d = din("cvec", [3, D])
    n1_d = din("norm1_g", [1, D])
    n2_d = din("norm2_g", [1, D])
    adaw_d = din("ada_w", [1, D, 6 * D])
    adab_d = din("ada_b", [1, 6 * D])
    win_d = din("w_in", [1, D, 8896])
    rdf_d = din("ret_decay_fwd", [1, 4])
    rdb_d = din("ret_decay_bwd", [1, 4])
    qg_d = din("mla_q_norm_g", [1, 384])
    wuq_d = din("mla_w_uq", [1, 384, 1536])
    kvg_d = din("mla_kv_norm_g", [1, 256])
    wukv_d = din("mla_w_ukv", [1, 256, 2048])
    wbr_d = din("w_branch_ret", [1, 2048, D])
    wbm_d = din("w_branch_mla", [1, D, D])
    wo_d = din("w_out", [1, D, D])
    rw_d = din("router_w", [1, D, 32])
    rb_d = din("router_b", [1, 32])
    egu_d = din("exp_w_gu", [1, 32, D, 2 * D])
    ebgu_d = din("exp_b_gu", [1, 32, 2 * D])
    edn_d = din("exp_w_down", [1, 32, D, D])
    ebd_d = din("exp_b_down", [1, 32, D])
    fg_d = din("final_norm_g", [D])
    cst_d = din("consts", [128, 12, 128])
    rc_d = din("rope_rc", [LT, 256])
    rs_d = din("rope_rs", [LT, 256])
    mc_d = din("rope_mc", [64, LT])
    ms_d = din("rope_ms", [64, LT])
    out_d = nc.dram_tensor("out", [NB, L, D], F32, kind="ExternalOutput").ap()

    hT_d = dscr("hT_s", [NT, 128, 8, 128], BF16)
    att_d = dscr("att_s", [8, 128, L], BF16)
    rT_d = dscr("rT_s", [4, 4, 128, L], BF16)
    of_d = dscr("of_s", [32, 128, 512], F32)
    x1_d = dscr("x1_s", [NB * 32, 128, D], F32)
    NROWS = NB * L * 4 + 32 * 512
    NBLK = NROWS // 512
    h2_d = dscr("h2_s", [NB * 32, 128, D], BF16)
    xs_d = dscr("xs_s", [NROWS, D], BF16)
    ys_d = dscr("ys_s", [NROWS, D], F32)
    bias_d = dscr("bias_s", [32, 3072], BF16)
    hT_t = [Tk() for _ in range(NT)]
    att_t = [Tk() for _ in range(8)]
    rT_t = [Tk() for _ in range(8)]
    of_t = [Tk() for _ in range(32)]
    x1_t = [Tk() for _ in range(NB * 32)]
    out_t = Tk()

    with contextlib.ExitStack() as gstack:
        S = Sched(nc, gstack)

        uid = [0]

        def sb(stack, name, shape, dt):
            uid[0] += 1
            return stack.enter_context(nc.sbuf_tensor(f"{name}_u{uid[0]}", list(shape), dt))

        def ps(stack, name, shape, dt=F32):
            uid[0] += 1
            return stack.enter_context(nc.psum_tensor(f"{name}_u{uid[0]}", list(shape), dt))

        cst = sb(gstack, "cst", [128, 12, 128], F32)
        cst_t = Tk()
        S.dma(cst[:], cst_d[:, :, :], writes=[cst_t])
        identf = cst[:, 0, :]
        onesf = cst[:, 1, :]
        identb = sb(gstack, "identb", [128, 128], BF16)
        onesb = sb(gstack, "onesb", [128, 128], BF16)
        cb_t = Tk()
        S.op("dve", lambda e: e.tensor_copy(out=identb[:], in_=identf), reads=[cst_t], writes=[cb_t])
        S.op("dve", lambda e: e.tensor_copy(out=onesb[:], in_=onesf), reads=[cst_t], writes=[cb_t])

        mT = sb(gstack, "mT", [128, 48, 3], F32)
        mT_t = Tk()
        gs1 = sb(gstack, "gs1", [128, 3, 8], F32)
        gs2 = sb(gstack, "gs2", [128, 3, 8], F32)
        gs_t = Tk()
        mix_stack = contextlib.ExitStack()
        G2 = sb(gstack, "G2", [128, NB, D], F32)
        FG = sb(gstack, "FG", [128, D], F32)
        G_t = Tk()
        gq = sb(gstack, "gq", [128, 8], F32)
        gq_t = Tk()
        KD = sb(gstack, "KD", [128, 4, 8], F32)
        G1 = sb(mix_stack, "G1", [128, NB, D], F32)
        MTf = sb(mix_stack, "MTf", [128, 4, 128], F32)
        MTb = sb(mix_stack, "MTb", [128, 4, 128], F32)
        QDf = sb(mix_stack, "QDf", [128, 4, 128], F32)
        QDb = sb(mix_stack, "QDb", [128, 4, 128], F32)
        dec_t = Tk()

        def featmajor_load(stack, pst, pst_t, dst_ap, src_rows_ap, nrows, tmpname):
            tmp = sb(stack, tmpname, [nrows, 128], F32)
            tt = Tk()
            S.dma(tmp[:], src_rows_ap, writes=[tt])
            S.op("pe", lambda e: e.transpose(out=pst[:, 0:nrows], in_=tmp[:], identity=cst[0:nrows, 0, 0:nrows]),
                 reads=[tt, cst_t], writes=[pst_t])
            return tmp

        with contextlib.ExitStack() as st:
            pA = ps(st, "pA0", [128, 512])
            pA_t = Tk()
            pB = ps(st, "pB0", [128, 2, 512])
            pB_t = Tk()
            cv = sb(st, "cv", [3, D], F32)
            cvs = sb(st, "cvs", [3, D], F32)
            cv_t = Tk()
            S.dma(cv[:], cv_d[:, :], writes=[cv_t])
            cvs_t = Tk()
            S.op("act", lambda e: e.activation(out=cvs[:], in_=cv[:], func=AF.Silu), reads=[cv_t], writes=[cvs_t])
            sT = sb(st, "sT", [128, 8, 3], F32)
            sT_t = Tk()
            for kc in range(8):
                S.op("pe", lambda e: e.transpose(out=pA[:, kc * 4:kc * 4 + 3], in_=cvs[:, kc * 128:(kc + 1) * 128],
                                                 identity=cst[0:3, 0, 0:3]), reads=[cvs_t, cst_t], writes=[pA_t])
            for kc in range(8):
                S.op("dve", lambda e: e.tensor_copy(out=sT[:, kc, :], in_=pA[:, kc * 4:kc * 4 + 3]),
                     reads=[pA_t], writes=[sT_t])
            abT = sb(st, "abT", [128, 48], F32)
            g12 = sb(st, "g12", [128, 16], F32)
            ab_t = Tk()
            featmajor_load(st, pA, pA_t, None, adab_d[0, :].rearrange("(r p) -> r p", p=128), 48, "t_ab")
            S.op("dve", lambda e: e.tensor_copy(out=abT[:], in_=pA[:, 0:48]), reads=[pA_t], writes=[ab_t])
            featmajor_load(st, pA, pA_t, None, n1_d[0, :].rearrange("(r p) -> r p", p=128), 8, "t_n1")
            S.op("dve", lambda e: e.tensor_copy(out=g12[:, 0:8], in_=pA[:, 0:8]), reads=[pA_t], writes=[ab_t])
            featmajor_load(st, pA, pA_t, None, n2_d[0, :].rearrange("(r p) -> r p", p=128), 8, "t_n2")
            S.op("dve", lambda e: e.tensor_copy(out=g12[:, 8:16], in_=pA[:, 0:8]), reads=[pA_t], writes=[ab_t])
            featmajor_load(st, pA, pA_t, None, qg_d[0, :].rearrange("(r p) -> r p", p=128), 3, "t_qg")
            S.op("dve", lambda e: e.tensor_copy(out=gq[:, 0:3], in_=pA[:, 0:3]), reads=[pA_t], writes=[gq_t])
            featmajor_load(st, pA, pA_t, None, kvg_d[0, :].rearrange("(r p) -> r p", p=128), 2, "t_kvg")
            S.op("dve", lambda e: e.tensor_copy(out=gq[:, 4:6], in_=pA[:, 0:2]), reads=[pA_t], writes=[gq_t])
            fgT = sb(st, "fgT", [128, 8], F32)
            fg_t = Tk()
            featmajor_load(st, pA, pA_t, None, fg_d.rearrange("(r p) -> r p", p=128), 8, "t_fg")
            S.op("dve", lambda e: e.tensor_copy(out=fgT[:], in_=pA[:, 0:8]), reads=[pA_t], writes=[fg_t])
            awv = adaw_d[0].rearrange("(kc p) n -> p kc n", p=128)
            aw = [sb(st, f"aw{i}", [128, 8, 512], F32) for i in range(2)]
            aw_t = [Tk(), Tk()]
            pm = ps(st, "pm", [128, 48, 4])
            pm_t = Tk()
            for blk in range(12):
                w = aw[blk % 2]
                wt = aw_t[blk % 2]
                S.dma(w[:], awv[:, :, blk * 512:(blk + 1) * 512], writes=[wt])
                for jj in range(4):
                    j = blk * 4 + jj
                    for kc in range(8):
                        S.op("pe", lambda e: e.matmul(out=pm[:, j, 0:3], lhsT=w[:, kc, jj * 128:(jj + 1) * 128],
                                                      rhs=sT[:, kc, :], start=(kc == 0), stop=(kc == 7)),
                             reads=[wt, sT_t], writes=[pm_t], signal=(kc == 7))
            for r in range(3):
                S.op("dve", lambda e: e.tensor_tensor(out=mT[:, :, r], in0=pm[:, :, r], in1=abT[:], op=ALU.add),
                     reads=[pm_t, ab_t], writes=[mT_t])
            for r in range(3):
                S.op("dve", lambda e: e.scalar_tensor_tensor(out=gs1[:, r, :], in0=mT[:, 8:16, r], scalar=1.0,
                                                             in1=g12[:, 0:8], op0=ALU.add, op1=ALU.mult),
                     reads=[mT_t, ab_t], writes=[gs_t])
                S.op("dve", lambda e: e.scalar_tensor_tensor(out=gs2[:, r, :], in0=mT[:, 32:40, r], scalar=1.0,
                                                             in1=g12[:, 8:16], op0=ALU.add, op1=ALU.mult),
                     reads=[mT_t, ab_t], writes=[gs_t])
            dg = sb(st, "dg", [128, 8, 128], F32)
            dg_t = Tk()

            def bcast_tile(dst_ap, vec_fn):
                for c in range(8):
                    S.op("dve", lambda e: e.tensor_scalar(out=dg[:, c, :], in0=identf, scalar1=vec_fn(c), scalar2=None,
                                                          op0=ALU.mult), reads=[cst_t, mT_t, fg_t], writes=[dg_t])
                for c in range(8):
                    S.op("pe", lambda e: e.matmul(out=pB[:, c // 4, (c % 4) * 128:(c % 4 + 1) * 128], lhsT=onesf,
                                                  rhs=dg[:, c, :], start=True, stop=True),
                         reads=[dg_t, cst_t], writes=[pB_t])
                S.op("act", lambda e: e.copy(out=dst_ap, in_=pB[:].rearrange("p a b -> p (a b)")),
                     reads=[pB_t], writes=[G_t])

            for b in range(NB):
                bcast_tile(G1[:, b, :], lambda c: mT[:, 16 + c, b:b + 1])
                bcast_tile(G2[:, b, :], lambda c: mT[:, 40 + c, b:b + 1])
            bcast_tile(FG[:], lambda c: fgT[:, c:c + 1])

            rd = sb(st, "rd", [1, 8], F32)
            rd_t = Tk()
            S.dma(rd[:, 0:4], rdf_d[:, :], writes=[rd_t])
            S.dma(rd[:, 4:8], rdb_d[:, :], writes=[rd_t])
            S.op("pe", lambda e: e.matmul(out=pA[:, 0:8], lhsT=cst[0:1, 1, :], rhs=rd[:], start=True, stop=True),
                 reads=[rd_t, cst_t], writes=[pA_t])
            lg = sb(st, "lg", [128, 8], F32)
            lg_t = Tk()
            S.op("act", lambda e: e.activation(out=lg[:], in_=pA[:, 0:8], func=AF.Exp, scale=-1.0),
                 reads=[pA_t], writes=[lg_t])
            S.op("act", lambda e: e.activation(out=lg[:], in_=lg[:], func=AF.Ln, bias=1.0, scale=1.0),
                 reads=[lg_t], writes=[lg_t])
            S.op("dve", lambda e: e.tensor_scalar(out=lg[:], in0=lg[:], scalar1=-1.0, scalar2=None, op0=ALU.mult),
                 reads=[lg_t], writes=[lg_t])
            tmpd = sb(st, "tmpd", [128, 128], F32)
            tmpd_t = Tk()
            for h in range(4):
                for (dst, dtab, mtab, col) in ((MTf, 2, 3, h), (MTb, 4, 5, 4 + h)):
                    S.op("act", lambda e: e.activation(out=tmpd[:], in_=cst[:, dtab, :], func=AF.Exp,
                                                       scale=lg[:, col:col + 1]),
                         reads=[cst_t, lg_t], writes=[tmpd_t])
                    S.op("dve", lambda e: e.tensor_tensor(out=dst[:, h, :], in0=tmpd[:], in1=cst[:, mtab, :],
                                                          op=ALU.mult), reads=[tmpd_t, cst_t], writes=[dec_t])
                S.op("act", lambda e: e.activation(out=QDf[:, h, :], in_=cst[:, 6, :], func=AF.Exp,
                                                   scale=lg[:, h:h + 1]), reads=[cst_t, lg_t], writes=[dec_t])
                S.op("act", lambda e: e.activation(out=QDb[:, h, :], in_=cst[:, 7, :], func=AF.Exp,
                                                   scale=lg[:, 4 + h:5 + h]), reads=[cst_t, lg_t], writes=[dec_t])
                S.op("act", lambda e: e.activation(out=KD[:, h, 0:1], in_=cst[:, 8, 0:1], func=AF.Exp,
                                                   scale=lg[:, h:h + 1]), reads=[cst_t, lg_t], writes=[dec_t])
                S.op("act", lambda e: e.activation(out=KD[:, h, 1:2], in_=cst[:, 8, 1:2], func=AF.Exp,
                                                   scale=lg[:, 4 + h:5 + h]), reads=[cst_t, lg_t], writes=[dec_t])
                S.op("act", lambda e: e.activation(out=KD[:, h, 2:3], in_=cst[:, 8, 2:3], func=AF.Exp,
                                                   scale=lg[:, h:h + 1]), reads=[cst_t, lg_t], writes=[dec_t])
                S.op("act", lambda e: e.activation(out=KD[:, h, 3:4], in_=cst[:, 8, 2:3], func=AF.Exp,
                                                   scale=lg[:, 4 + h:5 + h]), reads=[cst_t, lg_t], writes=[dec_t])
            S.barrier()

        def load_cast(stack_stage, dst, dst_t, src_ap, shape, stg, stg_t, eng, scale=None, dst_view=None):
            S.dma(stg, src_ap, writes=[stg_t])
            dv = dst if dst_view is None else dst_view
            if scale is None:
                S.op(eng, lambda e: e.tensor_copy(out=dv, in_=stg), reads=[stg_t], writes=[dst_t])
            else:
                S.op(eng, lambda e: e.tensor_scalar(out=dv, in0=stg, scalar1=scale, scalar2=None, op0=ALU.mult),
                     reads=[stg_t], writes=[dst_t])

        def norm_T(stack, pfx, src_tile, src_t, gs_ap, sh_ap, pT, pT_t, hTo, hTo_t, scr, ssb, tmp_t, xn, xn_t,
                   f32_out=None, f32_t=None):
            S.op("pool", lambda e: e.memset(ssb[:, 0:1], 0.0), writes=[tmp_t])
            S.op("act", lambda e: e.activation(out=scr[:], in_=src_tile, func=AF.Square, accum_out=ssb[:, 0:1]),
                 reads=[src_t, tmp_t], writes=[tmp_t])
            S.op("act", lambda e: e.activation(out=ssb[:, 1:2], in_=ssb[:, 0:1], func=AF.Sqrt, scale=1.0 / D, bias=EPS),
                 reads=[tmp_t], writes=[tmp_t])
            S.op("dve", lambda e: e.reciprocal(out=ssb[:, 2:3], in_=ssb[:, 1:2]), reads=[tmp_t], writes=[tmp_t])
            S.op("dve", lambda e: e.tensor_scalar(out=xn[:], in0=src_tile, scalar1=ssb[:, 2:3], scalar2=None,
                                                  op0=ALU.mult), reads=[src_t, tmp_t], writes=[xn_t])
            for c in range(8):
                S.op("pe", lambda e: e.transpose(out=pT[c // 4][:, (c % 4) * 128:(c % 4 + 1) * 128],
                                                 in_=xn[:, c * 128:(c + 1) * 128], identity=identf),
                     reads=[xn_t, cst_t], writes=[pT_t[c // 4]], signal=(c % 4 == 3))
            for c in range(8):
                src = pT[c // 4][:, (c % 4) * 128:(c % 4 + 1) * 128]
                if f32_out is not None:
                    S.op("dve", lambda e: e.tensor_scalar(out=f32_out[:, c, :], in0=src, scalar1=gs_ap[:, c:c + 1],
                                                          scalar2=sh_ap(c), op0=ALU.mult, op1=ALU.add),
                         reads=[pT_t[c // 4], gs_t, mT_t], writes=[f32_t])
                    S.op("pool", lambda e: e.tensor_copy(out=hTo[:, c, :], in_=f32_out[:, c, :]),
                         reads=[f32_t], writes=[hTo_t])
                else:
                    S.op("dve", lambda e: e.tensor_scalar(out=hTo[:, c, :], in0=src, scalar1=gs_ap[:, c:c + 1],
                                                          scalar2=sh_ap(c), op0=ALU.mult, op1=ALU.add),
                         reads=[pT_t[c // 4], gs_t, mT_t], writes=[hTo_t])

        winv = win_d[0].rearrange("(kc p) n -> p kc n", p=128)

        for b in range(NB):
            with contextlib.ExitStack() as st:
                xt = [sb(st, f"s0x{i}", [128, D], F32) for i in range(2)]
                xt_t = [Tk(), Tk()]
                xn = sb(st, "s0xn", [128, D], F32)
                xn_t = Tk()
                scr = sb(st, "s0scr", [128, D], BF16)
                ssb = sb(st, "s0ss", [128, 4], F32)
                tmp_t = Tk()
                hTo = [sb(st, f"s0h{i}", [128, 8, 128], BF16) for i in range(2)]
                hTo_t = [Tk(), Tk()]
                pT = [ps(st, f"s0p{i}", [128, 512]) for i in range(2)]
                pT_t = [Tk(), Tk()]

                def s0_load(t):
                    src = ctx_d[b, t * 128:(t + 1) * 128, :] if t < 2 else x_d[b, (t - 2) * 128:(t - 1) * 128, :]
                    S.dma(xt[t % 2][:], src, writes=[xt_t[t % 2]])

                s0_load(0)
                for t in range(NT):
                    if t + 1 < NT:
                        s0_load(t + 1)
                    r = 2 if t < 2 else b
                    norm_T(st, "s0", xt[t % 2][:], xt_t[t % 2], gs1[:, r, :], lambda c: mT[:, c, r:r + 1],
                           pT, pT_t, hTo[t % 2], hTo_t[t % 2], scr, ssb, tmp_t, xn, xn_t)
                    S.dma(hT_d[t], hTo[t % 2][:], reads=[hTo_t[t % 2]], writes=[hT_t[t]])
                S.barrier()

            with contextlib.ExitStack() as st:
                cqn = sb(st, "cqn", [128, 3, L], BF16)
                cqn_t = Tk()
                ckvn = sb(st, "ckvn", [128, 2, LT], BF16)
                ckvn_t = Tk()
                krT = sb(st, "krT", [64, LT], BF16)
                krT_t = Tk()
                with contextlib.ExitStack() as s1:
                    Wm = sb(s1, "Wm", [128, 8, 768], BF16)
                    Wm_t = Tk()
                    stg = sb(s1, "s1stg", [128, 8, 704], F32)
                    stg_t = Tk()
                    S.dma(stg[:], winv[:, :, 6144:6848], writes=[stg_t])
                    S.op("dve", lambda e: e.tensor_copy(out=Wm[:, :, 0:704], in_=stg[:]), reads=[stg_t], writes=[Wm_t])
                    for (dst, src) in ((704, 656), (720, 640), (736, 688), (752, 672)):
                        S.op("dve", lambda e: e.tensor_copy(out=Wm[:, :, dst:dst + 16], in_=stg[:, :, src:src + 16]),
                             reads=[stg_t], writes=[Wm_t])
                    hb = [sb(s1, f"s1h{i}", [128, 8, 512], BF16) for i in range(2)]
                    hb_t = [Tk(), Tk()]
                    tc_ = [sb(s1, f"s1tc{i}", [64, 2, 512], F32) for i in range(2)]
                    tc_t = [Tk(), Tk()]
                    pq = [ps(s1, f"s1pq{i}", [128, 512]) for i in range(3)]
                    pq_t = [Tk() for _ in range(3)]
                    pk = [ps(s1, f"s1pk{i}", [128, 512]) for i in range(2)]
                    pk_t = [Tk() for _ in range(2)]
                    pr = [ps(s1, f"s1pr{i}", [128, 512]) for i in range(2)]
                    pr_t = [Tk() for _ in range(2)]
                    pss = ps(s1, "s1pss", [128, 512])
                    pss_t = Tk()
                    xf = sb(s1, "s1xf", [128, 3, 512], F32)
                    xf_t = Tk()
                    sq = sb(s1, "s1sq", [128, 3, 512], F32)
                    sq_t = Tk()
                    rstd = sb(s1, "s1rstd", [128, 512], F32)
                    rstd_t = Tk()
                    r1 = sb(s1, "s1r1", [64, 512], F32)
                    r2 = sb(s1, "s1r2", [64, 512], F32)
                    r_t = Tk()

                    def blk_range(j):
                        return (0, 256) if j == 0 else (256 + (j - 1) * 512, 512)

                    def s1_load(j):
                        t0, n = blk_range(j)
                        for i in range(n // 128):
                            S.dma(hb[j % 2][:, :, i * 128:(i + 1) * 128], hT_d[t0 // 128 + i],
                                  reads=[hT_t[t0 // 128 + i]], writes=[hb_t[j % 2]])
                        S.dma(tc_[j % 2][:, 0, 0:n], mc_d[:, t0:t0 + n], writes=[tc_t[j % 2]])
                        S.dma(tc_[j % 2][:, 1, 0:n], ms_d[:, t0:t0 + n], writes=[tc_t[j % 2]])

                    def rms_T(pl, pl_t, nch, rank, gcol, dst, dst_t, dcol0, n):
                        for c in range(nch):
                            S.op("act", lambda e: e.copy(out=xf[:, c, 0:n], in_=pl[c][:, 0:n]),
                                 reads=[pl_t[c]], writes=[xf_t])
                            S.op("act", lambda e: e.activation(out=sq[:, c, 0:n], in_=pl[c][:, 0:n], func=AF.Square),
                                 reads=[pl_t[c]], writes=[sq_t])
                        for c in range(nch):
                            S.op("pe", lambda e: e.matmul(out=pss[:, 0:n], lhsT=onesf, rhs=sq[:, c, 0:n],
                                                          start=(c == 0), stop=(c == nch - 1)),
                                 reads=[sq_t, cst_t], writes=[pss_t], signal=(c == nch - 1))
                        S.op("act", lambda e: e.activation(out=rstd[:, 0:n], in_=pss[:, 0:n], func=AF.Sqrt,
                                                           scale=1.0 / rank, bias=EPS), reads=[pss_t], writes=[rstd_t])
                        S.op("dve", lambda e: e.reciprocal(out=rstd[:, 0:n], in_=rstd[:, 0:n]),
                             reads=[rstd_t], writes=[rstd_t])
                        for c in range(nch):
                            S.op("dve", lambda e: e.scalar_tensor_tensor(
                                out=dst[:, c, dcol0:dcol0 + n], in0=xf[:, c, 0:n], scalar=gq[:, gcol + c:gcol + c + 1],
                                in1=rstd[:, 0:n], op0=ALU.mult, op1=ALU.mult),
                                 reads=[xf_t, rstd_t, gq_t], writes=[dst_t])

                    s1_load(0)
                    for j in range(9):
                        if j + 1 < 9:
                            s1_load(j + 1)
                        t0, n = blk_range(j)
                        h_, h_t = hb[j % 2], hb_t[j % 2]
                        if j > 0:
                            for c in range(3):
                                for kc in range(8):
                                    S.op("pe", lambda e: e.matmul(out=pq[c][:, 0:n], lhsT=Wm[:, kc, c * 128:(c + 1) * 128],
                                                                  rhs=h_[:, kc, 0:n], start=(kc == 0), stop=(kc == 7)),
                                         reads=[Wm_t, h_t], writes=[pq_t[c]], signal=(kc == 7))
                        for c in range(2):
                            for kc in range(8):
                                S.op("pe", lambda e: e.matmul(out=pk[c][:, 0:n], lhsT=Wm[:, kc, 384 + c * 128:384 + (c + 1) * 128],
                                                              rhs=h_[:, kc, 0:n], start=(kc == 0), stop=(kc == 7)),
                                     reads=[Wm_t, h_t], writes=[pk_t[c]], signal=(kc == 7))
                        for c in range(2):
                            for kc in range(8):
                                S.op("pe", lambda e: e.matmul(out=pr[c][0:64, 0:n], lhsT=Wm[:, kc, 640 + c * 64:704 + c * 64],
                                                              rhs=h_[:, kc, 0:n], start=(kc == 0), stop=(kc == 7)),
                                     reads=[Wm_t, h_t], writes=[pr_t[c]], signal=(kc == 7))
                        if j > 0:
                            rms_T(pq, pq_t, 3, 384, 0, cqn, cqn_t, t0 - 256, n)
                        rms_T(pk, pk_t, 2, 256, 4, ckvn, ckvn_t, t0, n)
                        tcc, tcc_t = tc_[j % 2], tc_t[j % 2]
                        S.op("dve", lambda e: e.tensor_tensor(out=r1[:, 0:n], in0=pr[0][0:64, 0:n], in1=tcc[:, 0, 0:n],
                                                              op=ALU.mult), reads=[pr_t[0], tcc_t], writes=[r_t])
                        S.op("dve", lambda e: e.tensor_tensor(out=r2[:, 0:n], in0=pr[1][0:64, 0:n], in1=tcc[:, 1, 0:n],
                                                              op=ALU.mult), reads=[pr_t[1], tcc_t], writes=[r_t])
                        S.op("dve", lambda e: e.tensor_tensor(out=krT[:, t0:t0 + n], in0=r1[:, 0:n], in1=r2[:, 0:n],
                                                              op=ALU.add), reads=[r_t], writes=[krT_t])
                    S.barrier()

                with contextlib.ExitStack() as s2:
                    KT = sb(s2, "KT", [128, LT], BF16)
                    KT_t = Tk()
                    V = sb(s2, "V", [128, NT, 128], BF16)
                    V_t = Tk()
                    QT = sb(s2, "QT", [128, L], BF16)
                    QT_t = Tk()
                    QrT = sb(s2, "QrT", [64, L], BF16)
                    QrT_t = Tk()
                    Wq = sb(s2, "Wq", [128, 3, 256], BF16)
                    Wq_t = Tk()
                    Wkv = sb(s2, "Wkv", [128, 2, 256], BF16)
                    Wkv_t = Tk()
                    sq_ = sb(s2, "s2sq", [128, 3, 192], F32)
                    sq_t = Tk()
                    skv = sb(s2, "s2skv", [128, 2, 256], F32)
                    skv_t = Tk()
                    tq = [sb(s2, f"s2tq{i}", [64, 2, 512], F32) for i in range(2)]
                    tq_t = [Tk(), Tk()]
                    r1 = sb(s2, "s2r1", [64, 512], F32)
                    r2 = sb(s2, "s2r2", [64, 512], F32)
                    r_t = Tk()
                    PT = [sb(s2, f"PT{i}", [128, 512], BF16) for i in range(4)]
                    PT_t = [Tk() for _ in range(4)]
                    accA = [sb(s2, f"s2accA{i}", [128, 512], F32) for i in range(2)]
                    accB = [sb(s2, f"s2accB{i}", [128, 512], F32) for i in range(2)]
                    accA_t = [Tk(), Tk()]
                    accB_t = [Tk(), Tk()]
                    rs = sb(s2, "s2rs", [128, 512], F32)
                    rs_t = Tk()
                    ot = [sb(s2, f"s2ot{i}", [128, 512], BF16) for i in range(2)]
                    ot_t = [Tk(), Tk()]
                    pS = [ps(s2, f"pS{i}", [128, 512]) for i in range(3)]
                    pS_t = [Tk() for _ in range(3)]
                    pO = [ps(s2, f"pO{i}", [128, 512]) for i in range(2)]
                    pO_t = [Tk() for _ in range(2)]
                    pZ = [ps(s2, f"pZ{i}", [128, 512]) for i in range(2)]
                    pZ_t = [Tk() for _ in range(2)]
                    pX = ps(s2, "pX", [128, 512])
                    pX_t = Tk()
                    wuqv = wuq_d[0].rearrange("(kc p) n -> p kc n", p=128)
                    wukvv = wukv_d[0].rearrange("(kc p) n -> p kc n", p=128)
                    sc = 192.0 ** -0.5
                    cnt = 0
                    for h in range(8):
                        S.dma(sq_[:], wuqv[:, :, h * 192:(h + 1) * 192], writes=[sq_t])
                        S.op("pool", lambda e: e.tensor_copy(out=Wq[:, :, 0:192], in_=sq_[:]), reads=[sq_t], writes=[Wq_t])
                        for (dst, src) in ((192, 144), (208, 128), (224, 176), (240, 160)):
                            S.op("pool", lambda e: e.tensor_copy(out=Wq[:, :, dst:dst + 16], in_=sq_[:, :, src:src + 16]),
                                 reads=[sq_t], writes=[Wq_t])
                        S.dma(skv[:], wukvv[:, :, h * 256:(h + 1) * 256], writes=[skv_t])
                        S.op("pool", lambda e: e.tensor_copy(out=Wkv[:], in_=skv[:]), reads=[skv_t], writes=[Wkv_t])
                        for j in range(9):
                            t0, n = (0, 256) if j == 0 else (256 + (j - 1) * 512, 512)
                            for kc in range(2):
                                S.op("pe", lambda e: e.matmul(out=pX[:, 0:n], lhsT=Wkv[:, kc, 0:128], rhs=ckvn[:, kc, t0:t0 + n],
                                                              start=(kc == 0), stop=(kc == 1)),
                                     reads=[Wkv_t, ckvn_t], writes=[pX_t], signal=(kc == 1))
                            S.op("act" if j % 2 else "dve", (lambda e: e.copy(out=KT[:, t0:t0 + n], in_=pX[:, 0:n])) if j % 2
                                 else (lambda e: e.tensor_copy(out=KT[:, t0:t0 + n], in_=pX[:, 0:n])),
                                 reads=[pX_t], writes=[KT_t])
                        for g in range(9):
                            tiles = list(range(g * 4, min(g * 4 + 4, NT)))
                            for i, t in enumerate(tiles):
                                for kc in range(2):
                                    S.op("pe", lambda e: e.matmul(out=pX[:, i * 128:(i + 1) * 128],
                                                                  lhsT=ckvn[:, kc, t * 128:(t + 1) * 128], rhs=Wkv[:, kc, 128:256],
                                                                  start=(kc == 0), stop=(kc == 1)),
                                         reads=[Wkv_t, ckvn_t], writes=[pX_t], signal=(kc == 1 and i == len(tiles) - 1))
                            nn = len(tiles)
                            S.op("act" if g % 2 else "dve",
                                 (lambda e: e.copy(out=V[:, tiles[0]:tiles[0] + nn, :].rearrange("p a b -> p (a b)"),
                                                   in_=pX[:, 0:nn * 128])) if g % 2 else
                                 (lambda e: e.tensor_copy(out=V[:, tiles[0]:tiles[0] + nn, :].rearrange("p a b -> p (a b)"),
                                                          in_=pX[:, 0:nn * 128])),
                                 reads=[pX_t], writes=[V_t])
                        for j in range(8):
                            q0 = j * 512
                            S.dma(tq[j % 2][:, 0, :], mc_d[:, 256 + q0:256 + q0 + 512], writes=[tq_t[j % 2]])
                            S.dma(tq[j % 2][:, 1, :], ms_d[:, 256 + q0:256 + q0 + 512], writes=[tq_t[j % 2]])
                            for kc in range(3):
                                S.op("pe", lambda e: e.matmul(out=pX[:], lhsT=Wq[:, kc, 0:128], rhs=cqn[:, kc, q0:q0 + 512],
                                                              start=(kc == 0), stop=(kc == 2)),
                                     reads=[Wq_t, cqn_t], writes=[pX_t], signal=(kc == 2))
                            S.op("act", lambda e: e.copy(out=QT[:, q0:q0 + 512], in_=pX[:]), reads=[pX_t], writes=[QT_t])
                            for c in range(2):
                                for kc in range(3):
                                    S.op("pe", lambda e: e.matmul(out=pZ[c][0:64, :], lhsT=Wq[:, kc, 128 + c * 64:192 + c * 64],
                                                                  rhs=cqn[:, kc, q0:q0 + 512], start=(kc == 0), stop=(kc == 2)),
                                         reads=[Wq_t, cqn_t], writes=[pZ_t[c]], signal=(kc == 2))
                            tt_, tt_t = tq[j % 2], tq_t[j % 2]
                            S.op("dve", lambda e: e.tensor_tensor(out=r1[:], in0=pZ[0][0:64, :], in1=tt_[:, 0, :], op=ALU.mult),
                                 reads=[pZ_t[0], tt_t], writes=[r_t])
                            S.op("dve", lambda e: e.tensor_tensor(out=r2[:], in0=pZ[1][0:64, :], in1=tt_[:, 1, :], op=ALU.mult),
                                 reads=[pZ_t[1], tt_t], writes=[r_t])
                            S.op("dve", lambda e: e.tensor_tensor(out=QrT[:, q0:q0 + 512], in0=r1[:], in1=r2[:], op=ALU.add),
                                 reads=[r_t], writes=[QrT_t])
                        for qb in range(8):
                            q0 = qb * 512
                            po, po_t = pO[qb % 2], pO_t[qb % 2]
                            pz, pz_t = pZ[qb % 2], pZ_t[qb % 2]
                            def emit_st(kt, cn):
                                s_, s_t = pS[cn % 3], pS_t[cn % 3]
                                S.op("pe", lambda e: e.matmul(out=s_[:], lhsT=KT[:, kt * 128:(kt + 1) * 128], rhs=QT[:, q0:q0 + 512],
                                                              start=True, stop=False), reads=[KT_t, QT_t], writes=[s_t], signal=False)
                                S.op("pe", lambda e: e.matmul(out=s_[:], lhsT=krT[:, kt * 128:(kt + 1) * 128], rhs=QrT[:, q0:q0 + 512],
                                                              start=False, stop=True), reads=[krT_t, QrT_t], writes=[s_t])

                            emit_st(0, cnt)
                            for kt in range(NT):
                                s_, s_t = pS[cnt % 3], pS_t[cnt % 3]
                                p_, p_t = PT[cnt % 4], PT_t[cnt % 4]
                                if kt + 1 < NT:
                                    emit_st(kt + 1, cnt + 1)
                                cnt += 1
                                S.op("act", lambda e: e.activation(out=p_[:], in_=s_[:], func=AF.Exp, scale=sc),
                                     reads=[s_t], writes=[p_t])
                                S.op("pe", lambda e: e.matmul(out=po[:], lhsT=V[:, kt, :], rhs=p_[:], start=(kt == 0),
                                                              stop=(kt == NT - 1)), reads=[V_t, p_t], writes=[po_t])
                                aa, aa_t = accA[qb % 2], accA_t[qb % 2]
                                if kt == 0:
                                    S.op("dve", lambda e: e.tensor_copy(out=aa[:], in_=p_[:]), reads=[p_t], writes=[aa_t])
                                else:
                                    S.op("dve", lambda e: e.tensor_tensor(out=aa[:], in0=aa[:], in1=p_[:], op=ALU.add),
                                         reads=[p_t, aa_t], writes=[aa_t])
                            S.op("pe", lambda e: e.matmul(out=pz[:], lhsT=onesf, rhs=accA[qb % 2][:], start=True, stop=True),
                                 reads=[cst_t, accA_t[qb % 2]], writes=[pz_t])
                            S.op("dve", lambda e: e.reciprocal(out=rs[:], in_=pz[:]), reads=[pz_t], writes=[rs_t])
                            o_, o_t = ot[qb % 2], ot_t[qb % 2]
                            S.op("dve", lambda e: e.tensor_tensor(out=o_[:], in0=po[:], in1=rs[:], op=ALU.mult),
                                 reads=[po_t, rs_t], writes=[o_t])
                            S.dma(att_d[h, :, q0:q0 + 512], o_[:], reads=[o_t], writes=[att_t[qb]])
                    S.barrier()

            with contextlib.ExitStack() as st:
                Wqk = sb(st, "Wqk", [128, 8, 512], BF16)
                Wv = sb(st, "Wv", [128, 8, 512], BF16)
                Wg = sb(st, "Wg", [128, 8, 512], BF16)
                Wqk_t, Wv_t, Wg_t = Tk(), Tk(), Tk()
                stg = sb(st, "s3stg", [128, 8, 512], F32)
                stg_t = Tk()
                hb = [sb(st, f"s3h{i}", [128, 8, 128], BF16) for i in range(3)]
                hb_t = [Tk() for _ in range(3)]
                tb = [sb(st, f"s3tb{i}", [128, 2, 256], F32) for i in range(3)]
                tb_t = [Tk() for _ in range(3)]
                ofb = [sb(st, f"s3of{i}", [128, 512], F32) for i in range(3)]
                ofb_t = [Tk() for _ in range(3)]
                Sf = sb(st, "Sf", [128, 2, 512], F32)
                Sb_ = sb(st, "Sb", [128, 2, 512], BF16)
                Sf_t = Tk()
                Sb_t = Tk()
                t1 = sb(st, "s3t1", [128, 512], F32)
                t2 = sb(st, "s3t2", [128, 512], F32)
                t12_t = Tk()
                qk_all = sb(st, "s3qkall", [128, NT, 512], BF16)
                qka_t = [Tk() for _ in range(NT)]
                V_all = sb(st, "s3Vall", [128, NT, 512], BF16)
                Va_t = [Tk() for _ in range(NT)]
                Kdb = [sb(st, f"s3Kd{i}", [128, 256], BF16) for i in range(2)]
                Kdb_t = [Tk(), Tk()]
                KTt = sb(st, "s3KT", [128, 2, 128], BF16)
                QTt = sb(st, "s3QT", [128, 2, 128], BF16)
                QdT = sb(st, "s3QdT", [128, 2, 128], BF16)
                tr_t = Tk()
                STm = sb(st, "s3STm", [128, 128], BF16)
                STm_t = Tk()
                osb = sb(st, "s3o", [128, 512], F32)
                osb_t = Tk()
                sg = sb(st, "s3sg", [128, 512], F32)
                sg_t = Tk()
                scr = sb(st, "s3scr", [128, 512], BF16)
                ssb = sb(st, "s3ss", [128, 4], F32)
                ss_t = Tk()
                rr = sb(st, "s3r", [128, 512], BF16)
                rr_t = Tk()
                rTs = [sb(st, f"s3rT{i}", [128, 4, 128], BF16) for i in range(2)]
                rTs_t = [Tk(), Tk()]
                pAl = [ps(st, f"s3pA{i}", [128, 512]) for i in range(2)]
                pAl_t = [Tk(), Tk()]
                pBl = [ps(st, f"s3pB{i}", [128, 512]) for i in range(2)]
                pBl_t = [Tk(), Tk()]
                pC = ps(st, "s3pC", [128, 512])
                pD = ps(st, "s3pD", [128, 2, 512], BF16)
                pE = ps(st, "s3pE", [128, 512])
                pF = ps(st, "s3pF", [128, 512])
                pC_t, pD_t, pD2_t, pE_t, pF_t = (Tk() for _ in range(5))
                pH = [pC, pE]
                pH_t = [pC_t, pE_t]
                for h in range(4):
                    for (dst, c0, scl) in ((Wqk[:, :, 0:256], h * 256, None), (Wqk[:, :, 256:512], 1024 + h * 256, 0.0625)):
                        S.dma(stg[:, :, 0:256], winv[:, :, c0:c0 + 256], writes=[stg_t])
                        if scl is None:
                            S.op("act", lambda e: e.copy(out=dst, in_=stg[:, :, 0:256]), reads=[stg_t], writes=[Wqk_t])
                        else:
                            S.op("act", lambda e: e.mul(out=dst, in_=stg[:, :, 0:256], mul=scl), reads=[stg_t], writes=[Wqk_t])
                    for (dst, c0, wt_) in ((Wv, 2048 + h * 512, Wv_t), (Wg, 4096 + h * 512, Wg_t)):
                        S.dma(stg[:], winv[:, :, c0:c0 + 512], writes=[stg_t])
                        S.op("act", lambda e: e.copy(out=dst[:], in_=stg[:]), reads=[stg_t], writes=[wt_])
                    for di, dirn in enumerate(("f", "b")):
                        order = list(range(NT)) if dirn == "f" else [1, 0] + list(range(NT - 1, 1, -1))
                        MT = MTf if dirn == "f" else MTb
                        QD = QDf if dirn == "f" else QDb
                        kdc = KD[:, h, di:di + 1]
                        cdc = KD[:, h, 2 + di:3 + di]
                        S.op("pool", lambda e: e.memset(Sf[:], 0.0), writes=[Sf_t])
                        S.op("pool", lambda e: e.memset(Sb_[:], 0.0), writes=[Sb_t])

                        def s3_load(idx):
                            t = order[idx]
                            S.dma(hb[idx % 3][:], hT_d[t], reads=[hT_t[t]], writes=[hb_t[idx % 3]])
                            if dirn == "f":
                                S.dma(tb[idx % 3][:, 0, :], rc_d[t * 128:(t + 1) * 128, :], writes=[tb_t[idx % 3]])
                                S.dma(tb[idx % 3][:, 1, :], rs_d[t * 128:(t + 1) * 128, :], writes=[tb_t[idx % 3]])
                            if dirn == "b" and t >= 2:
                                S.dma(ofb[idx % 3][:], of_d[t - 2], reads=[of_t[t - 2]], writes=[ofb_t[idx % 3]])

                        def s3_proj(idx):
                            if dirn == "b":
                                return
                            h_, h_t = hb[idx % 3], hb_t[idx % 3]
                            pA, pA_t = pAl[idx % 2], pAl_t[idx % 2]
                            pB, pB_t = pBl[idx % 2], pBl_t[idx % 2]
                            for kc in range(8):
                                S.op("pe", lambda e: e.matmul(out=pA[:], lhsT=h_[:, kc, :], rhs=Wqk[:, kc, :], start=(kc == 0),
                                                              stop=(kc == 7)), reads=[h_t, Wqk_t], writes=[pA_t], signal=(kc == 7))
                            for kc in range(8):
                                S.op("pe", lambda e: e.matmul(out=pB[:], lhsT=h_[:, kc, :], rhs=Wv[:, kc, :], start=(kc == 0),
                                                              stop=(kc == 7)), reads=[h_t, Wv_t], writes=[pB_t], signal=(kc == 7))

                        def s3_A(idx):
                            t = order[idx]
                            tb_, tbt = tb[idx % 3], tb_t[idx % 3]
                            pA, pA_t = pAl[idx % 2], pAl_t[idx % 2]
                            pB, pB_t = pBl[idx % 2], pBl_t[idx % 2]
                            qk, qk_t = qk_all[:, t, :], qka_t[t]
                            Vb, Vb_t = V_all[:, t, :], Va_t[t]
                            Kd, Kd_t = Kdb[idx % 2], Kdb_t[idx % 2]
                            for half in (range(2) if dirn == "f" else ()):
                                o = half * 256
                                S.op("dve", lambda e: e.tensor_tensor(out=t1[:, o:o + 256], in0=pA[:, o:o + 256], in1=tb_[:, 0, :],
                                                                      op=ALU.mult), reads=[pA_t, tbt], writes=[t12_t])
                                for part in range(2):
                                    a = o + part * 128
                                    S.op("dve", lambda e: e.tensor_tensor(out=t2[:, a:a + 64], in0=pA[:, a + 64:a + 128],
                                                                          in1=tb_[:, 1, part * 128:part * 128 + 64], op=ALU.mult),
                                         reads=[pA_t, tbt], writes=[t12_t])
                                    S.op("dve", lambda e: e.tensor_tensor(out=t2[:, a + 64:a + 128], in0=pA[:, a:a + 64],
                                                                          in1=tb_[:, 1, part * 128 + 64:part * 128 + 128], op=ALU.mult),
                                         reads=[pA_t, tbt], writes=[t12_t])
                            if dirn == "f":
                                S.op("pool", lambda e: e.tensor_tensor(out=qk[:], in0=t1[:], in1=t2[:], op=ALU.add),
                                     reads=[t12_t], writes=[qk_t])
                                S.op("act", lambda e: e.copy(out=Vb[:], in_=pB[:]), reads=[pB_t], writes=[Vb_t])
                            S.op("pool", lambda e: e.tensor_scalar(out=Kd[:], in0=qk[:, 256:512], scalar1=kdc, scalar2=None, op0=ALU.mult),
                                 reads=[qk_t, dec_t], writes=[Kd_t])

                        s3_load(0)
                        s3_load(1)
                        s3_proj(0)
                        s3_A(0)
                        for idx in range(NT):
                            if idx + 2 < NT:
                                s3_load(idx + 2)
                            if idx + 1 < NT:
                                s3_proj(idx + 1)
                                s3_A(idx + 1)
                            t = order[idx]
                            lat = t >= 2
                            h_, h_t = hb[idx % 3], hb_t[idx % 3]
                            tb_, tbt = tb[idx % 3], tb_t[idx % 3]
                            pA, pA_t = pAl[idx % 2], pAl_t[idx % 2]
                            pB, pB_t = pBl[idx % 2], pBl_t[idx % 2]
                            qk, qk_t = qk_all[:, t, :], qka_t[t]
                            Vb, Vb_t = V_all[:, t, :], Va_t[t]
                            Kd, Kd_t = Kdb[idx % 2], Kdb_t[idx % 2]
                            if lat and dirn == "b":
                                for kc in range(8):
                                    S.op("pe", lambda e: e.matmul(out=pC[:], lhsT=h_[:, kc, :], rhs=Wg[:, kc, :], start=(kc == 0),
                                                                  stop=(kc == 7)), reads=[h_t, Wg_t], writes=[pC_t], signal=(kc == 7))
                                S.op("act", lambda e: e.activation(out=sg[:], in_=pC[:], func=AF.Silu), reads=[pC_t], writes=[sg_t])
                            if lat:
                                for c in range(4):
                                    S.op("pe", lambda e: e.transpose(out=pD[:, 0, c * 128:(c + 1) * 128], in_=qk[:, c * 128:(c + 1) * 128],
                                                                     identity=identb[:]), reads=[qk_t, cb_t], writes=[pD_t], signal=(c == 3))
                                S.op("act", lambda e: e.copy(out=KTt[:].rearrange("p a b -> p (a b)"), in_=pD[:, 0, 256:512]),
                                     reads=[pD_t], writes=[tr_t])
                                S.op("act", lambda e: e.copy(out=QTt[:].rearrange("p a b -> p (a b)"), in_=pD[:, 0, 0:256]),
                                     reads=[pD_t], writes=[tr_t])
                                for dc in range(2):
                                    S.op("dve", lambda e: e.tensor_tensor(out=QdT[:, dc, :], in0=pD[:, 0, dc * 128:(dc + 1) * 128],
                                                                          in1=QD[:, h, :], op=ALU.mult), reads=[pD_t, dec_t], writes=[tr_t])
                                for dc in range(2):
                                    S.op("pe", lambda e: e.matmul(out=pE[:, 0:128], lhsT=KTt[:, dc, :], rhs=QTt[:, dc, :], start=(dc == 0),
                                                                  stop=(dc == 1)), reads=[tr_t], writes=[pE_t], signal=(dc == 1))
                                S.op("dve", lambda e: e.tensor_tensor(out=STm[:], in0=pE[:, 0:128], in1=MT[:, h, :], op=ALU.mult),
                                     reads=[pE_t, dec_t], writes=[STm_t])
                                S.op("pe", lambda e: e.matmul(out=pF[:], lhsT=STm[:], rhs=Vb[:], start=True, stop=False),
                                     reads=[STm_t, Vb_t], writes=[pF_t], signal=False)
                                for dc in range(2):
                                    S.op("pe", lambda e: e.matmul(out=pF[:], lhsT=QdT[:, dc, :], rhs=Sb_[:, dc, :], start=False,
                                                                  stop=(dc == 1)), reads=[tr_t, Sb_t], writes=[pF_t], signal=(dc == 1))
                            for dc in range(2):
                                S.op("pe", lambda e: e.matmul(out=pH[dc][:], lhsT=Kd[:, dc * 128:(dc + 1) * 128], rhs=Vb[:], start=True,
                                                              stop=True), reads=[Kd_t, Vb_t], writes=[pH_t[dc]])
                            if lat:
                                if dirn == "f":
                                    S.op("act", lambda e: e.copy(out=osb[:], in_=pF[:]), reads=[pF_t], writes=[osb_t])
                                    S.dma(of_d[t - 2], osb[:], reads=[osb_t], writes=[of_t[t - 2]])
                                else:
                                    of_, of_tt = ofb[idx % 3], ofb_t[idx % 3]
                                    S.op("dve", lambda e: e.tensor_tensor(out=osb[:], in0=pF[:], in1=of_[:], op=ALU.add),
                                         reads=[pF_t, of_tt], writes=[osb_t])
                                    S.op("pool", lambda e: e.memset(ssb[:, 0:1], 0.0), writes=[ss_t])
                                    S.op("act", lambda e: e.activation(out=scr[:], in_=osb[:], func=AF.Square, accum_out=ssb[:, 0:1]),
                                         reads=[osb_t, ss_t], writes=[ss_t])
                                    S.op("act", lambda e: e.activation(out=ssb[:, 1:2], in_=ssb[:, 0:1], func=AF.Sqrt, scale=1.0 / 512,
                                                                       bias=EPS), reads=[ss_t], writes=[ss_t])
                                    S.op("dve", lambda e: e.reciprocal(out=ssb[:, 2:3], in_=ssb[:, 1:2]), reads=[ss_t], writes=[ss_t])
                                    S.op("dve", lambda e: e.scalar_tensor_tensor(out=rr[:], in0=osb[:], scalar=ssb[:, 2:3], in1=sg[:],
                                                                                 op0=ALU.mult, op1=ALU.mult),
                                         reads=[osb_t, ss_t, sg_t], writes=[rr_t])
                                    for c in range(4):
                                        S.op("pe", lambda e: e.transpose(out=pD[:, 1, c * 128:(c + 1) * 128], in_=rr[:, c * 128:(c + 1) * 128],
                                                                         identity=identb[:]), reads=[rr_t, cb_t], writes=[pD2_t], signal=(c == 3))
                                    rT_, rT_tt = rTs[idx % 2], rTs_t[idx % 2]
                                    S.op("act", lambda e: e.copy(out=rT_[:].rearrange("p a b -> p (a b)"), in_=pD[:, 1, :]),
                                         reads=[pD2_t], writes=[rT_tt])
                                    tk0 = (t - 2) * 128
                                    S.dma(rT_d[h, :, :, tk0:tk0 + 128].rearrange("c p t -> p c t"), rT_[:], reads=[rT_tt],
                                          writes=[rT_t[(t - 2) // 4]])
                            for dc in range(2):
                                S.op("dve", lambda e: e.scalar_tensor_tensor(out=Sb_[:, dc, :], in0=Sf[:, dc, :], scalar=cdc, in1=pH[dc][:],
                                                                             op0=ALU.mult, op1=ALU.add),
                                     reads=[Sf_t, pH_t[dc], dec_t], writes=[Sb_t])
                                S.op("dve", lambda e: e.scalar_tensor_tensor(out=Sf[:, dc, :], in0=Sf[:, dc, :], scalar=cdc, in1=pH[dc][:],
                                                                             op0=ALU.mult, op1=ALU.add),
                                     reads=[Sf_t, pH_t[dc], dec_t], writes=[Sf_t])
                S.barrier()

            with contextlib.ExitStack() as st:
                Wgr = sb(st, "Wgr", [128, 8, 1024], BF16)
                Wgm = sb(st, "Wgm", [128, 8, 1024], BF16)
                Wbr = sb(st, "Wbr", [128, 16, 1024], BF16)
                Wbm = sb(st, "Wbm", [128, 8, 1024], BF16)
                Wo = sb(st, "Wo", [128, 8, 1024], BF16)
                stg = [sb(st, f"s4stg{i}", [128, 8, 256], F32) for i in range(2)]
                stg_t = [Tk(), Tk()]
                wbrv = wbr_d[0].rearrange("(kc p) n -> p kc n", p=128)
                wbmv = wbm_d[0].rearrange("(kc p) n -> p kc n", p=128)
                wov = wo_d[0].rearrange("(kc p) n -> p kc n", p=128)
                k = 0
                jobs = []
                W4_t = [Tk() for _ in range(4)]
                Wo_t = Tk()
                for cb in range(4):
                    cs = slice(cb * 256, (cb + 1) * 256)
                    jobs.append((Wbr[:, 0:8, cs], wbrv[:, 0:8, cs], W4_t[cb]))
                    jobs.append((Wbr[:, 8:16, cs], wbrv[:, 8:16, cs], W4_t[cb]))
                    jobs.append((Wbm[:, :, cs], wbmv[:, :, cs], W4_t[cb]))
                    jobs.append((Wgr[:, :, cs], winv[:, :, 6848 + cb * 256:6848 + (cb + 1) * 256], W4_t[cb]))
                    jobs.append((Wgm[:, :, cs], winv[:, :, 7872 + cb * 256:7872 + (cb + 1) * 256], W4_t[cb]))
                for cb in range(4):
                    cs = slice(cb * 256, (cb + 1) * 256)
                    jobs.append((Wo[:, :, cs], wov[:, :, cs], Wo_t))
                for (dst, src, wt_) in jobs:
                    load_cast(st, dst, wt_, src, None, stg[k % 2][:], stg_t[k % 2], "pool" if k % 2 else "dve")
                    k += 1
                TK4 = 256
                hb = sb(st, "s4h", [128, 8, TK4], BF16)
                hb_t = Tk()
                rb = sb(st, "s4r", [128, 16, TK4], BF16)
                rb_t = Tk()
                ab = sb(st, "s4a", [128, 8, TK4], BF16)
                ab_t = Tk()
                mTb = sb(st, "s4m", [128, 8, TK4], BF16)
                mTb_t = Tk()
                s3_ = sb(st, "s4s3", [128, TK4], F32)
                s4_ = sb(st, "s4s4", [128, TK4], F32)
                sg_t = Tk()
                m1 = sb(st, "s4m1", [128, TK4], F32)
                m2 = sb(st, "s4m2", [128, TK4], F32)
                m_t = Tk()
                x_ = sb(st, "s4x", [128, D], F32)
                x_t = Tk()
                yt = sb(st, "s4y", [128, D], F32)
                yt_t = Tk()
                o_ = sb(st, "s4xo", [128, D], F32)
                o_t = Tk()
                P = [ps(st, f"s4p{i}", [128, 512]) for i in range(4)]
                P_t = [Tk() for _ in range(4)]
                PY = [ps(st, f"s4py{i}", [128, 512]) for i in range(2)]
                PY_t = [Tk() for _ in range(2)]
                for tbk in range(L // TK4):
                    q0 = tbk * TK4
                    for i in range(TK4 // 128):
                        tl = 2 + tbk * (TK4 // 128) + i
                        S.dma(hb[:, :, i * 128:(i + 1) * 128], hT_d[tl], reads=[hT_t[tl]], writes=[hb_t])
                    for hh in range(4):
                        S.dma(rb[:, hh * 4:(hh + 1) * 4, :], rT_d[hh, :, :, q0:q0 + TK4].rearrange("c p t -> p c t"),
                              reads=[rT_t[q0 // 512]], writes=[rb_t])
                    S.dma(ab[:], att_d[:, :, q0:q0 + TK4].rearrange("h p t -> p h t"), reads=[att_t[q0 // 512]], writes=[ab_t])
                    for fc in range(8):
                        fs = slice(fc * 128, (fc + 1) * 128)
                        for kc in range(16):
                            S.op("pe", lambda e: e.matmul(out=P[0][:, 0:TK4], lhsT=Wbr[:, kc, fs], rhs=rb[:, kc, :], start=(kc == 0), stop=(kc == 15)),
                                 reads=[W4_t[fc // 2], rb_t], writes=[P_t[0]], signal=(kc == 15))
                        for (pi, Wx, src, src_t) in ((1, Wbm, ab, ab_t), (2, Wgr, hb, hb_t), (3, Wgm, hb, hb_t)):
                            for kc in range(8):
                                S.op("pe", lambda e: e.matmul(out=P[pi][:, 0:TK4], lhsT=Wx[:, kc, fs], rhs=src[:, kc, :], start=(kc == 0),
                                                              stop=(kc == 7)), reads=[W4_t[fc // 2], src_t], writes=[P_t[pi]], signal=(kc == 7))
                        S.op("act", lambda e: e.activation(out=s3_[:], in_=P[2][:, 0:TK4], func=AF.Sigmoid), reads=[P_t[2]], writes=[sg_t])
                        S.op("act", lambda e: e.activation(out=s4_[:], in_=P[3][:, 0:TK4], func=AF.Sigmoid), reads=[P_t[3]], writes=[sg_t])
                        S.op("dve", lambda e: e.tensor_tensor(out=m1[:], in0=P[0][:, 0:TK4], in1=s3_[:], op=ALU.mult),
                             reads=[P_t[0], sg_t], writes=[m_t])
                        S.op("dve", lambda e: e.tensor_tensor(out=m2[:], in0=P[1][:, 0:TK4], in1=s4_[:], op=ALU.mult),
                             reads=[P_t[1], sg_t], writes=[m_t])
                        S.op("pool", lambda e: e.tensor_tensor(out=mTb[:, fc, :], in0=m1[:], in1=m2[:], op=ALU.add),
                             reads=[m_t], writes=[mTb_t])
                    for tt in range(TK4 // 128):
                        gt = tbk * (TK4 // 128) + tt
                        S.dma(x_[:], x_d[b, gt * 128:(gt + 1) * 128, :], writes=[x_t])
                        for cb in range(2):
                            for kc in range(8):
                                S.op("pe", lambda e: e.matmul(out=PY[cb][:], lhsT=mTb[:, kc, tt * 128:(tt + 1) * 128],
                                                              rhs=Wo[:, kc, cb * 512:(cb + 1) * 512], start=(kc == 0), stop=(kc == 7)),
                                     reads=[mTb_t, Wo_t], writes=[PY_t[cb]], signal=(kc == 7))
                            S.op("dve", lambda e: e.tensor_tensor(out=yt[:, cb * 512:(cb + 1) * 512], in0=PY[cb][:],
                                                                  in1=G1[:, b, cb * 512:(cb + 1) * 512], op=ALU.mult),
                                 reads=[PY_t[cb], G_t], writes=[yt_t])
                        S.op("pool", lambda e: e.tensor_tensor(out=o_[:], in0=yt[:], in1=x_[:], op=ALU.add),
                             reads=[yt_t, x_t], writes=[o_t])
                        S.dma(x1_d[b * 32 + gt], o_[:], reads=[o_t], writes=[x1_t[b * 32 + gt]])
                S.barrier()

        mix_stack.close()
        I32 = mybir.dt.int32
        NTI = NB * 32
        with contextlib.ExitStack() as st:
            posk = sb(st, "posk", [128, NTI * 4], F32)
            ekk = sb(st, "ekk", [128, NTI * 4], F32)
            g4 = sb(st, "g4", [128, NTI * 4], F32)
            pk_t = Tk()
            base = sb(st, "base", [128, 32], F32)
            base_t = Tk()
            desti = sb(st, "desti", [128, NTI * 4], I32)
            desti_t = Tk()
            widx = sb(st, "widx", [128, 8, NBLK], I32)
            bidx = sb(st, "bidx", [2, NBLK], I32)
            widx_t = Tk()
            iota32 = cst[:, 10, 0:32]
            h2d_t = [Tk() for _ in range(NTI)]
            with contextlib.ExitStack() as p1:
                GS2 = sb(p1, "GS2", [128, NB, D], F32)
                SH2 = sb(p1, "SH2", [128, NB, D], F32)
                GS_t = Tk()
                pB2 = [ps(p1, f"p1B{i}", [128, 512]) for i in range(2)]
                pB2_t = [Tk(), Tk()]
                pR = ps(p1, "p1R", [128, 512])
                pR_t = Tk()
                dg = sb(p1, "p1dg", [128, 8, 128], F32)
                dg_t = Tk()
                for bb in range(NB):
                    for (dst, vfn) in ((GS2, lambda c: gs2[:, bb, c:c + 1]), (SH2, lambda c: mT[:, 24 + c, bb:bb + 1])):
                        for c in range(8):
                            S.op("dve", lambda e: e.tensor_scalar(out=dg[:, c, :], in0=identf, scalar1=vfn(c), scalar2=None,
                                                                  op0=ALU.mult), reads=[cst_t, mT_t, gs_t], writes=[dg_t])
                        for c in range(8):
                            S.op("pe", lambda e: e.matmul(out=pB2[c // 4][:, (c % 4) * 128:(c % 4 + 1) * 128], lhsT=onesf,
                                                          rhs=dg[:, c, :], start=True, stop=True),
                                 reads=[dg_t, cst_t], writes=[pB2_t[c // 4]])
                        for hh in range(2):
                            S.op("act", lambda e: e.copy(out=dst[:, bb, hh * 512:(hh + 1) * 512], in_=pB2[hh][:]),
                                 reads=[pB2_t[hh]], writes=[GS_t])
                bfull = sb(p1, "p1bfull", [32, 3072], F32)
                bfb = sb(p1, "p1bfb", [32, 3072], BF16)
                bf_t = Tk()
                S.dma(bfull[:, 0:2048], ebgu_d[0], writes=[bf_t])
                S.dma(bfull[:, 2048:3072], ebd_d[0], writes=[bf_t])
                bfb_t = Tk()
                S.op("dve", lambda e: e.tensor_copy(out=bfb[:], in_=bfull[:]), reads=[bf_t], writes=[bfb_t])
                S.dma(bias_d[:, :], bfb[:], reads=[bfb_t])
                Wr = sb(p1, "Wr", [128, 8, 32], F32)
                Wr_t = Tk()
                S.dma(Wr[:], rw_d[0].rearrange("(kc p) n -> p kc n", p=128), writes=[Wr_t])
                rbs = sb(p1, "rbs", [1, 32], F32)
                rbs_t = Tk()
                S.dma(rbs[:], rb_d[:, :], writes=[rbs_t])
                xt = [sb(p1, f"p1x{i}", [128, D], F32) for i in range(2)]
                xt_t = [Tk(), Tk()]
                xn = sb(p1, "p1xn", [128, D], F32)
                xn_t = Tk()
                h2 = sb(p1, "p1h2", [128, D], F32)
                h2_t = Tk()
                h2bf = [sb(p1, f"p1h2b{i}", [128, D], BF16) for i in range(2)]
                h2bf_t = [Tk(), Tk()]
                h2f = sb(p1, "p1h2f", [128, 8, 128], F32)
                h2f_t = Tk()
                scr = sb(p1, "p1scr", [128, D], BF16)
                ssb = sb(p1, "p1ss", [128, 4], F32)
                tmp_t = Tk()
                lgt = sb(p1, "p1lg", [128, 32], F32)
                mk = sb(p1, "p1mk", [128, 32], F32)
                pos = sb(p1, "p1pos", [128, 32], F32)
                s32 = sb(p1, "p1s32", [128, 32], F32)
                m8 = sb(p1, "p1m8", [128, 8], F32)
                e4 = sb(p1, "p1e4", [128, 4], F32)
                sm = sb(p1, "p1sm", [128, 4], F32)
                rt_t = Tk()
                S.op("pool", lambda e: e.memset(base[:], 0.0), writes=[base_t])
                S.dma(xt[0][:], x1_d[0], reads=[x1_t[0]], writes=[xt_t[0]])
                for ti in range(NTI):
                    bb = ti // 32
                    if ti + 1 < NTI:
                        S.dma(xt[(ti + 1) % 2][:], x1_d[ti + 1], reads=[x1_t[ti + 1]], writes=[xt_t[(ti + 1) % 2]])
                    x_, x_t = xt[ti % 2], xt_t[ti % 2]
                    S.op("pool", lambda e: e.memset(ssb[:, 0:1], 0.0), writes=[tmp_t])
                    S.op("act", lambda e: e.activation(out=scr[:], in_=x_[:], func=AF.Square, accum_out=ssb[:, 0:1]),
                         reads=[x_t, tmp_t], writes=[tmp_t])
                    S.op("act", lambda e: e.activation(out=ssb[:, 1:2], in_=ssb[:, 0:1], func=AF.Sqrt, scale=1.0 / D, bias=EPS),
                         reads=[tmp_t], writes=[tmp_t])
                    S.op("dve", lambda e: e.reciprocal(out=ssb[:, 2:3], in_=ssb[:, 1:2]), reads=[tmp_t], writes=[tmp_t])
                    S.op("dve", lambda e: e.tensor_scalar(out=xn[:], in0=x_[:], scalar1=ssb[:, 2:3], scalar2=None, op0=ALU.mult),
                         reads=[x_t, tmp_t], writes=[xn_t])
                    S.op("dve", lambda e: e.tensor_tensor(out=xn[:], in0=xn[:], in1=GS2[:, bb, :], op=ALU.mult),
                         reads=[xn_t, GS_t], writes=[xn_t])
                    S.op("pool", lambda e: e.tensor_tensor(out=h2[:], in0=xn[:], in1=SH2[:, bb, :], op=ALU.add),
                         reads=[xn_t, GS_t], writes=[h2_t])
                    hb_, hb_t = h2bf[ti % 2], h2bf_t[ti % 2]
                    S.op("act", lambda e: e.copy(out=hb_[:], in_=h2[:]), reads=[h2_t], writes=[hb_t])
                    S.dma(h2_d[ti], hb_[:], reads=[hb_t], writes=[h2d_t[ti]])
                    for c in range(8):
                        S.op("pe", lambda e: e.transpose(out=pB2[c // 4][:, (c % 4) * 128:(c % 4 + 1) * 128],
                                                         in_=h2[:, c * 128:(c + 1) * 128], identity=identf),
                             reads=[h2_t, cst_t], writes=[pB2_t[c // 4]], signal=(c % 4 == 3))
                    S.op("dve", lambda e: e.tensor_copy(out=h2f[:, 0:4, :].rearrange("p a b -> p (a b)"), in_=pB2[0][:]),
                         reads=[pB2_t[0]], writes=[h2f_t])
                    S.op("act", lambda e: e.copy(out=h2f[:, 4:8, :].rearrange("p a b -> p (a b)"), in_=pB2[1][:]),
                         reads=[pB2_t[1]], writes=[h2f_t])
                    for kc in range(8):
                        S.op("pe", lambda e: e.matmul(out=pR[:, 0:32], lhsT=h2f[:, kc, :], rhs=Wr[:, kc, :], start=(kc == 0), stop=False),
                             reads=[h2f_t, Wr_t], writes=[pR_t], signal=False)
                    S.op("pe", lambda e: e.matmul(out=pR[:, 0:32], lhsT=cst[0:1, 1, :], rhs=rbs[:], start=False, stop=True),
                         reads=[cst_t, rbs_t], writes=[pR_t])
                    S.op("dve", lambda e: e.tensor_copy(out=lgt[:], in_=pR[:, 0:32]), reads=[pR_t], writes=[rt_t])
                    S.op("dve", lambda e: e.max(out=m8[:], in_=lgt[:]), reads=[rt_t], writes=[rt_t])
                    S.op("dve", lambda e: e.tensor_scalar(out=mk[:], in0=lgt[:], scalar1=m8[:, 3:4], scalar2=None, op0=ALU.is_ge),
                         reads=[rt_t], writes=[rt_t])
                    S.op("dve", lambda e: e.tensor_scalar(out=sm[:, 0:1], in0=m8[:, 0:1], scalar1=-1.0, scalar2=None, op0=ALU.mult),
                         reads=[rt_t], writes=[rt_t])
                    S.op("act", lambda e: e.activation(out=e4[:], in_=m8[:, 0:4], func=AF.Exp, bias=sm[:, 0:1], scale=1.0),
                         reads=[rt_t], writes=[rt_t])
                    S.op("dve", lambda e: e.reduce_sum(out=sm[:, 1:2], in_=e4[:], axis=mybir.AxisListType.X), reads=[rt_t], writes=[rt_t])
                    S.op("dve", lambda e: e.reciprocal(out=sm[:, 2:3], in_=sm[:, 1:2]), reads=[rt_t], writes=[rt_t])
                    S.op("dve", lambda e: e.tensor_scalar(out=g4[:, ti * 4:ti * 4 + 4], in0=e4[:], scalar1=sm[:, 2:3], scalar2=None, op0=ALU.mult),
                         reads=[rt_t], writes=[pk_t])
                    S.op("pe", lambda e: e.matmul(out=pR[:, 32:64], lhsT=cst[:, 9, :], rhs=mk[:], start=True, stop=True),
                         reads=[rt_t, cst_t], writes=[pR_t])
                    S.op("pe", lambda e: e.matmul(out=pR[:, 64:96], lhsT=onesf, rhs=mk[:], start=True, stop=True),
                         reads=[rt_t, cst_t], writes=[pR_t])
                    S.op("dve", lambda e: e.tensor_tensor(out=pos[:], in0=pR[:, 32:64], in1=base[:], op=ALU.add),
                         reads=[pR_t, base_t], writes=[rt_t])
                    for k in range(4):
                        S.op("dve", lambda e: e.scalar_tensor_tensor(out=s32[:], in0=lgt[:], scalar=m8[:, k:k + 1], in1=pos[:],
                                                                     op0=ALU.is_equal, op1=ALU.mult), reads=[rt_t], writes=[rt_t])
                        S.op("dve", lambda e: e.reduce_sum(out=posk[:, ti * 4 + k:ti * 4 + k + 1], in_=s32[:], axis=mybir.AxisListType.X),
                             reads=[rt_t], writes=[pk_t])
                        S.op("dve", lambda e: e.scalar_tensor_tensor(out=s32[:], in0=lgt[:], scalar=m8[:, k:k + 1], in1=iota32,
                                                                     op0=ALU.is_equal, op1=ALU.mult), reads=[rt_t, cst_t, pk_t], writes=[rt_t])
                        S.op("dve", lambda e: e.reduce_sum(out=ekk[:, ti * 4 + k:ti * 4 + k + 1], in_=s32[:], axis=mybir.AxisListType.X),
                             reads=[rt_t], writes=[pk_t])
                    S.op("dve", lambda e: e.tensor_tensor(out=base[:], in0=pR[:, 64:96], in1=base[:], op=ALU.add),
                         reads=[pR_t, base_t, rt_t], writes=[base_t])
                ci = sb(p1, "p2ci", [128, 32], I32)
                padded = sb(p1, "p2pad", [128, 32], F32)
                pst = sb(p1, "p2pst", [128, 32], F32)
                pend = sb(p1, "p2pend", [128, 32], F32)
                bst = sb(p1, "p2bst", [128, NBLK], F32)
                be = sb(p1, "p2be", [128, NBLK], F32)
                wf = sb(p1, "p2wf", [128, 8, NBLK], F32)
                df = sb(p1, "p2df", [128, NTI * 4], F32)
                p2_t = Tk()
                S.op("dve", lambda e: e.tensor_copy(out=ci[:], in_=base[:]), reads=[base_t], writes=[p2_t])
                S.op("dve", lambda e: e.tensor_scalar(out=ci[:], in0=ci[:], scalar1=511, scalar2=None, op0=ALU.add), reads=[p2_t], writes=[p2_t])
                S.op("dve", lambda e: e.tensor_scalar(out=ci[:], in0=ci[:], scalar1=-512, scalar2=None, op0=ALU.bitwise_and), reads=[p2_t], writes=[p2_t])
                S.op("dve", lambda e: e.tensor_copy(out=padded[:], in_=ci[:]), reads=[p2_t], writes=[p2_t])
                S.op("dve", lambda e: e.memset(pst[:], 0.0), reads=[p2_t], writes=[p2_t])
                for ee in range(1, 32):
                    S.op("dve", lambda e: e.tensor_tensor(out=pst[:, ee:ee + 1], in0=pst[:, ee - 1:ee], in1=padded[:, ee - 1:ee], op=ALU.add),
                         reads=[p2_t], writes=[p2_t])
                S.op("dve", lambda e: e.tensor_tensor(out=pend[:], in0=pst[:], in1=padded[:], op=ALU.add), reads=[p2_t], writes=[p2_t])
                S.op("dve", lambda e: e.tensor_scalar(out=bst[:], in0=cst[:, 10, 0:NBLK], scalar1=512.0, scalar2=None, op0=ALU.mult),
                     reads=[cst_t, p2_t], writes=[p2_t])
                S.op("dve", lambda e: e.memset(be[:], 0.0), reads=[p2_t], writes=[p2_t])
                for ee in range(32):
                    S.op("dve", lambda e: e.scalar_tensor_tensor(out=be[:], in0=bst[:], scalar=pend[:, ee:ee + 1], in1=be[:],
                                                                 op0=ALU.is_ge, op1=ALU.add), reads=[p2_t], writes=[p2_t])
                S.op("dve", lambda e: e.tensor_scalar(out=be[:], in0=be[:], scalar1=31.0, scalar2=None, op0=ALU.min), reads=[p2_t], writes=[p2_t])
                for kc in range(8):
                    S.op("dve", lambda e: e.tensor_scalar(out=wf[:, kc, :], in0=be[:], scalar1=1024.0, scalar2=cst[:, 11, kc:kc + 1],
                                                          op0=ALU.mult, op1=ALU.add), reads=[p2_t, cst_t], writes=[p2_t])
                S.op("dve", lambda e: e.tensor_copy(out=widx[:], in_=wf[:]), reads=[p2_t], writes=[widx_t])
                S.op("dve", lambda e: e.tensor_copy(out=bidx[:], in_=be[0:2, :]), reads=[p2_t], writes=[widx_t])
                for c in range(NTI * 4):
                    S.op("dve", lambda e: e.scalar_tensor_tensor(out=s32[:], in0=iota32, scalar=ekk[:, c:c + 1], in1=pst[:],
                                                                 op0=ALU.is_equal, op1=ALU.mult), reads=[p2_t, pk_t, cst_t, rt_t], writes=[rt_t])
                    S.op("dve", lambda e: e.reduce_sum(out=df[:, c:c + 1], in_=s32[:], axis=mybir.AxisListType.X), reads=[rt_t], writes=[p2_t])
                S.op("dve", lambda e: e.tensor_tensor(out=df[:], in0=df[:], in1=posk[:], op=ALU.add), reads=[p2_t, pk_t], writes=[p2_t])
                S.op("dve", lambda e: e.tensor_copy(out=desti[:], in_=df[:]), reads=[p2_t], writes=[desti_t])
                for ti in range(NTI):
                    hb_, hb_t = h2bf[ti % 2], h2bf_t[ti % 2]
                    S.dma(hb_[:], h2_d[ti], reads=[h2d_t[ti]], writes=[hb_t])
                    for k in range(4):
                        cidx = ti * 4 + k
                        S.dma_ind(lambda e: e.indirect_dma_start(
                            out=xs_d[:, :], out_offset=bass.IndirectOffsetOnAxis(ap=desti[:, cidx:cidx + 1], axis=0),
                            in_=hb_[:], in_offset=None),
                            reads=[hb_t, desti_t])
                S.barrier()

            with contextlib.ExitStack() as p4:
                egu2 = egu_d[0].rearrange("e k n -> (e k) n")
                edn2 = edn_d[0].rearrange("e k n -> (e k) n")
                Wgu = [sb(p4, f"Wgu{i}", [128, 8, 1024], BF16) for i in range(3)]
                Wgu_t = [Tk(), Tk(), Tk()]
                Wd = [sb(p4, f"Wd{i}", [128, 4, 1024], BF16) for i in range(2)]
                Wd_t = [Tk(), Tk()]
                stg = [sb(p4, f"p4stg{i}", [128, 2048], F32) for i in range(4)]
                stg_t = [Tk() for _ in range(4)]
                browb = [sb(p4, f"browb{i}", [2, 3072], BF16) for i in range(2)]
                browb_t = [Tk(), Tk()]
                xs = [sb(p4, f"p4xs{i}", [128, D], BF16) for i in range(4)]
                xs_t = [Tk() for _ in range(4)]
                xsT = sb(p4, "p4xsT", [128, 8, 512], BF16)
                xsT_t = Tk()
                aT = [sb(p4, f"aT{i}", [128, 4, 512], BF16) for i in range(2)]
                aT_t = [Tk(), Tk()]
                g1 = sb(p4, "p4g1", [128, 512], F32)
                u1 = sb(p4, "p4u1", [128, 512], F32)
                glu = sb(p4, "p4gl", [128, 512], F32)
                g1_t, u1_t, glu_t = Tk(), Tk(), Tk()
                ysb = sb(p4, "p4ys", [128, 4, D], F32)
                ysb_t = [[Tk(), Tk()] for _ in range(4)]
                pG = [ps(p4, f"p4G{i}", [128, 512]) for i in range(2)]
                pG_t = [Tk(), Tk()]
                pU = [ps(p4, f"p4U{i}", [128, 512]) for i in range(2)]
                pU_t = [Tk(), Tk()]
                pY = [ps(p4, f"p4Y{i}", [128, 512]) for i in range(2)]
                pY_t = [Tk(), Tk()]
                pX = [ps(p4, f"p4X{i}", [128, 2, 512], BF16) for i in range(2)]
                pX_t = [Tk(), Tk()]
                ISC = 1.0 / 1.702
                cnt = dict(sk=0, gk=0, yk=0)

                def w_load_gu_block(blk):
                    for kc in range(8):
                        si = cnt["sk"] % 4
                        cnt["sk"] += 1
                        S.dma_ind(lambda e: e.indirect_dma_start(
                            out=stg[si][:, :], out_offset=None, in_=egu2[:, :],
                            in_offset=bass.IndirectOffsetOnAxis(ap=widx[:, kc, blk:blk + 1], axis=0)),
                            reads=[widx_t], writes=[stg_t[si]])
                        for hf in range(2):
                            wi3 = (2 * blk + hf) % 3
                            S.op("act", lambda e: e.copy(out=Wgu[wi3][:, kc, :].rearrange("p (g n) -> p g n", g=2),
                                                         in_=stg[si][:].rearrange("p (g h n) -> p g h n", g=2, h=2)[:, :, hf, :]),
                                 reads=[stg_t[si]], writes=[Wgu_t[wi3]])

                def w_load_d(blk, hf, wi):
                    wd, wd_t = Wd[wi], Wd_t[wi]
                    for jq in range(2):
                        si = cnt["sk"] % 4
                        cnt["sk"] += 1
                        for i in range(2):
                            kc = hf * 4 + jq * 2 + i
                            S.dma_ind(lambda e: e.indirect_dma_start(
                                out=stg[si][:, i * 1024:(i + 1) * 1024], out_offset=None, in_=edn2[:, :],
                                in_offset=bass.IndirectOffsetOnAxis(ap=widx[:, kc, blk:blk + 1], axis=0)),
                                reads=[widx_t], writes=[stg_t[si]])
                        S.op("act", lambda e: e.mul(out=wd[:, jq * 2:(jq + 1) * 2, :], in_=stg[si][:].rearrange("p (a b) -> p a b", a=2), mul=ISC),
                             reads=[stg_t[si]], writes=[wd_t])

                def b_load(blk):
                    S.dma_ind(lambda e: e.indirect_dma_start(
                        out=browb[blk % 2][0:2, :], out_offset=None, in_=bias_d[:, :],
                        in_offset=bass.IndirectOffsetOnAxis(ap=bidx[0:2, blk:blk + 1], axis=0)),
                        reads=[widx_t], writes=[browb_t[blk % 2]])

                def x_dma(blk):
                    for tt in range(4):
                        r0 = blk * 512 + tt * 128
                        S.dma(xs[tt][:], xs_d[r0:r0 + 128, :], writes=[xs_t[tt]])

                def x_tr(blk):
                    for tt in range(4):
                        for kc in range(8):
                            S.op("pe", lambda e: e.transpose(out=pX[tt % 2][:, kc // 4, (kc % 4) * 128:(kc % 4 + 1) * 128],
                                                             in_=xs[tt][:, kc * 128:(kc + 1) * 128], identity=identb[:]),
                                 reads=[xs_t[tt], cb_t], writes=[pX_t[tt % 2]], signal=(kc == 7))
                        S.op("dve", lambda e: e.tensor_copy(out=xsT[:, 0:4, tt * 128:(tt + 1) * 128],
                                                            in_=pX[tt % 2][:, 0, :].rearrange("p (a b) -> p a b", a=4)),
                             reads=[pX_t[tt % 2]], writes=[xsT_t])
                        S.op("act", lambda e: e.copy(out=xsT[:, 4:8, tt * 128:(tt + 1) * 128],
                                                     in_=pX[tt % 2][:, 1, :].rearrange("p (a b) -> p a b", a=4)),
                             reads=[pX_t[tt % 2]], writes=[xsT_t])

                def gu_unit(blk, hf, wi, au):
                    wg, wg_t = Wgu[(2 * blk + hf) % 3], Wgu_t[(2 * blk + hf) % 3]
                    bb_, bb_t = browb[blk % 2], browb_t[blk % 2]
                    a_, a_t = aT[au], aT_t[au]
                    for j in range(4):
                        i2 = cnt["gk"] % 2
                        cnt["gk"] += 1
                        fcol = hf * 512 + j * 128
                        for kc in range(8):
                            S.op("pe", lambda e: e.matmul(out=pG[i2][:], lhsT=wg[:, kc, j * 128:(j + 1) * 128], rhs=xsT[:, kc, :],
                                                          start=(kc == 0), stop=False), reads=[wg_t, xsT_t], writes=[pG_t[i2]], signal=False)
                        S.op("pe", lambda e: e.matmul(out=pG[i2][:], lhsT=bb_[0:1, fcol:fcol + 128], rhs=cbones[0:1, :], start=False, stop=True),
                             reads=[bb_t, cb_t], writes=[pG_t[i2]])
                        for kc in range(8):
                            S.op("pe", lambda e: e.matmul(out=pU[i2][:], lhsT=wg[:, kc, 512 + j * 128:512 + (j + 1) * 128], rhs=xsT[:, kc, :],
                                                          start=(kc == 0), stop=False), reads=[wg_t, xsT_t], writes=[pU_t[i2]], signal=False)
                        S.op("pe", lambda e: e.matmul(out=pU[i2][:], lhsT=bb_[0:1, 1024 + fcol:1024 + fcol + 128], rhs=cbones[0:1, :],
                                                      start=False, stop=True), reads=[bb_t, cb_t], writes=[pU_t[i2]])
                        S.op("dve", lambda e: e.tensor_scalar(out=g1[:], in0=pG[i2][:], scalar1=7.0, scalar2=None, op0=ALU.min),
                             reads=[pG_t[i2]], writes=[g1_t])
                        S.op("act", lambda e: e.activation(out=glu[:], in_=g1[:], func=AF.Silu, scale=1.702), reads=[g1_t], writes=[glu_t])
                        S.op("dve", lambda e: e.tensor_scalar(out=u1[:], in0=pU[i2][:], scalar1=1.0, scalar2=8.0, op0=ALU.add, op1=ALU.min),
                             reads=[pU_t[i2]], writes=[u1_t])
                        S.op("dve", lambda e: e.scalar_tensor_tensor(out=a_[:, j, :], in0=u1[:], scalar=-6.0, in1=glu[:],
                                                                     op0=ALU.max, op1=ALU.mult), reads=[u1_t, glu_t], writes=[a_t])

                def dn_unit(blk, hf, wi, au):
                    wd, wd_t = Wd[wi], Wd_t[wi]
                    bb_, bb_t = browb[blk % 2], browb_t[blk % 2]
                    a_, a_t = aT[au], aT_t[au]
                    for tt in range(4):
                        for cb in range(2):
                            yi = cnt["yk"] % 2
                            cnt["yk"] += 1
                            for j in range(4):
                                S.op("pe", lambda e: e.matmul(out=pY[yi][:], lhsT=a_[:, j, tt * 128:(tt + 1) * 128],
                                                              rhs=wd[:, j, cb * 512:(cb + 1) * 512], start=(j == 0), stop=(j == 3 and hf == 1)),
                                     reads=[a_t, wd_t], writes=[pY_t[yi]], signal=(j == 3 and hf == 1))
                            yv = ysb[:, tt, cb * 512:(cb + 1) * 512]
                            if hf == 0:
                                S.op("pe", lambda e: e.matmul(out=pY[yi][:], lhsT=cbones[0:1, 0:128], rhs=bb_[0:1, 2048 + cb * 512:2048 + (cb + 1) * 512],
                                                              start=False, stop=True), reads=[bb_t, cb_t], writes=[pY_t[yi]])
                                S.op("act", lambda e: e.copy(out=yv, in_=pY[yi][:]), reads=[pY_t[yi]], writes=[ysb_t[tt][cb]])
                            else:
                                S.op("dve", lambda e: e.tensor_tensor(out=yv, in0=pY[yi][:], in1=yv, op=ALU.add),
                                     reads=[pY_t[yi], ysb_t[tt][cb]], writes=[ysb_t[tt][cb]])
                    if hf == 1:
                        S.dma(ys_d[blk * 512:(blk + 1) * 512, :].rearrange("(t p) c -> p t c", p=128), ysb[:],
                              reads=[ysb_t[tt][cb] for tt in range(4) for cb in range(2)])

                cbones = sb(p4, "cbones", [1, 512], BF16)
                S.op("dve", lambda e: e.memset(cbones[:], 1.0), reads=[cb_t], writes=[cb_t])
                units = [(blk, hf) for blk in range(NBLK) for hf in range(2)]
                NU = len(units)
                b_load(0)
                x_dma(0)
                w_load_gu_block(0)
                w_load_d(0, 0, 0)
                w_load_d(0, 1, 1)
                x_tr(0)
                gu_unit(0, 0, 0, 0)
                w_load_gu_block(1)
                b_load(1)
                x_dma(1)
                for ui, (blk, hf) in enumerate(units):
                    if ui + 1 < NU:
                        nb_, nh_ = units[ui + 1]
                        if nh_ == 0:
                            x_tr(nb_)
                        gu_unit(nb_, nh_, (ui + 1) % 2, (ui + 1) % 2)
                        if nh_ == 0 and nb_ + 1 < NBLK:
                            w_load_gu_block(nb_ + 1)
                        if nh_ == 0 and nb_ + 1 < NBLK:
                            b_load(nb_ + 1)
                            x_dma(nb_ + 1)
                    dn_unit(blk, hf, ui % 2, ui % 2)
                    if ui + 2 < NU:
                        b2, h2_ = units[ui + 2]
                        w_load_d(b2, h2_, (ui + 2) % 2)
                S.barrier()

            with contextlib.ExitStack() as p5:
                yk = [sb(p5, f"p5y{i}", [128, D], F32) for i in range(4)]
                yk_t = [Tk() for _ in range(4)]
                accm = sb(p5, "p5acc", [128, D], F32)
                acc_t = Tk()
                x_ = sb(p5, "p5x", [128, D], F32)
                x_t = Tk()
                scr = sb(p5, "p5scr", [128, D], BF16)
                ssb = sb(p5, "p5ss", [128, 4], F32)
                tmp_t = Tk()
                yo = sb(p5, "p5yo", [128, D], F32)
                yo_t = Tk()
                for ti in range(NTI):
                    bb = ti // 32
                    S.dma(x_[:], x1_d[ti], reads=[x1_t[ti]], writes=[x_t])
                    for k in range(4):
                        cidx = ti * 4 + k
                        S.dma_ind(lambda e: e.indirect_dma_start(
                            out=yk[k][:], out_offset=None, in_=ys_d[:, :],
                            in_offset=bass.IndirectOffsetOnAxis(ap=desti[:, cidx:cidx + 1], axis=0)),
                            reads=[desti_t], writes=[yk_t[k]])
                    S.op("dve", lambda e: e.tensor_scalar(out=accm[:], in0=yk[0][:], scalar1=g4[:, ti * 4:ti * 4 + 1], scalar2=None, op0=ALU.mult),
                         reads=[yk_t[0], pk_t], writes=[acc_t])
                    for k in range(1, 4):
                        S.op("dve", lambda e: e.scalar_tensor_tensor(out=accm[:], in0=yk[k][:], scalar=g4[:, ti * 4 + k:ti * 4 + k + 1], in1=accm[:],
                                                                     op0=ALU.mult, op1=ALU.add), reads=[yk_t[k], pk_t, acc_t], writes=[acc_t])
                    S.op("dve", lambda e: e.tensor_tensor(out=accm[:], in0=accm[:], in1=G2[:, bb, :], op=ALU.mult),
                         reads=[acc_t, G_t], writes=[acc_t])
                    S.op("dve", lambda e: e.tensor_tensor(out=accm[:], in0=accm[:], in1=x_[:], op=ALU.add), reads=[acc_t, x_t], writes=[acc_t])
                    S.op("dve", lambda e: e.memset(ssb[:, 0:1], 0.0), writes=[tmp_t])
                    S.op("act", lambda e: e.activation(out=scr[:], in_=accm[:], func=AF.Square, accum_out=ssb[:, 0:1]),
                         reads=[acc_t, tmp_t], writes=[tmp_t])
                    S.op("act", lambda e: e.activation(out=ssb[:, 1:2], in_=ssb[:, 0:1], func=AF.Sqrt, scale=1.0 / D, bias=EPS),
                         reads=[tmp_t], writes=[tmp_t])
                    S.op("dve", lambda e: e.reciprocal(out=ssb[:, 2:3], in_=ssb[:, 1:2]), reads=[tmp_t], writes=[tmp_t])
                    S.op("dve", lambda e: e.scalar_tensor_tensor(out=yo[:], in0=accm[:], scalar=ssb[:, 2:3], in1=FG[:], op0=ALU.mult, op1=ALU.mult),
                         reads=[acc_t, tmp_t, G_t], writes=[yo_t])
                    r0 = (ti % 32) * 128
                    S.dma(out_d[bb, r0:r0 + 128, :], yo[:], reads=[yo_t])
            S.final_wait()
        print("ops:", S.n_ops, "dmas:", S.dma_n, "sig:", S.cnt)
    return nc


_CACHE = {}


def make_in_maps(inputs, n_cores=8):
    RC, RS, MC, MS = rope_tables()
    cst, _ = const_tables()
    f = lambda a: np.ascontiguousarray(np.asarray(a, dtype=np.float32))
    shared = {k: f(inputs[k]) for k in ("norm1_g", "norm2_g", "ada_w", "ada_b", "w_in", "ret_decay_fwd", "ret_decay_bwd",
                                        "mla_q_norm_g", "mla_w_uq", "mla_kv_norm_g", "mla_w_ukv", "w_branch_ret",
                                        "w_branch_mla", "w_out", "router_w", "router_b", "exp_w_gu", "exp_b_gu",
                                        "exp_w_down", "exp_b_down", "final_norm_g")}
    shared.update(consts=cst, rope_rc=RC, rope_rs=RS, rope_mc=MC, rope_ms=MS)
    x, c, ctx, c_ctx = f(inputs["x"]), f(inputs["c"]), f(inputs["ctx"]), f(inputs["c_ctx"])
    maps = []
    for i in range(n_cores):
        m = dict(shared)
        m["x"] = x[i * NB:(i + 1) * NB]
        m["ctx"] = ctx[i * NB:(i + 1) * NB]
        m["cvec"] = np.ascontiguousarray(np.concatenate([c[i * NB:(i + 1) * NB], c_ctx[None, :]], axis=0))
        maps.append(m)
    return maps


def kernel(**inputs):
    if "nc" not in _CACHE:
        _CACHE["nc"] = build()
    nc = _CACHE["nc"]
    maps = make_in_maps(inputs)
    res = run_bass_kernel_spmd(nc, maps, core_ids=list(range(8)))
    return np.concatenate([r["out"] for r in res.results], axis=0).astype(np.float32)
```

```python
import contextlib
import numpy as np
import concourse.bass as bass
import concourse.mybir as mybir
from concourse.bass_utils import run_bass_kernel_spmd

F32 = mybir.dt.float32
BF16 = mybir.dt.bfloat16
AF = mybir.ActivationFunctionType
ALU = mybir.AluOpType

NB = 2
L = 4096
CT = 256
LT = L + CT
NT = LT // 128
D = 1024
EPS = 1e-6
NS_DMA = 16
ERA = 16000


class Tk:
    __slots__ = ("w", "r", "name")

    def __init__(self, name=""):
        self.w = []
        self.r = []
        self.name = name


class Sched:
    def __init__(self, nc, stack):
        self.nc = nc
        self.stack = stack
        self.engs = {"pe": nc.tensor, "act": nc.scalar, "dve": nc.vector, "pool": nc.gpsimd, "sp": nc.sync}
        self.sems = {}
        self.seq = {e: 0 for e in self.engs}
        self.sig = {e: [] for e in self.engs}
        self.cnt = {e: 0 for e in self.engs}
        self.waited = {e: {} for e in self.engs}
        self.waited_d = {e: {} for e in self.engs}
        self.ring = [stack.enter_context(nc.semaphore(f"dq{i}")) for i in range(NS_DMA)]
        self.dma_n = 0
        self.n_ops = 0

    def _sem(self, eng, era):
        k = (eng, era)
        if k not in self.sems:
            self.sems[k] = self.stack.enter_context(self.nc.semaphore(f"s_{eng}_{era}"))
        return self.sems[k]

    def _wait(self, eng, tk):
        e = self.engs[eng]
        if tk[0] == "d":
            _, ring, val = tk
            if self.waited_d[eng].get(ring, 0) >= val:
                return
            e.wait_ge(self.ring[ring], val)
            self.waited_d[eng][ring] = val
            return
        _, peng, seq = tk
        lst = self.sig[peng]
        lo, hi = 0, len(lst)
        while lo < hi:
            mid = (lo + hi) // 2
            if lst[mid][0] >= seq:
                hi = mid
            else:
                lo = mid + 1
        if lo >= len(lst):
            raise RuntimeError(f"no signalling op after seq {seq} on {peng}")
        count = lst[lo][1]
        if self.waited[eng].get(peng, 0) >= count:
            return
        era, val = (count - 1) // ERA, (count - 1) % ERA + 1
        e.wait_ge(self._sem(peng, era), val)
        self.waited[eng][peng] = count

    def op(self, eng, fn, reads=(), writes=(), signal=True):
        deps = []
        for t in reads:
            deps.extend(t.w)
        for t in writes:
            for tk in t.w:
                if tk[0] == "d" or tk[1] != eng:
                    deps.append(tk)
            for tk in t.r:
                if tk[0] == "d" or tk[1] != eng:
                    deps.append(tk)
        for tk in deps:
            self._wait(eng, tk)
        ins = fn(self.engs[eng])
        self.seq[eng] += 1
        seq = self.seq[eng]
        if signal:
            self.cnt[eng] += 1
            c = self.cnt[eng]
            era, val = (c - 1) // ERA, (c - 1) % ERA + 1
            ins.then_inc(self._sem(eng, era), 1)
            self.sig[eng].append((seq, c))
        tk = ("c", eng, seq)
        for t in reads:
            t.r = [x for x in t.r if not (x[0] == "c" and x[1] == eng)]
            t.r.append(tk)
        for t in writes:
            t.w = [tk]
            t.r = []
        self.n_ops += 1
        return ins

    def dma(self, out, in_, reads=(), writes=()):
        eng = "sp"
        deps = []
        for t in reads:
            deps.extend(t.w)
        for t in writes:
            deps.extend(t.w)
            deps.extend(t.r)
        n = self.dma_n
        ring = n % NS_DMA
        val = 16 * (n // NS_DMA + 1)
        if n >= NS_DMA:
            deps.append(("d", ring, val - 16))
        for tk in deps:
            self._wait(eng, tk)
        self.engs[eng].dma_start(out=out, in_=in_).then_inc(self.ring[ring], 16)
        self.dma_n += 1
        tk = ("d", ring, val)
        for t in reads:
            t.r.append(tk)
        for t in writes:
            t.w = [tk]
            t.r = []
        self.n_ops += 1
        return tk

    def dma_ind(self, fn, reads=(), writes=()):
        eng = "pool"
        deps = []
        for t in reads:
            deps.extend(t.w)
        for t in writes:
            deps.extend(t.w)
            deps.extend(t.r)
        n = self.dma_n
        ring = n % NS_DMA
        val = 16 * (n // NS_DMA + 1)
        if n >= NS_DMA:
            deps.append(("d", ring, val - 16))
        for tk in deps:
            self._wait(eng, tk)
        fn(self.engs[eng]).then_inc(self.ring[ring], 16)
        self.dma_n += 1
        tk = ("d", ring, val)
        for t in reads:
            t.r.append(tk)
        for t in writes:
            t.w = [tk]
            t.r = []
        self.n_ops += 1
        return tk

    def barrier(self):
        tks = []
        for e in self.engs:
            if self.sig[e]:
                tks.append(("c", e, self.sig[e][-1][0]))
        n = self.dma_n
        for k in range(max(0, n - NS_DMA), n):
            tks.append(("d", k % NS_DMA, 16 * (k // NS_DMA + 1)))
        for e in self.engs:
            for tk in tks:
                if tk[0] == "c" and tk[1] == e:
                    continue
                self._wait(e, tk)

    def final_wait(self):
        n = self.dma_n
        for k in range(max(0, n - NS_DMA), n):
            self._wait("sp", ("d", k % NS_DMA, 16 * (k // NS_DMA + 1)))


def rope_tables():
    pos = np.arange(L)
    rows = (pos // 64).astype(np.float32)
    cols = (pos % 64).astype(np.float32)

    def tab(dr):
        half = dr // 2
        hh = half // 2
        freqs = (10000.0 ** (-np.arange(hh, dtype=np.float32) / hh)).astype(np.float32)
        C = np.ones((LT, dr), np.float32)
        S = np.zeros((LT, dr), np.float32)
        for part, p in enumerate((rows, cols)):
            ang = (p[:, None] * freqs[None, :]).astype(np.float32)
            c, s = np.cos(ang).astype(np.float32), np.sin(ang).astype(np.float32)
            o = part * half
            C[CT:, o:o + hh] = c
            C[CT:, o + hh:o + half] = c
            S[CT:, o:o + hh] = -s
            S[CT:, o + hh:o + half] = s
        return C, S

    RC, RS = tab(256)
    MC, MS = tab(64)
    return RC, RS, np.ascontiguousarray(MC.T), np.ascontiguousarray(MS.T)


def const_tables():
    j = np.arange(128, dtype=np.float32)[:, None]
    i = np.arange(128, dtype=np.float32)[None, :]
    c = {}
    c["ident"] = np.eye(128, dtype=np.float32)
    c["ones"] = np.ones((128, 128), np.float32)
    c["d1"] = np.maximum(i - j, 0.0) + 0 * j
    c["mf"] = (i >= j).astype(np.float32)
    c["d2"] = np.maximum(j - i, 0.0)
    c["mb"] = (i < j).astype(np.float32)
    c["ip1"] = (i + 1.0) + 0 * j
    c["rev"] = (128.0 - i) + 0 * j
    col = np.zeros((128, 128), np.float32)
    col[:, 0] = 127.0 - np.arange(128)
    col[:, 1] = np.arange(128)
    col[:, 2] = 128.0
    c["col"] = col
    c["lt"] = (i > j).astype(np.float32)
    c["iota"] = i + 0 * j
    rowb = np.zeros((128, 128), np.float32)
    for kc in range(8):
        rowb[:, kc] = kc * 128 + np.arange(128)
    c["rowb"] = rowb
    names = ["ident", "ones", "d1", "mf", "d2", "mb", "ip1", "rev", "col", "lt", "iota", "rowb"]
    return np.stack([c[n].astype(np.float32) for n in names], axis=1), names


def build(dbg=()):
    nc = bass.Bass("TRN2", target_bir_lowering=False)
    try:
        nc.allow_low_precision("bf16 matmul operands with fp32 accumulation")
    except Exception:
        pass
    try:
        nc.allow_non_contiguous_dma("strided weight/activation tiles")
    except Exception:
        pass

    def din(name, shape, dt=F32):
        return nc.dram_tensor(name, list(shape), dt, kind="ExternalInput").ap()

    def dscr(name, shape, dt):
        kind = "ExternalOutput" if name in dbg else "Internal"
        return nc.dram_tensor(name, list(shape), dt, kind=kind).ap()

    x_d = din("x", [NB, L, D])
    ctx_d = din("ctx", [NB, CT, D])
    cv_d = din("cvec", [3, D])
    n1_d = din("norm1_g", [1, D])
    n2_d = din("norm2_g", [1, D])
    adaw_d = din("ada_w", [1, D, 6 * D])
    adab_d = din("ada_b", [1, 6 * D])
    win_d = din("w_in", [1, D, 8896])
    rdf_d = din("ret_decay_fwd", [1, 4])
    rdb_d = din("ret_decay_bwd", [1, 4])
    qg_d = din("mla_q_norm_g", [1, 384])
    wuq_d = din("mla_w_uq", [1, 384, 1536])
    kvg_d = din("mla_kv_norm_g", [1, 256])
    wukv_d = din("mla_w_ukv", [1, 256, 2048])
    wbr_d = din("w_branch_ret", [1, 2048, D])
    wbm_d = din("w_branch_mla", [1, D, D])
    wo_d = din("w_out", [1, D, D])
    rw_d = din("router_w", [1, D, 32])
    rb_d = din("router_b", [1, 32])
    egu_d = din("exp_w_gu", [1, 32, D, 2 * D])
    ebgu_d = din("exp_b_gu", [1, 32, 2 * D])
    edn_d = din("exp_w_down", [1, 32, D, D])
    ebd_d = din("exp_b_down", [1, 32, D])
    fg_d = din("final_norm_g", [D])
    cst_d = din("consts", [128, 12, 128])
    rc_d = din("rope_rc", [LT, 256])
    rs_d = din("rope_rs", [LT, 256])
    mc_d = din("rope_mc", [64, LT])
    ms_d = din("rope_ms", [64, LT])
    out_d = nc.dram_tensor("out", [NB, L, D], F32, kind="ExternalOutput").ap()

    hT_d = dscr("hT_s", [NT, 128, 8, 128], BF16)
    att_d = dscr("att_s", [8, 128, L], BF16)
    rT_d = dscr("rT_s", [4, 4, 128, L], BF16)
    of_d = dscr("of_s", [32, 128, 512], F32)
    x1_d = dscr("x1_s", [NB * 32, 128, D], F32)
    NROWS = NB * L * 4 + 32 * 512
    NBLK = NROWS // 512
    h2_d = dscr("h2_s", [NB * 32, 128, D], BF16)
    xs_d = dscr("xs_s", [NROWS, D], BF16)
    ys_d = dscr("ys_s", [NROWS, D], F32)
    bias_d = dscr("bias_s", [32, 3072], BF16)
    hT_t = [Tk() for _ in range(NT)]
    att_t = [Tk() for _ in range(8)]
    rT_t = [Tk() for _ in range(8)]
    of_t = [Tk() for _ in range(32)]
    x1_t = [Tk() for _ in range(NB * 32)]
    out_t = Tk()

    with contextlib.ExitStack() as gstack:
        S = Sched(nc, gstack)

        uid = [0]

        def sb(stack, name, shape, dt):
            uid[0] += 1
            return stack.enter_context(nc.sbuf_tensor(f"{name}_u{uid[0]}", list(shape), dt))

        def ps(stack, name, shape, dt=F32):
            uid[0] += 1
            return stack.enter_context(nc.psum_tensor(f"{name}_u{uid[0]}", list(shape), dt))

        cst = sb(gstack, "cst", [128, 12, 128], F32)
        cst_t = Tk()
        S.dma(cst[:], cst_d[:, :, :], writes=[cst_t])
        identf = cst[:, 0, :]
        onesf = cst[:, 1, :]
        identb = sb(gstack, "identb", [128, 128], BF16)
        onesb = sb(gstack, "onesb", [128, 128], BF16)
        cb_t = Tk()
        S.op("dve", lambda e: e.tensor_copy(out=identb[:], in_=identf), reads=[cst_t], writes=[cb_t])
        S.op("dve", lambda e: e.tensor_copy(out=onesb[:], in_=onesf), reads=[cst_t], writes=[cb_t])

        mT = sb(gstack, "mT", [128, 48, 3], F32)
        mT_t = Tk()
        gs1 = sb(gstack, "gs1", [128, 3, 8], F32)
        gs2 = sb(gstack, "gs2", [128, 3, 8], F32)
        gs_t = Tk()
        mix_stack = contextlib.ExitStack()
        G2 = sb(gstack, "G2", [128, NB, D], F32)
        FG = sb(gstack, "FG", [128, D], F32)
        G_t = Tk()
        gq = sb(gstack, "gq", [128, 8], F32)
        gq_t = Tk()
        KD = sb(gstack, "KD", [128, 4, 8], F32)
        G1 = sb(mix_stack, "G1", [128, NB, D], F32)
        MTf = sb(mix_stack, "MTf", [128, 4, 128], F32)
        MTb = sb(mix_stack, "MTb", [128, 4, 128], F32)
        QDf = sb(mix_stack, "QDf", [128, 4, 128], F32)
        QDb = sb(mix_stack, "QDb", [128, 4, 128], F32)
        dec_t = Tk()

        def featmajor_load(stack, pst, pst_t, dst_ap, src_rows_ap, nrows, tmpname):
            tmp = sb(stack, tmpname, [nrows, 128], F32)
            tt = Tk()
            S.dma(tmp[:], src_rows_ap, writes=[tt])
            S.op("pe", lambda e: e.transpose(out=pst[:, 0:nrows], in_=tmp[:], identity=cst[0:nrows, 0, 0:nrows]),
                 reads=[tt, cst_t], writes=[pst_t])
            return tmp

        with contextlib.ExitStack() as st:
            pA = ps(st, "pA0", [128, 512])
            pA_t = Tk()
            pB = ps(st, "pB0", [128, 2, 512])
            pB_t = Tk()
            cv = sb(st, "cv", [3, D], F32)
            cvs = sb(st, "cvs", [3, D], F32)
            cv_t = Tk()
            S.dma(cv[:], cv_d[:, :], writes=[cv_t])
            cvs_t = Tk()
            S.op("act", lambda e: e.activation(out=cvs[:], in_=cv[:], func=AF.Silu), reads=[cv_t], writes=[cvs_t])
            sT = sb(st, "sT", [128, 8, 3], F32)
            sT_t = Tk()
            for kc in range(8):
                S.op("pe", lambda e: e.transpose(out=pA[:, kc * 4:kc * 4 + 3], in_=cvs[:, kc * 128:(kc + 1) * 128],
                                                 identity=cst[0:3, 0, 0:3]), reads=[cvs_t, cst_t], writes=[pA_t])
            for kc in range(8):
                S.op("dve", lambda e: e.tensor_copy(out=sT[:, kc, :], in_=pA[:, kc * 4:kc * 4 + 3]),
                     reads=[pA_t], writes=[sT_t])
            abT = sb(st, "abT", [128, 48], F32)
            g12 = sb(st, "g12", [128, 16], F32)
            ab_t = Tk()
            featmajor_load(st, pA, pA_t, None, adab_d[0, :].rearrange("(r p) -> r p", p=128), 48, "t_ab")
            S.op("dve", lambda e: e.tensor_copy(out=abT[:], in_=pA[:, 0:48]), reads=[pA_t], writes=[ab_t])
            featmajor_load(st, pA, pA_t, None, n1_d[0, :].rearrange("(r p) -> r p", p=128), 8, "t_n1")
            S.op("dve", lambda e: e.tensor_copy(out=g12[:, 0:8], in_=pA[:, 0:8]), reads=[pA_t], writes=[ab_t])
            featmajor_load(st, pA, pA_t, None, n2_d[0, :].rearrange("(r p) -> r p", p=128), 8, "t_n2")
            S.op("dve", lambda e: e.tensor_copy(out=g12[:, 8:16], in_=pA[:, 0:8]), reads=[pA_t], writes=[ab_t])
            featmajor_load(st, pA, pA_t, None, qg_d[0, :].rearrange("(r p) -> r p", p=128), 3, "t_qg")
            S.op("dve", lambda e: e.tensor_copy(out=gq[:, 0:3], in_=pA[:, 0:3]), reads=[pA_t], writes=[gq_t])
            featmajor_load(st, pA, pA_t, None, kvg_d[0, :].rearrange("(r p) -> r p", p=128), 2, "t_kvg")
            S.op("dve", lambda e: e.tensor_copy(out=gq[:, 4:6], in_=pA[:, 0:2]), reads=[pA_t], writes=[gq_t])
            fgT = sb(st, "fgT", [128, 8], F32)
            fg_t = Tk()
            featmajor_load(st, pA, pA_t, None, fg_d.rearrange("(r p) -> r p", p=128), 8, "t_fg")
            S.op("dve", lambda e: e.tensor_copy(out=fgT[:], in_=pA[:, 0:8]), reads=[pA_t], writes=[fg_t])
            awv = adaw_d[0].rearrange("(kc p) n -> p kc n", p=128)
            aw = [sb(st, f"aw{i}", [128, 8, 512], F32) for i in range(2)]
            aw_t = [Tk(), Tk()]
            pm = ps(st, "pm", [128, 48, 4])
            pm_t = Tk()
            for blk in range(12):
                w = aw[blk % 2]
                wt = aw_t[blk % 2]
                S.dma(w[:], awv[:, :, blk * 512:(blk + 1) * 512], writes=[wt])
                for jj in range(4):
                    j = blk * 4 + jj
                    for kc in range(8):
                        S.op("pe", lambda e: e.matmul(out=pm[:, j, 0:3], lhsT=w[:, kc, jj * 128:(jj + 1) * 128],
                                                      rhs=sT[:, kc, :], start=(kc == 0), stop=(kc == 7)),
                             reads=[wt, sT_t], writes=[pm_t], signal=(kc == 7))
            for r in range(3):
                S.op("dve", lambda e: e.tensor_tensor(out=mT[:, :, r], in0=pm[:, :, r], in1=abT[:], op=ALU.add),
                     reads=[pm_t, ab_t], writes=[mT_t])
            for r in range(3):
                S.op("dve", lambda e: e.scalar_tensor_tensor(out=gs1[:, r, :], in0=mT[:, 8:16, r], scalar=1.0,
                                                             in1=g12[:, 0:8], op0=ALU.add, op1=ALU.mult),
                     reads=[mT_t, ab_t], writes=[gs_t])
                S.op("dve", lambda e: e.scalar_tensor_tensor(out=gs2[:, r, :], in0=mT[:, 32:40, r], scalar=1.0,
                                                             in1=g12[:, 8:16], op0=ALU.add, op1=ALU.mult),
                     reads=[mT_t, ab_t], writes=[gs_t])
            dg = sb(st, "dg", [128, 8, 128], F32)
            dg_t = Tk()

            def bcast_tile(dst_ap, vec_fn):
                for c in range(8):
                    S.op("dve", lambda e: e.tensor_scalar(out=dg[:, c, :], in0=identf, scalar1=vec_fn(c), scalar2=None,
                                                          op0=ALU.mult), reads=[cst_t, mT_t, fg_t], writes=[dg_t])
                for c in range(8):
                    S.op("pe", lambda e: e.matmul(out=pB[:, c // 4, (c % 4) * 128:(c % 4 + 1) * 128], lhsT=onesf,
                                                  rhs=dg[:, c, :], start=True, stop=True),
                         reads=[dg_t, cst_t], writes=[pB_t])
                S.op("act", lambda e: e.copy(out=dst_ap, in_=pB[:].rearrange("p a b -> p (a b)")),
                     reads=[pB_t], writes=[G_t])

            for b in range(NB):
                bcast_tile(G1[:, b, :], lambda c: mT[:, 16 + c, b:b + 1])
                bcast_tile(G2[:, b, :], lambda c: mT[:, 40 + c, b:b + 1])
            bcast_tile(FG[:], lambda c: fgT[:, c:c + 1])

            rd = sb(st, "rd", [1, 8], F32)
            rd_t = Tk()
            S.dma(rd[:, 0:4], rdf_d[:, :], writes=[rd_t])
            S.dma(rd[:, 4:8], rdb_d[:, :], writes=[rd_t])
            S.op("pe", lambda e: e.matmul(out=pA[:, 0:8], lhsT=cst[0:1, 1, :], rhs=rd[:], start=True, stop=True),
                 reads=[rd_t, cst_t], writes=[pA_t])
            lg = sb(st, "lg", [128, 8], F32)
            lg_t = Tk()
            S.op("act", lambda e: e.activation(out=lg[:], in_=pA[:, 0:8], func=AF.Exp, scale=-1.0),
                 reads=[pA_t], writes=[lg_t])
            S.op("act", lambda e: e.activation(out=lg[:], in_=lg[:], func=AF.Ln, bias=1.0, scale=1.0),
                 reads=[lg_t], writes=[lg_t])
            S.op("dve", lambda e: e.tensor_scalar(out=lg[:], in0=lg[:], scalar1=-1.0, scalar2=None, op0=ALU.mult),
                 reads=[lg_t], writes=[lg_t])
            tmpd = sb(st, "tmpd", [128, 128], F32)
            tmpd_t = Tk()
            for h in range(4):
                for (dst, dtab, mtab, col) in ((MTf, 2, 3, h), (MTb, 4, 5, 4 + h)):
                    S.op("act", lambda e: e.activation(out=tmpd[:], in_=cst[:, dtab, :], func=AF.Exp,
                                                       scale=lg[:, col:col + 1]),
                         reads=[cst_t, lg_t], writes=[tmpd_t])
                    S.op("dve", lambda e: e.tensor_tensor(out=dst[:, h, :], in0=tmpd[:], in1=cst[:, mtab, :],
                                                          op=ALU.mult), reads=[tmpd_t, cst_t], writes=[dec_t])
                S.op("act", lambda e: e.activation(out=QDf[:, h, :], in_=cst[:, 6, :], func=AF.Exp,
                                                   scale=lg[:, h:h + 1]), reads=[cst_t, lg_t], writes=[dec_t])
                S.op("act", lambda e: e.activation(out=QDb[:, h, :], in_=cst[:, 7, :], func=AF.Exp,
                                                   scale=lg[:, 4 + h:5 + h]), reads=[cst_t, lg_t], writes=[dec_t])
                S.op("act", lambda e: e.activation(out=KD[:, h, 0:1], in_=cst[:, 8, 0:1], func=AF.Exp,
                                                   scale=lg[:, h:h + 1]), reads=[cst_t, lg_t], writes=[dec_t])
                S.op("act", lambda e: e.activation(out=KD[:, h, 1:2], in_=cst[:, 8, 1:2], func=AF.Exp,
                                                   scale=lg[:, 4 + h:5 + h]), reads=[cst_t, lg_t], writes=[dec_t])
                S.op("act", lambda e: e.activation(out=KD[:, h, 2:3], in_=cst[:, 8, 2:3], func=AF.Exp,
                                                   scale=lg[:, h:h + 1]), reads=[cst_t, lg_t], writes=[dec_t])
                S.op("act", lambda e: e.activation(out=KD[:, h, 3:4], in_=cst[:, 8, 2:3], func=AF.Exp,
                                                   scale=lg[:, 4 + h:5 + h]), reads=[cst_t, lg_t], writes=[dec_t])
            S.barrier()

        def load_cast(stack_stage, dst, dst_t, src_ap, shape, stg, stg_t, eng, scale=None, dst_view=None):
            S.dma(stg, src_ap, writes=[stg_t])
            dv = dst if dst_view is None else dst_view
            if scale is None:
                S.op(eng, lambda e: e.tensor_copy(out=dv, in_=stg), reads=[stg_t], writes=[dst_t])
            else:
                S.op(eng, lambda e: e.tensor_scalar(out=dv, in0=stg, scalar1=scale, scalar2=None, op0=ALU.mult),
                     reads=[stg_t], writes=[dst_t])

        def norm_T(stack, pfx, src_tile, src_t, gs_ap, sh_ap, pT, pT_t, hTo, hTo_t, scr, ssb, tmp_t, xn, xn_t,
                   f32_out=None, f32_t=None):
            S.op("pool", lambda e: e.memset(ssb[:, 0:1], 0.0), writes=[tmp_t])
            S.op("act", lambda e: e.activation(out=scr[:], in_=src_tile, func=AF.Square, accum_out=ssb[:, 0:1]),
                 reads=[src_t, tmp_t], writes=[tmp_t])
            S.op("act", lambda e: e.activation(out=ssb[:, 1:2], in_=ssb[:, 0:1], func=AF.Sqrt, scale=1.0 / D, bias=EPS),
                 reads=[tmp_t], writes=[tmp_t])
            S.op("dve", lambda e: e.reciprocal(out=ssb[:, 2:3], in_=ssb[:, 1:2]), reads=[tmp_t], writes=[tmp_t])
            S.op("dve", lambda e: e.tensor_scalar(out=xn[:], in0=src_tile, scalar1=ssb[:, 2:3], scalar2=None,
                                                  op0=ALU.mult), reads=[src_t, tmp_t], writes=[xn_t])
            for c in range(8):
                S.op("pe", lambda e: e.transpose(out=pT[c // 4][:, (c % 4) * 128:(c % 4 + 1) * 128],
                                                 in_=xn[:, c * 128:(c + 1) * 128], identity=identf),
                     reads=[xn_t, cst_t], writes=[pT_t[c // 4]], signal=(c % 4 == 3))
            for c in range(8):
                src = pT[c // 4][:, (c % 4) * 128:(c % 4 + 1) * 128]
                if f32_out is not None:
                    S.op("dve", lambda e: e.tensor_scalar(out=f32_out[:, c, :], in0=src, scalar1=gs_ap[:, c:c + 1],
                                                          scalar2=sh_ap(c), op0=ALU.mult, op1=ALU.add),
                         reads=[pT_t[c // 4], gs_t, mT_t], writes=[f32_t])
                    S.op("pool", lambda e: e.tensor_copy(out=hTo[:, c, :], in_=f32_out[:, c, :]),
                         reads=[f32_t], writes=[hTo_t])
                else:
                    S.op("dve", lambda e: e.tensor_scalar(out=hTo[:, c, :], in0=src, scalar1=gs_ap[:, c:c + 1],
                                                          scalar2=sh_ap(c), op0=ALU.mult, op1=ALU.add),
                         reads=[pT_t[c // 4], gs_t, mT_t], writes=[hTo_t])

        winv = win_d[0].rearrange("(kc p) n -> p kc n", p=128)

        for b in range(NB):
            with contextlib.ExitStack() as st:
                xt = [sb(st, f"s0x{i}", [128, D], F32) for i in range(2)]
                xt_t = [Tk(), Tk()]
                xn = sb(st, "s0xn", [128, D], F32)
                xn_t = Tk()
                scr = sb(st, "s0scr", [128, D], BF16)
                ssb = sb(st, "s0ss", [128, 4], F32)
                tmp_t = Tk()
                hTo = [sb(st, f"s0h{i}", [128, 8, 128], BF16) for i in range(2)]
                hTo_t = [Tk(), Tk()]
                pT = [ps(st, f"s0p{i}", [128, 512]) for i in range(2)]
                pT_t = [Tk(), Tk()]

                def s0_load(t):
                    src = ctx_d[b, t * 128:(t + 1) * 128, :] if t < 2 else x_d[b, (t - 2) * 128:(t - 1) * 128, :]
                    S.dma(xt[t % 2][:], src, writes=[xt_t[t % 2]])

                s0_load(0)
                for t in range(NT):
                    if t + 1 < NT:
                        s0_load(t + 1)
                    r = 2 if t < 2 else b
                    norm_T(st, "s0", xt[t % 2][:], xt_t[t % 2], gs1[:, r, :], lambda c: mT[:, c, r:r + 1],
                           pT, pT_t, hTo[t % 2], hTo_t[t % 2], scr, ssb, tmp_t, xn, xn_t)
                    S.dma(hT_d[t], hTo[t % 2][:], reads=[hTo_t[t % 2]], writes=[hT_t[t]])
                S.barrier()

            with contextlib.ExitStack() as st:
                cqn = sb(st, "cqn", [128, 3, L], BF16)
                cqn_t = Tk()
                ckvn = sb(st, "ckvn", [128, 2, LT], BF16)
                ckvn_t = Tk()
                krT = sb(st, "krT", [64, LT], BF16)
                krT_t = Tk()
                with contextlib.ExitStack() as s1:
                    Wm = sb(s1, "Wm", [128, 8, 768], BF16)
                    Wm_t = Tk()
                    stg = sb(s1, "s1stg", [128, 8, 704], F32)
                    stg_t = Tk()
                    S.dma(stg[:], winv[:, :, 6144:6848], writes=[stg_t])
                    S.op("dve", lambda e: e.tensor_copy(out=Wm[:, :, 0:704], in_=stg[:]), reads=[stg_t], writes=[Wm_t])
                    for (dst, src) in ((704, 656), (720, 640), (736, 688), (752, 672)):
                        S.op("dve", lambda e: e.tensor_copy(out=Wm[:, :, dst:dst + 16], in_=stg[:, :, src:src + 16]),
                             reads=[stg_t], writes=[Wm_t])
                    hb = [sb(s1, f"s1h{i}", [128, 8, 512], BF16) for i in range(2)]
                    hb_t = [Tk(), Tk()]
                    tc_ = [sb(s1, f"s1tc{i}", [64, 2, 512], F32) for i in range(2)]
                    tc_t = [Tk(), Tk()]
                    pq = [ps(s1, f"s1pq{i}", [128, 512]) for i in range(3)]
                    pq_t = [Tk() for _ in range(3)]
                    pk = [ps(s1, f"s1pk{i}", [128, 512]) for i in range(2)]
                    pk_t = [Tk() for _ in range(2)]
                    pr = [ps(s1, f"s1pr{i}", [128, 512]) for i in range(2)]
                    pr_t = [Tk() for _ in range(2)]
                    pss = ps(s1, "s1pss", [128, 512])
                    pss_t = Tk()
                    xf = sb(s1, "s1xf", [128, 3, 512], F32)
                    xf_t = Tk()
                    sq = sb(s1, "s1sq", [128, 3, 512], F32)
                    sq_t = Tk()
                    rstd = sb(s1, "s1rstd", [128, 512], F32)
                    rstd_t = Tk()
                    r1 = sb(s1, "s1r1", [64, 512], F32)
                    r2 = sb(s1, "s1r2", [64, 512], F32)
                    r_t = Tk()

                    def blk_range(j):
                        return (0, 256) if j == 0 else (256 + (j - 1) * 512, 512)

                    def s1_load(j):
                        t0, n = blk_range(j)
                        for i in range(n // 128):
                            S.dma(hb[j % 2][:, :, i * 128:(i + 1) * 128], hT_d[t0 // 128 + i],
                                  reads=[hT_t[t0 // 128 + i]], writes=[hb_t[j % 2]])
                        S.dma(tc_[j % 2][:, 0, 0:n], mc_d[:, t0:t0 + n], writes=[tc_t[j % 2]])
                        S.dma(tc_[j % 2][:, 1, 0:n], ms_d[:, t0:t0 + n], writes=[tc_t[j % 2]])

                    def rms_T(pl, pl_t, nch, rank, gcol, dst, dst_t, dcol0, n):
                        for c in range(nch):
                            S.op("act", lambda e: e.copy(out=xf[:, c, 0:n], in_=pl[c][:, 0:n]),
                                 reads=[pl_t[c]], writes=[xf_t])
                            S.op("act", lambda e: e.activation(out=sq[:, c, 0:n], in_=pl[c][:, 0:n], func=AF.Square),
                                 reads=[pl_t[c]], writes=[sq_t])
                        for c in range(nch):
                            S.op("pe", lambda e: e.matmul(out=pss[:, 0:n], lhsT=onesf, rhs=sq[:, c, 0:n],
                                                          start=(c == 0), stop=(c == nch - 1)),
                                 reads=[sq_t, cst_t], writes=[pss_t], signal=(c == nch - 1))
                        S.op("act", lambda e: e.activation(out=rstd[:, 0:n], in_=pss[:, 0:n], func=AF.Sqrt,
                                                           scale=1.0 / rank, bias=EPS), reads=[pss_t], writes=[rstd_t])
                        S.op("dve", lambda e: e.reciprocal(out=rstd[:, 0:n], in_=rstd[:, 0:n]),
                             reads=[rstd_t], writes=[rstd_t])
                        for c in range(nch):
                            S.op("dve", lambda e: e.scalar_tensor_tensor(
                                out=dst[:, c, dcol0:dcol0 + n], in0=xf[:, c, 0:n], scalar=gq[:, gcol + c:gcol + c + 1],
                                in1=rstd[:, 0:n], op0=ALU.mult, op1=ALU.mult),
                                 reads=[xf_t, rstd_t, gq_t], writes=[dst_t])

                    s1_load(0)
                    for j in range(9):
                        if j + 1 < 9:
                            s1_load(j + 1)
                        t0, n = blk_range(j)
                        h_, h_t = hb[j % 2], hb_t[j % 2]
                        if j > 0:
                            for c in range(3):
                                for kc in range(8):
                                    S.op("pe", lambda e: e.matmul(out=pq[c][:, 0:n], lhsT=Wm[:, kc, c * 128:(c + 1) * 128],
                                                                  rhs=h_[:, kc, 0:n], start=(kc == 0), stop=(kc == 7)),
                                         reads=[Wm_t, h_t], writes=[pq_t[c]], signal=(kc == 7))
                        for c in range(2):
                            for kc in range(8):
                                S.op("pe", lambda e: e.matmul(out=pk[c][:, 0:n], lhsT=Wm[:, kc, 384 + c * 128:384 + (c + 1) * 128],
                                                              rhs=h_[:, kc, 0:n], start=(kc == 0), stop=(kc == 7)),
                                     reads=[Wm_t, h_t], writes=[pk_t[c]], signal=(kc == 7))
                        for c in range(2):
                            for kc in range(8):
                                S.op("pe", lambda e: e.matmul(out=pr[c][0:64, 0:n], lhsT=Wm[:, kc, 640 + c * 64:704 + c * 64],
                                                              rhs=h_[:, kc, 0:n], start=(kc == 0), stop=(kc == 7)),
                                     reads=[Wm_t, h_t], writes=[pr_t[c]], signal=(kc == 7))
                        if j > 0:
                            rms_T(pq, pq_t, 3, 384, 0, cqn, cqn_t, t0 - 256, n)
                        rms_T(pk, pk_t, 2, 256, 4, ckvn, ckvn_t, t0, n)
                        tcc, tcc_t = tc_[j % 2], tc_t[j % 2]
                        S.op("dve", lambda e: e.tensor_tensor(out=r1[:, 0:n], in0=pr[0][0:64, 0:n], in1=tcc[:, 0, 0:n],
                                                              op=ALU.mult), reads=[pr_t[0], tcc_t], writes=[r_t])
                        S.op("dve", lambda e: e.tensor_tensor(out=r2[:, 0:n], in0=pr[1][0:64, 0:n], in1=tcc[:, 1, 0:n],
                                                              op=ALU.mult), reads=[pr_t[1], tcc_t], writes=[r_t])
                        S.op("dve", lambda e: e.tensor_tensor(out=krT[:, t0:t0 + n], in0=r1[:, 0:n], in1=r2[:, 0:n],
                                                              op=ALU.add), reads=[r_t], writes=[krT_t])
                    S.barrier()

                with contextlib.ExitStack() as s2:
                    KT = sb(s2, "KT", [128, LT], BF16)
                    KT_t = Tk()
                    V = sb(s2, "V", [128, NT, 128], BF16)
                    V_t = Tk()
                    QT = sb(s2, "QT", [128, L], BF16)
                    QT_t = Tk()
                    QrT = sb(s2, "QrT", [64, L], BF16)
                    QrT_t = Tk()
                    Wq = sb(s2, "Wq", [128, 3, 256], BF16)
                    Wq_t = Tk()
                    Wkv = sb(s2, "Wkv", [128, 2, 256], BF16)
                    Wkv_t = Tk()
                    sq_ = sb(s2, "s2sq", [128, 3, 192], F32)
                    sq_t = Tk()
                    skv = sb(s2, "s2skv", [128, 2, 256], F32)
                    skv_t = Tk()
                    tq = [sb(s2, f"s2tq{i}", [64, 2, 512], F32) for i in range(2)]
                    tq_t = [Tk(), Tk()]
                    r1 = sb(s2, "s2r1", [64, 512], F32)
                    r2 = sb(s2, "s2r2", [64, 512], F32)
                    r_t = Tk()
                    PT = [sb(s2, f"PT{i}", [128, 512], BF16) for i in range(4)]
                    PT_t = [Tk() for _ in range(4)]
                    accA = [sb(s2, f"s2accA{i}", [128, 512], F32) for i in range(2)]
                    accB = [sb(s2, f"s2accB{i}", [128, 512], F32) for i in range(2)]
                    accA_t = [Tk(), Tk()]
                    accB_t = [Tk(), Tk()]
                    rs = sb(s2, "s2rs", [128, 512], F32)
                    rs_t = Tk()
                    ot = [sb(s2, f"s2ot{i}", [128, 512], BF16) for i in range(2)]
                    ot_t = [Tk(), Tk()]
                    pS = [ps(s2, f"pS{i}", [128, 512]) for i in range(3)]
                    pS_t = [Tk() for _ in range(3)]
                    pO = [ps(s2, f"pO{i}", [128, 512]) for i in range(2)]
                    pO_t = [Tk() for _ in range(2)]
                    pZ = [ps(s2, f"pZ{i}", [128, 512]) for i in range(2)]
                    pZ_t = [Tk() for _ in range(2)]
                    pX = ps(s2, "pX", [128, 512])
                    pX_t = Tk()
                    wuqv = wuq_d[0].rearrange("(kc p) n -> p kc n", p=128)
                    wukvv = wukv_d[0].rearrange("(kc p) n -> p kc n", p=128)
                    sc = 192.0 ** -0.5
                    cnt = 0
                    for h in range(8):
                        S.dma(sq_[:], wuqv[:, :, h * 192:(h + 1) * 192], writes=[sq_t])
                        S.op("pool", lambda e: e.tensor_copy(out=Wq[:, :, 0:192], in_=sq_[:]), reads=[sq_t], writes=[Wq_t])
                        for (dst, src) in ((192, 144), (208, 128), (224, 176), (240, 160)):
                            S.op("pool", lambda e: e.tensor_copy(out=Wq[:, :, dst:dst + 16], in_=sq_[:, :, src:src + 16]),
                                 reads=[sq_t], writes=[Wq_t])
                        S.dma(skv[:], wukvv[:, :, h * 256:(h + 1) * 256], writes=[skv_t])
                        S.op("pool", lambda e: e.tensor_copy(out=Wkv[:], in_=skv[:]), reads=[skv_t], writes=[Wkv_t])
                        for j in range(9):
                            t0, n = (0, 256) if j == 0 else (256 + (j - 1) * 512, 512)
                            for kc in range(2):
                                S.op("pe", lambda e: e.matmul(out=pX[:, 0:n], lhsT=Wkv[:, kc, 0:128], rhs=ckvn[:, kc, t0:t0 + n],
                                                              start=(kc == 0), stop=(kc == 1)),
                                     reads=[Wkv_t, ckvn_t], writes=[pX_t], signal=(kc == 1))
                            S.op("act" if j % 2 else "dve", (lambda e: e.copy(out=KT[:, t0:t0 + n], in_=pX[:, 0:n])) if j % 2
                                 else (lambda e: e.tensor_copy(out=KT[:, t0:t0 + n], in_=pX[:, 0:n])),
                                 reads=[pX_t], writes=[KT_t])
                        for g in range(9):
                            tiles = list(range(g * 4, min(g * 4 + 4, NT)))
                            for i, t in enumerate(tiles):
                                for kc in range(2):
                                    S.op("pe", lambda e: e.matmul(out=pX[:, i * 128:(i + 1) * 128],
                                                                  lhsT=ckvn[:, kc, t * 128:(t + 1) * 128], rhs=Wkv[:, kc, 128:256],
                                                                  start=(kc == 0), stop=(kc == 1)),
                                         reads=[Wkv_t, ckvn_t], writes=[pX_t], signal=(kc == 1 and i == len(tiles) - 1))
                            nn = len(tiles)
                            S.op("act" if g % 2 else "dve",
                                 (lambda e: e.copy(out=V[:, tiles[0]:tiles[0] + nn, :].rearrange("p a b -> p (a b)"),
                                                   in_=pX[:, 0:nn * 128])) if g % 2 else
                                 (lambda e: e.tensor_copy(out=V[:, tiles[0]:tiles[0] + nn, :].rearrange("p a b -> p (a b)"),
                                                          in_=pX[:, 0:nn * 128])),
                                 reads=[pX_t], writes=[V_t])
                        for j in range(8):
                            q0 = j * 512
                            S.dma(tq[j % 2][:, 0, :], mc_d[:, 256 + q0:256 + q0 + 512], writes=[tq_t[j % 2]])
                            S.dma(tq[j % 2][:, 1, :], ms_d[:, 256 + q0:256 + q0 + 512], writes=[tq_t[j % 2]])
                            for kc in range(3):
                                S.op("pe", lambda e: e.matmul(out=pX[:], lhsT=Wq[:, kc, 0:128], rhs=cqn[:, kc, q0:q0 + 512],
                                                              start=(kc == 0), stop=(kc == 2)),
                                     reads=[Wq_t, cqn_t], writes=[pX_t], signal=(kc == 2))
                            S.op("act", lambda e: e.copy(out=QT[:, q0:q0 + 512], in_=pX[:]), reads=[pX_t], writes=[QT_t])
                            for c in range(2):
                                for kc in range(3):
                                    S.op("pe", lambda e: e.matmul(out=pZ[c][0:64, :], lhsT=Wq[:, kc, 128 + c * 64:192 + c * 64],
                                                                  rhs=cqn[:, kc, q0:q0 + 512], start=(kc == 0), stop=(kc == 2)),
                                         reads=[Wq_t, cqn_t], writes=[pZ_t[c]], signal=(kc == 2))
                            tt_, tt_t = tq[j % 2], tq_t[j % 2]
                            S.op("dve", lambda e: e.tensor_tensor(out=r1[:], in0=pZ[0][0:64, :], in1=tt_[:, 0, :], op=ALU.mult),
                                 reads=[pZ_t[0], tt_t], writes=[r_t])
                            S.op("dve", lambda e: e.tensor_tensor(out=r2[:], in0=pZ[1][0:64, :], in1=tt_[:, 1, :], op=ALU.mult),
                                 reads=[pZ_t[1], tt_t], writes=[r_t])
                            S.op("dve", lambda e: e.tensor_tensor(out=QrT[:, q0:q0 + 512], in0=r1[:], in1=r2[:], op=ALU.add),
                                 reads=[r_t], writes=[QrT_t])
                        for qb in range(8):
                            q0 = qb * 512
                            po, po_t = pO[qb % 2], pO_t[qb % 2]
                            pz, pz_t = pZ[qb % 2], pZ_t[qb % 2]
                            def emit_st(kt, cn):
                                s_, s_t = pS[cn % 3], pS_t[cn % 3]
                                S.op("pe", lambda e: e.matmul(out=s_[:], lhsT=KT[:, kt * 128:(kt + 1) * 128], rhs=QT[:, q0:q0 + 512],
                                                              start=True, stop=False), reads=[KT_t, QT_t], writes=[s_t], signal=False)
                                S.op("pe", lambda e: e.matmul(out=s_[:], lhsT=krT[:, kt * 128:(kt + 1) * 128], rhs=QrT[:, q0:q0 + 512],
                                                              start=False, stop=True), reads=[krT_t, QrT_t], writes=[s_t])

                            emit_st(0, cnt)
                            for kt in range(NT):
                                s_, s_t = pS[cnt % 3], pS_t[cnt % 3]
                                p_, p_t = PT[cnt % 4], PT_t[cnt % 4]
                                if kt + 1 < NT:
                                    emit_st(kt + 1, cnt + 1)
                                cnt += 1
                                S.op("act", lambda e: e.activation(out=p_[:], in_=s_[:], func=AF.Exp, scale=sc),
                                     reads=[s_t], writes=[p_t])
                                S.op("pe", lambda e: e.matmul(out=po[:], lhsT=V[:, kt, :], rhs=p_[:], start=(kt == 0),
                                                              stop=(kt == NT - 1)), reads=[V_t, p_t], writes=[po_t])
                                aa, aa_t = accA[qb % 2], accA_t[qb % 2]
                                if kt == 0:
                                    S.op("dve", lambda e: e.tensor_copy(out=aa[:], in_=p_[:]), reads=[p_t], writes=[aa_t])
                                else:
                                    S.op("dve", lambda e: e.tensor_tensor(out=aa[:], in0=aa[:], in1=p_[:], op=ALU.add),
                                         reads=[p_t, aa_t], writes=[aa_t])
                            S.op("pe", lambda e: e.matmul(out=pz[:], lhsT=onesf, rhs=accA[qb % 2][:], start=True, stop=True),
                                 reads=[cst_t, accA_t[qb % 2]], writes=[pz_t])
                            S.op("dve", lambda e: e.reciprocal(out=rs[:], in_=pz[:]), reads=[pz_t], writes=[rs_t])
                            o_, o_t = ot[qb % 2], ot_t[qb % 2]
                            S.op("dve", lambda e: e.tensor_tensor(out=o_[:], in0=po[:], in1=rs[:], op=ALU.mult),
                                 reads=[po_t, rs_t], writes=[o_t])
                            S.dma(att_d[h, :, q0:q0 + 512], o_[:], reads=[o_t], writes=[att_t[qb]])
                    S.barrier()

            with contextlib.ExitStack() as st:
                Wqk = sb(st, "Wqk", [128, 8, 512], BF16)
                Wv = sb(st, "Wv", [128, 8, 512], BF16)
                Wg = sb(st, "Wg", [128, 8, 512], BF16)
                Wqk_t, Wv_t, Wg_t = Tk(), Tk(), Tk()
                stg = sb(st, "s3stg", [128, 8, 512], F32)
                stg_t = Tk()
                hb = [sb(st, f"s3h{i}", [128, 8, 128], BF16) for i in range(3)]
                hb_t = [Tk() for _ in range(3)]
                tb = [sb(st, f"s3tb{i}", [128, 2, 256], F32) for i in range(3)]
                tb_t = [Tk() for _ in range(3)]
                ofb = [sb(st, f"s3of{i}", [128, 512], F32) for i in range(3)]
                ofb_t = [Tk() for _ in range(3)]
                Sf = sb(st, "Sf", [128, 2, 512], F32)
                Sb_ = sb(st, "Sb", [128, 2, 512], BF16)
                Sf_t = Tk()
                Sb_t = Tk()
                t1 = sb(st, "s3t1", [128, 512], F32)
                t2 = sb(st, "s3t2", [128, 512], F32)
                t12_t = Tk()
                qk_all = sb(st, "s3qkall", [128, NT, 512], BF16)
                qka_t = [Tk() for _ in range(NT)]
                V_all = sb(st, "s3Vall", [128, NT, 512], BF16)
                Va_t = [Tk() for _ in range(NT)]
                Kdb = [sb(st, f"s3Kd{i}", [128, 256], BF16) for i in range(2)]
                Kdb_t = [Tk(), Tk()]
                KTt = sb(st, "s3KT", [128, 2, 128], BF16)
                QTt = sb(st, "s3QT", [128, 2, 128], BF16)
                QdT = sb(st, "s3QdT", [128, 2, 128], BF16)
                tr_t = Tk()
                STm = sb(st, "s3STm", [128, 128], BF16)
                STm_t = Tk()
                osb = sb(st, "s3o", [128, 512], F32)
                osb_t = Tk()
                sg = sb(st, "s3sg", [128, 512], F32)
                sg_t = Tk()
                scr = sb(st, "s3scr", [128, 512], BF16)
                ssb = sb(st, "s3ss", [128, 4], F32)
                ss_t = Tk()
                rr = sb(st, "s3r", [128, 512], BF16)
                rr_t = Tk()
                rTs = [sb(st, f"s3rT{i}", [128, 4, 128], BF16) for i in range(2)]
                rTs_t = [Tk(), Tk()]
                pAl = [ps(st, f"s3pA{i}", [128, 512]) for i in range(2)]
                pAl_t = [Tk(), Tk()]
                pBl = [ps(st, f"s3pB{i}", [128, 512]) for i in range(2)]
                pBl_t = [Tk(), Tk()]
                pC = ps(st, "s3pC", [128, 512])
                pD = ps(st, "s3pD", [128, 2, 512], BF16)
                pE = ps(st, "s3pE", [128, 512])
                pF = ps(st, "s3pF", [128, 512])
                pC_t, pD_t, pD2_t, pE_t, pF_t = (Tk() for _ in range(5))
                pH = [pC, pE]
                pH_t = [pC_t, pE_t]
                for h in range(4):
                    for (dst, c0, scl) in ((Wqk[:, :, 0:256], h * 256, None), (Wqk[:, :, 256:512], 1024 + h * 256, 0.0625)):
                        S.dma(stg[:, :, 0:256], winv[:, :, c0:c0 + 256], writes=[stg_t])
                        if scl is None:
                            S.op("act", lambda e: e.copy(out=dst, in_=stg[:, :, 0:256]), reads=[stg_t], writes=[Wqk_t])
                        else:
                            S.op("act", lambda e: e.mul(out=dst, in_=stg[:, :, 0:256], mul=scl), reads=[stg_t], writes=[Wqk_t])
                    for (dst, c0, wt_) in ((Wv, 2048 + h * 512, Wv_t), (Wg, 4096 + h * 512, Wg_t)):
                        S.dma(stg[:], winv[:, :, c0:c0 + 512], writes=[stg_t])
                        S.op("act", lambda e: e.copy(out=dst[:], in_=stg[:]), reads=[stg_t], writes=[wt_])
                    for di, dirn in enumerate(("f", "b")):
                        order = list(range(NT)) if dirn == "f" else [1, 0] + list(range(NT - 1, 1, -1))
                        MT = MTf if dirn == "f" else MTb
                        QD = QDf if dirn == "f" else QDb
                        kdc = KD[:, h, di:di + 1]
                        cdc = KD[:, h, 2 + di:3 + di]
                        S.op("pool", lambda e: e.memset(Sf[:], 0.0), writes=[Sf_t])
                        S.op("pool", lambda e: e.memset(Sb_[:], 0.0), writes=[Sb_t])

                        def s3_load(idx):
                            t = order[idx]
                            S.dma(hb[idx % 3][:], hT_d[t], reads=[hT_t[t]], writes=[hb_t[idx % 3]])
                            if dirn == "f":
                                S.dma(tb[idx % 3][:, 0, :], rc_d[t * 128:(t + 1) * 128, :], writes=[tb_t[idx % 3]])
                                S.dma(tb[idx % 3][:, 1, :], rs_d[t * 128:(t + 1) * 128, :], writes=[tb_t[idx % 3]])
                            if dirn == "b" and t >= 2:
                                S.dma(ofb[idx % 3][:], of_d[t - 2], reads=[of_t[t - 2]], writes=[ofb_t[idx % 3]])

                        def s3_proj(idx):
                            if dirn == "b":
                                return
                            h_, h_t = hb[idx % 3], hb_t[idx % 3]
                            pA, pA_t = pAl[idx % 2], pAl_t[idx % 2]
                            pB, pB_t = pBl[idx % 2], pBl_t[idx % 2]
                            for kc in range(8):
                                S.op("pe", lambda e: e.matmul(out=pA[:], lhsT=h_[:, kc, :], rhs=Wqk[:, kc, :], start=(kc == 0),
                                                              stop=(kc == 7)), reads=[h_t, Wqk_t], writes=[pA_t], signal=(kc == 7))
                            for kc in range(8):
                                S.op("pe", lambda e: e.matmul(out=pB[:], lhsT=h_[:, kc, :], rhs=Wv[:, kc, :], start=(kc == 0),
                                                              stop=(kc == 7)), reads=[h_t, Wv_t], writes=[pB_t], signal=(kc == 7))

                        def s3_A(idx):
                            t = order[idx]
                            tb_, tbt = tb[idx % 3], tb_t[idx % 3]
                            pA, pA_t = pAl[idx % 2], pAl_t[idx % 2]
                            pB, pB_t = pBl[idx % 2], pBl_t[idx % 2]
                            qk, qk_t = qk_all[:, t, :], qka_t[t]
                            Vb, Vb_t = V_all[:, t, :], Va_t[t]
                            Kd, Kd_t = Kdb[idx % 2], Kdb_t[idx % 2]
                            for half in (range(2) if dirn == "f" else ()):
                                o = half * 256
                                S.op("dve", lambda e: e.tensor_tensor(out=t1[:, o:o + 256], in0=pA[:, o:o + 256], in1=tb_[:, 0, :],
                                                                      op=ALU.mult), reads=[pA_t, tbt], writes=[t12_t])
                                for part in range(2):
                                    a = o + part * 128
                                    S.op("dve", lambda e: e.tensor_tensor(out=t2[:, a:a + 64], in0=pA[:, a + 64:a + 128],
                                                                          in1=tb_[:, 1, part * 128:part * 128 + 64], op=ALU.mult),
                                         reads=[pA_t, tbt], writes=[t12_t])
                                    S.op("dve", lambda e: e.tensor_tensor(out=t2[:, a + 64:a + 128], in0=pA[:, a:a + 64],
                                                                          in1=tb_[:, 1, part * 128 + 64:part * 128 + 128], op=ALU.mult),
                                         reads=[pA_t, tbt], writes=[t12_t])
                            if dirn == "f":
                                S.op("pool", lambda e: e.tensor_tensor(out=qk[:], in0=t1[:], in1=t2[:], op=ALU.add),
                                     reads=[t12_t], writes=[qk_t])
                                S.op("act", lambda e: e.copy(out=Vb[:], in_=pB[:]), reads=[pB_t], writes=[Vb_t])
                            S.op("pool", lambda e: e.tensor_scalar(out=Kd[:], in0=qk[:, 256:512], scalar1=kdc, scalar2=None, op0=ALU.mult),
                                 reads=[qk_t, dec_t], writes=[Kd_t])

                        s3_load(0)
                        s3_load(1)
                        s3_proj(0)
                        s3_A(0)
                        for idx in range(NT):
                            if idx + 2 < NT:
                                s3_load(idx + 2)
                            if idx + 1 < NT:
                                s3_proj(idx + 1)
                                s3_A(idx + 1)
                            t = order[idx]
                            lat = t >= 2
                            h_, h_t = hb[idx % 3], hb_t[idx % 3]
                            tb_, tbt = tb[idx % 3], tb_t[idx % 3]
                            pA, pA_t = pAl[idx % 2], pAl_t[idx % 2]
                            pB, pB_t = pBl[idx % 2], pBl_t[idx % 2]
                            qk, qk_t = qk_all[:, t, :], qka_t[t]
                            Vb, Vb_t = V_all[:, t, :], Va_t[t]
                            Kd, Kd_t = Kdb[idx % 2], Kdb_t[idx % 2]
                            if lat and dirn == "b":
                                for kc in range(8):
                                    S.op("pe", lambda e: e.matmul(out=pC[:], lhsT=h_[:, kc, :], rhs=Wg[:, kc, :], start=(kc == 0),
                                                                  stop=(kc == 7)), reads=[h_t, Wg_t], writes=[pC_t], signal=(kc == 7))
                                S.op("act", lambda e: e.activation(out=sg[:], in_=pC[:], func=AF.Silu), reads=[pC_t], writes=[sg_t])
                            if lat:
                                for c in range(4):
                                    S.op("pe", lambda e: e.transpose(out=pD[:, 0, c * 128:(c + 1) * 128], in_=qk[:, c * 128:(c + 1) * 128],
                                                                     identity=identb[:]), reads=[qk_t, cb_t], writes=[pD_t], signal=(c == 3))
                                S.op("act", lambda e: e.copy(out=KTt[:].rearrange("p a b -> p (a b)"), in_=pD[:, 0, 256:512]),
                                     reads=[pD_t], writes=[tr_t])
                                S.op("act", lambda e: e.copy(out=QTt[:].rearrange("p a b -> p (a b)"), in_=pD[:, 0, 0:256]),
                                     reads=[pD_t], writes=[tr_t])
                                for dc in range(2):
                                    S.op("dve", lambda e: e.tensor_tensor(out=QdT[:, dc, :], in0=pD[:, 0, dc * 128:(dc + 1) * 128],
                                                                          in1=QD[:, h, :], op=ALU.mult), reads=[pD_t, dec_t], writes=[tr_t])
                                for dc in range(2):
                                    S.op("pe", lambda e: e.matmul(out=pE[:, 0:128], lhsT=KTt[:, dc, :], rhs=QTt[:, dc, :], start=(dc == 0),
                                                                  stop=(dc == 1)), reads=[tr_t], writes=[pE_t], signal=(dc == 1))
                                S.op("dve", lambda e: e.tensor_tensor(out=STm[:], in0=pE[:, 0:128], in1=MT[:, h, :], op=ALU.mult),
                                     reads=[pE_t, dec_t], writes=[STm_t])
                                S.op("pe", lambda e: e.matmul(out=pF[:], lhsT=STm[:], rhs=Vb[:], start=True, stop=False),
                                     reads=[STm_t, Vb_t], writes=[pF_t], signal=False)
                                for dc in range(2):
                                    S.op("pe", lambda e: e.matmul(out=pF[:], lhsT=QdT[:, dc, :], rhs=Sb_[:, dc, :], start=False,
                                                                  stop=(dc == 1)), reads=[tr_t, Sb_t], writes=[pF_t], signal=(dc == 1))
                            for dc in range(2):
                                S.op("pe", lambda e: e.matmul(out=pH[dc][:], lhsT=Kd[:, dc * 128:(dc + 1) * 128], rhs=Vb[:], start=True,
                                                              stop=True), reads=[Kd_t, Vb_t], writes=[pH_t[dc]])
                            if lat:
                                if dirn == "f":
                                    S.op("act", lambda e: e.copy(out=osb[:], in_=pF[:]), reads=[pF_t], writes=[osb_t])
                                    S.dma(of_d[t - 2], osb[:], reads=[osb_t], writes=[of_t[t - 2]])
                                else:
                                    of_, of_tt = ofb[idx % 3], ofb_t[idx % 3]
                                    S.op("dve", lambda e: e.tensor_tensor(out=osb[:], in0=pF[:], in1=of_[:], op=ALU.add),
                                         reads=[pF_t, of_tt], writes=[osb_t])
                                    S.op("pool", lambda e: e.memset(ssb[:, 0:1], 0.0), writes=[ss_t])
                                    S.op("act", lambda e: e.activation(out=scr[:], in_=osb[:], func=AF.Square, accum_out=ssb[:, 0:1]),
                                         reads=[osb_t, ss_t], writes=[ss_t])
                                    S.op("act", lambda e: e.activation(out=ssb[:, 1:2], in_=ssb[:, 0:1], func=AF.Sqrt, scale=1.0 / 512,
                                                                       bias=EPS), reads=[ss_t], writes=[ss_t])
                                    S.op("dve", lambda e: e.reciprocal(out=ssb[:, 2:3], in_=ssb[:, 1:2]), reads=[ss_t], writes=[ss_t])
                                    S.op("dve", lambda e: e.scalar_tensor_tensor(out=rr[:], in0=osb[:], scalar=ssb[:, 2:3], in1=sg[:],
                                                                                 op0=ALU.mult, op1=ALU.mult),
                                         reads=[osb_t, ss_t, sg_t], writes=[rr_t])
                                    for c in range(4):
                                        S.op("pe", lambda e: e.transpose(out=pD[:, 1, c * 128:(c + 1) * 128], in_=rr[:, c * 128:(c + 1) * 128],
                                                                         identity=identb[:]), reads=[rr_t, cb_t], writes=[pD2_t], signal=(c == 3))
                                    rT_, rT_tt = rTs[idx % 2], rTs_t[idx % 2]
                                    S.op("act", lambda e: e.copy(out=rT_[:].rearrange("p a b -> p (a b)"), in_=pD[:, 1, :]),
                                         reads=[pD2_t], writes=[rT_tt])
                                    tk0 = (t - 2) * 128
                                    S.dma(rT_d[h, :, :, tk0:tk0 + 128].rearrange("c p t -> p c t"), rT_[:], reads=[rT_tt],
                                          writes=[rT_t[(t - 2) // 4]])
                            for dc in range(2):
                                S.op("dve", lambda e: e.scalar_tensor_tensor(out=Sb_[:, dc, :], in0=Sf[:, dc, :], scalar=cdc, in1=pH[dc][:],
                                                                             op0=ALU.mult, op1=ALU.add),
                                     reads=[Sf_t, pH_t[dc], dec_t], writes=[Sb_t])
                                S.op("dve", lambda e: e.scalar_tensor_tensor(out=Sf[:, dc, :], in0=Sf[:, dc, :], scalar=cdc, in1=pH[dc][:],
                                                                             op0=ALU.mult, op1=ALU.add),
                                     reads=[Sf_t, pH_t[dc], dec_t], writes=[Sf_t])
                S.barrier()

            with contextlib.ExitStack() as st:
                Wgr = sb(st, "Wgr", [128, 8, 1024], BF16)
                Wgm = sb(st, "Wgm", [128, 8, 1024], BF16)
                Wbr = sb(st, "Wbr", [128, 16, 1024], BF16)
                Wbm = sb(st, "Wbm", [128, 8, 1024], BF16)
                Wo = sb(st, "Wo", [128, 8, 1024], BF16)
                stg = [sb(st, f"s4stg{i}", [128, 8, 256], F32) for i in range(2)]
                stg_t = [Tk(), Tk()]
                wbrv = wbr_d[0].rearrange("(kc p) n -> p kc n", p=128)
                wbmv = wbm_d[0].rearrange("(kc p) n -> p kc n", p=128)
                wov = wo_d[0].rearrange("(kc p) n -> p kc n", p=128)
                k = 0
                jobs = []
                W4_t = [Tk() for _ in range(4)]
                Wo_t = Tk()
                for cb in range(4):
                    cs = slice(cb * 256, (cb + 1) * 256)
                    jobs.append((Wbr[:, 0:8, cs], wbrv[:, 0:8, cs], W4_t[cb]))
                    jobs.append((Wbr[:, 8:16, cs], wbrv[:, 8:16, cs], W4_t[cb]))
                    jobs.append((Wbm[:, :, cs], wbmv[:, :, cs], W4_t[cb]))
                    jobs.append((Wgr[:, :, cs], winv[:, :, 6848 + cb * 256:6848 + (cb + 1) * 256], W4_t[cb]))
                    jobs.append((Wgm[:, :, cs], winv[:, :, 7872 + cb * 256:7872 + (cb + 1) * 256], W4_t[cb]))
                for cb in range(4):
                    cs = slice(cb * 256, (cb + 1) * 256)
                    jobs.append((Wo[:, :, cs], wov[:, :, cs], Wo_t))
                for (dst, src, wt_) in jobs:
                    load_cast(st, dst, wt_, src, None, stg[k % 2][:], stg_t[k % 2], "pool" if k % 2 else "dve")
                    k += 1
                TK4 = 256
                hb = sb(st, "s4h", [128, 8, TK4], BF16)
                hb_t = Tk()
                rb = sb(st, "s4r", [128, 16, TK4], BF16)
                rb_t = Tk()
                ab = sb(st, "s4a", [128, 8, TK4], BF16)
                ab_t = Tk()
                mTb = sb(st, "s4m", [128, 8, TK4], BF16)
                mTb_t = Tk()
                s3_ = sb(st, "s4s3", [128, TK4], F32)
                s4_ = sb(st, "s4s4", [128, TK4], F32)
                sg_t = Tk()
                m1 = sb(st, "s4m1", [128, TK4], F32)
                m2 = sb(st, "s4m2", [128, TK4], F32)
                m_t = Tk()
                x_ = sb(st, "s4x", [128, D], F32)
                x_t = Tk()
                yt = sb(st, "s4y", [128, D], F32)
                yt_t = Tk()
                o_ = sb(st, "s4xo", [128, D], F32)
                o_t = Tk()
                P = [ps(st, f"s4p{i}", [128, 512]) for i in range(4)]
                P_t = [Tk() for _ in range(4)]
                PY = [ps(st, f"s4py{i}", [128, 512]) for i in range(2)]
                PY_t = [Tk() for _ in range(2)]
                for tbk in range(L // TK4):
                    q0 = tbk * TK4
                    for i in range(TK4 // 128):
                        tl = 2 + tbk * (TK4 // 128) + i
                        S.dma(hb[:, :, i * 128:(i + 1) * 128], hT_d[tl], reads=[hT_t[tl]], writes=[hb_t])
                    for hh in range(4):
                        S.dma(rb[:, hh * 4:(hh + 1) * 4, :], rT_d[hh, :, :, q0:q0 + TK4].rearrange("c p t -> p c t"),
                              reads=[rT_t[q0 // 512]], writes=[rb_t])
                    S.dma(ab[:], att_d[:, :, q0:q0 + TK4].rearrange("h p t -> p h t"), reads=[att_t[q0 // 512]], writes=[ab_t])
                    for fc in range(8):
                        fs = slice(fc * 128, (fc + 1) * 128)
                        for kc in range(16):
                            S.op("pe", lambda e: e.matmul(out=P[0][:, 0:TK4], lhsT=Wbr[:, kc, fs], rhs=rb[:, kc, :], start=(kc == 0), stop=(kc == 15)),
                                 reads=[W4_t[fc // 2], rb_t], writes=[P_t[0]], signal=(kc == 15))
                        for (pi, Wx, src, src_t) in ((1, Wbm, ab, ab_t), (2, Wgr, hb, hb_t), (3, Wgm, hb, hb_t)):
                            for kc in range(8):
                                S.op("pe", lambda e: e.matmul(out=P[pi][:, 0:TK4], lhsT=Wx[:, kc, fs], rhs=src[:, kc, :], start=(kc == 0),
                                                              stop=(kc == 7)), reads=[W4_t[fc // 2], src_t], writes=[P_t[pi]], signal=(kc == 7))
                        S.op("act", lambda e: e.activation(out=s3_[:], in_=P[2][:, 0:TK4], func=AF.Sigmoid), reads=[P_t[2]], writes=[sg_t])
                        S.op("act", lambda e: e.activation(out=s4_[:], in_=P[3][:, 0:TK4], func=AF.Sigmoid), reads=[P_t[3]], writes=[sg_t])
                        S.op("dve", lambda e: e.tensor_tensor(out=m1[:], in0=P[0][:, 0:TK4], in1=s3_[:], op=ALU.mult),
                             reads=[P_t[0], sg_t], writes=[m_t])
                        S.op("dve", lambda e: e.tensor_tensor(out=m2[:], in0=P[1][:, 0:TK4], in1=s4_[:], op=ALU.mult),
                             reads=[P_t[1], sg_t], writes=[m_t])
                        S.op("pool", lambda e: e.tensor_tensor(out=mTb[:, fc, :], in0=m1[:], in1=m2[:], op=ALU.add),
                             reads=[m_t], writes=[mTb_t])
                    for tt in range(TK4 // 128):
                        gt = tbk * (TK4 // 128) + tt
                        S.dma(x_[:], x_d[b, gt * 128:(gt + 1) * 128, :], writes=[x_t])
                        for cb in range(2):
                            for kc in range(8):
                                S.op("pe", lambda e: e.matmul(out=PY[cb][:], lhsT=mTb[:, kc, tt * 128:(tt + 1) * 128],
                                                              rhs=Wo[:, kc, cb * 512:(cb + 1) * 512], start=(kc == 0), stop=(kc == 7)),
                                     reads=[mTb_t, Wo_t], writes=[PY_t[cb]], signal=(kc == 7))
                            S.op("dve", lambda e: e.tensor_tensor(out=yt[:, cb * 512:(cb + 1) * 512], in0=PY[cb][:],
                                                                  in1=G1[:, b, cb * 512:(cb + 1) * 512], op=ALU.mult),
                                 reads=[PY_t[cb], G_t], writes=[yt_t])
                        S.op("pool", lambda e: e.tensor_tensor(out=o_[:], in0=yt[:], in1=x_[:], op=ALU.add),
                             reads=[yt_t, x_t], writes=[o_t])
                        S.dma(x1_d[b * 32 + gt], o_[:], reads=[o_t], writes=[x1_t[b * 32 + gt]])
                S.barrier()

        mix_stack.close()
        I32 = mybir.dt.int32
        NTI = NB * 32
        with contextlib.ExitStack() as st:
            posk = sb(st, "posk", [128, NTI * 4], F32)
            ekk = sb(st, "ekk", [128, NTI * 4], F32)
            g4 = sb(st, "g4", [128, NTI * 4], F32)
            pk_t = Tk()
            base = sb(st, "base", [128, 32], F32)
            base_t = Tk()
            desti = sb(st, "desti", [128, NTI * 4], I32)
            desti_t = Tk()
            widx = sb(st, "widx", [128, 8, NBLK], I32)
            bidx = sb(st, "bidx", [2, NBLK], I32)
            widx_t = Tk()
            iota32 = cst[:, 10, 0:32]
            h2d_t = [Tk() for _ in range(NTI)]
            with contextlib.ExitStack() as p1:
                GS2 = sb(p1, "GS2", [128, NB, D], F32)
                SH2 = sb(p1, "SH2", [128, NB, D], F32)
                GS_t = Tk()
                pB2 = [ps(p1, f"p1B{i}", [128, 512]) for i in range(2)]
                pB2_t = [Tk(), Tk()]
                pR = ps(p1, "p1R", [128, 512])
                pR_t = Tk()
                dg = sb(p1, "p1dg", [128, 8, 128], F32)
                dg_t = Tk()
                for bb in range(NB):
                    for (dst, vfn) in ((GS2, lambda c: gs2[:, bb, c:c + 1]), (SH2, lambda c: mT[:, 24 + c, bb:bb + 1])):
                        for c in range(8):
                            S.op("dve", lambda e: e.tensor_scalar(out=dg[:, c, :], in0=identf, scalar1=vfn(c), scalar2=None,
                                                                  op0=ALU.mult), reads=[cst_t, mT_t, gs_t], writes=[dg_t])
                        for c in range(8):
                            S.op("pe", lambda e: e.matmul(out=pB2[c // 4][:, (c % 4) * 128:(c % 4 + 1) * 128], lhsT=onesf,
                                                          rhs=dg[:, c, :], start=True, stop=True),
                                 reads=[dg_t, cst_t], writes=[pB2_t[c // 4]])
                        for hh in range(2):
                            S.op("act", lambda e: e.copy(out=dst[:, bb, hh * 512:(hh + 1) * 512], in_=pB2[hh][:]),
                                 reads=[pB2_t[hh]], writes=[GS_t])
                bfull = sb(p1, "p1bfull", [32, 3072], F32)
                bfb = sb(p1, "p1bfb", [32, 3072], BF16)
                bf_t = Tk()
                S.dma(bfull[:, 0:2048], ebgu_d[0], writes=[bf_t])
                S.dma(bfull[:, 2048:3072], ebd_d[0], writes=[bf_t])
                bfb_t = Tk()
                S.op("dve", lambda e: e.tensor_copy(out=bfb[:], in_=bfull[:]), reads=[bf_t], writes=[bfb_t])
                S.dma(bias_d[:, :], bfb[:], reads=[bfb_t])
                Wr = sb(p1, "Wr", [128, 8, 32], F32)
                Wr_t = Tk()
                S.dma(Wr[:], rw_d[0].rearrange("(kc p) n -> p kc n", p=128), writes=[Wr_t])
                rbs = sb(p1, "rbs", [1, 32], F32)
                rbs_t = Tk()
                S.dma(rbs[:], rb_d[:, :], writes=[rbs_t])
                xt = [sb(p1, f"p1x{i}", [128, D], F32) for i in range(2)]
                xt_t = [Tk(), Tk()]
                xn = sb(p1, "p1xn", [128, D], F32)
                xn_t = Tk()
                h2 = sb(p1, "p1h2", [128, D], F32)
                h2_t = Tk()
                h2bf = [sb(p1, f"p1h2b{i}", [128, D], BF16) for i in range(2)]
                h2bf_t = [Tk(), Tk()]
                h2f = sb(p1, "p1h2f", [128, 8, 128], F32)
                h2f_t = Tk()
                scr = sb(p1, "p1scr", [128, D], BF16)
                ssb = sb(p1, "p1ss", [128, 4], F32)
                tmp_t = Tk()
                lgt = sb(p1, "p1lg", [128, 32], F32)
                mk = sb(p1, "p1mk", [128, 32], F32)
                pos = sb(p1, "p1pos", [128, 32], F32)
                s32 = sb(p1, "p1s32", [128, 32], F32)
                m8 = sb(p1, "p1m8", [128, 8], F32)
                e4 = sb(p1, "p1e4", [128, 4], F32)
                sm = sb(p1, "p1sm", [128, 4], F32)
                rt_t = Tk()
                S.op("pool", lambda e: e.memset(base[:], 0.0), writes=[base_t])
                S.dma(xt[0][:], x1_d[0], reads=[x1_t[0]], writes=[xt_t[0]])
                for ti in range(NTI):
                    bb = ti // 32
                    if ti + 1 < NTI:
                        S.dma(xt[(ti + 1) % 2][:], x1_d[ti + 1], reads=[x1_t[ti + 1]], writes=[xt_t[(ti + 1) % 2]])
                    x_, x_t = xt[ti % 2], xt_t[ti % 2]
                    S.op("pool", lambda e: e.memset(ssb[:, 0:1], 0.0), writes=[tmp_t])
                    S.op("act", lambda e: e.activation(out=scr[:], in_=x_[:], func=AF.Square, accum_out=ssb[:, 0:1]),
                         reads=[x_t, tmp_t], writes=[tmp_t])
                    S.op("act", lambda e: e.activation(out=ssb[:, 1:2], in_=ssb[:, 0:1], func=AF.Sqrt, scale=1.0 / D, bias=EPS),
                         reads=[tmp_t], writes=[tmp_t])
                    S.op("dve", lambda e: e.reciprocal(out=ssb[:, 2:3], in_=ssb[:, 1:2]), reads=[tmp_t], writes=[tmp_t])
                    S.op("dve", lambda e: e.tensor_scalar(out=xn[:], in0=x_[:], scalar1=ssb[:, 2:3], scalar2=None, op0=ALU.mult),
                         reads=[x_t, tmp_t], writes=[xn_t])
                    S.op("dve", lambda e: e.tensor_tensor(out=xn[:], in0=xn[:], in1=GS2[:, bb, :], op=ALU.mult),
                         reads=[xn_t, GS_t], writes=[xn_t])
                    S.op("pool", lambda e: e.tensor_tensor(out=h2[:], in0=xn[:], in1=SH2[:, bb, :], op=ALU.add),
                         reads=[xn_t, GS_t], writes=[h2_t])
                    hb_, hb_t = h2bf[ti % 2], h2bf_t[ti % 2]
                    S.op("act", lambda e: e.copy(out=hb_[:], in_=h2[:]), reads=[h2_t], writes=[hb_t])
                    S.dma(h2_d[ti], hb_[:], reads=[hb_t], writes=[h2d_t[ti]])
                    for c in range(8):
                        S.op("pe", lambda e: e.transpose(out=pB2[c // 4][:, (c % 4) * 128:(c % 4 + 1) * 128],
                                                         in_=h2[:, c * 128:(c + 1) * 128], identity=identf),
                             reads=[h2_t, cst_t], writes=[pB2_t[c // 4]], signal=(c % 4 == 3))
                    S.op("dve", lambda e: e.tensor_copy(out=h2f[:, 0:4, :].rearrange("p a b -> p (a b)"), in_=pB2[0][:]),
                         reads=[pB2_t[0]], writes=[h2f_t])
                    S.op("act", lambda e: e.copy(out=h2f[:, 4:8, :].rearrange("p a b -> p (a b)"), in_=pB2[1][:]),
                         reads=[pB2_t[1]], writes=[h2f_t])
                    for kc in range(8):
                        S.op("pe", lambda e: e.matmul(out=pR[:, 0:32], lhsT=h2f[:, kc, :], rhs=Wr[:, kc, :], start=(kc == 0), stop=False),
                             reads=[h2f_t, Wr_t], writes=[pR_t], signal=False)
                    S.op("pe", lambda e: e.matmul(out=pR[:, 0:32], lhsT=cst[0:1, 1, :], rhs=rbs[:], start=False, stop=True),
                         reads=[cst_t, rbs_t], writes=[pR_t])
                    S.op("dve", lambda e: e.tensor_copy(out=lgt[:], in_=pR[:, 0:32]), reads=[pR_t], writes=[rt_t])
                    S.op("dve", lambda e: e.max(out=m8[:], in_=lgt[:]), reads=[rt_t], writes=[rt_t])
                    S.op("dve", lambda e: e.tensor_scalar(out=mk[:], in0=lgt[:], scalar1=m8[:, 3:4], scalar2=None, op0=ALU.is_ge),
                         reads=[rt_t], writes=[rt_t])
                    S.op("dve", lambda e: e.tensor_scalar(out=sm[:, 0:1], in0=m8[:, 0:1], scalar1=-1.0, scalar2=None, op0=ALU.mult),
                         reads=[rt_t], writes=[rt_t])
                    S.op("act", lambda e: e.activation(out=e4[:], in_=m8[:, 0:4], func=AF.Exp, bias=sm[:, 0:1], scale=1.0),
                         reads=[rt_t], writes=[rt_t])
                    S.op("dve", lambda e: e.reduce_sum(out=sm[:, 1:2], in_=e4[:], axis=mybir.AxisListType.X), reads=[rt_t], writes=[rt_t])
                    S.op("dve", lambda e: e.reciprocal(out=sm[:, 2:3], in_=sm[:, 1:2]), reads=[rt_t], writes=[rt_t])
                    S.op("dve", lambda e: e.tensor_scalar(out=g4[:, ti * 4:ti * 4 + 4], in0=e4[:], scalar1=sm[:, 2:3], scalar2=None, op0=ALU.mult),
                         reads=[rt_t], writes=[pk_t])
                    S.op("pe", lambda e: e.matmul(out=pR[:, 32:64], lhsT=cst[:, 9, :], rhs=mk[:], start=True, stop=True),
                         reads=[rt_t, cst_t], writes=[pR_t])
                    S.op("pe", lambda e: e.matmul(out=pR[:, 64:96], lhsT=onesf, rhs=mk[:], start=True, stop=True),
                         reads=[rt_t, cst_t], writes=[pR_t])
                    S.op("dve", lambda e: e.tensor_tensor(out=pos[:], in0=pR[:, 32:64], in1=base[:], op=ALU.add),
                         reads=[pR_t, base_t], writes=[rt_t])
                    for k in range(4):
                        S.op("dve", lambda e: e.scalar_tensor_tensor(out=s32[:], in0=lgt[:], scalar=m8[:, k:k + 1], in1=pos[:],
                                                                     op0=ALU.is_equal, op1=ALU.mult), reads=[rt_t], writes=[rt_t])
                        S.op("dve", lambda e: e.reduce_sum(out=posk[:, ti * 4 + k:ti * 4 + k + 1], in_=s32[:], axis=mybir.AxisListType.X),
                             reads=[rt_t], writes=[pk_t])
                        S.op("dve", lambda e: e.scalar_tensor_tensor(out=s32[:], in0=lgt[:], scalar=m8[:, k:k + 1], in1=iota32,
                                                                     op0=ALU.is_equal, op1=ALU.mult), reads=[rt_t, cst_t, pk_t], writes=[rt_t])
                        S.op("dve", lambda e: e.reduce_sum(out=ekk[:, ti * 4 + k:ti * 4 + k + 1], in_=s32[:], axis=mybir.AxisListType.X),
                             reads=[rt_t], writes=[pk_t])
                    S.op("dve", lambda e: e.tensor_tensor(out=base[:], in0=pR[:, 64:96], in1=base[:], op=ALU.add),
                         reads=[pR_t, base_t, rt_t], writes=[base_t])
                ci = sb(p1, "p2ci", [128, 32], I32)
                padded = sb(p1, "p2pad", [128, 32], F32)
                pst = sb(p1, "p2pst", [128, 32], F32)
                pend = sb(p1, "p2pend", [128, 32], F32)
                bst = sb(p1, "p2bst", [128, NBLK], F32)
                be = sb(p1, "p2be", [128, NBLK], F32)
                wf = sb(p1, "p2wf", [128, 8, NBLK], F32)
                df = sb(p1, "p2df", [128, NTI * 4], F32)
                p2_t = Tk()
                S.op("dve", lambda e: e.tensor_copy(out=ci[:], in_=base[:]), reads=[base_t], writes=[p2_t])
                S.op("dve", lambda e: e.tensor_scalar(out=ci[:], in0=ci[:], scalar1=511, scalar2=None, op0=ALU.add), reads=[p2_t], writes=[p2_t])
                S.op("dve", lambda e: e.tensor_scalar(out=ci[:], in0=ci[:], scalar1=-512, scalar2=None, op0=ALU.bitwise_and), reads=[p2_t], writes=[p2_t])
                S.op("dve", lambda e: e.tensor_copy(out=padded[:], in_=ci[:]), reads=[p2_t], writes=[p2_t])
                S.op("dve", lambda e: e.memset(pst[:], 0.0), reads=[p2_t], writes=[p2_t])
                for ee in range(1, 32):
                    S.op("dve", lambda e: e.tensor_tensor(out=pst[:, ee:ee + 1], in0=pst[:, ee - 1:ee], in1=padded[:, ee - 1:ee], op=ALU.add),
                         reads=[p2_t], writes=[p2_t])
                S.op("dve", lambda e: e.tensor_tensor(out=pend[:], in0=pst[:], in1=padded[:], op=ALU.add), reads=[p2_t], writes=[p2_t])
                S.op("dve", lambda e: e.tensor_scalar(out=bst[:], in0=cst[:, 10, 0:NBLK], scalar1=512.0, scalar2=None, op0=ALU.mult),
                     reads=[cst_t, p2_t], writes=[p2_t])
                S.op("dve", lambda e: e.memset(be[:], 0.0), reads=[p2_t], writes=[p2_t])
                for ee in range(32):
                    S.op("dve", lambda e: e.scalar_tensor_tensor(out=be[:], in0=bst[:], scalar=pend[:, ee:ee + 1], in1=be[:],
                                                                 op0=ALU.is_ge, op1=ALU.add), reads=[p2_t], writes=[p2_t])
                S.op("dve", lambda e: e.tensor_scalar(out=be[:], in0=be[:], scalar1=31.0, scalar2=None, op0=ALU.min), reads=[p2_t], writes=[p2_t])
                for kc in range(8):
                    S.op("dve", lambda e: e.tensor_scalar(out=wf[:, kc, :], in0=be[:], scalar1=1024.0, scalar2=cst[:, 11, kc:kc + 1],
                                                          op0=ALU.mult, op1=ALU.add), reads=[p2_t, cst_t], writes=[p2_t])
                S.op("dve", lambda e: e.tensor_copy(out=widx[:], in_=wf[:]), reads=[p2_t], writes=[widx_t])
                S.op("dve", lambda e: e.tensor_copy(out=bidx[:], in_=be[0:2, :]), reads=[p2_t], writes=[widx_t])
                for c in range(NTI * 4):
                    S.op("dve", lambda e: e.scalar_tensor_tensor(out=s32[:], in0=iota32, scalar=ekk[:, c:c + 1], in1=pst[:],
                                                                 op0=ALU.is_equal, op1=ALU.mult), reads=[p2_t, pk_t, cst_t, rt_t], writes=[rt_t])
                    S.op("dve", lambda e: e.reduce_sum(out=df[:, c:c + 1], in_=s32[:], axis=mybir.AxisListType.X), reads=[rt_t], writes=[p2_t])
                S.op("dve", lambda e: e.tensor_tensor(out=df[:], in0=df[:], in1=posk[:], op=ALU.add), reads=[p2_t, pk_t], writes=[p2_t])
                S.op("dve", lambda e: e.tensor_copy(out=desti[:], in_=df[:]), reads=[p2_t], writes=[desti_t])
                for ti in range(NTI):
                    hb_, hb_t = h2bf[ti % 2], h2bf_t[ti % 2]
                    S.dma(hb_[:], h2_d[ti], reads=[h2d_t[ti]], writes=[hb_t])
                    for k in range(4):
                        cidx = ti * 4 + k
                        S.dma_ind(lambda e: e.indirect_dma_start(
                            out=xs_d[:, :], out_offset=bass.IndirectOffsetOnAxis(ap=desti[:, cidx:cidx + 1], axis=0),
                            in_=hb_[:], in_offset=None),
                            reads=[hb_t, desti_t])
                S.barrier()

            with contextlib.ExitStack() as p4:
                egu2 = egu_d[0].rearrange("e k n -> (e k) n")
                edn2 = edn_d[0].rearrange("e k n -> (e k) n")
                Wgu = [sb(p4, f"Wgu{i}", [128, 8, 1024], BF16) for i in range(3)]
                Wgu_t = [Tk(), Tk(), Tk()]
                Wd = [sb(p4, f"Wd{i}", [128, 4, 1024], BF16) for i in range(2)]
                Wd_t = [Tk(), Tk()]
                stg = [sb(p4, f"p4stg{i}", [128, 2048], F32) for i in range(4)]
                stg_t = [Tk() for _ in range(4)]
                browb = [sb(p4, f"browb{i}", [2, 3072], BF16) for i in range(2)]
                browb_t = [Tk(), Tk()]
                xs = [sb(p4, f"p4xs{i}", [128, D], BF16) for i in range(4)]
                xs_t = [Tk() for _ in range(4)]
                xsT = sb(p4, "p4xsT", [128, 8, 512], BF16)
                xsT_t = Tk()
                aT = [sb(p4, f"aT{i}", [128, 4, 512], BF16) for i in range(2)]
                aT_t = [Tk(), Tk()]
                g1 = sb(p4, "p4g1", [128, 512], F32)
                u1 = sb(p4, "p4u1", [128, 512], F32)
                glu = sb(p4, "p4gl", [128, 512], F32)
                g1_t, u1_t, glu_t = Tk(), Tk(), Tk()
                ysb = sb(p4, "p4ys", [128, 4, D], F32)
                ysb_t = [[Tk(), Tk()] for _ in range(4)]
                pG = [ps(p4, f"p4G{i}", [128, 512]) for i in range(2)]
                pG_t = [Tk(), Tk()]
                pU = [ps(p4, f"p4U{i}", [128, 512]) for i in range(2)]
                pU_t = [Tk(), Tk()]
                pY = [ps(p4, f"p4Y{i}", [128, 512]) for i in range(2)]
                pY_t = [Tk(), Tk()]
                pX = [ps(p4, f"p4X{i}", [128, 2, 512], BF16) for i in range(2)]
                pX_t = [Tk(), Tk()]
                ISC = 1.0 / 1.702
                cnt = dict(sk=0, gk=0, yk=0)

                def w_load_gu_block(blk):
                    for kc in range(8):
                        si = cnt["sk"] % 4
                        cnt["sk"] += 1
                        S.dma_ind(lambda e: e.indirect_dma_start(
                            out=stg[si][:, :], out_offset=None, in_=egu2[:, :],
                            in_offset=bass.IndirectOffsetOnAxis(ap=widx[:, kc, blk:blk + 1], axis=0)),
                            reads=[widx_t], writes=[stg_t[si]])
                        for hf in range(2):
                            wi3 = (2 * blk + hf) % 3
                            S.op("act", lambda e: e.copy(out=Wgu[wi3][:, kc, :].rearrange("p (g n) -> p g n", g=2),
                                                         in_=stg[si][:].rearrange("p (g h n) -> p g h n", g=2, h=2)[:, :, hf, :]),
                                 reads=[stg_t[si]], writes=[Wgu_t[wi3]])

                def w_load_d(blk, hf, wi):
                    wd, wd_t = Wd[wi], Wd_t[wi]
                    for jq in range(2):
                        si = cnt["sk"] % 4
                        cnt["sk"] += 1
                        for i in range(2):
                            kc = hf * 4 + jq * 2 + i
                            S.dma_ind(lambda e: e.indirect_dma_start(
                                out=stg[si][:, i * 1024:(i + 1) * 1024], out_offset=None, in_=edn2[:, :],
                                in_offset=bass.IndirectOffsetOnAxis(ap=widx[:, kc, blk:blk + 1], axis=0)),
                                reads=[widx_t], writes=[stg_t[si]])
                        S.op("act", lambda e: e.mul(out=wd[:, jq * 2:(jq + 1) * 2, :], in_=stg[si][:].rearrange("p (a b) -> p a b", a=2), mul=ISC),
                             reads=[stg_t[si]], writes=[wd_t])

                def b_load(blk):
                    S.dma_ind(lambda e: e.indirect_dma_start(
                        out=browb[blk % 2][0:2, :], out_offset=None, in_=bias_d[:, :],
                        in_offset=bass.IndirectOffsetOnAxis(ap=bidx[0:2, blk:blk + 1], axis=0)),
                        reads=[widx_t], writes=[browb_t[blk % 2]])

                def x_dma(blk):
                    for tt in range(4):
                        r0 = blk * 512 + tt * 128
                        S.dma(xs[tt][:], xs_d[r0:r0 + 128, :], writes=[xs_t[tt]])

                def x_tr(blk):
                    for tt in range(4):
                        for kc in range(8):
                            S.op("pe", lambda e: e.transpose(out=pX[tt % 2][:, kc // 4, (kc % 4) * 128:(kc % 4 + 1) * 128],
                                                             in_=xs[tt][:, kc * 128:(kc + 1) * 128], identity=identb[:]),
                                 reads=[xs_t[tt], cb_t], writes=[pX_t[tt % 2]], signal=(kc == 7))
                        S.op("dve", lambda e: e.tensor_copy(out=xsT[:, 0:4, tt * 128:(tt + 1) * 128],
                                                            in_=pX[tt % 2][:, 0, :].rearrange("p (a b) -> p a b", a=4)),
                             reads=[pX_t[tt % 2]], writes=[xsT_t])
                        S.op("dve", lambda e: e.tensor_copy(out=xsT[:, 4:8, tt * 128:(tt + 1) * 128],
                                                            in_=pX[tt % 2][:, 1, :].rearrange("p (a b) -> p a b", a=4)),
                             reads=[pX_t[tt % 2]], writes=[xsT_t])

                def gu_unit(blk, hf, wi, au):
                    wg, wg_t = Wgu[(2 * blk + hf) % 3], Wgu_t[(2 * blk + hf) % 3]
                    bb_, bb_t = browb[blk % 2], browb_t[blk % 2]
                    a_, a_t = aT[au], aT_t[au]
                    for j in range(4):
                        i2 = cnt["gk"] % 2
                        cnt["gk"] += 1
                        fcol = hf * 512 + j * 128
                        for kc in range(8):
                            S.op("pe", lambda e: e.matmul(out=pG[i2][:], lhsT=wg[:, kc, j * 128:(j + 1) * 128], rhs=xsT[:, kc, :],
                                                          start=(kc == 0), stop=False), reads=[wg_t, xsT_t], writes=[pG_t[i2]], signal=False)
                        S.op("pe", lambda e: e.matmul(out=pG[i2][:], lhsT=bb_[0:1, fcol:fcol + 128], rhs=cbones[0:1, :], start=False, stop=True),
                             reads=[bb_t, cb_t], writes=[pG_t[i2]])
                        for kc in range(8):
                            S.op("pe", lambda e: e.matmul(out=pU[i2][:], lhsT=wg[:, kc, 512 + j * 128:512 + (j + 1) * 128], rhs=xsT[:, kc, :],
                                                          start=(kc == 0), stop=False), reads=[wg_t, xsT_t], writes=[pU_t[i2]], signal=False)
                        S.op("pe", lambda e: e.matmul(out=pU[i2][:], lhsT=bb_[0:1, 1024 + fcol:1024 + fcol + 128], rhs=cbones[0:1, :],
                                                      start=False, stop=True), reads=[bb_t, cb_t], writes=[pU_t[i2]])
                        S.op("dve", lambda e: e.tensor_scalar(out=g1[:], in0=pG[i2][:], scalar1=7.0, scalar2=None, op0=ALU.min),
                             reads=[pG_t[i2]], writes=[g1_t])
                        S.op("act", lambda e: e.activation(out=glu[:], in_=g1[:], func=AF.Silu, scale=1.702), reads=[g1_t], writes=[glu_t])
                        S.op("dve", lambda e: e.tensor_scalar(out=u1[:], in0=pU[i2][:], scalar1=1.0, scalar2=8.0, op0=ALU.add, op1=ALU.min),
                             reads=[pU_t[i2]], writes=[u1_t])
                        S.op("dve", lambda e: e.scalar_tensor_tensor(out=a_[:, j, :], in0=u1[:], scalar=-6.0, in1=glu[:],
                                                                     op0=ALU.max, op1=ALU.mult), reads=[u1_t, glu_t], writes=[a_t])

                def dn_unit(blk, hf, wi, au):
                    wd, wd_t = Wd[wi], Wd_t[wi]
                    bb_, bb_t = browb[blk % 2], browb_t[blk % 2]
                    a_, a_t = aT[au], aT_t[au]
                    for tt in range(4):
                        for cb in range(2):
                            yi = cnt["yk"] % 2
                            cnt["yk"] += 1
                            for j in range(4):
                                S.op("pe", lambda e: e.matmul(out=pY[yi][:], lhsT=a_[:, j, tt * 128:(tt + 1) * 128],
                                                              rhs=wd[:, j, cb * 512:(cb + 1) * 512], start=(j == 0), stop=(j == 3 and hf == 1)),
                                     reads=[a_t, wd_t], writes=[pY_t[yi]], signal=(j == 3 and hf == 1))
                            yv = ysb[:, tt, cb * 512:(cb + 1) * 512]
                            if hf == 0:
                                S.op("pe", lambda e: e.matmul(out=pY[yi][:], lhsT=cbones[0:1, 0:128], rhs=bb_[0:1, 2048 + cb * 512:2048 + (cb + 1) * 512],
                                                              start=False, stop=True), reads=[bb_t, cb_t], writes=[pY_t[yi]])
                                S.op("dve", lambda e: e.tensor_copy(out=yv, in_=pY[yi][:]), reads=[pY_t[yi]], writes=[ysb_t[tt][cb]])
                            else:
                                S.op("dve", lambda e: e.tensor_tensor(out=yv, in0=pY[yi][:], in1=yv, op=ALU.add),
                                     reads=[pY_t[yi], ysb_t[tt][cb]], writes=[ysb_t[tt][cb]])
                    if hf == 1:
                        S.dma(ys_d[blk * 512:(blk + 1) * 512, :].rearrange("(t p) c -> p t c", p=128), ysb[:],
                              reads=[ysb_t[tt][cb] for tt in range(4) for cb in range(2)])

                cbones = sb(p4, "cbones", [1, 512], BF16)
                S.op("dve", lambda e: e.memset(cbones[:], 1.0), reads=[cb_t], writes=[cb_t])
                units = [(blk, hf) for blk in range(NBLK) for hf in range(2)]
                NU = len(units)
                b_load(0)
                x_dma(0)
                w_load_gu_block(0)
                w_load_d(0, 0, 0)
                w_load_d(0, 1, 1)
                x_tr(0)
                gu_unit(0, 0, 0, 0)
                w_load_gu_block(1)
                b_load(1)
                x_dma(1)
                for ui, (blk, hf) in enumerate(units):
                    if ui + 1 < NU:
                        nb_, nh_ = units[ui + 1]
                        if nh_ == 0:
                            x_tr(nb_)
                        gu_unit(nb_, nh_, (ui + 1) % 2, (ui + 1) % 2)
                        if nh_ == 0 and nb_ + 1 < NBLK:
                            w_load_gu_block(nb_ + 1)
                        if nh_ == 0 and nb_ + 1 < NBLK:
                            b_load(nb_ + 1)
                            x_dma(nb_ + 1)
                    dn_unit(blk, hf, ui % 2, ui % 2)
                    if ui + 2 < NU:
                        b2, h2_ = units[ui + 2]
                        w_load_d(b2, h2_, (ui + 2) % 2)
                S.barrier()

            with contextlib.ExitStack() as p5:
                yk = [sb(p5, f"p5y{i}", [128, D], F32) for i in range(4)]
                yk_t = [Tk() for _ in range(4)]
                accm = sb(p5, "p5acc", [128, D], F32)
                acc_t = Tk()
                x_ = sb(p5, "p5x", [128, D], F32)
                x_t = Tk()
                scr = sb(p5, "p5scr", [128, D], BF16)
                ssb = sb(p5, "p5ss", [128, 4], F32)
                tmp_t = Tk()
                yo = sb(p5, "p5yo", [128, D], F32)
                yo_t = Tk()
                for ti in range(NTI):
                    bb = ti // 32
                    S.dma(x_[:], x1_d[ti], reads=[x1_t[ti]], writes=[x_t])
                    for k in range(4):
                        cidx = ti * 4 + k
                        S.dma_ind(lambda e: e.indirect_dma_start(
                            out=yk[k][:], out_offset=None, in_=ys_d[:, :],
                            in_offset=bass.IndirectOffsetOnAxis(ap=desti[:, cidx:cidx + 1], axis=0)),
                            reads=[desti_t], writes=[yk_t[k]])
                    S.op("dve", lambda e: e.tensor_scalar(out=accm[:], in0=yk[0][:], scalar1=g4[:, ti * 4:ti * 4 + 1], scalar2=None, op0=ALU.mult),
                         reads=[yk_t[0], pk_t], writes=[acc_t])
                    for k in range(1, 4):
                        S.op("dve", lambda e: e.scalar_tensor_tensor(out=accm[:], in0=yk[k][:], scalar=g4[:, ti * 4 + k:ti * 4 + k + 1], in1=accm[:],
                                                                     op0=ALU.mult, op1=ALU.add), reads=[yk_t[k], pk_t, acc_t], writes=[acc_t])
                    S.op("dve", lambda e: e.tensor_tensor(out=accm[:], in0=accm[:], in1=G2[:, bb, :], op=ALU.mult),
                         reads=[acc_t, G_t], writes=[acc_t])
                    S.op("dve", lambda e: e.tensor_tensor(out=accm[:], in0=accm[:], in1=x_[:], op=ALU.add), reads=[acc_t, x_t], writes=[acc_t])
                    S.op("dve", lambda e: e.memset(ssb[:, 0:1], 0.0), writes=[tmp_t])
                    S.op("act", lambda e: e.activation(out=scr[:], in_=accm[:], func=AF.Square, accum_out=ssb[:, 0:1]),
                         reads=[acc_t, tmp_t], writes=[tmp_t])
                    S.op("act", lambda e: e.activation(out=ssb[:, 1:2], in_=ssb[:, 0:1], func=AF.Sqrt, scale=1.0 / D, bias=EPS),
                         reads=[tmp_t], writes=[tmp_t])
                    S.op("dve", lambda e: e.reciprocal(out=ssb[:, 2:3], in_=ssb[:, 1:2]), reads=[tmp_t], writes=[tmp_t])
                    S.op("dve", lambda e: e.scalar_tensor_tensor(out=yo[:], in0=accm[:], scalar=ssb[:, 2:3], in1=FG[:], op0=ALU.mult, op1=ALU.mult),
                         reads=[acc_t, tmp_t, G_t], writes=[yo_t])
                    r0 = (ti % 32) * 128
                    S.dma(out_d[bb, r0:r0 + 128, :], yo[:], reads=[yo_t])
            S.final_wait()
        print("ops:", S.n_ops, "dmas:", S.dma_n, "sig:", S.cnt)
    return nc


_CACHE = {}


def make_in_maps(inputs, n_cores=8):
    RC, RS, MC, MS = rope_tables()
    cst, _ = const_tables()
    f = lambda a: np.ascontiguousarray(np.asarray(a, dtype=np.float32))
    shared = {k: f(inputs[k]) for k in ("norm1_g", "norm2_g", "ada_w", "ada_b", "w_in", "ret_decay_fwd", "ret_decay_bwd",
                                        "mla_q_norm_g", "mla_w_uq", "mla_kv_norm_g", "mla_w_ukv", "w_branch_ret",
                                        "w_branch_mla", "w_out", "router_w", "router_b", "exp_w_gu", "exp_b_gu",
                                        "exp_w_down", "exp_b_down", "final_norm_g")}
    shared.update(consts=cst, rope_rc=RC, rope_rs=RS, rope_mc=MC, rope_ms=MS)
    x, c, ctx, c_ctx = f(inputs["x"]), f(inputs["c"]), f(inputs["ctx"]), f(inputs["c_ctx"])
    maps = []
    for i in range(n_cores):
        m = dict(shared)
        m["x"] = x[i * NB:(i + 1) * NB]
        m["ctx"] = ctx[i * NB:(i + 1) * NB]
        m["cvec"] = np.ascontiguousarray(np.concatenate([c[i * NB:(i + 1) * NB], c_ctx[None, :]], axis=0))
        maps.append(m)
    return maps


def kernel(**inputs):
    if "nc" not in _CACHE:
        _CACHE["nc"] = build()
    nc = _CACHE["nc"]
    maps = make_in_maps(inputs)
    res = run_bass_kernel_spmd(nc, maps, core_ids=list(range(8)))
    return np.concatenate([r["out"] for r in res.results], axis=0).astype(np.float32)
```

```python
import contextlib
import numpy as np
import concourse.bass as bass
import concourse.mybir as mybir
from concourse.bass_utils import run_bass_kernel_spmd

F32 = mybir.dt.float32
BF16 = mybir.dt.bfloat16
AF = mybir.ActivationFunctionType
ALU = mybir.AluOpType

NB = 2
L = 4096
CT = 256
LT = L + CT
NT = LT // 128
D = 1024
EPS = 1e-6
NS_DMA = 16
ERA = 16000


class Tk:
    __slots__ = ("w", "r", "name")

    def __init__(self, name=""):
        self.w = []
        self.r = []
        self.name = name


class Sched:
    def __init__(self, nc, stack):
        self.nc = nc
        self.stack = stack
        self.engs = {"pe": nc.tensor, "act": nc.scalar, "dve": nc.vector, "pool": nc.gpsimd, "sp": nc.sync}
        self.sems = {}
        self.seq = {e: 0 for e in self.engs}
        self.sig = {e: [] for e in self.engs}
        self.cnt = {e: 0 for e in self.engs}
        self.waited = {e: {} for e in self.engs}
        self.waited_d = {e: {} for e in self.engs}
        self.ring = [stack.enter_context(nc.semaphore(f"dq{i}")) for i in range(NS_DMA)]
        self.dma_n = 0
        self.n_ops = 0

    def _sem(self, eng, era):
        k = (eng, era)
        if k not in self.sems:
            self.sems[k] = self.stack.enter_context(self.nc.semaphore(f"s_{eng}_{era}"))
        return self.sems[k]

    def _wait(self, eng, tk):
        e = self.engs[eng]
        if tk[0] == "d":
            _, ring, val = tk
            if self.waited_d[eng].get(ring, 0) >= val:
                return
            e.wait_ge(self.ring[ring], val)
            self.waited_d[eng][ring] = val
            return
        _, peng, seq = tk
        lst = self.sig[peng]
        lo, hi = 0, len(lst)
        while lo < hi:
            mid = (lo + hi) // 2
            if lst[mid][0] >= seq:
                hi = mid
            else:
                lo = mid + 1
        if lo >= len(lst):
            raise RuntimeError(f"no signalling op after seq {seq} on {peng}")
        count = lst[lo][1]
        if self.waited[eng].get(peng, 0) >= count:
            return
        era, val = (count - 1) // ERA, (count - 1) % ERA + 1
        e.wait_ge(self._sem(peng, era), val)
        self.waited[eng][peng] = count

    def op(self, eng, fn, reads=(), writes=(), signal=True):
        deps = []
        for t in reads:
            deps.extend(t.w)
        for t in writes:
            for tk in t.w:
                if tk[0] == "d" or tk[1] != eng:
                    deps.append(tk)
            for tk in t.r:
                if tk[0] == "d" or tk[1] != eng:
                    deps.append(tk)
        for tk in deps:
            self._wait(eng, tk)
        ins = fn(self.engs[eng])
        self.seq[eng] += 1
        seq = self.seq[eng]
        if signal:
            self.cnt[eng] += 1
            c = self.cnt[eng]
            era, val = (c - 1) // ERA, (c - 1) % ERA + 1
            ins.then_inc(self._sem(eng, era), 1)
            self.sig[eng].append((seq, c))
        tk = ("c", eng, seq)
        for t in reads:
            t.r = [x for x in t.r if not (x[0] == "c" and x[1] == eng)]
            t.r.append(tk)
        for t in writes:
            t.w = [tk]
            t.r = []
        self.n_ops += 1
        return ins

    def dma(self, out, in_, reads=(), writes=()):
        eng = "sp"
        deps = []
        for t in reads:
            deps.extend(t.w)
        for t in writes:
            deps.extend(t.w)
            deps.extend(t.r)
        n = self.dma_n
        ring = n % NS_DMA
        val = 16 * (n // NS_DMA + 1)
        if n >= NS_DMA:
            deps.append(("d", ring, val - 16))
        for tk in deps:
            self._wait(eng, tk)
        self.engs[eng].dma_start(out=out, in_=in_).then_inc(self.ring[ring], 16)
        self.dma_n += 1
        tk = ("d", ring, val)
        for t in reads:
            t.r.append(tk)
        for t in writes:
            t.w = [tk]
            t.r = []
        self.n_ops += 1
        return tk

    def dma_ind(self, fn, reads=(), writes=()):
        eng = "pool"
        deps = []
        for t in reads:
            deps.extend(t.w)
        for t in writes:
            deps.extend(t.w)
            deps.extend(t.r)
        n = self.dma_n
        ring = n % NS_DMA
        val = 16 * (n // NS_DMA + 1)
        if n >= NS_DMA:
            deps.append(("d", ring, val - 16))
        for tk in deps:
            self._wait(eng, tk)
        fn(self.engs[eng]).then_inc(self.ring[ring], 16)
        self.dma_n += 1
        tk = ("d", ring, val)
        for t in reads:
            t.r.append(tk)
        for t in writes:
            t.w = [tk]
            t.r = []
        self.n_ops += 1
        return tk

    def barrier(self):
        tks = []
        for e in self.engs:
            if self.sig[e]:
                tks.append(("c", e, self.sig[e][-1][0]))
        n = self.dma_n
        for k in range(max(0, n - NS_DMA), n):
            tks.append(("d", k % NS_DMA, 16 * (k // NS_DMA + 1)))
        for e in self.engs:
            for tk in tks:
                if tk[0] == "c" and tk[1] == e:
                    continue
                self._wait(e, tk)

    def final_wait(self):
        n = self.dma_n
        for k in range(max(0, n - NS_DMA), n):
            self._wait("sp", ("d", k % NS_DMA, 16 * (k // NS_DMA + 1)))


def rope_tables():
    pos = np.arange(L)
    rows = (pos // 64).astype(np.float32)
    cols = (pos % 64).astype(np.float32)

    def tab(dr):
        half = dr // 2
        hh = half // 2
        freqs = (10000.0 ** (-np.arange(hh, dtype=np.float32) / hh)).astype(np.float32)
        C = np.ones((LT, dr), np.float32)
        S = np.zeros((LT, dr), np.float32)
        for part, p in enumerate((rows, cols)):
            ang = (p[:, None] * freqs[None, :]).astype(np.float32)
            c, s = np.cos(ang).astype(np.float32), np.sin(ang).astype(np.float32)
            o = part * half
            C[CT:, o:o + hh] = c
            C[CT:, o + hh:o + half] = c
            S[CT:, o:o + hh] = -s
            S[CT:, o + hh:o + half] = s
        return C, S

    RC, RS = tab(256)
    MC, MS = tab(64)
    return RC, RS, np.ascontiguousarray(MC.T), np.ascontiguousarray(MS.T)


def const_tables():
    j = np.arange(128, dtype=np.float32)[:, None]
    i = np.arange(128, dtype=np.float32)[None, :]
    c = {}
    c["ident"] = np.eye(128, dtype=np.float32)
    c["ones"] = np.ones((128, 128), np.float32)
    c["d1"] = np.maximum(i - j, 0.0) + 0 * j
    c["mf"] = (i >= j).astype(np.float32)
    c["d2"] = np.maximum(j - i, 0.0)
    c["mb"] = (i < j).astype(np.float32)
    c["ip1"] = (i + 1.0) + 0 * j
    c["rev"] = (128.0 - i) + 0 * j
    col = np.zeros((128, 128), np.float32)
    col[:, 0] = 127.0 - np.arange(128)
    col[:, 1] = np.arange(128)
    col[:, 2] = 128.0
    c["col"] = col
    c["lt"] = (i > j).astype(np.float32)
    c["iota"] = i + 0 * j
    rowb = np.zeros((128, 128), np.float32)
    for kc in range(8):
        rowb[:, kc] = kc * 128 + np.arange(128)
    c["rowb"] = rowb
    names = ["ident", "ones", "d1", "mf", "d2", "mb", "ip1", "rev", "col", "lt", "iota", "rowb"]
    return np.stack([c[n].astype(np.float32) for n in names], axis=1), names


def build(dbg=()):
    nc = bass.Bass("TRN2", target_bir_lowering=False)
    try:
        nc.allow_low_precision("bf16 matmul operands with fp32 accumulation")
    except Exception:
        pass
    try:
        nc.allow_non_contiguous_dma("strided weight/activation tiles")
    except Exception:
        pass

    def din(name, shape, dt=F32):
        return nc.dram_tensor(name, list(shape), dt, kind="ExternalInput").ap()

    def dscr(name, shape, dt):
        kind = "ExternalOutput" if name in dbg else "Internal"
        return nc.dram_tensor(name, list(shape), dt, kind=kind).ap()

    x_d = din("x", [NB, L, D])
    ctx_d = din("ctx", [NB, CT, D])
    cv_d = din("cvec", [3, D])
    n1_d = din("norm1_g", [1, D])
    n2_d = din("norm2_g", [1, D])
    adaw_d = din("ada_w", [1, D, 6 * D])
    adab_d = din("ada_b", [1, 6 * D])
    win_d = din("w_in", [1, D, 8896])
    rdf_d = din("ret_decay_fwd", [1, 4])
    rdb_d = din("ret_decay_bwd", [1, 4])
    qg_d = din("mla_q_norm_g", [1, 384])
    wuq_d = din("mla_w_uq", [1, 384, 1536])
    kvg_d = din("mla_kv_norm_g", [1, 256])
    wukv_d = din("mla_w_ukv", [1, 256, 2048])
    wbr_d = din("w_branch_ret", [1, 2048, D])
    wbm_d = din("w_branch_mla", [1, D, D])
    wo_d = din("w_out", [1, D, D])
    rw_d = din("router_w", [1, D, 32])
    rb_d = din("router_b", [1, 32])
    egu_d = din("exp_w_gu", [1, 32, D, 2 * D])
    ebgu_d = din("exp_b_gu", [1, 32, 2 * D])
    edn_d = din("exp_w_down", [1, 32, D, D])
    ebd_d = din("exp_b_down", [1, 32, D])
    fg_d = din("final_norm_g", [D])
    cst_d = din("consts", [128, 12, 128])
    rc_d = din("rope_rc", [LT, 256])
    rs_d = din("rope_rs", [LT, 256])
    mc_d = din("rope_mc", [64, LT])
    ms_d = din("rope_ms", [64, LT])
    out_d = nc.dram_tensor("out", [NB, L, D], F32, kind="ExternalOutput").ap()

    hT_d = dscr("hT_s", [NT, 128, 8, 128], BF16)
    att_d = dscr("att_s", [8, 128, L], BF16)
    rT_d = dscr("rT_s", [4, 4, 128, L], BF16)
    of_d = dscr("of_s", [32, 128, 512], F32)
    x1_d = dscr("x1_s", [NB * 32, 128, D], F32)
    NROWS = NB * L * 4 + 32 * 512
    NBLK = NROWS // 512
    h2_d = dscr("h2_s", [NB * 32, 128, D], BF16)
    xs_d = dscr("xs_s", [NROWS, D], BF16)
    ys_d = dscr("ys_s", [NROWS, D], F32)
    bias_d = dscr("bias_s", [32, 3072], BF16)
    hT_t = [Tk() for _ in range(NT)]
    att_t = [Tk() for _ in range(8)]
    rT_t = [Tk() for _ in range(8)]
    of_t = [Tk() for _ in range(32)]
    x1_t = [Tk() for _ in range(NB * 32)]
    out_t = Tk()

    with contextlib.ExitStack() as gstack:
        S = Sched(nc, gstack)

        uid = [0]

        def sb(stack, name, shape, dt):
            uid[0] += 1
            return stack.enter_context(nc.sbuf_tensor(f"{name}_u{uid[0]}", list(shape), dt))

        def ps(stack, name, shape, dt=F32):
            uid[0] += 1
            return stack.enter_context(nc.psum_tensor(f"{name}_u{uid[0]}", list(shape), dt))

        cst = sb(gstack, "cst", [128, 12, 128], F32)
        cst_t = Tk()
        S.dma(cst[:], cst_d[:, :, :], writes=[cst_t])
        identf = cst[:, 0, :]
        onesf = cst[:, 1, :]
        identb = sb(gstack, "identb", [128, 128], BF16)
        onesb = sb(gstack, "onesb", [128, 128], BF16)
        cb_t = Tk()
        S.op("dve", lambda e: e.tensor_copy(out=identb[:], in_=identf), reads=[cst_t], writes=[cb_t])
        S.op("dve", lambda e: e.tensor_copy(out=onesb[:], in_=onesf), reads=[cst_t], writes=[cb_t])

        mT = sb(gstack, "mT", [128, 48, 3], F32)
        mT_t = Tk()
        gs1 = sb(gstack, "gs1", [128, 3, 8], F32)
        gs2 = sb(gstack, "gs2", [128, 3, 8], F32)
        gs_t = Tk()
        mix_stack = contextlib.ExitStack()
        G2 = sb(gstack, "G2", [128, NB, D], F32)
        FG = sb(gstack, "FG", [128, D], F32)
        G_t = Tk()
        gq = sb(gstack, "gq", [128, 8], F32)
        gq_t = Tk()
        KD = sb(gstack, "KD", [128, 4, 8], F32)
        G1 = sb(mix_stack, "G1", [128, NB, D], F32)
        MTf = sb(mix_stack, "MTf", [128, 4, 128], F32)
        MTb = sb(mix_stack, "MTb", [128, 4, 128], F32)
        QDf = sb(mix_stack, "QDf", [128, 4, 128], F32)
        QDb = sb(mix_stack, "QDb", [128, 4, 128], F32)
        dec_t = Tk()

        def featmajor_load(stack, pst, pst_t, dst_ap, src_rows_ap, nrows, tmpname):
            tmp = sb(stack, tmpname, [nrows, 128], F32)
            tt = Tk()
            S.dma(tmp[:], src_rows_ap, writes=[tt])
            S.op("pe", lambda e: e.transpose(out=pst[:, 0:nrows], in_=tmp[:], identity=cst[0:nrows, 0, 0:nrows]),
                 reads=[tt, cst_t], writes=[pst_t])
            return tmp

        with contextlib.ExitStack() as st:
            pA = ps(st, "pA0", [128, 512])
            pA_t = Tk()
            pB = ps(st, "pB0", [128, 2, 512])
            pB_t = Tk()
            cv = sb(st, "cv", [3, D], F32)
            cvs = sb(st, "cvs", [3, D], F32)
            cv_t = Tk()
            S.dma(cv[:], cv_d[:, :], writes=[cv_t])
            cvs_t = Tk()
            S.op("act", lambda e: e.activation(out=cvs[:], in_=cv[:], func=AF.Silu), reads=[cv_t], writes=[cvs_t])
            sT = sb(st, "sT", [128, 8, 3], F32)
            sT_t = Tk()
            for kc in range(8):
                S.op("pe", lambda e: e.transpose(out=pA[:, kc * 4:kc * 4 + 3], in_=cvs[:, kc * 128:(kc + 1) * 128],
                                                 identity=cst[0:3, 0, 0:3]), reads=[cvs_t, cst_t], writes=[pA_t])
            for kc in range(8):
                S.op("dve", lambda e: e.tensor_copy(out=sT[:, kc, :], in_=pA[:, kc * 4:kc * 4 + 3]),
                     reads=[pA_t], writes=[sT_t])
            abT = sb(st, "abT", [128, 48], F32)
            g12 = sb(st, "g12", [128, 16], F32)
            ab_t = Tk()
            featmajor_load(st, pA, pA_t, None, adab_d[0, :].rearrange("(r p) -> r p", p=128), 48, "t_ab")
            S.op("dve", lambda e: e.tensor_copy(out=abT[:], in_=pA[:, 0:48]), reads=[pA_t], writes=[ab_t])
            featmajor_load(st, pA, pA_t, None, n1_d[0, :].rearrange("(r p) -> r p", p=128), 8, "t_n1")
            S.op("dve", lambda e: e.tensor_copy(out=g12[:, 0:8], in_=pA[:, 0:8]), reads=[pA_t], writes=[ab_t])
            featmajor_load(st, pA, pA_t, None, n2_d[0, :].rearrange("(r p) -> r p", p=128), 8, "t_n2")
            S.op("dve", lambda e: e.tensor_copy(out=g12[:, 8:16], in_=pA[:, 0:8]), reads=[pA_t], writes=[ab_t])
            featmajor_load(st, pA, pA_t, None, qg_d[0, :].rearrange("(r p) -> r p", p=128), 3, "t_qg")
            S.op("dve", lambda e: e.tensor_copy(out=gq[:, 0:3], in_=pA[:, 0:3]), reads=[pA_t], writes=[gq_t])
            featmajor_load(st, pA, pA_t, None, kvg_d[0, :].rearrange("(r p) -> r p", p=128), 2, "t_kvg")
            S.op("dve", lambda e: e.tensor_copy(out=gq[:, 4:6], in_=pA[:, 0:2]), reads=[pA_t], writes=[gq_t])
            fgT = sb(st, "fgT", [128, 8], F32)
            fg_t = Tk()
            featmajor_load(st, pA, pA_t, None, fg_d.rearrange("(r p) -> r p", p=128), 8, "t_fg")
            S.op("dve", lambda e: e.tensor_copy(out=fgT[:], in_=pA[:, 0:8]), reads=[pA_t], writes=[fg_t])
            awv = adaw_d[0].rearrange("(kc p) n -> p kc n", p=128)
            aw = [sb(st, f"aw{i}", [128, 8, 512], F32) for i in range(2)]
            aw_t = [Tk(), Tk()]
            pm = ps(st, "pm", [128, 48, 4])
            pm_t = Tk()
            for blk in range(12):
                w = aw[blk % 2]
                wt = aw_t[blk % 2]
                S.dma(w[:], awv[:, :, blk * 512:(blk + 1) * 512], writes=[wt])
                for jj in range(4):
                    j = blk * 4 + jj
                    for kc in range(8):
                        S.op("pe", lambda e: e.matmul(out=pm[:, j, 0:3], lhsT=w[:, kc, jj * 128:(jj + 1) * 128],
                                                      rhs=sT[:, kc, :], start=(kc == 0), stop=(kc == 7)),
                             reads=[wt, sT_t], writes=[pm_t], signal=(kc == 7))
            for r in range(3):
                S.op("dve", lambda e: e.tensor_tensor(out=mT[:, :, r], in0=pm[:, :, r], in1=abT[:], op=ALU.add),
                     reads=[pm_t, ab_t], writes=[mT_t])
            for r in range(3):
                S.op("dve", lambda e: e.scalar_tensor_tensor(out=gs1[:, r, :], in0=mT[:, 8:16, r], scalar=1.0,
                                                             in1=g12[:, 0:8], op0=ALU.add, op1=ALU.mult),
                     reads=[mT_t, ab_t], writes=[gs_t])
                S.op("dve", lambda e: e.scalar_tensor_tensor(out=gs2[:, r, :], in0=mT[:, 32:40, r], scalar=1.0,
                                                             in1=g12[:, 8:16], op0=ALU.add, op1=ALU.mult),
                     reads=[mT_t, ab_t], writes=[gs_t])
            dg = sb(st, "dg", [128, 8, 128], F32)
            dg_t = Tk()

            def bcast_tile(dst_ap, vec_fn):
                for c in range(8):
                    S.op("dve", lambda e: e.tensor_scalar(out=dg[:, c, :], in0=identf, scalar1=vec_fn(c), scalar2=None,
                                                          op0=ALU.mult), reads=[cst_t, mT_t, fg_t], writes=[dg_t])
                for c in range(8):
                    S.op("pe", lambda e: e.matmul(out=pB[:, c // 4, (c % 4) * 128:(c % 4 + 1) * 128], lhsT=onesf,
                                                  rhs=dg[:, c, :], start=True, stop=True),
                         reads=[dg_t, cst_t], writes=[pB_t])
                S.op("act", lambda e: e.copy(out=dst_ap, in_=pB[:].rearrange("p a b -> p (a b)")),
                     reads=[pB_t], writes=[G_t])

            for b in range(NB):
                bcast_tile(G1[:, b, :], lambda c: mT[:, 16 + c, b:b + 1])
                bcast_tile(G2[:, b, :], lambda c: mT[:, 40 + c, b:b + 1])
            bcast_tile(FG[:], lambda c: fgT[:, c:c + 1])

            rd = sb(st, "rd", [1, 8], F32)
            rd_t = Tk()
            S.dma(rd[:, 0:4], rdf_d[:, :], writes=[rd_t])
            S.dma(rd[:, 4:8], rdb_d[:, :], writes=[rd_t])
            S.op("pe", lambda e: e.matmul(out=pA[:, 0:8], lhsT=cst[0:1, 1, :], rhs=rd[:], start=True, stop=True),
                 reads=[rd_t, cst_t], writes=[pA_t])
            lg = sb(st, "lg", [128, 8], F32)
            lg_t = Tk()
            S.op("act", lambda e: e.activation(out=lg[:], in_=pA[:, 0:8], func=AF.Exp, scale=-1.0),
                 reads=[pA_t], writes=[lg_t])
            S.op("act", lambda e: e.activation(out=lg[:], in_=lg[:], func=AF.Ln, bias=1.0, scale=1.0),
                 reads=[lg_t], writes=[lg_t])
            S.op("dve", lambda e: e.tensor_scalar(out=lg[:], in0=lg[:], scalar1=-1.0, scalar2=None, op0=ALU.mult),
                 reads=[lg_t], writes=[lg_t])
            tmpd = sb(st, "tmpd", [128, 128], F32)
            tmpd_t = Tk()
            for h in range(4):
                for (dst, dtab, mtab, col) in ((MTf, 2, 3, h), (MTb, 4, 5, 4 + h)):
                    S.op("act", lambda e: e.activation(out=tmpd[:], in_=cst[:, dtab, :], func=AF.Exp,
                                                       scale=lg[:, col:col + 1]),
                         reads=[cst_t, lg_t], writes=[tmpd_t])
                    S.op("dve", lambda e: e.tensor_tensor(out=dst[:, h, :], in0=tmpd[:], in1=cst[:, mtab, :],
                                                          op=ALU.mult), reads=[tmpd_t, cst_t], writes=[dec_t])
                S.op("act", lambda e: e.activation(out=QDf[:, h, :], in_=cst[:, 6, :], func=AF.Exp,
                                                   scale=lg[:, h:h + 1]), reads=[cst_t, lg_t], writes=[dec_t])
                S.op("act", lambda e: e.activation(out=QDb[:, h, :], in_=cst[:, 7, :], func=AF.Exp,
                                                   scale=lg[:, 4 + h:5 + h]), reads=[cst_t, lg_t], writes=[dec_t])
                S.op("act", lambda e: e.activation(out=KD[:, h, 0:1], in_=cst[:, 8, 0:1], func=AF.Exp,
                                                   scale=lg[:, h:h + 1]), reads=[cst_t, lg_t], writes=[dec_t])
                S.op("act", lambda e: e.activation(out=KD[:, h, 1:2], in_=cst[:, 8, 1:2], func=AF.Exp,
                                                   scale=lg[:, 4 + h:5 + h]), reads=[cst_t, lg_t], writes=[dec_t])
                S.op("act", lambda e: e.activation(out=KD[:, h, 2:3], in_=cst[:, 8, 2:3], func=AF.Exp,
                                                   scale=lg[:, h:h + 1]), reads=[cst_t, lg_t], writes=[dec_t])
                S.op("act", lambda e: e.activation(out=KD[:, h, 3:4], in_=cst[:, 8, 2:3], func=AF.Exp,
                                                   scale=lg[:, 4 + h:5 + h]), reads=[cst_t, lg_t], writes=[dec_t])
            S.barrier()

        def load_cast(stack_stage, dst, dst_t, src_ap, shape, stg, stg_t, eng, scale=None, dst_view=None):
            S.dma(stg, src_ap, writes=[stg_t])
            dv = dst if dst_view is None else dst_view
            if scale is None:
                S.op(eng, lambda e: e.tensor_copy(out=dv, in_=stg), reads=[stg_t], writes=[dst_t])
            else:
                S.op(eng, lambda e: e.tensor_scalar(out=dv, in0=stg, scalar1=scale, scalar2=None, op0=ALU.mult),
                     reads=[stg_t], writes=[dst_t])

        def norm_T(stack, pfx, src_tile, src_t, gs_ap, sh_ap, pT, pT_t, hTo, hTo_t, scr, ssb, tmp_t, xn, xn_t,
                   f32_out=None, f32_t=None):
            S.op("pool", lambda e: e.memset(ssb[:, 0:1], 0.0), writes=[tmp_t])
            S.op("act", lambda e: e.activation(out=scr[:], in_=src_tile, func=AF.Square, accum_out=ssb[:, 0:1]),
                 reads=[src_t, tmp_t], writes=[tmp_t])
            S.op("act", lambda e: e.activation(out=ssb[:, 1:2], in_=ssb[:, 0:1], func=AF.Sqrt, scale=1.0 / D, bias=EPS),
                 reads=[tmp_t], writes=[tmp_t])
            S.op("dve", lambda e: e.reciprocal(out=ssb[:, 2:3], in_=ssb[:, 1:2]), reads=[tmp_t], writes=[tmp_t])
            S.op("dve", lambda e: e.tensor_scalar(out=xn[:], in0=src_tile, scalar1=ssb[:, 2:3], scalar2=None,
                                                  op0=ALU.mult), reads=[src_t, tmp_t], writes=[xn_t])
            for c in range(8):
                S.op("pe", lambda e: e.transpose(out=pT[c // 4][:, (c % 4) * 128:(c % 4 + 1) * 128],
                                                 in_=xn[:, c * 128:(c + 1) * 128], identity=identf),
                     reads=[xn_t, cst_t], writes=[pT_t[c // 4]], signal=(c % 4 == 3))
            for c in range(8):
                src = pT[c // 4][:, (c % 4) * 128:(c % 4 + 1) * 128]
                if f32_out is not None:
                    S.op("dve", lambda e: e.tensor_scalar(out=f32_out[:, c, :], in0=src, scalar1=gs_ap[:, c:c + 1],
                                                          scalar2=sh_ap(c), op0=ALU.mult, op1=ALU.add),
                         reads=[pT_t[c // 4], gs_t, mT_t], writes=[f32_t])
                    S.op("pool", lambda e: e.tensor_copy(out=hTo[:, c, :], in_=f32_out[:, c, :]),
                         reads=[f32_t], writes=[hTo_t])
                else:
                    S.op("dve", lambda e: e.tensor_scalar(out=hTo[:, c, :], in0=src, scalar1=gs_ap[:, c:c + 1],
                                                          scalar2=sh_ap(c), op0=ALU.mult, op1=ALU.add),
                         reads=[pT_t[c // 4], gs_t, mT_t], writes=[hTo_t])

        winv = win_d[0].rearrange("(kc p) n -> p kc n", p=128)

        for b in range(NB):
            with contextlib.ExitStack() as st:
                xt = [sb(st, f"s0x{i}", [128, D], F32) for i in range(2)]
                xt_t = [Tk(), Tk()]
                xn = sb(st, "s0xn", [128, D], F32)
                xn_t = Tk()
                scr = sb(st, "s0scr", [128, D], BF16)
                ssb = sb(st, "s0ss", [128, 4], F32)
                tmp_t = Tk()
                hTo = [sb(st, f"s0h{i}", [128, 8, 128], BF16) for i in range(2)]
                hTo_t = [Tk(), Tk()]
                pT = [ps(st, f"s0p{i}", [128, 512]) for i in range(2)]
                pT_t = [Tk(), Tk()]

                def s0_load(t):
                    src = ctx_d[b, t * 128:(t + 1) * 128, :] if t < 2 else x_d[b, (t - 2) * 128:(t - 1) * 128, :]
                    S.dma(xt[t % 2][:], src, writes=[xt_t[t % 2]])

                s0_load(0)
                for t in range(NT):
                    if t + 1 < NT:
                        s0_load(t + 1)
                    r = 2 if t < 2 else b
                    norm_T(st, "s0", xt[t % 2][:], xt_t[t % 2], gs1[:, r, :], lambda c: mT[:, c, r:r + 1],
                           pT, pT_t, hTo[t % 2], hTo_t[t % 2], scr, ssb, tmp_t, xn, xn_t)
                    S.dma(hT_d[t], hTo[t % 2][:], reads=[hTo_t[t % 2]], writes=[hT_t[t]])
                S.barrier()

            with contextlib.ExitStack() as st:
                cqn = sb(st, "cqn", [128, 3, L], BF16)
                cqn_t = Tk()
                ckvn = sb(st, "ckvn", [128, 2, LT], BF16)
                ckvn_t = Tk()
                krT = sb(st, "krT", [64, LT], BF16)
                krT_t = Tk()
                with contextlib.ExitStack() as s1:
                    Wm = sb(s1, "Wm", [128, 8, 768], BF16)
                    Wm_t = Tk()
                    stg = sb(s1, "s1stg", [128, 8, 704], F32)
                    stg_t = Tk()
                    S.dma(stg[:], winv[:, :, 6144:6848], writes=[stg_t])
                    S.op("dve", lambda e: e.tensor_copy(out=Wm[:, :, 0:704], in_=stg[:]), reads=[stg_t], writes=[Wm_t])
                    for (dst, src) in ((704, 656), (720, 640), (736, 688), (752, 672)):
                        S.op("dve", lambda e: e.tensor_copy(out=Wm[:, :, dst:dst + 16], in_=stg[:, :, src:src + 16]),
                             reads=[stg_t], writes=[Wm_t])
                    hb = [sb(s1, f"s1h{i}", [128, 8, 512], BF16) for i in range(2)]
                    hb_t = [Tk(), Tk()]
                    tc_ = [sb(s1, f"s1tc{i}", [64, 2, 512], F32) for i in range(2)]
                    tc_t = [Tk(), Tk()]
                    pq = [ps(s1, f"s1pq{i}", [128, 512]) for i in range(3)]
                    pq_t = [Tk() for _ in range(3)]
                    pk = [ps(s1, f"s1pk{i}", [128, 512]) for i in range(2)]
                    pk_t = [Tk() for _ in range(2)]
                    pr = [ps(s1, f"s1pr{i}", [128, 512]) for i in range(2)]
                    pr_t = [Tk() for _ in range(2)]
                    pss = ps(s1, "s1pss", [128, 512])
                    pss_t = Tk()
                    xf = sb(s1, "s1xf", [128, 3, 512], F32)
                    xf_t = Tk()
                    sq = sb(s1, "s1sq", [128, 3, 512], F32)
                    sq_t = Tk()
                    rstd = sb(s1, "s1rstd", [128, 512], F32)
                    rstd_t = Tk()
                    r1 = sb(s1, "s1r1", [64, 512], F32)
                    r2 = sb(s1, "s1r2", [64, 512], F32)
                    r_t = Tk()

                    def blk_range(j):
                        return (0, 256) if j == 0 else (256 + (j - 1) * 512, 512)

                    def s1_load(j):
                        t0, n = blk_range(j)
                        for i in range(n // 128):
                            S.dma(hb[j % 2][:, :, i * 128:(i + 1) * 128], hT_d[t0 // 128 + i],
                                  reads=[hT_t[t0 // 128 + i]], writes=[hb_t[j % 2]])
                        S.dma(tc_[j % 2][:, 0, 0:n], mc_d[:, t0:t0 + n], writes=[tc_t[j % 2]])
                        S.dma(tc_[j % 2][:, 1, 0:n], ms_d[:, t0:t0 + n], writes=[tc_t[j % 2]])

                    def rms_T(pl, pl_t, nch, rank, gcol, dst, dst_t, dcol0, n):
                        for c in range(nch):
                            S.op("act", lambda e: e.copy(out=xf[:, c, 0:n], in_=pl[c][:, 0:n]),
                                 reads=[pl_t[c]], writes=[xf_t])
                            S.op("act", lambda e: e.activation(out=sq[:, c, 0:n], in_=pl[c][:, 0:n], func=AF.Square),
                                 reads=[pl_t[c]], writes=[sq_t])
                        for c in range(nch):
                            S.op("pe", lambda e: e.matmul(out=pss[:, 0:n], lhsT=onesf, rhs=sq[:, c, 0:n],
                                                          start=(c == 0), stop=(c == nch - 1)),
                                 reads=[sq_t, cst_t], writes=[pss_t], signal=(c == nch - 1))
                        S.op("act", lambda e: e.activation(out=rstd[:, 0:n], in_=pss[:, 0:n], func=AF.Sqrt,
                                                           scale=1.0 / rank, bias=EPS), reads=[pss_t], writes=[rstd_t])
                        S.op("dve", lambda e: e.reciprocal(out=rstd[:, 0:n], in_=rstd[:, 0:n]),
                             reads=[rstd_t], writes=[rstd_t])
                        for c in range(nch):
                            S.op("dve", lambda e: e.scalar_tensor_tensor(
                                out=dst[:, c, dcol0:dcol0 + n], in0=xf[:, c, 0:n], scalar=gq[:, gcol + c:gcol + c + 1],
                                in1=rstd[:, 0:n], op0=ALU.mult, op1=ALU.mult),
                                 reads=[xf_t, rstd_t, gq_t], writes=[dst_t])

                    s1_load(0)
                    for j in range(9):
                        if j + 1 < 9:
                            s1_load(j + 1)
                        t0, n = blk_range(j)
                        h_, h_t = hb[j % 2], hb_t[j % 2]
                        if j > 0:
                            for c in range(3):
                                for kc in range(8):
                                    S.op("pe", lambda e: e.matmul(out=pq[c][:, 0:n], lhsT=Wm[:, kc, c * 128:(c + 1) * 128],
                                                                  rhs=h_[:, kc, 0:n], start=(kc == 0), stop=(kc == 7)),
                                         reads=[Wm_t, h_t], writes=[pq_t[c]], signal=(kc == 7))
                        for c in range(2):
                            for kc in range(8):
                                S.op("pe", lambda e: e.matmul(out=pk[c][:, 0:n], lhsT=Wm[:, kc, 384 + c * 128:384 + (c + 1) * 128],
                                                              rhs=h_[:, kc, 0:n], start=(kc == 0), stop=(kc == 7)),
                                     reads=[Wm_t, h_t], writes=[pk_t[c]], signal=(kc == 7))
                        for c in range(2):
                            for kc in range(8):
                                S.op("pe", lambda e: e.matmul(out=pr[c][0:64, 0:n], lhsT=Wm[:, kc, 640 + c * 64:704 + c * 64],
                                                              rhs=h_[:, kc, 0:n], start=(kc == 0), stop=(kc == 7)),
                                     reads=[Wm_t, h_t], writes=[pr_t[c]], signal=(kc == 7))
                        if j > 0:
                            rms_T(pq, pq_t, 3, 384, 0, cqn, cqn_t, t0 - 256, n)
                        rms_T(pk, pk_t, 2, 256, 4, ckvn, ckvn_t, t0, n)
                        tcc, tcc_t = tc_[j % 2], tc_t[j % 2]
                        S.op("dve", lambda e: e.tensor_tensor(out=r1[:, 0:n], in0=pr[0][0:64, 0:n], in1=tcc[:, 0, 0:n],
                                                              op=ALU.mult), reads=[pr_t[0], tcc_t], writes=[r_t])
                        S.op("dve", lambda e: e.tensor_tensor(out=r2[:, 0:n], in0=pr[1][0:64, 0:n], in1=tcc[:, 1, 0:n],
                                                              op=ALU.mult), reads=[pr_t[1], tcc_t], writes=[r_t])
                        S.op("dve", lambda e: e.tensor_tensor(out=krT[:, t0:t0 + n], in0=r1[:, 0:n], in1=r2[:, 0:n],
                                                              op=ALU.add), reads=[r_t], writes=[krT_t])
                    S.barrier()

                with contextlib.ExitStack() as s2:
                    KT = sb(s2, "KT", [128, LT], BF16)
                    KT_t = Tk()
                    V = sb(s2, "V", [128, NT, 128], BF16)
                    V_t = Tk()
                    QT = sb(s2, "QT", [128, L], BF16)
                    QT_t = Tk()
                    QrT = sb(s2, "QrT", [64, L], BF16)
                    QrT_t = Tk()
                    Wq = sb(s2, "Wq", [128, 3, 256], BF16)
                    Wq_t = Tk()
                    Wkv = sb(s2, "Wkv", [128, 2, 256], BF16)
                    Wkv_t = Tk()
                    sq_ = sb(s2, "s2sq", [128, 3, 192], F32)
                    sq_t = Tk()
                    skv = sb(s2, "s2skv", [128, 2, 256], F32)
                    skv_t = Tk()
                    tq = [sb(s2, f"s2tq{i}", [64, 2, 512], F32) for i in range(2)]
                    tq_t = [Tk(), Tk()]
                    r1 = sb(s2, "s2r1", [64, 512], F32)
                    r2 = sb(s2, "s2r2", [64, 512], F32)
                    r_t = Tk()
                    PT = [sb(s2, f"PT{i}", [128, 512], BF16) for i in range(4)]
                    PT_t = [Tk() for _ in range(4)]
                    accA = [sb(s2, f"s2accA{i}", [128, 512], F32) for i in range(2)]
                    accB = [sb(s2, f"s2accB{i}", [128, 512], F32) for i in range(2)]
                    accA_t = [Tk(), Tk()]
                    accB_t = [Tk(), Tk()]
                    rs = sb(s2, "s2rs", [128, 512], F32)
                    rs_t = Tk()
                    ot = [sb(s2, f"s2ot{i}", [128, 512], BF16) for i in range(2)]
                    ot_t = [Tk(), Tk()]
                    pS = [ps(s2, f"pS{i}", [128, 512]) for i in range(3)]
                    pS_t = [Tk() for _ in range(3)]
                    pO = [ps(s2, f"pO{i}", [128, 512]) for i in range(2)]
                    pO_t = [Tk() for _ in range(2)]
                    pZ = [ps(s2, f"pZ{i}", [128, 512]) for i in range(2)]
                    pZ_t = [Tk() for _ in range(2)]
                    pX = ps(s2, "pX", [128, 512])
                    pX_t = Tk()
                    wuqv = wuq_d[0].rearrange("(kc p) n -> p kc n", p=128)
                    wukvv = wukv_d[0].rearrange("(kc p) n -> p kc n", p=128)
                    sc = 192.0 ** -0.5
                    cnt = 0
                    for h in range(8):
                        S.dma(sq_[:], wuqv[:, :, h * 192:(h + 1) * 192], writes=[sq_t])
                        S.op("pool", lambda e: e.tensor_copy(out=Wq[:, :, 0:192], in_=sq_[:]), reads=[sq_t], writes=[Wq_t])
                        for (dst, src) in ((192, 144), (208, 128), (224, 176), (240, 160)):
                            S.op("pool", lambda e: e.tensor_copy(out=Wq[:, :, dst:dst + 16], in_=sq_[:, :, src:src + 16]),
                                 reads=[sq_t], writes=[Wq_t])
                        S.dma(skv[:], wukvv[:, :, h * 256:(h + 1) * 256], writes=[skv_t])
                        S.op("pool", lambda e: e.tensor_copy(out=Wkv[:], in_=skv[:]), reads=[skv_t], writes=[Wkv_t])
                        for j in range(9):
                            t0, n = (0, 256) if j == 0 else (256 + (j - 1) * 512, 512)
                            for kc in range(2):
                                S.op("pe", lambda e: e.matmul(out=pX[:, 0:n], lhsT=Wkv[:, kc, 0:128], rhs=ckvn[:, kc, t0:t0 + n],
                                                              start=(kc == 0), stop=(kc == 1)),
                                     reads=[Wkv_t, ckvn_t], writes=[pX_t], signal=(kc == 1))
                            S.op("act" if j % 2 else "dve", (lambda e: e.copy(out=KT[:, t0:t0 + n], in_=pX[:, 0:n])) if j % 2
                                 else (lambda e: e.tensor_copy(out=KT[:, t0:t0 + n], in_=pX[:, 0:n])),
                                 reads=[pX_t], writes=[KT_t])
                        for g in range(9):
                            tiles = list(range(g * 4, min(g * 4 + 4, NT)))
                            for i, t in enumerate(tiles):
                                for kc in range(2):
                                    S.op("pe", lambda e: e.matmul(out=pX[:, i * 128:(i + 1) * 128],
                                                                  lhsT=ckvn[:, kc, t * 128:(t + 1) * 128], rhs=Wkv[:, kc, 128:256],
                                                                  start=(kc == 0), stop=(kc == 1)),
                                         reads=[Wkv_t, ckvn_t], writes=[pX_t], signal=(kc == 1 and i == len(tiles) - 1))
                            nn = len(tiles)
                            S.op("act" if g % 2 else "dve",
                                 (lambda e: e.copy(out=V[:, tiles[0]:tiles[0] + nn, :].rearrange("p a b -> p (a b)"),
                                                   in_=pX[:, 0:nn * 128])) if g % 2 else
                                 (lambda e: e.tensor_copy(out=V[:, tiles[0]:tiles[0] + nn, :].rearrange("p a b -> p (a b)"),
                                                          in_=pX[:, 0:nn * 128])),
                                 reads=[pX_t], writes=[V_t])
                        for j in range(8):
                            q0 = j * 512
                            S.dma(tq[j % 2][:, 0, :], mc_d[:, 256 + q0:256 + q0 + 512], writes=[tq_t[j % 2]])
                            S.dma(tq[j % 2][:, 1, :], ms_d[:, 256 + q0:256 + q0 + 512], writes=[tq_t[j % 2]])
                            for kc in range(3):
                                S.op("pe", lambda e: e.matmul(out=pX[:], lhsT=Wq[:, kc, 0:128], rhs=cqn[:, kc, q0:q0 + 512],
                                                              start=(kc == 0), stop=(kc == 2)),
                                     reads=[Wq_t, cqn_t], writes=[pX_t], signal=(kc == 2))
                            S.op("act", lambda e: e.copy(out=QT[:, q0:q0 + 512], in_=pX[:]), reads=[pX_t], writes=[QT_t])
                            for c in range(2):
                                for kc in range(3):
                                    S.op("pe", lambda e: e.matmul(out=pZ[c][0:64, :], lhsT=Wq[:, kc, 128 + c * 64:192 + c * 64],
                                                                  rhs=cqn[:, kc, q0:q0 + 512], start=(kc == 0), stop=(kc == 2)),
                                         reads=[Wq_t, cqn_t], writes=[pZ_t[c]], signal=(kc == 2))
                            tt_, tt_t = tq[j % 2], tq_t[j % 2]
                            S.op("dve", lambda e: e.tensor_tensor(out=r1[:], in0=pZ[0][0:64, :], in1=tt_[:, 0, :], op=ALU.mult),
                                 reads=[pZ_t[0], tt_t], writes=[r_t])
                            S.op("dve", lambda e: e.tensor_tensor(out=r2[:], in0=pZ[1][0:64, :], in1=tt_[:, 1, :], op=ALU.mult),
                                 reads=[pZ_t[1], tt_t], writes=[r_t])
                            S.op("dve", lambda e: e.tensor_tensor(out=QrT[:, q0:q0 + 512], in0=r1[:], in1=r2[:], op=ALU.add),
                                 reads=[r_t], writes=[QrT_t])
                        for qb in range(8):
                            q0 = qb * 512
                            po, po_t = pO[qb % 2], pO_t[qb % 2]
                            pz, pz_t = pZ[qb % 2], pZ_t[qb % 2]
                            def emit_st(kt, cn):
                                s_, s_t = pS[cn % 3], pS_t[cn % 3]
                                S.op("pe", lambda e: e.matmul(out=s_[:], lhsT=KT[:, kt * 128:(kt + 1) * 128], rhs=QT[:, q0:q0 + 512],
                                                              start=True, stop=False), reads=[KT_t, QT_t], writes=[s_t], signal=False)
                                S.op("pe", lambda e: e.matmul(out=s_[:], lhsT=krT[:, kt * 128:(kt + 1) * 128], rhs=QrT[:, q0:q0 + 512],
                                                              start=False, stop=True), reads=[krT_t, QrT_t], writes=[s_t])

                            emit_st(0, cnt)
                            for kt in range(NT):
                                s_, s_t = pS[cnt % 3], pS_t[cnt % 3]
                                p_, p_t = PT[cnt % 4], PT_t[cnt % 4]
                                if kt + 1 < NT:
                                    emit_st(kt + 1, cnt + 1)
                                cnt += 1
                                S.op("act", lambda e: e.activation(out=p_[:], in_=s_[:], func=AF.Exp, scale=sc),
                                     reads=[s_t], writes=[p_t])
                                S.op("pe", lambda e: e.matmul(out=po[:], lhsT=V[:, kt, :], rhs=p_[:], start=(kt == 0),
                                                              stop=(kt == NT - 1)), reads=[V_t, p_t], writes=[po_t])
                                aa, aa_t = accA[qb % 2], accA_t[qb % 2]
                                if kt == 0:
                                    S.op("dve", lambda e: e.tensor_copy(out=aa[:], in_=p_[:]), reads=[p_t], writes=[aa_t])
                                else:
                                    S.op("dve", lambda e: e.tensor_tensor(out=aa[:], in0=aa[:], in1=p_[:], op=ALU.add),
                                         reads=[p_t, aa_t], writes=[aa_t])
                            S.op("pe", lambda e: e.matmul(out=pz[:], lhsT=onesf, rhs=accA[qb % 2][:], start=True, stop=True),
                                 reads=[cst_t, accA_t[qb % 2]], writes=[pz_t])
                            S.op("dve", lambda e: e.reciprocal(out=rs[:], in_=pz[:]), reads=[pz_t], writes=[rs_t])
                            o_, o_t = ot[qb % 2], ot_t[qb % 2]
                            S.op("dve", lambda e: e.tensor_tensor(out=o_[:], in0=po[:], in1=rs[:], op=ALU.mult),
                                 reads=[po_t, rs_t], writes=[o_t])
                            S.dma(att_d[h, :, q0:q0 + 512], o_[:], reads=[o_t], writes=[att_t[qb]])
                    S.barrier()

            with contextlib.ExitStack() as st:
                Wqk = sb(st, "Wqk", [128, 8, 512], BF16)
                Wv = sb(st, "Wv", [128, 8, 512], BF16)
                Wg = sb(st, "Wg", [128, 8, 512], BF16)
                Wqk_t, Wv_t, Wg_t = Tk(), Tk(), Tk()
                stg = sb(st, "s3stg", [128, 8, 512], F32)
                stg_t = Tk()
                hb = [sb(st, f"s3h{i}", [128, 8, 128], BF16) for i in range(3)]
                hb_t = [Tk() for _ in range(3)]
                tb = [sb(st, f"s3tb{i}", [128, 2, 256], F32) for i in range(3)]
                tb_t = [Tk() for _ in range(3)]
                ofb = [sb(st, f"s3of{i}", [128, 512], F32) for i in range(3)]
                ofb_t = [Tk() for _ in range(3)]
                Sf = sb(st, "Sf", [128, 2, 512], F32)
                Sb_ = sb(st, "Sb", [128, 2, 512], BF16)
                Sf_t = Tk()
                Sb_t = Tk()
                t1 = sb(st, "s3t1", [128, 512], F32)
                t2 = sb(st, "s3t2", [128, 512], F32)
                t12_t = Tk()
                qk_all = sb(st, "s3qkall", [128, NT, 512], BF16)
                qka_t = [Tk() for _ in range(NT)]
                V_all = sb(st, "s3Vall", [128, NT, 512], BF16)
                Va_t = [Tk() for _ in range(NT)]
                Kdb = [sb(st, f"s3Kd{i}", [128, 256], BF16) for i in range(2)]
                Kdb_t = [Tk(), Tk()]
                KTt = sb(st, "s3KT", [128, 2, 128], BF16)
                QTt = sb(st, "s3QT", [128, 2, 128], BF16)
                QdT = sb(st, "s3QdT", [128, 2, 128], BF16)
                tr_t = Tk()
                STm = sb(st, "s3STm", [128, 128], BF16)
                STm_t = Tk()
                osb = sb(st, "s3o", [128, 512], F32)
                osb_t = Tk()
                sg = sb(st, "s3sg", [128, 512], F32)
                sg_t = Tk()
                scr = sb(st, "s3scr", [128, 512], BF16)
                ssb = sb(st, "s3ss", [128, 4], F32)
                ss_t = Tk()
                rr = sb(st, "s3r", [128, 512], BF16)
                rr_t = Tk()
                rTs = [sb(st, f"s3rT{i}", [128, 4, 128], BF16) for i in range(2)]
                rTs_t = [Tk(), Tk()]
                pAl = [ps(st, f"s3pA{i}", [128, 512]) for i in range(2)]
                pAl_t = [Tk(), Tk()]
                pBl = [ps(st, f"s3pB{i}", [128, 512]) for i in range(2)]
                pBl_t = [Tk(), Tk()]
                pC = ps(st, "s3pC", [128, 512])
                pD = ps(st, "s3pD", [128, 2, 512], BF16)
                pE = ps(st, "s3pE", [128, 512])
                pF = ps(st, "s3pF", [128, 512])
                pC_t, pD_t, pD2_t, pE_t, pF_t = (Tk() for _ in range(5))
                pH = [pC, pE]
                pH_t = [pC_t, pE_t]
                for h in range(4):
                    for (dst, c0, scl) in ((Wqk[:, :, 0:256], h * 256, None), (Wqk[:, :, 256:512], 1024 + h * 256, 0.0625)):
                        S.dma(stg[:, :, 0:256], winv[:, :, c0:c0 + 256], writes=[stg_t])
                        if scl is None:
                            S.op("act", lambda e: e.copy(out=dst, in_=stg[:, :, 0:256]), reads=[stg_t], writes=[Wqk_t])
                        else:
                            S.op("act", lambda e: e.mul(out=dst, in_=stg[:, :, 0:256], mul=scl), reads=[stg_t], writes=[Wqk_t])
                    for (dst, c0, wt_) in ((Wv, 2048 + h * 512, Wv_t), (Wg, 4096 + h * 512, Wg_t)):
                        S.dma(stg[:], winv[:, :, c0:c0 + 512], writes=[stg_t])
                        S.op("act", lambda e: e.copy(out=dst[:], in_=stg[:]), reads=[stg_t], writes=[wt_])
                    for di, dirn in enumerate(("f", "b")):
                        order = list(range(NT)) if dirn == "f" else [1, 0] + list(range(NT - 1, 1, -1))
                        MT = MTf if dirn == "f" else MTb
                        QD = QDf if dirn == "f" else QDb
                        kdc = KD[:, h, di:di + 1]
                        cdc = KD[:, h, 2 + di:3 + di]
                        S.op("pool", lambda e: e.memset(Sf[:], 0.0), writes=[Sf_t])
                        S.op("pool", lambda e: e.memset(Sb_[:], 0.0), writes=[Sb_t])

                        def s3_load(idx):
                            t = order[idx]
                            S.dma(hb[idx % 3][:], hT_d[t], reads=[hT_t[t]], writes=[hb_t[idx % 3]])
                            if dirn == "f":
                                S.dma(tb[idx % 3][:, 0, :], rc_d[t * 128:(t + 1) * 128, :], writes=[tb_t[idx % 3]])
                                S.dma(tb[idx % 3][:, 1, :], rs_d[t * 128:(t + 1) * 128, :], writes=[tb_t[idx % 3]])
                            if dirn == "b" and t >= 2:
                                S.dma(ofb[idx % 3][:], of_d[t - 2], reads=[of_t[t - 2]], writes=[ofb_t[idx % 3]])

                        def s3_proj(idx):
                            if dirn == "b":
                                return
                            h_, h_t = hb[idx % 3], hb_t[idx % 3]
                            pA, pA_t = pAl[idx % 2], pAl_t[idx % 2]
                            pB, pB_t = pBl[idx % 2], pBl_t[idx % 2]
                            for kc in range(8):
                                S.op("pe", lambda e: e.matmul(out=pA[:], lhsT=h_[:, kc, :], rhs=Wqk[:, kc, :], start=(kc == 0),
                                                              stop=(kc == 7)), reads=[h_t, Wqk_t], writes=[pA_t], signal=(kc == 7))
                            for kc in range(8):
                                S.op("pe", lambda e: e.matmul(out=pB[:], lhsT=h_[:, kc, :], rhs=Wv[:, kc, :], start=(kc == 0),
                                                              stop=(kc == 7)), reads=[h_t, Wv_t], writes=[pB_t], signal=(kc == 7))

                        def s3_A(idx):
                            t = order[idx]
                            tb_, tbt = tb[idx % 3], tb_t[idx % 3]
                            pA, pA_t = pAl[idx % 2], pAl_t[idx % 2]
                            pB, pB_t = pBl[idx % 2], pBl_t[idx % 2]
                            qk, qk_t = qk_all[:, t, :], qka_t[t]
                            Vb, Vb_t = V_all[:, t, :], Va_t[t]
                            Kd, Kd_t = Kdb[idx % 2], Kdb_t[idx % 2]
                            for half in (range(2) if dirn == "f" else ()):
                                o = half * 256
                                S.op("dve", lambda e: e.tensor_tensor(out=t1[:, o:o + 256], in0=pA[:, o:o + 256], in1=tb_[:, 0, :],
                                                                      op=ALU.mult), reads=[pA_t, tbt], writes=[t12_t])
                                for part in range(2):
                                    a = o + part * 128
                                    S.op("dve", lambda e: e.tensor_tensor(out=t2[:, a:a + 64], in0=pA[:, a + 64:a + 128],
                                                                          in1=tb_[:, 1, part * 128:part * 128 + 64], op=ALU.mult),
                                         reads=[pA_t, tbt], writes=[t12_t])
                                    S.op("dve", lambda e: e.tensor_tensor(out=t2[:, a + 64:a + 128], in0=pA[:, a:a + 64],
                                                                          in1=tb_[:, 1, part * 128 + 64:part * 128 + 128], op=ALU.mult),
                                         reads=[pA_t, tbt], writes=[t12_t])
                            if dirn == "f":
                                S.op("pool", lambda e: e.tensor_tensor(out=qk[:], in0=t1[:], in1=t2[:], op=ALU.add),
                                     reads=[t12_t], writes=[qk_t])
                                S.op("act", lambda e: e.copy(out=Vb[:], in_=pB[:]), reads=[pB_t], writes=[Vb_t])
                            S.op("pool", lambda e: e.tensor_scalar(out=Kd[:], in0=qk[:, 256:512], scalar1=kdc, scalar2=None, op0=ALU.mult),
                                 reads=[qk_t, dec_t], writes=[Kd_t])

                        s3_load(0)
                        s3_load(1)
                        s3_proj(0)
                        s3_A(0)
                        for idx in range(NT):
                            if idx + 2 < NT:
                                s3_load(idx + 2)
                            if idx + 1 < NT:
                                s3_proj(idx + 1)
                                s3_A(idx + 1)
                            t = order[idx]
                            lat = t >= 2
                            h_, h_t = hb[idx % 3], hb_t[idx % 3]
                            tb_, tbt = tb[idx % 3], tb_t[idx % 3]
                            pA, pA_t = pAl[idx % 2], pAl_t[idx % 2]
                            pB, pB_t = pBl[idx % 2], pBl_t[idx % 2]
                            qk, qk_t = qk_all[:, t, :], qka_t[t]
                            Vb, Vb_t = V_all[:, t, :], Va_t[t]
                            Kd, Kd_t = Kdb[idx % 2], Kdb_t[idx % 2]
                            if lat and dirn == "b":
                                for kc in range(8):
                                    S.op("pe", lambda e: e.matmul(out=pC[:], lhsT=h_[:, kc, :], rhs=Wg[:, kc, :], start=(kc == 0),
                                                                  stop=(kc == 7)), reads=[h_t, Wg_t], writes=[pC_t], signal=(kc == 7))
                                S.op("act", lambda e: e.activation(out=sg[:], in_=pC[:], func=AF.Silu), reads=[pC_t], writes=[sg_t])
                            if lat:
                                for c in range(4):
                                    S.op("pe", lambda e: e.transpose(out=pD[:, 0, c * 128:(c + 1) * 128], in_=qk[:, c * 128:(c + 1) * 128],
                                                                     identity=identb[:]), reads=[qk_t, cb_t], writes=[pD_t], signal=(c == 3))
                                S.op("act", lambda e: e.copy(out=KTt[:].rearrange("p a b -> p (a b)"), in_=pD[:, 0, 256:512]),
                                     reads=[pD_t], writes=[tr_t])
                                S.op("act", lambda e: e.copy(out=QTt[:].rearrange("p a b -> p (a b)"), in_=pD[:, 0, 0:256]),
                                     reads=[pD_t], writes=[tr_t])
                                for dc in range(2):
                                    S.op("dve", lambda e: e.tensor_tensor(out=QdT[:, dc, :], in0=pD[:, 0, dc * 128:(dc + 1) * 128],
                                                                          in1=QD[:, h, :], op=ALU.mult), reads=[pD_t, dec_t], writes=[tr_t])
                                for dc in range(2):
                                    S.op("pe", lambda e: e.matmul(out=pE[:, 0:128], lhsT=KTt[:, dc, :], rhs=QTt[:, dc, :], start=(dc == 0),
                                                                  stop=(dc == 1)), reads=[tr_t], writes=[pE_t], signal=(dc == 1))
                                S.op("dve", lambda e: e.tensor_tensor(out=STm[:], in0=pE[:, 0:128], in1=MT[:, h, :], op=ALU.mult),
                                     reads=[pE_t, dec_t], writes=[STm_t])
                                S.op("pe", lambda e: e.matmul(out=pF[:], lhsT=STm[:], rhs=Vb[:], start=True, stop=False),
                                     reads=[STm_t, Vb_t], writes=[pF_t], signal=False)
                                for dc in range(2):
                                    S.op("pe", lambda e: e.matmul(out=pF[:], lhsT=QdT[:, dc, :], rhs=Sb_[:, dc, :], start=False,
                                                                  stop=(dc == 1)), reads=[tr_t, Sb_t], writes=[pF_t], signal=(dc == 1))
                            for dc in range(2):
                                S.op("pe", lambda e: e.matmul(out=pH[dc][:], lhsT=Kd[:, dc * 128:(dc + 1) * 128], rhs=Vb[:], start=True,
                                                              stop=True), reads=[Kd_t, Vb_t], writes=[pH_t[dc]])
                            if lat:
                                if dirn == "f":
                                    S.op("act", lambda e: e.copy(out=osb[:], in_=pF[:]), reads=[pF_t], writes=[osb_t])
                                    S.dma(of_d[t - 2], osb[:], reads=[osb_t], writes=[of_t[t - 2]])
                                else:
                                    of_, of_tt = ofb[idx % 3], ofb_t[idx % 3]
                                    S.op("dve", lambda e: e.tensor_tensor(out=osb[:], in0=pF[:], in1=of_[:], op=ALU.add),
                                         reads=[pF_t, of_tt], writes=[osb_t])
                                    S.op("pool", lambda e: e.memset(ssb[:, 0:1], 0.0), writes=[ss_t])
                                    S.op("act", lambda e: e.activation(out=scr[:], in_=osb[:], func=AF.Square, accum_out=ssb[:, 0:1]),
                                         reads=[osb_t, ss_t], writes=[ss_t])
                                    S.op("act", lambda e: e.activation(out=ssb[:, 1:2], in_=ssb[:, 0:1], func=AF.Sqrt, scale=1.0 / 512,
                                                                       bias=EPS), reads=[ss_t], writes=[ss_t])
                                    S.op("dve", lambda e: e.reciprocal(out=ssb[:, 2:3], in_=ssb[:, 1:2]), reads=[ss_t], writes=[ss_t])
                                    S.op("dve", lambda e: e.scalar_tensor_tensor(out=rr[:], in0=osb[:], scalar=ssb[:, 2:3], in1=sg[:],
                                                                                 op0=ALU.mult, op1=ALU.mult),
                                         reads=[osb_t, ss_t, sg_t], writes=[rr_t])
                                    for c in range(4):
                                        S.op("pe", lambda e: e.transpose(out=pD[:, 1, c * 128:(c + 1) * 128], in_=rr[:, c * 128:(c + 1) * 128],
                                                                         identity=identb[:]), reads=[rr_t, cb_t], writes=[pD2_t], signal=(c == 3))
                                    rT_, rT_tt = rTs[idx % 2], rTs_t[idx % 2]
                                    S.op("act", lambda e: e.copy(out=rT_[:].rearrange("p a b -> p (a b)"), in_=pD[:, 1, :]),
                                         reads=[pD2_t], writes=[rT_tt])
                                    tk0 = (t - 2) * 128
                                    S.dma(rT_d[h, :, :, tk0:tk0 + 128].rearrange("c p t -> p c t"), rT_[:], reads=[rT_tt],
                                          writes=[rT_t[(t - 2) // 4]])
                            for dc in range(2):
                                S.op("dve", lambda e: e.scalar_tensor_tensor(out=Sb_[:, dc, :], in0=Sf[:, dc, :], scalar=cdc, in1=pH[dc][:],
                                                                             op0=ALU.mult, op1=ALU.add),
                                     reads=[Sf_t, pH_t[dc], dec_t], writes=[Sb_t])
                                S.op("dve", lambda e: e.scalar_tensor_tensor(out=Sf[:, dc, :], in0=Sf[:, dc, :], scalar=cdc, in1=pH[dc][:],
                                                                             op0=ALU.mult, op1=ALU.add),
                                     reads=[Sf_t, pH_t[dc], dec_t], writes=[Sf_t])
                S.barrier()

            with contextlib.ExitStack() as st:
                Wgr = sb(st, "Wgr", [128, 8, 1024], BF16)
                Wgm = sb(st, "Wgm", [128, 8, 1024], BF16)
                Wbr = sb(st, "Wbr", [128, 16, 1024], BF16)
                Wbm = sb(st, "Wbm", [128, 8, 1024], BF16)
                Wo = sb(st, "Wo", [128, 8, 1024], BF16)
                stg = [sb(st, f"s4stg{i}", [128, 8, 256], F32) for i in range(2)]
                stg_t = [Tk(), Tk()]
                wbrv = wbr_d[0].rearrange("(kc p) n -> p kc n", p=128)
                wbmv = wbm_d[0].rearrange("(kc p) n -> p kc n", p=128)
                wov = wo_d[0].rearrange("(kc p) n -> p kc n", p=128)
                k = 0
                jobs = []
                W4_t = [Tk() for _ in range(4)]
                Wo_t = Tk()
                for cb in range(4):
                    cs = slice(cb * 256, (cb + 1) * 256)
                    jobs.append((Wbr[:, 0:8, cs], wbrv[:, 0:8, cs], W4_t[cb]))
                    jobs.append((Wbr[:, 8:16, cs], wbrv[:, 8:16, cs], W4_t[cb]))
                    jobs.append((Wbm[:, :, cs], wbmv[:, :, cs], W4_t[cb]))
                    jobs.append((Wgr[:, :, cs], winv[:, :, 6848 + cb * 256:6848 + (cb + 1) * 256], W4_t[cb]))
                    jobs.append((Wgm[:, :, cs], winv[:, :, 7872 + cb * 256:7872 + (cb + 1) * 256], W4_t[cb]))
                for cb in range(4):
                    cs = slice(cb * 256, (cb + 1) * 256)
                    jobs.append((Wo[:, :, cs], wov[:, :, cs], Wo_t))
                for (dst, src, wt_) in jobs:
                    load_cast(st, dst, wt_, src, None, stg[k % 2][:], stg_t[k % 2], "pool" if k % 2 else "dve")
                    k += 1
                TK4 = 256
                hb = sb(st, "s4h", [128, 8, TK4], BF16)
                hb_t = Tk()
                rb = sb(st, "s4r", [128, 16, TK4], BF16)
                rb_t = Tk()
                ab = sb(st, "s4a", [128, 8, TK4], BF16)
                ab_t = Tk()
                mTb = sb(st, "s4m", [128, 8, TK4], BF16)
                mTb_t = Tk()
                s3_ = sb(st, "s4s3", [128, TK4], F32)
                s4_ = sb(st, "s4s4", [128, TK4], F32)
                sg_t = Tk()
                m1 = sb(st, "s4m1", [128, TK4], F32)
                m2 = sb(st, "s4m2", [128, TK4], F32)
                m_t = Tk()
                x_ = sb(st, "s4x", [128, D], F32)
                x_t = Tk()
                yt = sb(st, "s4y", [128, D], F32)
                yt_t = Tk()
                o_ = sb(st, "s4xo", [128, D], F32)
                o_t = Tk()
                P = [ps(st, f"s4p{i}", [128, 512]) for i in range(4)]
                P_t = [Tk() for _ in range(4)]
                PY = [ps(st, f"s4py{i}", [128, 512]) for i in range(2)]
                PY_t = [Tk() for _ in range(2)]
                for tbk in range(L // TK4):
                    q0 = tbk * TK4
                    for i in range(TK4 // 128):
                        tl = 2 + tbk * (TK4 // 128) + i
                        S.dma(hb[:, :, i * 128:(i + 1) * 128], hT_d[tl], reads=[hT_t[tl]], writes=[hb_t])
                    for hh in range(4):
                        S.dma(rb[:, hh * 4:(hh + 1) * 4, :], rT_d[hh, :, :, q0:q0 + TK4].rearrange("c p t -> p c t"),
                              reads=[rT_t[q0 // 512]], writes=[rb_t])
                    S.dma(ab[:], att_d[:, :, q0:q0 + TK4].rearrange("h p t -> p h t"), reads=[att_t[q0 // 512]], writes=[ab_t])
                    for fc in range(8):
                        fs = slice(fc * 128, (fc + 1) * 128)
                        for kc in range(16):
                            S.op("pe", lambda e: e.matmul(out=P[0][:, 0:TK4], lhsT=Wbr[:, kc, fs], rhs=rb[:, kc, :], start=(kc == 0), stop=(kc == 15)),
                                 reads=[W4_t[fc // 2], rb_t], writes=[P_t[0]], signal=(kc == 15))
                        for (pi, Wx, src, src_t) in ((1, Wbm, ab, ab_t), (2, Wgr, hb, hb_t), (3, Wgm, hb, hb_t)):
                            for kc in range(8):
                                S.op("pe", lambda e: e.matmul(out=P[pi][:, 0:TK4], lhsT=Wx[:, kc, fs], rhs=src[:, kc, :], start=(kc == 0),
                                                              stop=(kc == 7)), reads=[W4_t[fc // 2], src_t], writes=[P_t[pi]], signal=(kc == 7))
                        S.op("act", lambda e: e.activation(out=s3_[:], in_=P[2][:, 0:TK4], func=AF.Sigmoid), reads=[P_t[2]], writes=[sg_t])
                        S.op("act", lambda e: e.activation(out=s4_[:], in_=P[3][:, 0:TK4], func=AF.Sigmoid), reads=[P_t[3]], writes=[sg_t])
                        S.op("dve", lambda e: e.tensor_tensor(out=m1[:], in0=P[0][:, 0:TK4], in1=s3_[:], op=ALU.mult),
                             reads=[P_t[0], sg_t], writes=[m_t])
                        S.op("dve", lambda e: e.tensor_tensor(out=m2[:], in0=P[1][:, 0:TK4], in1=s4_[:], op=ALU.mult),
                             reads=[P_t[1], sg_t], writes=[m_t])
                        S.op("pool", lambda e: e.tensor_tensor(out=mTb[:, fc, :], in0=m1[:], in1=m2[:], op=ALU.add),
                             reads=[m_t], writes=[mTb_t])
                    for tt in range(TK4 // 128):
                        gt = tbk * (TK4 // 128) + tt
                        S.dma(x_[:], x_d[b, gt * 128:(gt + 1) * 128, :], writes=[x_t])
                        for cb in range(2):
                            for kc in range(8):
                                S.op("pe", lambda e: e.matmul(out=PY[cb][:], lhsT=mTb[:, kc, tt * 128:(tt + 1) * 128],
                                                              rhs=Wo[:, kc, cb * 512:(cb + 1) * 512], start=(kc == 0), stop=(kc == 7)),
                                     reads=[mTb_t, Wo_t], writes=[PY_t[cb]], signal=(kc == 7))
                            S.op("dve", lambda e: e.tensor_tensor(out=yt[:, cb * 512:(cb + 1) * 512], in0=PY[cb][:],
                                                                  in1=G1[:, b, cb * 512:(cb + 1) * 512], op=ALU.mult),
                                 reads=[PY_t[cb], G_t], writes=[yt_t])
                        S.op("pool", lambda e: e.tensor_tensor(out=o_[:], in0=yt[:], in1=x_[:], op=ALU.add),
                             reads=[yt_t, x_t], writes=[o_t])
                        S.dma(x1_d[b * 32 + gt], o_[:], reads=[o_t], writes=[x1_t[b * 32 + gt]])
                S.barrier()

        mix_stack.close()
        I32 = mybir.dt.int32
        NTI = NB * 32
        with contextlib.ExitStack() as st:
            posk = sb(st, "posk", [128, NTI * 4], F32)
            ekk = sb(st, "ekk", [128, NTI * 4], F32)
            g4 = sb(st, "g4", [128, NTI * 4], F32)
            pk_t = Tk()
            base = sb(st, "base", [128, 32], F32)
            base_t = Tk()
            desti = sb(st, "desti", [128, NTI * 4], I32)
            desti_t = Tk()
            widx = sb(st, "widx", [128, 8, NBLK], I32)
            bidx = sb(st, "bidx", [2, NBLK], I32)
            widx_t = Tk()
            iota32 = cst[:, 10, 0:32]
            h2d_t = [Tk() for _ in range(NTI)]
            with contextlib.ExitStack() as p1:
                GS2 = sb(p1, "GS2", [128, NB, D], F32)
                SH2 = sb(p1, "SH2", [128, NB, D], F32)
                GS_t = Tk()
                pB2 = [ps(p1, f"p1B{i}", [128, 512]) for i in range(2)]
                pB2_t = [Tk(), Tk()]
                pR = ps(p1, "p1R", [128, 512])
                pR_t = Tk()
                dg = sb(p1, "p1dg", [128, 8, 128], F32)
                dg_t = Tk()
                for bb in range(NB):
                    for (dst, vfn) in ((GS2, lambda c: gs2[:, bb, c:c + 1]), (SH2, lambda c: mT[:, 24 + c, bb:bb + 1])):
                        for c in range(8):
                            S.op("dve", lambda e: e.tensor_scalar(out=dg[:, c, :], in0=identf, scalar1=vfn(c), scalar2=None,
                                                                  op0=ALU.mult), reads=[cst_t, mT_t, gs_t], writes=[dg_t])
                        for c in range(8):
                            S.op("pe", lambda e: e.matmul(out=pB2[c // 4][:, (c % 4) * 128:(c % 4 + 1) * 128], lhsT=onesf,
                                                          rhs=dg[:, c, :], start=True, stop=True),
                                 reads=[dg_t, cst_t], writes=[pB2_t[c // 4]])
                        for hh in range(2):
                            S.op("act", lambda e: e.copy(out=dst[:, bb, hh * 512:(hh + 1) * 512], in_=pB2[hh][:]),
                                 reads=[pB2_t[hh]], writes=[GS_t])
                bfull = sb(p1, "p1bfull", [32, 3072], F32)
                bfb = sb(p1, "p1bfb", [32, 3072], BF16)
                bf_t = Tk()
                S.dma(bfull[:, 0:2048], ebgu_d[0], writes=[bf_t])
                S.dma(bfull[:, 2048:3072], ebd_d[0], writes=[bf_t])
                bfb_t = Tk()
                S.op("dve", lambda e: e.tensor_copy(out=bfb[:], in_=bfull[:]), reads=[bf_t], writes=[bfb_t])
                S.dma(bias_d[:, :], bfb[:], reads=[bfb_t])
                Wr = sb(p1, "Wr", [128, 8, 32], F32)
                Wr_t = Tk()
                S.dma(Wr[:], rw_d[0].rearrange("(kc p) n -> p kc n", p=128), writes=[Wr_t])
                rbs = sb(p1, "rbs", [1, 32], F32)
                rbs_t = Tk()
                S.dma(rbs[:], rb_d[:, :], writes=[rbs_t])
                xt = [sb(p1, f"p1x{i}", [128, D], F32) for i in range(2)]
                xt_t = [Tk(), Tk()]
                xn = sb(p1, "p1xn", [128, D], F32)
                xn_t = Tk()
                h2 = sb(p1, "p1h2", [128, D], F32)
                h2_t = Tk()
                h2bf = [sb(p1, f"p1h2b{i}", [128, D], BF16) for i in range(2)]
                h2bf_t = [Tk(), Tk()]
                h2f = sb(p1, "p1h2f", [128, 8, 128], F32)
                h2f_t = Tk()
                scr = sb(p1, "p1scr", [128, D], BF16)
                ssb = sb(p1, "p1ss", [128, 4], F32)
                tmp_t = Tk()
                lgt = sb(p1, "p1lg", [128, 32], F32)
                mk = sb(p1, "p1mk", [128, 32], F32)
                pos = sb(p1, "p1pos", [128, 32], F32)
                s32 = sb(p1, "p1s32", [128, 32], F32)
                m8 = sb(p1, "p1m8", [128, 8], F32)
                e4 = sb(p1, "p1e4", [128, 4], F32)
                sm = sb(p1, "p1sm", [128, 4], F32)
                rt_t = Tk()
                S.op("pool", lambda e: e.memset(base[:], 0.0), writes=[base_t])
                S.dma(xt[0][:], x1_d[0], reads=[x1_t[0]], writes=[xt_t[0]])
                for ti in range(NTI):
                    bb = ti // 32
                    if ti + 1 < NTI:
                        S.dma(xt[(ti + 1) % 2][:], x1_d[ti + 1], reads=[x1_t[ti + 1]], writes=[xt_t[(ti + 1) % 2]])
                    x_, x_t = xt[ti % 2], xt_t[ti % 2]
                    S.op("pool", lambda e: e.memset(ssb[:, 0:1], 0.0), writes=[tmp_t])
                    S.op("act", lambda e: e.activation(out=scr[:], in_=x_[:], func=AF.Square, accum_out=ssb[:, 0:1]),
                         reads=[x_t, tmp_t], writes=[tmp_t])
                    S.op("act", lambda e: e.activation(out=ssb[:, 1:2], in_=ssb[:, 0:1], func=AF.Sqrt, scale=1.0 / D, bias=EPS),
                         reads=[tmp_t], writes=[tmp_t])
                    S.op("dve", lambda e: e.reciprocal(out=ssb[:, 2:3], in_=ssb[:, 1:2]), reads=[tmp_t], writes=[tmp_t])
                    S.op("dve", lambda e: e.tensor_scalar(out=xn[:], in0=x_[:], scalar1=ssb[:, 2:3], scalar2=None, op0=ALU.mult),
                         reads=[x_t, tmp_t], writes=[xn_t])
                    S.op("dve", lambda e: e.tensor_tensor(out=xn[:], in0=xn[:], in1=GS2[:, bb, :], op=ALU.mult),
                         reads=[xn_t, GS_t], writes=[xn_t])
                    S.op("pool", lambda e: e.tensor_tensor(out=h2[:], in0=xn[:], in1=SH2[:, bb, :], op=ALU.add),
                         reads=[xn_t, GS_t], writes=[h2_t])
                    hb_, hb_t = h2bf[ti % 2], h2bf_t[ti % 2]
                    S.op("act", lambda e: e.copy(out=hb_[:], in_=h2[:]), reads=[h2_t], writes=[hb_t])
                    S.dma(h2_d[ti], hb_[:], reads=[hb_t], writes=[h2d_t[ti]])
                    for c in range(8):
                        S.op("pe", lambda e: e.transpose(out=pB2[c // 4][:, (c % 4) * 128:(c % 4 + 1) * 128],
                                                         in_=h2[:, c * 128:(c + 1) * 128], identity=identf),
                             reads=[h2_t, cst_t], writes=[pB2_t[c // 4]], signal=(c % 4 == 3))
                    S.op("dve", lambda e: e.tensor_copy(out=h2f[:, 0:4, :].rearrange("p a b -> p (a b)"), in_=pB2[0][:]),
                         reads=[pB2_t[0]], writes=[h2f_t])
                    S.op("act", lambda e: e.copy(out=h2f[:, 4:8, :].rearrange("p a b -> p (a b)"), in_=pB2[1][:]),
                         reads=[pB2_t[1]], writes=[h2f_t])
                    for kc in range(8):
                        S.op("pe", lambda e: e.matmul(out=pR[:, 0:32], lhsT=h2f[:, kc, :], rhs=Wr[:, kc, :], start=(kc == 0), stop=False),
                             reads=[h2f_t, Wr_t], writes=[pR_t], signal=False)
                    S.op("pe", lambda e: e.matmul(out=pR[:, 0:32], lhsT=cst[0:1, 1, :], rhs=rbs[:], start=False, stop=True),
                         reads=[cst_t, rbs_t], writes=[pR_t])
                    S.op("dve", lambda e: e.tensor_copy(out=lgt[:], in_=pR[:, 0:32]), reads=[pR_t], writes=[rt_t])
                    S.op("dve", lambda e: e.max(out=m8[:], in_=lgt[:]), reads=[rt_t], writes=[rt_t])
                    S.op("dve", lambda e: e.tensor_scalar(out=mk[:], in0=lgt[:], scalar1=m8[:, 3:4], scalar2=None, op0=ALU.is_ge),
                         reads=[rt_t], writes=[rt_t])
                    S.op("dve", lambda e: e.tensor_scalar(out=sm[:, 0:1], in0=m8[:, 0:1], scalar1=-1.0, scalar2=None, op0=ALU.mult),
                         reads=[rt_t], writes=[rt_t])
                    S.op("act", lambda e: e.activation(out=e4[:], in_=m8[:, 0:4], func=AF.Exp, bias=sm[:, 0:1], scale=1.0),
                         reads=[rt_t], writes=[rt_t])
                    S.op("dve", lambda e: e.reduce_sum(out=sm[:, 1:2], in_=e4[:], axis=mybir.AxisListType.X), reads=[rt_t], writes=[rt_t])
                    S.op("dve", lambda e: e.reciprocal(out=sm[:, 2:3], in_=sm[:, 1:2]), reads=[rt_t], writes=[rt_t])
                    S.op("dve", lambda e: e.tensor_scalar(out=g4[:, ti * 4:ti * 4 + 4], in0=e4[:], scalar1=sm[:, 2:3], scalar2=None, op0=ALU.mult),
                         reads=[rt_t], writes=[pk_t])
                    S.op("pe", lambda e: e.matmul(out=pR[:, 32:64], lhsT=cst[:, 9, :], rhs=mk[:], start=True, stop=True),
                         reads=[rt_t, cst_t], writes=[pR_t])
                    S.op("pe", lambda e: e.matmul(out=pR[:, 64:96], lhsT=onesf, rhs=mk[:], start=True, stop=True),
                         reads=[rt_t, cst_t], writes=[pR_t])
                    S.op("dve", lambda e: e.tensor_tensor(out=pos[:], in0=pR[:, 32:64], in1=base[:], op=ALU.add),
                         reads=[pR_t, base_t], writes=[rt_t])
                    for k in range(4):
                        S.op("dve", lambda e: e.scalar_tensor_tensor(out=s32[:], in0=lgt[:], scalar=m8[:, k:k + 1], in1=pos[:],
                                                                     op0=ALU.is_equal, op1=ALU.mult), reads=[rt_t], writes=[rt_t])
                        S.op("dve", lambda e: e.reduce_sum(out=posk[:, ti * 4 + k:ti * 4 + k + 1], in_=s32[:], axis=mybir.AxisListType.X),
                             reads=[rt_t], writes=[pk_t])
                        S.op("dve", lambda e: e.scalar_tensor_tensor(out=s32[:], in0=lgt[:], scalar=m8[:, k:k + 1], in1=iota32,
                                                                     op0=ALU.is_equal, op1=ALU.mult), reads=[rt_t, cst_t, pk_t], writes=[rt_t])
                        S.op("dve", lambda e: e.reduce_sum(out=ekk[:, ti * 4 + k:ti * 4 + k + 1], in_=s32[:], axis=mybir.AxisListType.X),
                             reads=[rt_t], writes=[pk_t])
                    S.op("dve", lambda e: e.tensor_tensor(out=base[:], in0=pR[:, 64:96], in1=base[:], op=ALU.add),
                         reads=[pR_t, base_t, rt_t], writes=[base_t])
                ci = sb(p1, "p2ci", [128, 32], I32)
                padded = sb(p1, "p2pad", [128, 32], F32)
                pst = sb(p1, "p2pst", [128, 32], F32)
                pend = sb(p1, "p2pend", [128, 32], F32)
                bst = sb(p1, "p2bst", [128, NBLK], F32)
                be = sb(p1, "p2be", [128, NBLK], F32)
                wf = sb(p1, "p2wf", [128, 8, NBLK], F32)
                df = sb(p1, "p2df", [128, NTI * 4], F32)
                p2_t = Tk()
                S.op("dve", lambda e: e.tensor_copy(out=ci[:], in_=base[:]), reads=[base_t], writes=[p2_t])
                S.op("dve", lambda e: e.tensor_scalar(out=ci[:], in0=ci[:], scalar1=511, scalar2=None, op0=ALU.add), reads=[p2_t], writes=[p2_t])
                S.op("dve", lambda e: e.tensor_scalar(out=ci[:], in0=ci[:], scalar1=-512, scalar2=None, op0=ALU.bitwise_and), reads=[p2_t], writes=[p2_t])
                S.op("dve", lambda e: e.tensor_copy(out=padded[:], in_=ci[:]), reads=[p2_t], writes=[p2_t])
                S.op("dve", lambda e: e.memset(pst[:], 0.0), reads=[p2_t], writes=[p2_t])
                for ee in range(1, 32):
                    S.op("dve", lambda e: e.tensor_tensor(out=pst[:, ee:ee + 1], in0=pst[:, ee - 1:ee], in1=padded[:, ee - 1:ee], op=ALU.add),
                         reads=[p2_t], writes=[p2_t])
                S.op("dve", lambda e: e.tensor_tensor(out=pend[:], in0=pst[:], in1=padded[:], op=ALU.add), reads=[p2_t], writes=[p2_t])
                S.op("dve", lambda e: e.tensor_scalar(out=bst[:], in0=cst[:, 10, 0:NBLK], scalar1=512.0, scalar2=None, op0=ALU.mult),
                     reads=[cst_t, p2_t], writes=[p2_t])
                S.op("dve", lambda e: e.memset(be[:], 0.0), reads=[p2_t], writes=[p2_t])
                for ee in range(32):
                    S.op("dve", lambda e: e.scalar_tensor_tensor(out=be[:], in0=bst[:], scalar=pend[:, ee:ee + 1], in1=be[:],
                                                                 op0=ALU.is_ge, op1=ALU.add), reads=[p2_t], writes=[p2_t])
                S.op("dve", lambda e: e.tensor_scalar(out=be[:], in0=be[:], scalar1=31.0, scalar2=None, op0=ALU.min), reads=[p2_t], writes=[p2_t])
                for kc in range(8):
                    S.op("dve", lambda e: e.tensor_scalar(out=wf[:, kc, :], in0=be[:], scalar1=1024.0, scalar2=cst[:, 11, kc:kc + 1],
                                                          op0=ALU.mult, op1=ALU.add), reads=[p2_t, cst_t], writes=[p2_t])
                S.op("dve", lambda e: e.tensor_copy(out=widx[:], in_=wf[:]), reads=[p2_t], writes=[widx_t])
                S.op("dve", lambda e: e.tensor_copy(out=bidx[:], in_=be[0:2, :]), reads=[p2_t], writes=[widx_t])
                for c in range(NTI * 4):
                    S.op("dve", lambda e: e.scalar_tensor_tensor(out=s32[:], in0=iota32, scalar=ekk[:, c:c + 1], in1=pst[:],
                                                                 op0=ALU.is_equal, op1=ALU.mult), reads=[p2_t, pk_t, cst_t, rt_t], writes=[rt_t])
                    S.op("dve", lambda e: e.reduce_sum(out=df[:, c:c + 1], in_=s32[:], axis=mybir.AxisListType.X), reads=[rt_t], writes=[p2_t])
                S.op("dve", lambda e: e.tensor_tensor(out=df[:], in0=df[:], in1=posk[:], op=ALU.add), reads=[p2_t, pk_t], writes=[p2_t])
                S.op("dve", lambda e: e.tensor_copy(out=desti[:], in_=df[:]), reads=[p2_t], writes=[desti_t])
                for ti in range(NTI):
                    hb_, hb_t = h2bf[ti % 2], h2bf_t[ti % 2]
                    S.dma(hb_[:], h2_d[ti], reads=[h2d_t[ti]], writes=[hb_t])
                    for k in range(4):
                        cidx = ti * 4 + k
                        S.dma_ind(lambda e: e.indirect_dma_start(
                            out=xs_d[:, :], out_offset=bass.IndirectOffsetOnAxis(ap=desti[:, cidx:cidx + 1], axis=0),
                            in_=hb_[:], in_offset=None),
                            reads=[hb_t, desti_t])
                S.barrier()

            with contextlib.ExitStack() as p4:
                egu2 = egu_d[0].rearrange("e k n -> (e k) n")
                edn2 = edn_d[0].rearrange("e k n -> (e k) n")
                Wgu = [sb(p4, f"Wgu{i}", [128, 8, 1024], BF16) for i in range(3)]
                Wgu_t = [Tk(), Tk(), Tk()]
                Wd = [sb(p4, f"Wd{i}", [128, 4, 1024], BF16) for i in range(2)]
                Wd_t = [Tk(), Tk()]
                stg = [sb(p4, f"p4stg{i}", [128, 2048], F32) for i in range(5)]
                stg_t = [Tk() for _ in range(5)]
                browb = [sb(p4, f"browb{i}", [2, 3072], BF16) for i in range(2)]
                browb_t = [Tk(), Tk()]
                xs = [sb(p4, f"p4xs{i}", [128, D], BF16) for i in range(4)]
                xs_t = [Tk() for _ in range(4)]
                xsT = sb(p4, "p4xsT", [128, 8, 512], BF16)
                xsT_t = Tk()
                aT = [sb(p4, f"aT{i}", [128, 4, 512], BF16) for i in range(2)]
                aT_t = [Tk(), Tk()]
                g1 = sb(p4, "p4g1", [128, 512], F32)
                u1 = sb(p4, "p4u1", [128, 512], F32)
                glu = sb(p4, "p4gl", [128, 512], F32)
                g1_t, u1_t, glu_t = Tk(), Tk(), Tk()
                ysb = sb(p4, "p4ys", [128, 4, D], F32)
                ysb_t = [[Tk(), Tk()] for _ in range(4)]
                pG = [ps(p4, f"p4G{i}", [128, 512]) for i in range(2)]
                pG_t = [Tk(), Tk()]
                pU = [ps(p4, f"p4U{i}", [128, 512]) for i in range(2)]
                pU_t = [Tk(), Tk()]
                pY = [ps(p4, f"p4Y{i}", [128, 512]) for i in range(2)]
                pY_t = [Tk(), Tk()]
                pX = [ps(p4, f"p4X{i}", [128, 2, 512], BF16) for i in range(2)]
                pX_t = [Tk(), Tk()]
                ISC = 1.0 / 1.702
                cnt = dict(sk=0, gk=0, yk=0)

                def w_load_gu_block(blk):
                    for kc in range(8):
                        si = cnt["sk"] % 5
                        cnt["sk"] += 1
                        S.dma_ind(lambda e: e.indirect_dma_start(
                            out=stg[si][:, :], out_offset=None, in_=egu2[:, :],
                            in_offset=bass.IndirectOffsetOnAxis(ap=widx[:, kc, blk:blk + 1], axis=0)),
                            reads=[widx_t], writes=[stg_t[si]])
                        for hf in range(2):
                            wi3 = (2 * blk + hf) % 3
                            S.op("act", lambda e: e.copy(out=Wgu[wi3][:, kc, :].rearrange("p (g n) -> p g n", g=2),
                                                         in_=stg[si][:].rearrange("p (g h n) -> p g h n", g=2, h=2)[:, :, hf, :]),
                                 reads=[stg_t[si]], writes=[Wgu_t[wi3]])

                def w_load_d(blk, hf, wi):
                    wd, wd_t = Wd[wi], Wd_t[wi]
                    for jq in range(2):
                        si = cnt["sk"] % 5
                        cnt["sk"] += 1
                        for i in range(2):
                            kc = hf * 4 + jq * 2 + i
                            S.dma_ind(lambda e: e.indirect_dma_start(
                                out=stg[si][:, i * 1024:(i + 1) * 1024], out_offset=None, in_=edn2[:, :],
                                in_offset=bass.IndirectOffsetOnAxis(ap=widx[:, kc, blk:blk + 1], axis=0)),
                                reads=[widx_t], writes=[stg_t[si]])
                        S.op("act", lambda e: e.mul(out=wd[:, jq * 2:(jq + 1) * 2, :], in_=stg[si][:].rearrange("p (a b) -> p a b", a=2), mul=ISC),
                             reads=[stg_t[si]], writes=[wd_t])

                def b_load(blk):
                    S.dma_ind(lambda e: e.indirect_dma_start(
                        out=browb[blk % 2][0:2, :], out_offset=None, in_=bias_d[:, :],
                        in_offset=bass.IndirectOffsetOnAxis(ap=bidx[0:2, blk:blk + 1], axis=0)),
                        reads=[widx_t], writes=[browb_t[blk % 2]])

                def x_dma(blk):
                    for tt in range(4):
                        r0 = blk * 512 + tt * 128
                        S.dma(xs[tt][:], xs_d[r0:r0 + 128, :], writes=[xs_t[tt]])

                def x_tr(blk):
                    for tt in range(4):
                        for kc in range(8):
                            S.op("pe", lambda e: e.transpose(out=pX[tt % 2][:, kc // 4, (kc % 4) * 128:(kc % 4 + 1) * 128],
                                                             in_=xs[tt][:, kc * 128:(kc + 1) * 128], identity=identb[:]),
                                 reads=[xs_t[tt], cb_t], writes=[pX_t[tt % 2]], signal=(kc == 7))
                        S.op("dve", lambda e: e.tensor_copy(out=xsT[:, 0:4, tt * 128:(tt + 1) * 128],
                                                            in_=pX[tt % 2][:, 0, :].rearrange("p (a b) -> p a b", a=4)),
                             reads=[pX_t[tt % 2]], writes=[xsT_t])
                        S.op("dve", lambda e: e.tensor_copy(out=xsT[:, 4:8, tt * 128:(tt + 1) * 128],
                                                            in_=pX[tt % 2][:, 1, :].rearrange("p (a b) -> p a b", a=4)),
                             reads=[pX_t[tt % 2]], writes=[xsT_t])

                def gu_unit(blk, hf, wi, au):
                    wg, wg_t = Wgu[(2 * blk + hf) % 3], Wgu_t[(2 * blk + hf) % 3]
                    bb_, bb_t = browb[blk % 2], browb_t[blk % 2]
                    a_, a_t = aT[au], aT_t[au]
                    for j in range(4):
                        i2 = cnt["gk"] % 2
                        cnt["gk"] += 1
                        fcol = hf * 512 + j * 128
                        for kc in range(8):
                            S.op("pe", lambda e: e.matmul(out=pG[i2][:], lhsT=wg[:, kc, j * 128:(j + 1) * 128], rhs=xsT[:, kc, :],
                                                          start=(kc == 0), stop=False), reads=[wg_t, xsT_t], writes=[pG_t[i2]], signal=False)
                        S.op("pe", lambda e: e.matmul(out=pG[i2][:], lhsT=bb_[0:1, fcol:fcol + 128], rhs=cbones[0:1, :], start=False, stop=True),
                             reads=[bb_t, cb_t], writes=[pG_t[i2]])
                        for kc in range(8):
                            S.op("pe", lambda e: e.matmul(out=pU[i2][:], lhsT=wg[:, kc, 512 + j * 128:512 + (j + 1) * 128], rhs=xsT[:, kc, :],
                                                          start=(kc == 0), stop=False), reads=[wg_t, xsT_t], writes=[pU_t[i2]], signal=False)
                        S.op("pe", lambda e: e.matmul(out=pU[i2][:], lhsT=bb_[0:1, 1024 + fcol:1024 + fcol + 128], rhs=cbones[0:1, :],
                                                      start=False, stop=True), reads=[bb_t, cb_t], writes=[pU_t[i2]])
                        S.op("dve", lambda e: e.tensor_scalar(out=g1[:], in0=pG[i2][:], scalar1=7.0, scalar2=None, op0=ALU.min),
                             reads=[pG_t[i2]], writes=[g1_t])
                        S.op("act", lambda e: e.activation(out=glu[:], in_=g1[:], func=AF.Silu, scale=1.702), reads=[g1_t], writes=[glu_t])
                        S.op("dve", lambda e: e.tensor_scalar(out=u1[:], in0=pU[i2][:], scalar1=1.0, scalar2=8.0, op0=ALU.add, op1=ALU.min),
                             reads=[pU_t[i2]], writes=[u1_t])
                        S.op("dve", lambda e: e.scalar_tensor_tensor(out=a_[:, j, :], in0=u1[:], scalar=-6.0, in1=glu[:],
                                                                     op0=ALU.max, op1=ALU.mult), reads=[u1_t, glu_t], writes=[a_t])

                def dn_unit(blk, hf, wi, au):
                    wd, wd_t = Wd[wi], Wd_t[wi]
                    bb_, bb_t = browb[blk % 2], browb_t[blk % 2]
                    a_, a_t = aT[au], aT_t[au]
                    for tt in range(4):
                        for cb in range(2):
                            yi = cnt["yk"] % 2
                            cnt["yk"] += 1
                            for j in range(4):
                                S.op("pe", lambda e: e.matmul(out=pY[yi][:], lhsT=a_[:, j, tt * 128:(tt + 1) * 128],
                                                              rhs=wd[:, j, cb * 512:(cb + 1) * 512], start=(j == 0), stop=(j == 3 and hf == 1)),
                                     reads=[a_t, wd_t], writes=[pY_t[yi]], signal=(j == 3 and hf == 1))
                            yv = ysb[:, tt, cb * 512:(cb + 1) * 512]
                            if hf == 0:
                                S.op("pe", lambda e: e.matmul(out=pY[yi][:], lhsT=cbones[0:1, 0:128], rhs=bb_[0:1, 2048 + cb * 512:2048 + (cb + 1) * 512],
                                                              start=False, stop=True), reads=[bb_t, cb_t], writes=[pY_t[yi]])
                                S.op("dve", lambda e: e.tensor_copy(out=yv, in_=pY[yi][:]), reads=[pY_t[yi]], writes=[ysb_t[tt][cb]])
                            else:
                                S.op("dve", lambda e: e.tensor_tensor(out=yv, in0=pY[yi][:], in1=yv, op=ALU.add),
                                     reads=[pY_t[yi], ysb_t[tt][cb]], writes=[ysb_t[tt][cb]])
                    if hf == 1:
                        S.dma(ys_d[blk * 512:(blk + 1) * 512, :].rearrange("(t p) c -> p t c", p=128), ysb[:],
                              reads=[ysb_t[tt][cb] for tt in range(4) for cb in range(2)])

                cbones = sb(p4, "cbones", [1, 512], BF16)
                S.op("dve", lambda e: e.memset(cbones[:], 1.0), reads=[cb_t], writes=[cb_t])
                units = [(blk, hf) for blk in range(NBLK) for hf in range(2)]
                NU = len(units)
                b_load(0)
                x_dma(0)
                w_load_gu_block(0)
                w_load_d(0, 0, 0)
                w_load_d(0, 1, 1)
                x_tr(0)
                gu_unit(0, 0, 0, 0)
                w_load_gu_block(1)
                b_load(1)
                x_dma(1)
                for ui, (blk, hf) in enumerate(units):
                    if ui + 1 < NU:
                        nb_, nh_ = units[ui + 1]
                        if nh_ == 0:
                            x_tr(nb_)
                        gu_unit(nb_, nh_, (ui + 1) % 2, (ui + 1) % 2)
                        if nh_ == 0 and nb_ + 1 < NBLK:
                            w_load_gu_block(nb_ + 1)
                        if nh_ == 0 and nb_ + 1 < NBLK:
                            b_load(nb_ + 1)
                            x_dma(nb_ + 1)
                    dn_unit(blk, hf, ui % 2, ui % 2)
                    if ui + 2 < NU:
                        b2, h2_ = units[ui + 2]
                        w_load_d(b2, h2_, (ui + 2) % 2)
                S.barrier()

            with contextlib.ExitStack() as p5:
                yk = [sb(p5, f"p5y{i}", [128, D], F32) for i in range(4)]
                yk_t = [Tk() for _ in range(4)]
                accm = sb(p5, "p5acc", [128, D], F32)
                acc_t = Tk()
                x_ = sb(p5, "p5x", [128, D], F32)
                x_t = Tk()
                scr = sb(p5, "p5scr", [128, D], BF16)
                ssb = sb(p5, "p5ss", [128, 4], F32)
                tmp_t = Tk()
                yo = sb(p5, "p5yo", [128, D], F32)
                yo_t = Tk()
                for ti in range(NTI):
                    bb = ti // 32
                    S.dma(x_[:], x1_d[ti], reads=[x1_t[ti]], writes=[x_t])
                    for k in range(4):
                        cidx = ti * 4 + k
                        S.dma_ind(lambda e: e.indirect_dma_start(
                            out=yk[k][:], out_offset=None, in_=ys_d[:, :],
                            in_offset=bass.IndirectOffsetOnAxis(ap=desti[:, cidx:cidx + 1], axis=0)),
                            reads=[desti_t], writes=[yk_t[k]])
                    S.op("dve", lambda e: e.tensor_scalar(out=accm[:], in0=yk[0][:], scalar1=g4[:, ti * 4:ti * 4 + 1], scalar2=None, op0=ALU.mult),
                         reads=[yk_t[0], pk_t], writes=[acc_t])
                    for k in range(1, 4):
                        S.op("dve", lambda e: e.scalar_tensor_tensor(out=accm[:], in0=yk[k][:], scalar=g4[:, ti * 4 + k:ti * 4 + k + 1], in1=accm[:],
                                                                     op0=ALU.mult, op1=ALU.add), reads=[yk_t[k], pk_t, acc_t], writes=[acc_t])
                    S.op("dve", lambda e: e.tensor_tensor(out=accm[:], in0=accm[:], in1=G2[:, bb, :], op=ALU.mult),
                         reads=[acc_t, G_t], writes=[acc_t])
                    S.op("dve", lambda e: e.tensor_tensor(out=accm[:], in0=accm[:], in1=x_[:], op=ALU.add), reads=[acc_t, x_t], writes=[acc_t])
                    S.op("dve", lambda e: e.memset(ssb[:, 0:1], 0.0), writes=[tmp_t])
                    S.op("act", lambda e: e.activation(out=scr[:], in_=accm[:], func=AF.Square, accum_out=ssb[:, 0:1]),
                         reads=[acc_t, tmp_t], writes=[tmp_t])
                    S.op("act", lambda e: e.activation(out=ssb[:, 1:2], in_=ssb[:, 0:1], func=AF.Sqrt, scale=1.0 / D, bias=EPS),
                         reads=[tmp_t], writes=[tmp_t])
                    S.op("dve", lambda e: e.reciprocal(out=ssb[:, 2:3], in_=ssb[:, 1:2]), reads=[tmp_t], writes=[tmp_t])
                    S.op("dve", lambda e: e.scalar_tensor_tensor(out=yo[:], in0=accm[:], scalar=ssb[:, 2:3], in1=FG[:], op0=ALU.mult, op1=ALU.mult),
                         reads=[acc_t, tmp_t, G_t], writes=[yo_t])
                    r0 = (ti % 32) * 128
                    S.dma(out_d[bb, r0:r0 + 128, :], yo[:], reads=[yo_t])
            S.final_wait()
        print("ops:", S.n_ops, "dmas:", S.dma_n, "sig:", S.cnt)
    return nc


_CACHE = {}


def make_in_maps(inputs, n_cores=8):
    RC, RS, MC, MS = rope_tables()
    cst, _ = const_tables()
    f = lambda a: np.ascontiguousarray(np.asarray(a, dtype=np.float32))
    shared = {k: f(inputs[k]) for k in ("norm1_g", "norm2_g", "ada_w", "ada_b", "w_in", "ret_decay_fwd", "ret_decay_bwd",
                                        "mla_q_norm_g", "mla_w_uq", "mla_kv_norm_g", "mla_w_ukv", "w_branch_ret",
                                        "w_branch_mla", "w_out", "router_w", "router_b", "exp_w_gu", "exp_b_gu",
                                        "exp_w_down", "exp_b_down", "final_norm_g")}
    shared.update(consts=cst, rope_rc=RC, rope_rs=RS, rope_mc=MC, rope_ms=MS)
    x, c, ctx, c_ctx = f(inputs["x"]), f(inputs["c"]), f(inputs["ctx"]), f(inputs["c_ctx"])
    maps = []
    for i in range(n_cores):
        m = dict(shared)
        m["x"] = x[i * NB:(i + 1) * NB]
        m["ctx"] = ctx[i * NB:(i + 1) * NB]
        m["cvec"] = np.ascontiguousarray(np.concatenate([c[i * NB:(i + 1) * NB], c_ctx[None, :]], axis=0))
        maps.append(m)
    return maps


def kernel(**inputs):
    if "nc" not in _CACHE:
        _CACHE["nc"] = build()
    nc = _CACHE["nc"]
    maps = make_in_maps(inputs)
    res = run_bass_kernel_spmd(nc, maps, core_ids=list(range(8)))
    return np.concatenate([r["out"] for r in res.results], axis=0).astype(np.float32)
```

```python
import contextlib
import numpy as np
import concourse.bass as bass
import concourse.mybir as mybir
from concourse.bass_utils import run_bass_kernel_spmd

F32 = mybir.dt.float32
BF16 = mybir.dt.bfloat16
AF = mybir.ActivationFunctionType
ALU = mybir.AluOpType

NB = 2
L = 4096
CT = 256
LT = L + CT
NT = LT // 128
D = 1024
EPS = 1e-6
NS_DMA = 16
ERA = 16000


class Tk:
    __slots__ = ("w", "r", "name")

    def __init__(self, name=""):
        self.w = []
        self.r = []
        self.name = name


class Sched:
    def __init__(self, nc, stack):
        self.nc = nc
        self.stack = stack
        self.engs = {"pe": nc.tensor, "act": nc.scalar, "dve": nc.vector, "pool": nc.gpsimd, "sp": nc.sync}
        self.sems = {}
        self.seq = {e: 0 for e in self.engs}
        self.sig = {e: [] for e in self.engs}
        self.cnt = {e: 0 for e in self.engs}
        self.waited = {e: {} for e in self.engs}
        self.waited_d = {e: {} for e in self.engs}
        self.ring = [stack.enter_context(nc.semaphore(f"dq{i}")) for i in range(NS_DMA)]
        self.dma_n = 0
        self.n_ops = 0

    def _sem(self, eng, era):
        k = (eng, era)
        if k not in self.sems:
            self.sems[k] = self.stack.enter_context(self.nc.semaphore(f"s_{eng}_{era}"))
        return self.sems[k]

    def _wait(self, eng, tk):
        e = self.engs[eng]
        if tk[0] == "d":
            _, ring, val = tk
            if self.waited_d[eng].get(ring, 0) >= val:
                return
            e.wait_ge(self.ring[ring], val)
            self.waited_d[eng][ring] = val
            return
        _, peng, seq = tk
        lst = self.sig[peng]
        lo, hi = 0, len(lst)
        while lo < hi:
            mid = (lo + hi) // 2
            if lst[mid][0] >= seq:
                hi = mid
            else:
                lo = mid + 1
        if lo >= len(lst):
            raise RuntimeError(f"no signalling op after seq {seq} on {peng}")
        count = lst[lo][1]
        if self.waited[eng].get(peng, 0) >= count:
            return
        era, val = (count - 1) // ERA, (count - 1) % ERA + 1
        e.wait_ge(self._sem(peng, era), val)
        self.waited[eng][peng] = count

    def op(self, eng, fn, reads=(), writes=(), signal=True):
        deps = []
        for t in reads:
            deps.extend(t.w)
        for t in writes:
            for tk in t.w:
                if tk[0] == "d" or tk[1] != eng:
                    deps.append(tk)
            for tk in t.r:
                if tk[0] == "d" or tk[1] != eng:
                    deps.append(tk)
        for tk in deps:
            self._wait(eng, tk)
        ins = fn(self.engs[eng])
        self.seq[eng] += 1
        seq = self.seq[eng]
        if signal:
            self.cnt[eng] += 1
            c = self.cnt[eng]
            era, val = (c - 1) // ERA, (c - 1) % ERA + 1
            ins.then_inc(self._sem(eng, era), 1)
            self.sig[eng].append((seq, c))
        tk = ("c", eng, seq)
        for t in reads:
            t.r = [x for x in t.r if not (x[0] == "c" and x[1] == eng)]
            t.r.append(tk)
        for t in writes:
            t.w = [tk]
            t.r = []
        self.n_ops += 1
        return ins

    def dma(self, out, in_, reads=(), writes=()):
        eng = "sp"
        deps = []
        for t in reads:
            deps.extend(t.w)
        for t in writes:
            deps.extend(t.w)
            deps.extend(t.r)
        n = self.dma_n
        ring = n % NS_DMA
        val = 16 * (n // NS_DMA + 1)
        if n >= NS_DMA:
            deps.append(("d", ring, val - 16))
        for tk in deps:
            self._wait(eng, tk)
        self.engs[eng].dma_start(out=out, in_=in_).then_inc(self.ring[ring], 16)
        self.dma_n += 1
        tk = ("d", ring, val)
        for t in reads:
            t.r.append(tk)
        for t in writes:
            t.w = [tk]
            t.r = []
        self.n_ops += 1
        return tk

    def dma_ind(self, fn, reads=(), writes=()):
        eng = "pool"
        deps = []
        for t in reads:
            deps.extend(t.w)
        for t in writes:
            deps.extend(t.w)
            deps.extend(t.r)
        n = self.dma_n
        ring = n % NS_DMA
        val = 16 * (n // NS_DMA + 1)
        if n >= NS_DMA:
            deps.append(("d", ring, val - 16))
        for tk in deps:
            self._wait(eng, tk)
        fn(self.engs[eng]).then_inc(self.ring[ring], 16)
        self.dma_n += 1
        tk = ("d", ring, val)
        for t in reads:
            t.r.append(tk)
        for t in writes:
            t.w = [tk]
            t.r = []
        self.n_ops += 1
        return tk

    def barrier(self):
        tks = []
        for e in self.engs:
            if self.sig[e]:
                tks.append(("c", e, self.sig[e][-1][0]))
        n = self.dma_n
        for k in range(max(0, n - NS_DMA), n):
            tks.append(("d", k % NS_DMA, 16 * (k // NS_DMA + 1)))
        for e in self.engs:
            for tk in tks:
                if tk[0] == "c" and tk[1] == e:
                    continue
                self._wait(e, tk)

    def final_wait(self):
        n = self.dma_n
        for k in range(max(0, n - NS_DMA), n):
            self._wait("sp", ("d", k % NS_DMA, 16 * (k // NS_DMA + 1)))


def rope_tables():
    pos = np.arange(L)
    rows = (pos // 64).astype(np.float32)
    cols = (pos % 64).astype(np.float32)

    def tab(dr):
        half = dr // 2
        hh = half // 2
        freqs = (10000.0 ** (-np.arange(hh, dtype=np.float32) / hh)).astype(np.float32)
        C = np.ones((LT, dr), np.float32)
        S = np.zeros((LT, dr), np.float32)
        for part, p in enumerate((rows, cols)):
            ang = (p[:, None] * freqs[None, :]).astype(np.float32)
            c, s = np.cos(ang).astype(np.float32), np.sin(ang).astype(np.float32)
            o = part * half
            C[CT:, o:o + hh] = c
            C[CT:, o + hh:o + half] = c
            S[CT:, o:o + hh] = -s
            S[CT:, o + hh:o + half] = s
        return C, S

    RC, RS = tab(256)
    MC, MS = tab(64)
    return RC, RS, np.ascontiguousarray(MC.T), np.ascontiguousarray(MS.T)


def const_tables():
    j = np.arange(128, dtype=np.float32)[:, None]
    i = np.arange(128, dtype=np.float32)[None, :]
    c = {}
    c["ident"] = np.eye(128, dtype=np.float32)
    c["ones"] = np.ones((128, 128), np.float32)
    c["d1"] = np.maximum(i - j, 0.0) + 0 * j
    c["mf"] = (i >= j).astype(np.float32)
    c["d2"] = np.maximum(j - i, 0.0)
    c["mb"] = (i < j).astype(np.float32)
    c["ip1"] = (i + 1.0) + 0 * j
    c["rev"] = (128.0 - i) + 0 * j
    col = np.zeros((128, 128), np.float32)
    col[:, 0] = 127.0 - np.arange(128)
    col[:, 1] = np.arange(128)
    col[:, 2] = 128.0
    c["col"] = col
    c["lt"] = (i > j).astype(np.float32)
    c["iota"] = i + 0 * j
    rowb = np.zeros((128, 128), np.float32)
    for kc in range(8):
        rowb[:, kc] = kc * 128 + np.arange(128)
    c["rowb"] = rowb
    names = ["ident", "ones", "d1", "mf", "d2", "mb", "ip1", "rev", "col", "lt", "iota", "rowb"]
    return np.stack([c[n].astype(np.float32) for n in names], axis=1), names


def build(dbg=()):
    nc = bass.Bass("TRN2", target_bir_lowering=False)
    try:
        nc.allow_low_precision("bf16 matmul operands with fp32 accumulation")
    except Exception:
        pass
    try:
        nc.allow_non_contiguous_dma("strided weight/activation tiles")
    except Exception:
        pass

    def din(name, shape, dt=F32):
        return nc.dram_tensor(name, list(shape), dt, kind="ExternalInput").ap()

    def dscr(name, shape, dt):
        kind = "ExternalOutput" if name in dbg else "Internal"
        return nc.dram_tensor(name, list(shape), dt, kind=kind).ap()

    x_d = din("x", [NB, L, D])
    ctx_d = din("ctx", [NB, CT, D])
    cv_d = din("cvec", [3, D])
    n1_d = din("norm1_g", [1, D])
    n2_d = din("norm2_g", [1, D])
    adaw_d = din("ada_w", [1, D, 6 * D])
    adab_d = din("ada_b", [1, 6 * D])
    win_d = din("w_in", [1, D, 8896])
    rdf_d = din("ret_decay_fwd", [1, 4])
    rdb_d = din("ret_decay_bwd", [1, 4])
    qg_d = din("mla_q_norm_g", [1, 384])
    wuq_d = din("mla_w_uq", [1, 384, 1536])
    kvg_d = din("mla_kv_norm_g", [1, 256])
    wukv_d = din("mla_w_ukv", [1, 256, 2048])
    wbr_d = din("w_branch_ret", [1, 2048, D])
    wbm_d = din("w_branch_mla", [1, D, D])
    wo_d = din("w_out", [1, D, D])
    rw_d = din("router_w", [1, D, 32])
    rb_d = din("router_b", [1, 32])
    egu_d = din("exp_w_gu", [1, 32, D, 2 * D])
    ebgu_d = din("exp_b_gu", [1, 32, 2 * D])
    edn_d = din("exp_w_down", [1, 32, D, D])
    ebd_d = din("exp_b_down", [1, 32, D])
    fg_d = din("final_norm_g", [D])
    cst_d = din("consts", [128, 12, 128])
    rc_d = din("rope_rc", [LT, 256])
    rs_d = din("rope_rs", [LT, 256])
    mc_d = din("rope_mc", [64, LT])
    ms_d = din("rope_ms", [64, LT])
    out_d = nc.dram_tensor("out", [NB, L, D], F32, kind="ExternalOutput").ap()

    hT_d = dscr("hT_s", [NT, 128, 8, 128], BF16)
    att_d = dscr("att_s", [8, 128, L], BF16)
    rT_d = dscr("rT_s", [4, 4, 128, L], BF16)
    of_d = dscr("of_s", [32, 128, 512], F32)
    x1_d = dscr("x1_s", [NB * 32, 128, D], F32)
    NROWS = NB * L * 4 + 32 * 512
    NBLK = NROWS // 512
    h2_d = dscr("h2_s", [NB * 32, 128, D], BF16)
    xs_d = dscr("xs_s", [NROWS, D], BF16)
    ys_d = dscr("ys_s", [NROWS, D], F32)
    bias_d = dscr("bias_s", [32, 3072], BF16)
    hT_t = [Tk() for _ in range(NT)]
    att_t = [Tk() for _ in range(8)]
    rT_t = [Tk() for _ in range(8)]
    of_t = [Tk() for _ in range(32)]
    x1_t = [Tk() for _ in range(NB * 32)]
    out_t = Tk()

    with contextlib.ExitStack() as gstack:
        S = Sched(nc, gstack)

        uid = [0]

        def sb(stack, name, shape, dt):
            uid[0] += 1
            return stack.enter_context(nc.sbuf_tensor(f"{name}_u{uid[0]}", list(shape), dt))

        def ps(stack, name, shape, dt=F32):
            uid[0] += 1
            return stack.enter_context(nc.psum_tensor(f"{name}_u{uid[0]}", list(shape), dt))

        cst = sb(gstack, "cst", [128, 12, 128], F32)
        cst_t = Tk()
        S.dma(cst[:], cst_d[:, :, :], writes=[cst_t])
        identf = cst[:, 0, :]
        onesf = cst[:, 1, :]
        identb = sb(gstack, "identb", [128, 128], BF16)
        onesb = sb(gstack, "onesb", [128, 128], BF16)
        cb_t = Tk()
        S.op("dve", lambda e: e.tensor_copy(out=identb[:], in_=identf), reads=[cst_t], writes=[cb_t])
        S.op("dve", lambda e: e.tensor_copy(out=onesb[:], in_=onesf), reads=[cst_t], writes=[cb_t])

        mT = sb(gstack, "mT", [128, 48, 3], F32)
        mT_t = Tk()
        gs1 = sb(gstack, "gs1", [128, 3, 8], F32)
        gs2 = sb(gstack, "gs2", [128, 3, 8], F32)
        gs_t = Tk()
        mix_stack = contextlib.ExitStack()
        G2 = sb(gstack, "G2", [128, NB, D], F32)
        FG = sb(gstack, "FG", [128, D], F32)
        G_t = Tk()
        gq = sb(gstack, "gq", [128, 8], F32)
        gq_t = Tk()
        KD = sb(gstack, "KD", [128, 4, 8], F32)
        G1 = sb(mix_stack, "G1", [128, NB, D], F32)
        MTf = sb(mix_stack, "MTf", [128, 4, 128], F32)
        MTb = sb(mix_stack, "MTb", [128, 4, 128], F32)
        QDf = sb(mix_stack, "QDf", [128, 4, 128], F32)
        QDb = sb(mix_stack, "QDb", [128, 4, 128], F32)
        dec_t = Tk()

        def featmajor_load(stack, pst, pst_t, dst_ap, src_rows_ap, nrows, tmpname):
            tmp = sb(stack, tmpname, [nrows, 128], F32)
            tt = Tk()
            S.dma(tmp[:], src_rows_ap, writes=[tt])
            S.op("pe", lambda e: e.transpose(out=pst[:, 0:nrows], in_=tmp[:], identity=cst[0:nrows, 0, 0:nrows]),
                 reads=[tt, cst_t], writes=[pst_t])
            return tmp

        with contextlib.ExitStack() as st:
            pA = ps(st, "pA0", [128, 512])
            pA_t = Tk()
            pB = ps(st, "pB0", [128, 2, 512])
            pB_t = Tk()
            cv = sb(st, "cv", [3, D], F32)
            cvs = sb(st, "cvs", [3, D], F32)
            cv_t = Tk()
            S.dma(cv[:], cv_d[:, :], writes=[cv_t])
            cvs_t = Tk()
            S.op("act", lambda e: e.activation(out=cvs[:], in_=cv[:], func=AF.Silu), reads=[cv_t], writes=[cvs_t])
            sT = sb(st, "sT", [128, 8, 3], F32)
            sT_t = Tk()
            for kc in range(8):
                S.op("pe", lambda e: e.transpose(out=pA[:, kc * 4:kc * 4 + 3], in_=cvs[:, kc * 128:(kc + 1) * 128],
                                                 identity=cst[0:3, 0, 0:3]), reads=[cvs_t, cst_t], writes=[pA_t])
            for kc in range(8):
                S.op("dve", lambda e: e.tensor_copy(out=sT[:, kc, :], in_=pA[:, kc * 4:kc * 4 + 3]),
                     reads=[pA_t], writes=[sT_t])
            abT = sb(st, "abT", [128, 48], F32)
            g12 = sb(st, "g12", [128, 16], F32)
            ab_t = Tk()
            featmajor_load(st, pA, pA_t, None, adab_d[0, :].rearrange("(r p) -> r p", p=128), 48, "t_ab")
            S.op("dve", lambda e: e.tensor_copy(out=abT[:], in_=pA[:, 0:48]), reads=[pA_t], writes=[ab_t])
            featmajor_load(st, pA, pA_t, None, n1_d[0, :].rearrange("(r p) -> r p", p=128), 8, "t_n1")
            S.op("dve", lambda e: e.tensor_copy(out=g12[:, 0:8], in_=pA[:, 0:8]), reads=[pA_t], writes=[ab_t])
            featmajor_load(st, pA, pA_t, None, n2_d[0, :].rearrange("(r p) -> r p", p=128), 8, "t_n2")
            S.op("dve", lambda e: e.tensor_copy(out=g12[:, 8:16], in_=pA[:, 0:8]), reads=[pA_t], writes=[ab_t])
            featmajor_load(st, pA, pA_t, None, qg_d[0, :].rearrange("(r p) -> r p", p=128), 3, "t_qg")
            S.op("dve", lambda e: e.tensor_copy(out=gq[:, 0:3], in_=pA[:, 0:3]), reads=[pA_t], writes=[gq_t])
            featmajor_load(st, pA, pA_t, None, kvg_d[0, :].rearrange("(r p) -> r p", p=128), 2, "t_kvg")
            S.op("dve", lambda e: e.tensor_copy(out=gq[:, 4:6], in_=pA[:, 0:2]), reads=[pA_t], writes=[gq_t])
            fgT = sb(st, "fgT", [128, 8], F32)
            fg_t = Tk()
            featmajor_load(st, pA, pA_t, None, fg_d.rearrange("(r p) -> r p", p=128), 8, "t_fg")
            S.op("dve", lambda e: e.tensor_copy(out=fgT[:], in_=pA[:, 0:8]), reads=[pA_t], writes=[fg_t])
            awv = adaw_d[0].rearrange("(kc p) n -> p kc n", p=128)
            aw = [sb(st, f"aw{i}", [128, 8, 512], F32) for i in range(2)]
            aw_t = [Tk(), Tk()]
            pm = ps(st, "pm", [128, 48, 4])
            pm_t = Tk()
            for blk in range(12):
                w = aw[blk % 2]
                wt = aw_t[blk % 2]
                S.dma(w[:], awv[:, :, blk * 512:(blk + 1) * 512], writes=[wt])
                for jj in range(4):
                    j = blk * 4 + jj
                    for kc in range(8):
                        S.op("pe", lambda e: e.matmul(out=pm[:, j, 0:3], lhsT=w[:, kc, jj * 128:(jj + 1) * 128],
                                                      rhs=sT[:, kc, :], start=(kc == 0), stop=(kc == 7)),
                             reads=[wt, sT_t], writes=[pm_t], signal=(kc == 7))
            for r in range(3):
                S.op("dve", lambda e: e.tensor_tensor(out=mT[:, :, r], in0=pm[:, :, r], in1=abT[:], op=ALU.add),
                     reads=[pm_t, ab_t], writes=[mT_t])
            for r in range(3):
                S.op("dve", lambda e: e.scalar_tensor_tensor(out=gs1[:, r, :], in0=mT[:, 8:16, r], scalar=1.0,
                                                             in1=g12[:, 0:8], op0=ALU.add, op1=ALU.mult),
                     reads=[mT_t, ab_t], writes=[gs_t])
                S.op("dve", lambda e: e.scalar_tensor_tensor(out=gs2[:, r, :], in0=mT[:, 32:40, r], scalar=1.0,
                                                             in1=g12[:, 8:16], op0=ALU.add, op1=ALU.mult),
                     reads=[mT_t, ab_t], writes=[gs_t])
            dg = sb(st, "dg", [128, 8, 128], F32)
            dg_t = Tk()

            def bcast_tile(dst_ap, vec_fn):
                for c in range(8):
                    S.op("dve", lambda e: e.tensor_scalar(out=dg[:, c, :], in0=identf, scalar1=vec_fn(c), scalar2=None,
                                                          op0=ALU.mult), reads=[cst_t, mT_t, fg_t], writes=[dg_t])
                for c in range(8):
                    S.op("pe", lambda e: e.matmul(out=pB[:, c // 4, (c % 4) * 128:(c % 4 + 1) * 128], lhsT=onesf,
                                                  rhs=dg[:, c, :], start=True, stop=True),
                         reads=[dg_t, cst_t], writes=[pB_t])
                S.op("act", lambda e: e.copy(out=dst_ap, in_=pB[:].rearrange("p a b -> p (a b)")),
                     reads=[pB_t], writes=[G_t])

            for b in range(NB):
                bcast_tile(G1[:, b, :], lambda c: mT[:, 16 + c, b:b + 1])
                bcast_tile(G2[:, b, :], lambda c: mT[:, 40 + c, b:b + 1])
            bcast_tile(FG[:], lambda c: fgT[:, c:c + 1])

            rd = sb(st, "rd", [1, 8], F32)
            rd_t = Tk()
            S.dma(rd[:, 0:4], rdf_d[:, :], writes=[rd_t])
            S.dma(rd[:, 4:8], rdb_d[:, :], writes=[rd_t])
            S.op("pe", lambda e: e.matmul(out=pA[:, 0:8], lhsT=cst[0:1, 1, :], rhs=rd[:], start=True, stop=True),
                 reads=[rd_t, cst_t], writes=[pA_t])
            lg = sb(st, "lg", [128, 8], F32)
            lg_t = Tk()
            S.op("act", lambda e: e.activation(out=lg[:], in_=pA[:, 0:8], func=AF.Exp, scale=-1.0),
                 reads=[pA_t], writes=[lg_t])
            S.op("act", lambda e: e.activation(out=lg[:], in_=lg[:], func=AF.Ln, bias=1.0, scale=1.0),
                 reads=[lg_t], writes=[lg_t])
            S.op("dve", lambda e: e.tensor_scalar(out=lg[:], in0=lg[:], scalar1=-1.0, scalar2=None, op0=ALU.mult),
                 reads=[lg_t], writes=[lg_t])
            tmpd = sb(st, "tmpd", [128, 128], F32)
            tmpd_t = Tk()
            for h in range(4):
                for (dst, dtab, mtab, col) in ((MTf, 2, 3, h), (MTb, 4, 5, 4 + h)):
                    S.op("act", lambda e: e.activation(out=tmpd[:], in_=cst[:, dtab, :], func=AF.Exp,
                                                       scale=lg[:, col:col + 1]),
                         reads=[cst_t, lg_t], writes=[tmpd_t])
                    S.op("dve", lambda e: e.tensor_tensor(out=dst[:, h, :], in0=tmpd[:], in1=cst[:, mtab, :],
                                                          op=ALU.mult), reads=[tmpd_t, cst_t], writes=[dec_t])
                S.op("act", lambda e: e.activation(out=QDf[:, h, :], in_=cst[:, 6, :], func=AF.Exp,
                                                   scale=lg[:, h:h + 1]), reads=[cst_t, lg_t], writes=[dec_t])
                S.op("act", lambda e: e.activation(out=QDb[:, h, :], in_=cst[:, 7, :], func=AF.Exp,
                                                   scale=lg[:, 4 + h:5 + h]), reads=[cst_t, lg_t], writes=[dec_t])
                S.op("act", lambda e: e.activation(out=KD[:, h, 0:1], in_=cst[:, 8, 0:1], func=AF.Exp,
                                                   scale=lg[:, h:h + 1]), reads=[cst_t, lg_t], writes=[dec_t])
                S.op("act", lambda e: e.activation(out=KD[:, h, 1:2], in_=cst[:, 8, 1:2], func=AF.Exp,
                                                   scale=lg[:, 4 + h:5 + h]), reads=[cst_t, lg_t], writes=[dec_t])
                S.op("act", lambda e: e.activation(out=KD[:, h, 2:3], in_=cst[:, 8, 2:3], func=AF.Exp,
                                                   scale=lg[:, h:h + 1]), reads=[cst_t, lg_t], writes=[dec_t])
                S.op("act", lambda e: e.activation(out=KD[:, h, 3:4], in_=cst[:, 8, 2:3], func=AF.Exp,
                                                   scale=lg[:, 4 + h:5 + h]), reads=[cst_t, lg_t], writes=[dec_t])
            S.barrier()

        def load_cast(stack_stage, dst, dst_t, src_ap, shape, stg, stg_t, eng, scale=None, dst_view=None):
            S.dma(stg, src_ap, writes=[stg_t])
            dv = dst if dst_view is None else dst_view
            if scale is None:
                S.op(eng, lambda e: e.tensor_copy(out=dv, in_=stg), reads=[stg_t], writes=[dst_t])
            else:
                S.op(eng, lambda e: e.tensor_scalar(out=dv, in0=stg, scalar1=scale, scalar2=None, op0=ALU.mult),
                     reads=[stg_t], writes=[dst_t])

        def norm_T(stack, pfx, src_tile, src_t, gs_ap, sh_ap, pT, pT_t, hTo, hTo_t, scr, ssb, tmp_t, xn, xn_t,
                   f32_out=None, f32_t=None):
            S.op("pool", lambda e: e.memset(ssb[:, 0:1], 0.0), writes=[tmp_t])
            S.op("act", lambda e: e.activation(out=scr[:], in_=src_tile, func=AF.Square, accum_out=ssb[:, 0:1]),
                 reads=[src_t, tmp_t], writes=[tmp_t])
            S.op("act", lambda e: e.activation(out=ssb[:, 1:2], in_=ssb[:, 0:1], func=AF.Sqrt, scale=1.0 / D, bias=EPS),
                 reads=[tmp_t], writes=[tmp_t])
            S.op("dve", lambda e: e.reciprocal(out=ssb[:, 2:3], in_=ssb[:, 1:2]), reads=[tmp_t], writes=[tmp_t])
            S.op("dve", lambda e: e.tensor_scalar(out=xn[:], in0=src_tile, scalar1=ssb[:, 2:3], scalar2=None,
                                                  op0=ALU.mult), reads=[src_t, tmp_t], writes=[xn_t])
            for c in range(8):
                S.op("pe", lambda e: e.transpose(out=pT[c // 4][:, (c % 4) * 128:(c % 4 + 1) * 128],
                                                 in_=xn[:, c * 128:(c + 1) * 128], identity=identf),
                     reads=[xn_t, cst_t], writes=[pT_t[c // 4]], signal=(c % 4 == 3))
            for c in range(8):
                src = pT[c // 4][:, (c % 4) * 128:(c % 4 + 1) * 128]
                if f32_out is not None:
                    S.op("dve", lambda e: e.tensor_scalar(out=f32_out[:, c, :], in0=src, scalar1=gs_ap[:, c:c + 1],
                                                          scalar2=sh_ap(c), op0=ALU.mult, op1=ALU.add),
                         reads=[pT_t[c // 4], gs_t, mT_t], writes=[f32_t])
                    S.op("pool", lambda e: e.tensor_copy(out=hTo[:, c, :], in_=f32_out[:, c, :]),
                         reads=[f32_t], writes=[hTo_t])
                else:
                    S.op("dve", lambda e: e.tensor_scalar(out=hTo[:, c, :], in0=src, scalar1=gs_ap[:, c:c + 1],
                                                          scalar2=sh_ap(c), op0=ALU.mult, op1=ALU.add),
                         reads=[pT_t[c // 4], gs_t, mT_t], writes=[hTo_t])

        winv = win_d[0].rearrange("(kc p) n -> p kc n", p=128)

        for b in range(NB):
            with contextlib.ExitStack() as st:
                xt = [sb(st, f"s0x{i}", [128, D], F32) for i in range(2)]
                xt_t = [Tk(), Tk()]
                xn = sb(st, "s0xn", [128, D], F32)
                xn_t = Tk()
                scr = sb(st, "s0scr", [128, D], BF16)
                ssb = sb(st, "s0ss", [128, 4], F32)
                tmp_t = Tk()
                hTo = [sb(st, f"s0h{i}", [128, 8, 128], BF16) for i in range(2)]
                hTo_t = [Tk(), Tk()]
                pT = [ps(st, f"s0p{i}", [128, 512]) for i in range(2)]
                pT_t = [Tk(), Tk()]

                def s0_load(t):
                    src = ctx_d[b, t * 128:(t + 1) * 128, :] if t < 2 else x_d[b, (t - 2) * 128:(t - 1) * 128, :]
                    S.dma(xt[t % 2][:], src, writes=[xt_t[t % 2]])

                s0_load(0)
                for t in range(NT):
                    if t + 1 < NT:
                        s0_load(t + 1)
                    r = 2 if t < 2 else b
                    norm_T(st, "s0", xt[t % 2][:], xt_t[t % 2], gs1[:, r, :], lambda c: mT[:, c, r:r + 1],
                           pT, pT_t, hTo[t % 2], hTo_t[t % 2], scr, ssb, tmp_t, xn, xn_t)
                    S.dma(hT_d[t], hTo[t % 2][:], reads=[hTo_t[t % 2]], writes=[hT_t[t]])
                S.barrier()

            with contextlib.ExitStack() as st:
                cqn = sb(st, "cqn", [128, 3, L], BF16)
                cqn_t = Tk()
                ckvn = sb(st, "ckvn", [128, 2, LT], BF16)
                ckvn_t = Tk()
                krT = sb(st, "krT", [64, LT], BF16)
                krT_t = Tk()
                with contextlib.ExitStack() as s1:
                    Wm = sb(s1, "Wm", [128, 8, 768], BF16)
                    Wm_t = Tk()
                    stg = sb(s1, "s1stg", [128, 8, 704], F32)
                    stg_t = Tk()
                    S.dma(stg[:], winv[:, :, 6144:6848], writes=[stg_t])
                    S.op("dve", lambda e: e.tensor_copy(out=Wm[:, :, 0:704], in_=stg[:]), reads=[stg_t], writes=[Wm_t])
                    for (dst, src) in ((704, 656), (720, 640), (736, 688), (752, 672)):
                        S.op("dve", lambda e: e.tensor_copy(out=Wm[:, :, dst:dst + 16], in_=stg[:, :, src:src + 16]),
                             reads=[stg_t], writes=[Wm_t])
                    hb = [sb(s1, f"s1h{i}", [128, 8, 512], BF16) for i in range(2)]
                    hb_t = [Tk(), Tk()]
                    tc_ = [sb(s1, f"s1tc{i}", [64, 2, 512], F32) for i in range(2)]
                    tc_t = [Tk(), Tk()]
                    pq = [ps(s1, f"s1pq{i}", [128, 512]) for i in range(3)]
                    pq_t = [Tk() for _ in range(3)]
                    pk = [ps(s1, f"s1pk{i}", [128, 512]) for i in range(2)]
                    pk_t = [Tk() for _ in range(2)]
                    pr = [ps(s1, f"s1pr{i}", [128, 512]) for i in range(2)]
                    pr_t = [Tk() for _ in range(2)]
                    pss = ps(s1, "s1pss", [128, 512])
                    pss_t = Tk()
                    xf = sb(s1, "s1xf", [128, 3, 512], F32)
                    xf_t = Tk()
                    sq = sb(s1, "s1sq", [128, 3, 512], F32)
                    sq_t = Tk()
                    rstd = sb(s1, "s1rstd", [128, 512], F32)
                    rstd_t = Tk()
                    r1 = sb(s1, "s1r1", [64, 512], F32)
                    r2 = sb(s1, "s1r2", [64, 512], F32)
                    r_t = Tk()

                    def blk_range(j):
                        return (0, 256) if j == 0 else (256 + (j - 1) * 512, 512)

                    def s1_load(j):
                        t0, n = blk_range(j)
                        for i in range(n // 128):
                            S.dma(hb[j % 2][:, :, i * 128:(i + 1) * 128], hT_d[t0 // 128 + i],
                                  reads=[hT_t[t0 // 128 + i]], writes=[hb_t[j % 2]])
                        S.dma(tc_[j % 2][:, 0, 0:n], mc_d[:, t0:t0 + n], writes=[tc_t[j % 2]])
                        S.dma(tc_[j % 2][:, 1, 0:n], ms_d[:, t0:t0 + n], writes=[tc_t[j % 2]])

                    def rms_T(pl, pl_t, nch, rank, gcol, dst, dst_t, dcol0, n):
                        for c in range(nch):
                            S.op("act", lambda e: e.copy(out=xf[:, c, 0:n], in_=pl[c][:, 0:n]),
                                 reads=[pl_t[c]], writes=[xf_t])
                            S.op("act", lambda e: e.activation(out=sq[:, c, 0:n], in_=pl[c][:, 0:n], func=AF.Square),
                                 reads=[pl_t[c]], writes=[sq_t])
                        for c in range(nch):
                            S.op("pe", lambda e: e.matmul(out=pss[:, 0:n], lhsT=onesf, rhs=sq[:, c, 0:n],
                                                          start=(c == 0), stop=(c == nch - 1)),
                                 reads=[sq_t, cst_t], writes=[pss_t], signal=(c == nch - 1))
                        S.op("act", lambda e: e.activation(out=rstd[:, 0:n], in_=pss[:, 0:n], func=AF.Sqrt,
                                                           scale=1.0 / rank, bias=EPS), reads=[pss_t], writes=[rstd_t])
                        S.op("dve", lambda e: e.reciprocal(out=rstd[:, 0:n], in_=rstd[:, 0:n]),
                             reads=[rstd_t], writes=[rstd_t])
                        for c in range(nch):
                            S.op("dve", lambda e: e.scalar_tensor_tensor(
                                out=dst[:, c, dcol0:dcol0 + n], in0=xf[:, c, 0:n], scalar=gq[:, gcol + c:gcol + c + 1],
                                in1=rstd[:, 0:n], op0=ALU.mult, op1=ALU.mult),
                                 reads=[xf_t, rstd_t, gq_t], writes=[dst_t])

                    s1_load(0)
                    for j in range(9):
                        if j + 1 < 9:
                            s1_load(j + 1)
                        t0, n = blk_range(j)
                        h_, h_t = hb[j % 2], hb_t[j % 2]
                        if j > 0:
                            for c in range(3):
                                for kc in range(8):
                                    S.op("pe", lambda e: e.matmul(out=pq[c][:, 0:n], lhsT=Wm[:, kc, c * 128:(c + 1) * 128],
                                                                  rhs=h_[:, kc, 0:n], start=(kc == 0), stop=(kc == 7)),
                                         reads=[Wm_t, h_t], writes=[pq_t[c]], signal=(kc == 7))
                        for c in range(2):
                            for kc in range(8):
                                S.op("pe", lambda e: e.matmul(out=pk[c][:, 0:n], lhsT=Wm[:, kc, 384 + c * 128:384 + (c + 1) * 128],
                                                              rhs=h_[:, kc, 0:n], start=(kc == 0), stop=(kc == 7)),
                                     reads=[Wm_t, h_t], writes=[pk_t[c]], signal=(kc == 7))
                        for c in range(2):
                            for kc in range(8):
                                S.op("pe", lambda e: e.matmul(out=pr[c][0:64, 0:n], lhsT=Wm[:, kc, 640 + c * 64:704 + c * 64],
                                                              rhs=h_[:, kc, 0:n], start=(kc == 0), stop=(kc == 7)),
                                     reads=[Wm_t, h_t], writes=[pr_t[c]], signal=(kc == 7))
                        if j > 0:
                            rms_T(pq, pq_t, 3, 384, 0, cqn, cqn_t, t0 - 256, n)
                        rms_T(pk, pk_t, 2, 256, 4, ckvn, ckvn_t, t0, n)
                        tcc, tcc_t = tc_[j % 2], tc_t[j % 2]
                        S.op("dve", lambda e: e.tensor_tensor(out=r1[:, 0:n], in0=pr[0][0:64, 0:n], in1=tcc[:, 0, 0:n],
                                                              op=ALU.mult), reads=[pr_t[0], tcc_t], writes=[r_t])
                        S.op("dve", lambda e: e.tensor_tensor(out=r2[:, 0:n], in0=pr[1][0:64, 0:n], in1=tcc[:, 1, 0:n],
                                                              op=ALU.mult), reads=[pr_t[1], tcc_t], writes=[r_t])
                        S.op("dve", lambda e: e.tensor_tensor(out=krT[:, t0:t0 + n], in0=r1[:, 0:n], in1=r2[:, 0:n],
                                                              op=ALU.add), reads=[r_t], writes=[krT_t])
                    S.barrier()

                with contextlib.ExitStack() as s2:
                    KT = sb(s2, "KT", [128, LT], BF16)
                    KT_t = Tk()
                    V = sb(s2, "V", [128, NT, 128], BF16)
                    V_t = Tk()
                    QT = sb(s2, "QT", [128, L], BF16)
                    QT_t = Tk()
                    QrT = sb(s2, "QrT", [64, L], BF16)
                    QrT_t = Tk()
                    Wq = sb(s2, "Wq", [128, 3, 256], BF16)
                    Wq_t = Tk()
                    Wkv = sb(s2, "Wkv", [128, 2, 256], BF16)
                    Wkv_t = Tk()
                    sq_ = sb(s2, "s2sq", [128, 3, 192], F32)
                    sq_t = Tk()
                    skv = sb(s2, "s2skv", [128, 2, 256], F32)
                    skv_t = Tk()
                    tq = [sb(s2, f"s2tq{i}", [64, 2, 512], F32) for i in range(2)]
                    tq_t = [Tk(), Tk()]
                    r1 = sb(s2, "s2r1", [64, 512], F32)
                    r2 = sb(s2, "s2r2", [64, 512], F32)
                    r_t = Tk()
                    PT = [sb(s2, f"PT{i}", [128, 512], BF16) for i in range(4)]
                    PT_t = [Tk() for _ in range(4)]
                    accA = [sb(s2, f"s2accA{i}", [128, 512], F32) for i in range(2)]
                    accB = [sb(s2, f"s2accB{i}", [128, 512], F32) for i in range(2)]
                    accA_t = [Tk(), Tk()]
                    accB_t = [Tk(), Tk()]
                    rs = sb(s2, "s2rs", [128, 512], F32)
                    rs_t = Tk()
                    ot = [sb(s2, f"s2ot{i}", [128, 512], BF16) for i in range(2)]
                    ot_t = [Tk(), Tk()]
                    pS = [ps(s2, f"pS{i}", [128, 512]) for i in range(3)]
                    pS_t = [Tk() for _ in range(3)]
                    pO = [ps(s2, f"pO{i}", [128, 512]) for i in range(2)]
                    pO_t = [Tk() for _ in range(2)]
                    pZ = [ps(s2, f"pZ{i}", [128, 512]) for i in range(2)]
                    pZ_t = [Tk() for _ in range(2)]
                    pX = ps(s2, "pX", [128, 512])
                    pX_t = Tk()
                    wuqv = wuq_d[0].rearrange("(kc p) n -> p kc n", p=128)
                    wukvv = wukv_d[0].rearrange("(kc p) n -> p kc n", p=128)
                    sc = 192.0 ** -0.5
                    cnt = 0
                    for h in range(8):
                        S.dma(sq_[:], wuqv[:, :, h * 192:(h + 1) * 192], writes=[sq_t])
                        S.op("pool", lambda e: e.tensor_copy(out=Wq[:, :, 0:192], in_=sq_[:]), reads=[sq_t], writes=[Wq_t])
                        for (dst, src) in ((192, 144), (208, 128), (224, 176), (240, 160)):
                            S.op("pool", lambda e: e.tensor_copy(out=Wq[:, :, dst:dst + 16], in_=sq_[:, :, src:src + 16]),
                                 reads=[sq_t], writes=[Wq_t])
                        S.dma(skv[:], wukvv[:, :, h * 256:(h + 1) * 256], writes=[skv_t])
                        S.op("pool", lambda e: e.tensor_copy(out=Wkv[:], in_=skv[:]), reads=[skv_t], writes=[Wkv_t])
                        for j in range(9):
                            t0, n = (0, 256) if j == 0 else (256 + (j - 1) * 512, 512)
                            for kc in range(2):
                                S.op("pe", lambda e: e.matmul(out=pX[:, 0:n], lhsT=Wkv[:, kc, 0:128], rhs=ckvn[:, kc, t0:t0 + n],
                                                              start=(kc == 0), stop=(kc == 1)),
                                     reads=[Wkv_t, ckvn_t], writes=[pX_t], signal=(kc == 1))
                            S.op("act" if j % 2 else "dve", (lambda e: e.copy(out=KT[:, t0:t0 + n], in_=pX[:, 0:n])) if j % 2
                                 else (lambda e: e.tensor_copy(out=KT[:, t0:t0 + n], in_=pX[:, 0:n])),
                                 reads=[pX_t], writes=[KT_t])
                        for g in range(9):
                            tiles = list(range(g * 4, min(g * 4 + 4, NT)))
                            for i, t in enumerate(tiles):
                                for kc in range(2):
                                    S.op("pe", lambda e: e.matmul(out=pX[:, i * 128:(i + 1) * 128],
                                                                  lhsT=ckvn[:, kc, t * 128:(t + 1) * 128], rhs=Wkv[:, kc, 128:256],
                                                                  start=(kc == 0), stop=(kc == 1)),
                                         reads=[Wkv_t, ckvn_t], writes=[pX_t], signal=(kc == 1 and i == len(tiles) - 1))
                            nn = len(tiles)
                            S.op("act" if g % 2 else "dve",
                                 (lambda e: e.copy(out=V[:, tiles[0]:tiles[0] + nn, :].rearrange("p a b -> p (a b)"),
                                                   in_=pX[:, 0:nn * 128])) if g % 2 else
                                 (lambda e: e.tensor_copy(out=V[:, tiles[0]:tiles[0] + nn, :].rearrange("p a b -> p (a b)"),
                                                          in_=pX[:, 0:nn * 128])),
                                 reads=[pX_t], writes=[V_t])
                        for j in range(8):
                            q0 = j * 512
                            S.dma(tq[j % 2][:, 0, :], mc_d[:, 256 + q0:256 + q0 + 512], writes=[tq_t[j % 2]])
                            S.dma(tq[j % 2][:, 1, :], ms_d[:, 256 + q0:256 + q0 + 512], writes=[tq_t[j % 2]])
                            for kc in range(3):
                                S.op("pe", lambda e: e.matmul(out=pX[:], lhsT=Wq[:, kc, 0:128], rhs=cqn[:, kc, q0:q0 + 512],
                                                              start=(kc == 0), stop=(kc == 2)),
                                     reads=[Wq_t, cqn_t], writes=[pX_t], signal=(kc == 2))
                            S.op("act", lambda e: e.copy(out=QT[:, q0:q0 + 512], in_=pX[:]), reads=[pX_t], writes=[QT_t])
                            for c in range(2):
                                for kc in range(3):
                                    S.op("pe", lambda e: e.matmul(out=pZ[c][0:64, :], lhsT=Wq[:, kc, 128 + c * 64:192 + c * 64],
                                                                  rhs=cqn[:, kc, q0:q0 + 512], start=(kc == 0), stop=(kc == 2)),
                                         reads=[Wq_t, cqn_t], writes=[pZ_t[c]], signal=(kc == 2))
                            tt_, tt_t = tq[j % 2], tq_t[j % 2]
                            S.op("dve", lambda e: e.tensor_tensor(out=r1[:], in0=pZ[0][0:64, :], in1=tt_[:, 0, :], op=ALU.mult),
                                 reads=[pZ_t[0], tt_t], writes=[r_t])
                            S.op("dve", lambda e: e.tensor_tensor(out=r2[:], in0=pZ[1][0:64, :], in1=tt_[:, 1, :], op=ALU.mult),
                                 reads=[pZ_t[1], tt_t], writes=[r_t])
                            S.op("dve", lambda e: e.tensor_tensor(out=QrT[:, q0:q0 + 512], in0=r1[:], in1=r2[:], op=ALU.add),
                                 reads=[r_t], writes=[QrT_t])
                        for qb in range(8):
                            q0 = qb * 512
                            po, po_t = pO[qb % 2], pO_t[qb % 2]
                            pz, pz_t = pZ[qb % 2], pZ_t[qb % 2]
                            def emit_st(kt, cn):
                                s_, s_t = pS[cn % 3], pS_t[cn % 3]
                                S.op("pe", lambda e: e.matmul(out=s_[:], lhsT=KT[:, kt * 128:(kt + 1) * 128], rhs=QT[:, q0:q0 + 512],
                                                              start=True, stop=False), reads=[KT_t, QT_t], writes=[s_t], signal=False)
                                S.op("pe", lambda e: e.matmul(out=s_[:], lhsT=krT[:, kt * 128:(kt + 1) * 128], rhs=QrT[:, q0:q0 + 512],
                                                              start=False, stop=True), reads=[krT_t, QrT_t], writes=[s_t])

                            emit_st(0, cnt)
                            for kt in range(NT):
                                s_, s_t = pS[cnt % 3], pS_t[cnt % 3]
                                p_, p_t = PT[cnt % 4], PT_t[cnt % 4]
                                if kt + 1 < NT:
                                    emit_st(kt + 1, cnt + 1)
                                cnt += 1
                                S.op("act", lambda e: e.activation(out=p_[:], in_=s_[:], func=AF.Exp, scale=sc),
                                     reads=[s_t], writes=[p_t])
                                S.op("pe", lambda e: e.matmul(out=po[:], lhsT=V[:, kt, :], rhs=p_[:], start=(kt == 0),
                                                              stop=(kt == NT - 1)), reads=[V_t, p_t], writes=[po_t])
                                aa, aa_t = accA[qb % 2], accA_t[qb % 2]
                                if kt == 0:
                                    S.op("dve", lambda e: e.tensor_copy(out=aa[:], in_=p_[:]), reads=[p_t], writes=[aa_t])
                                else:
                                    S.op("dve", lambda e: e.tensor_tensor(out=aa[:], in0=aa[:], in1=p_[:], op=ALU.add),
                                         reads=[p_t, aa_t], writes=[aa_t])
                            S.op("pe", lambda e: e.matmul(out=pz[:], lhsT=onesf, rhs=accA[qb % 2][:], start=True, stop=True),
                                 reads=[cst_t, accA_t[qb % 2]], writes=[pz_t])
                            S.op("dve", lambda e: e.reciprocal(out=rs[:], in_=pz[:]), reads=[pz_t], writes=[rs_t])
                            o_, o_t = ot[qb % 2], ot_t[qb % 2]
                            S.op("dve", lambda e: e.tensor_tensor(out=o_[:], in0=po[:], in1=rs[:], op=ALU.mult),
                                 reads=[po_t, rs_t], writes=[o_t])
                            S.dma(att_d[h, :, q0:q0 + 512], o_[:], reads=[o_t], writes=[att_t[qb]])
                    S.barrier()

            with contextlib.ExitStack() as st:
                Wqk = sb(st, "Wqk", [128, 8, 512], BF16)
                Wv = sb(st, "Wv", [128, 8, 512], BF16)
                Wg = sb(st, "Wg", [128, 8, 512], BF16)
                Wqk_t, Wv_t, Wg_t = Tk(), Tk(), Tk()
                stg = sb(st, "s3stg", [128, 8, 512], F32)
                stg_t = Tk()
                hb = [sb(st, f"s3h{i}", [128, 8, 128], BF16) for i in range(3)]
                hb_t = [Tk() for _ in range(3)]
                tb = [sb(st, f"s3tb{i}", [128, 2, 256], F32) for i in range(3)]
                tb_t = [Tk() for _ in range(3)]
                ofb = [sb(st, f"s3of{i}", [128, 512], F32) for i in range(3)]
                ofb_t = [Tk() for _ in range(3)]
                Sf = sb(st, "Sf", [128, 2, 512], F32)
                Sb_ = sb(st, "Sb", [128, 2, 512], BF16)
                Sf_t = Tk()
                Sb_t = Tk()
                t1 = sb(st, "s3t1", [128, 512], F32)
                t2 = sb(st, "s3t2", [128, 512], F32)
                t12_t = Tk()
                qk_all = sb(st, "s3qkall", [128, NT, 512], BF16)
                qka_t = [Tk() for _ in range(NT)]
                V_all = sb(st, "s3Vall", [128, NT, 512], BF16)
                Va_t = [Tk() for _ in range(NT)]
                Kdb = [sb(st, f"s3Kd{i}", [128, 256], BF16) for i in range(2)]
                Kdb_t = [Tk(), Tk()]
                KTt = sb(st, "s3KT", [128, 2, 128], BF16)
                QTt = sb(st, "s3QT", [128, 2, 128], BF16)
                QdT = sb(st, "s3QdT", [128, 2, 128], BF16)
                tr_t = Tk()
                STm = sb(st, "s3STm", [128, 128], BF16)
                STm_t = Tk()
                osb = sb(st, "s3o", [128, 512], F32)
                osb_t = Tk()
                sg = sb(st, "s3sg", [128, 512], F32)
                sg_t = Tk()
                scr = sb(st, "s3scr", [128, 512], BF16)
                ssb = sb(st, "s3ss", [128, 4], F32)
                ss_t = Tk()
                rr = sb(st, "s3r", [128, 512], BF16)
                rr_t = Tk()
                rTs = [sb(st, f"s3rT{i}", [128, 4, 128], BF16) for i in range(2)]
                rTs_t = [Tk(), Tk()]
                pAl = [ps(st, f"s3pA{i}", [128, 512]) for i in range(2)]
                pAl_t = [Tk(), Tk()]
                pBl = [ps(st, f"s3pB{i}", [128, 512]) for i in range(2)]
                pBl_t = [Tk(), Tk()]
                pC = ps(st, "s3pC", [128, 512])
                pD = ps(st, "s3pD", [128, 2, 512], BF16)
                pE = ps(st, "s3pE", [128, 512])
                pF = ps(st, "s3pF", [128, 512])
                pC_t, pD_t, pD2_t, pE_t, pF_t = (Tk() for _ in range(5))
                pH = [pC, pE]
                pH_t = [pC_t, pE_t]
                for h in range(4):
                    for (dst, c0, scl) in ((Wqk[:, :, 0:256], h * 256, None), (Wqk[:, :, 256:512], 1024 + h * 256, 0.0625)):
                        S.dma(stg[:, :, 0:256], winv[:, :, c0:c0 + 256], writes=[stg_t])
                        if scl is None:
                            S.op("act", lambda e: e.copy(out=dst, in_=stg[:, :, 0:256]), reads=[stg_t], writes=[Wqk_t])
                        else:
                            S.op("act", lambda e: e.mul(out=dst, in_=stg[:, :, 0:256], mul=scl), reads=[stg_t], writes=[Wqk_t])
                    for (dst, c0, wt_) in ((Wv, 2048 + h * 512, Wv_t), (Wg, 4096 + h * 512, Wg_t)):
                        S.dma(stg[:], winv[:, :, c0:c0 + 512], writes=[stg_t])
                        S.op("act", lambda e: e.copy(out=dst[:], in_=stg[:]), reads=[stg_t], writes=[wt_])
                    for di, dirn in enumerate(("f", "b")):
                        order = list(range(NT)) if dirn == "f" else [1, 0] + list(range(NT - 1, 1, -1))
                        MT = MTf if dirn == "f" else MTb
                        QD = QDf if dirn == "f" else QDb
                        kdc = KD[:, h, di:di + 1]
                        cdc = KD[:, h, 2 + di:3 + di]
                        S.op("pool", lambda e: e.memset(Sf[:], 0.0), writes=[Sf_t])
                        S.op("pool", lambda e: e.memset(Sb_[:], 0.0), writes=[Sb_t])

                        def s3_load(idx):
                            t = order[idx]
                            S.dma(hb[idx % 3][:], hT_d[t], reads=[hT_t[t]], writes=[hb_t[idx % 3]])
                            if dirn == "f":
                                S.dma(tb[idx % 3][:, 0, :], rc_d[t * 128:(t + 1) * 128, :], writes=[tb_t[idx % 3]])
                                S.dma(tb[idx % 3][:, 1, :], rs_d[t * 128:(t + 1) * 128, :], writes=[tb_t[idx % 3]])
                            if dirn == "b" and t >= 2:
                                S.dma(ofb[idx % 3][:], of_d[t - 2], reads=[of_t[t - 2]], writes=[ofb_t[idx % 3]])

                        def s3_proj(idx):
                            if dirn == "b":
                                return
                            h_, h_t = hb[idx % 3], hb_t[idx % 3]
                            pA, pA_t = pAl[idx % 2], pAl_t[idx % 2]
                            pB, pB_t = pBl[idx % 2], pBl_t[idx % 2]
                            for kc in range(8):
                                S.op("pe", lambda e: e.matmul(out=pA[:], lhsT=h_[:, kc, :], rhs=Wqk[:, kc, :], start=(kc == 0),
                                                              stop=(kc == 7)), reads=[h_t, Wqk_t], writes=[pA_t], signal=(kc == 7))
                            for kc in range(8):
                                S.op("pe", lambda e: e.matmul(out=pB[:], lhsT=h_[:, kc, :], rhs=Wv[:, kc, :], start=(kc == 0),
                                                              stop=(kc == 7)), reads=[h_t, Wv_t], writes=[pB_t], signal=(kc == 7))

                        def s3_A(idx):
                            t = order[idx]
                            tb_, tbt = tb[idx % 3], tb_t[idx % 3]
                            pA, pA_t = pAl[idx % 2], pAl_t[idx % 2]
                            pB, pB_t = pBl[idx % 2], pBl_t[idx % 2]
                            qk, qk_t = qk_all[:, t, :], qka_t[t]
                            Vb, Vb_t = V_all[:, t, :], Va_t[t]
                            Kd, Kd_t = Kdb[idx % 2], Kdb_t[idx % 2]
                            for half in (range(2) if dirn == "f" else ()):
                                o = half * 256
                                S.op("dve", lambda e: e.tensor_tensor(out=t1[:, o:o + 256], in0=pA[:, o:o + 256], in1=tb_[:, 0, :],
                                                                      op=ALU.mult), reads=[pA_t, tbt], writes=[t12_t])
                                for part in range(2):
                                    a = o + part * 128
                                    S.op("dve", lambda e: e.tensor_tensor(out=t2[:, a:a + 64], in0=pA[:, a + 64:a + 128],
                                                                          in1=tb_[:, 1, part * 128:part * 128 + 64], op=ALU.mult),
                                         reads=[pA_t, tbt], writes=[t12_t])
                                    S.op("dve", lambda e: e.tensor_tensor(out=t2[:, a + 64:a + 128], in0=pA[:, a:a + 64],
                                                                          in1=tb_[:, 1, part * 128 + 64:part * 128 + 128], op=ALU.mult),
                                         reads=[pA_t, tbt], writes=[t12_t])
                            if dirn == "f":
                                S.op("pool", lambda e: e.tensor_tensor(out=qk[:], in0=t1[:], in1=t2[:], op=ALU.add),
                                     reads=[t12_t], writes=[qk_t])
                                S.op("act", lambda e: e.copy(out=Vb[:], in_=pB[:]), reads=[pB_t], writes=[Vb_t])
                            S.op("pool", lambda e: e.tensor_scalar(out=Kd[:], in0=qk[:, 256:512], scalar1=kdc, scalar2=None, op0=ALU.mult),
                                 reads=[qk_t, dec_t], writes=[Kd_t])

                        s3_load(0)
                        s3_load(1)
                        s3_proj(0)
                        s3_A(0)
                        for idx in range(NT):
                            if idx + 2 < NT:
                                s3_load(idx + 2)
                            if idx + 1 < NT:
                                s3_proj(idx + 1)
                                s3_A(idx + 1)
                            t = order[idx]
                            lat = t >= 2
                            h_, h_t = hb[idx % 3], hb_t[idx % 3]
                            tb_, tbt = tb[idx % 3], tb_t[idx % 3]
                            pA, pA_t = pAl[idx % 2], pAl_t[idx % 2]
                            pB, pB_t = pBl[idx % 2], pBl_t[idx % 2]
                            qk, qk_t = qk_all[:, t, :], qka_t[t]
                            Vb, Vb_t = V_all[:, t, :], Va_t[t]
                            Kd, Kd_t = Kdb[idx % 2], Kdb_t[idx % 2]
                            if lat and dirn == "b":
                                for kc in range(8):
                                    S.op("pe", lambda e: e.matmul(out=pC[:], lhsT=h_[:, kc, :], rhs=Wg[:, kc, :], start=(kc == 0),
                                                                  stop=(kc == 7)), reads=[h_t, Wg_t], writes=[pC_t], signal=(kc == 7))
                                S.op("act", lambda e: e.activation(out=sg[:], in_=pC[:], func=AF.Silu), reads=[pC_t], writes=[sg_t])
                            if lat:
                                for c in range(4):
                                    S.op("pe", lambda e: e.transpose(out=pD[:, 0, c * 128:(c + 1) * 128], in_=qk[:, c * 128:(c + 1) * 128],
                                                                     identity=identb[:]), reads=[qk_t, cb_t], writes=[pD_t], signal=(c == 3))
                                S.op("act", lambda e: e.copy(out=KTt[:].rearrange("p a b -> p (a b)"), in_=pD[:, 0, 256:512]),
                                     reads=[pD_t], writes=[tr_t])
                                S.op("act", lambda e: e.copy(out=QTt[:].rearrange("p a b -> p (a b)"), in_=pD[:, 0, 0:256]),
                                     reads=[pD_t], writes=[tr_t])
                                for dc in range(2):
                                    S.op("dve", lambda e: e.tensor_tensor(out=QdT[:, dc, :], in0=pD[:, 0, dc * 128:(dc + 1) * 128],
                                                                          in1=QD[:, h, :], op=ALU.mult), reads=[pD_t, dec_t], writes=[tr_t])
                                for dc in range(2):
                                    S.op("pe", lambda e: e.matmul(out=pE[:, 0:128], lhsT=KTt[:, dc, :], rhs=QTt[:, dc, :], start=(dc == 0),
                                                                  stop=(dc == 1)), reads=[tr_t], writes=[pE_t], signal=(dc == 1))
                                S.op("dve", lambda e: e.tensor_tensor(out=STm[:], in0=pE[:, 0:128], in1=MT[:, h, :], op=ALU.mult),
                                     reads=[pE_t, dec_t], writes=[STm_t])
                                S.op("pe", lambda e: e.matmul(out=pF[:], lhsT=STm[:], rhs=Vb[:], start=True, stop=False),
                                     reads=[STm_t, Vb_t], writes=[pF_t], signal=False)
                                for dc in range(2):
                                    S.op("pe", lambda e: e.matmul(out=pF[:], lhsT=QdT[:, dc, :], rhs=Sb_[:, dc, :], start=False,
                                                                  stop=(dc == 1)), reads=[tr_t, Sb_t], writes=[pF_t], signal=(dc == 1))
                            for dc in range(2):
                                S.op("pe", lambda e: e.matmul(out=pH[dc][:], lhsT=Kd[:, dc * 128:(dc + 1) * 128], rhs=Vb[:], start=True,
                                                              stop=True), reads=[Kd_t, Vb_t], writes=[pH_t[dc]])
                            if lat:
                                if dirn == "f":
                                    S.op("act", lambda e: e.copy(out=osb[:], in_=pF[:]), reads=[pF_t], writes=[osb_t])
                                    S.dma(of_d[t - 2], osb[:], reads=[osb_t], writes=[of_t[t - 2]])
                                else:
                                    of_, of_tt = ofb[idx % 3], ofb_t[idx % 3]
                                    S.op("dve", lambda e: e.tensor_tensor(out=osb[:], in0=pF[:], in1=of_[:], op=ALU.add),
                                         reads=[pF_t, of_tt], writes=[osb_t])
                                    S.op("pool", lambda e: e.memset(ssb[:, 0:1], 0.0), writes=[ss_t])
                                    S.op("act", lambda e: e.activation(out=scr[:], in_=osb[:], func=AF.Square, accum_out=ssb[:, 0:1]),
                                         reads=[osb_t, ss_t], writes=[ss_t])
                                    S.op("act", lambda e: e.activation(out=ssb[:, 1:2], in_=ssb[:, 0:1], func=AF.Sqrt, scale=1.0 / 512,
                                                                       bias=EPS), reads=[ss_t], writes=[ss_t])
                                    S.op("dve", lambda e: e.reciprocal(out=ssb[:, 2:3], in_=ssb[:, 1:2]), reads=[ss_t], writes=[ss_t])
                                    S.op("dve", lambda e: e.scalar_tensor_tensor(out=rr[:], in0=osb[:], scalar=ssb[:, 2:3], in1=sg[:],
                                                                                 op0=ALU.mult, op1=ALU.mult),
                                         reads=[osb_t, ss_t, sg_t], writes=[rr_t])
                                    for c in range(4):
                                        S.op("pe", lambda e: e.transpose(out=pD[:, 1, c * 128:(c + 1) * 128], in_=rr[:, c * 128:(c + 1) * 128],
                                                                         identity=identb[:]), reads=[rr_t, cb_t], writes=[pD2_t], signal=(c == 3))
                                    rT_, rT_tt = rTs[idx % 2], rTs_t[idx % 2]
                                    S.op("act", lambda e: e.copy(out=rT_[:].rearrange("p a b -> p (a b)"), in_=pD[:, 1, :]),
                                         reads=[pD2_t], writes=[rT_tt])
                                    tk0 = (t - 2) * 128
                                    S.dma(rT_d[h, :, :, tk0:tk0 + 128].rearrange("c p t -> p c t"), rT_[:], reads=[rT_tt],
                                          writes=[rT_t[(t - 2) // 4]])
                            for dc in range(2):
                                S.op("dve", lambda e: e.scalar_tensor_tensor(out=Sb_[:, dc, :], in0=Sf[:, dc, :], scalar=cdc, in1=pH[dc][:],
                                                                             op0=ALU.mult, op1=ALU.add),
                                     reads=[Sf_t, pH_t[dc], dec_t], writes=[Sb_t])
                                S.op("dve", lambda e: e.scalar_tensor_tensor(out=Sf[:, dc, :], in0=Sf[:, dc, :], scalar=cdc, in1=pH[dc][:],
                                                                             op0=ALU.mult, op1=ALU.add),
                                     reads=[Sf_t, pH_t[dc], dec_t], writes=[Sf_t])
                S.barrier()

            with contextlib.ExitStack() as st:
                Wgr = sb(st, "Wgr", [128, 8, 1024], BF16)
                Wgm = sb(st, "Wgm", [128, 8, 1024], BF16)
                Wbr = sb(st, "Wbr", [128, 16, 1024], BF16)
                Wbm = sb(st, "Wbm", [128, 8, 1024], BF16)
                Wo = sb(st, "Wo", [128, 8, 1024], BF16)
                stg = [sb(st, f"s4stg{i}", [128, 8, 256], F32) for i in range(2)]
                stg_t = [Tk(), Tk()]
                wbrv = wbr_d[0].rearrange("(kc p) n -> p kc n", p=128)
                wbmv = wbm_d[0].rearrange("(kc p) n -> p kc n", p=128)
                wov = wo_d[0].rearrange("(kc p) n -> p kc n", p=128)
                k = 0
                jobs = []
                W4_t = [Tk() for _ in range(4)]
                Wo_t = Tk()
                for cb in range(4):
                    cs = slice(cb * 256, (cb + 1) * 256)
                    jobs.append((Wbr[:, 0:8, cs], wbrv[:, 0:8, cs], W4_t[cb]))
                    jobs.append((Wbr[:, 8:16, cs], wbrv[:, 8:16, cs], W4_t[cb]))
                    jobs.append((Wbm[:, :, cs], wbmv[:, :, cs], W4_t[cb]))
                    jobs.append((Wgr[:, :, cs], winv[:, :, 6848 + cb * 256:6848 + (cb + 1) * 256], W4_t[cb]))
                    jobs.append((Wgm[:, :, cs], winv[:, :, 7872 + cb * 256:7872 + (cb + 1) * 256], W4_t[cb]))
                for cb in range(4):
                    cs = slice(cb * 256, (cb + 1) * 256)
                    jobs.append((Wo[:, :, cs], wov[:, :, cs], Wo_t))
                for (dst, src, wt_) in jobs:
                    load_cast(st, dst, wt_, src, None, stg[k % 2][:], stg_t[k % 2], "pool" if k % 2 else "dve")
                    k += 1
                TK4 = 256
                hb = sb(st, "s4h", [128, 8, TK4], BF16)
                hb_t = Tk()
                rb = sb(st, "s4r", [128, 16, TK4], BF16)
                rb_t = Tk()
                ab = sb(st, "s4a", [128, 8, TK4], BF16)
                ab_t = Tk()
                mTb = sb(st, "s4m", [128, 8, TK4], BF16)
                mTb_t = Tk()
                s3_ = sb(st, "s4s3", [128, TK4], F32)
                s4_ = sb(st, "s4s4", [128, TK4], F32)
                sg_t = Tk()
                m1 = sb(st, "s4m1", [128, TK4], F32)
                m2 = sb(st, "s4m2", [128, TK4], F32)
                m_t = Tk()
                x_ = sb(st, "s4x", [128, D], F32)
                x_t = Tk()
                yt = sb(st, "s4y", [128, D], F32)
                yt_t = Tk()
                o_ = sb(st, "s4xo", [128, D], F32)
                o_t = Tk()
                P = [ps(st, f"s4p{i}", [128, 512]) for i in range(4)]
                P_t = [Tk() for _ in range(4)]
                PY = [ps(st, f"s4py{i}", [128, 512]) for i in range(2)]
                PY_t = [Tk() for _ in range(2)]
                for tbk in range(L // TK4):
                    q0 = tbk * TK4
                    for i in range(TK4 // 128):
                        tl = 2 + tbk * (TK4 // 128) + i
                        S.dma(hb[:, :, i * 128:(i + 1) * 128], hT_d[tl], reads=[hT_t[tl]], writes=[hb_t])
                    for hh in range(4):
                        S.dma(rb[:, hh * 4:(hh + 1) * 4, :], rT_d[hh, :, :, q0:q0 + TK4].rearrange("c p t -> p c t"),
                              reads=[rT_t[q0 // 512]], writes=[rb_t])
                    S.dma(ab[:], att_d[:, :, q0:q0 + TK4].rearrange("h p t -> p h t"), reads=[att_t[q0 // 512]], writes=[ab_t])
                    for fc in range(8):
                        fs = slice(fc * 128, (fc + 1) * 128)
                        for kc in range(16):
                            S.op("pe", lambda e: e.matmul(out=P[0][:, 0:TK4], lhsT=Wbr[:, kc, fs], rhs=rb[:, kc, :], start=(kc == 0), stop=(kc == 15)),
                                 reads=[W4_t[fc // 2], rb_t], writes=[P_t[0]], signal=(kc == 15))
                        for (pi, Wx, src, src_t) in ((1, Wbm, ab, ab_t), (2, Wgr, hb, hb_t), (3, Wgm, hb, hb_t)):
                            for kc in range(8):
                                S.op("pe", lambda e: e.matmul(out=P[pi][:, 0:TK4], lhsT=Wx[:, kc, fs], rhs=src[:, kc, :], start=(kc == 0),
                                                              stop=(kc == 7)), reads=[W4_t[fc // 2], src_t], writes=[P_t[pi]], signal=(kc == 7))
                        S.op("act", lambda e: e.activation(out=s3_[:], in_=P[2][:, 0:TK4], func=AF.Sigmoid), reads=[P_t[2]], writes=[sg_t])
                        S.op("act", lambda e: e.activation(out=s4_[:], in_=P[3][:, 0:TK4], func=AF.Sigmoid), reads=[P_t[3]], writes=[sg_t])
                        S.op("dve", lambda e: e.tensor_tensor(out=m1[:], in0=P[0][:, 0:TK4], in1=s3_[:], op=ALU.mult),
                             reads=[P_t[0], sg_t], writes=[m_t])
                        S.op("dve", lambda e: e.tensor_tensor(out=m2[:], in0=P[1][:, 0:TK4], in1=s4_[:], op=ALU.mult),
                             reads=[P_t[1], sg_t], writes=[m_t])
                        S.op("pool", lambda e: e.tensor_tensor(out=mTb[:, fc, :], in0=m1[:], in1=m2[:], op=ALU.add),
                             reads=[m_t], writes=[mTb_t])
                    for tt in range(TK4 // 128):
                        gt = tbk * (TK4 // 128) + tt
                        S.dma(x_[:], x_d[b, gt * 128:(gt + 1) * 128, :], writes=[x_t])
                        for cb in range(2):
                            for kc in range(8):
                                S.op("pe", lambda e: e.matmul(out=PY[cb][:], lhsT=mTb[:, kc, tt * 128:(tt + 1) * 128],
                                                              rhs=Wo[:, kc, cb * 512:(cb + 1) * 512], start=(kc == 0), stop=(kc == 7)),
                                     reads=[mTb_t, Wo_t], writes=[PY_t[cb]], signal=(kc == 7))
                            S.op("dve", lambda e: e.tensor_tensor(out=yt[:, cb * 512:(cb + 1) * 512], in0=PY[cb][:],
                                                                  in1=G1[:, b, cb * 512:(cb + 1) * 512], op=ALU.mult),
                                 reads=[PY_t[cb], G_t], writes=[yt_t])
                        S.op("pool", lambda e: e.tensor_tensor(out=o_[:], in0=yt[:], in1=x_[:], op=ALU.add),
                             reads=[yt_t, x_t], writes=[o_t])
                        S.dma(x1_d[b * 32 + gt], o_[:], reads=[o_t], writes=[x1_t[b * 32 + gt]])
                S.barrier()

        mix_stack.close()
        I32 = mybir.dt.int32
        NTI = NB * 32
        with contextlib.ExitStack() as st:
            posk = sb(st, "posk", [128, NTI * 4], F32)
            ekk = sb(st, "ekk", [128, NTI * 4], F32)
            g4 = sb(st, "g4", [128, NTI * 4], F32)
            pk_t = Tk()
            base = sb(st, "base", [128, 32], F32)
            base_t = Tk()
            desti = sb(st, "desti", [128, NTI * 4], I32)
            desti_t = Tk()
            widx = sb(st, "widx", [128, 8, NBLK], I32)
            bidx = sb(st, "bidx", [2, NBLK], I32)
            widx_t = Tk()
            iota32 = cst[:, 10, 0:32]
            h2d_t = [Tk() for _ in range(NTI)]
            with contextlib.ExitStack() as p1:
                GS2 = sb(p1, "GS2", [128, NB, D], F32)
                SH2 = sb(p1, "SH2", [128, NB, D], F32)
                GS_t = Tk()
                pB2 = [ps(p1, f"p1B{i}", [128, 512]) for i in range(2)]
                pB2_t = [Tk(), Tk()]
                pR = ps(p1, "p1R", [128, 512])
                pR_t = Tk()
                dg = sb(p1, "p1dg", [128, 8, 128], F32)
                dg_t = Tk()
                for bb in range(NB):
                    for (dst, vfn) in ((GS2, lambda c: gs2[:, bb, c:c + 1]), (SH2, lambda c: mT[:, 24 + c, bb:bb + 1])):
                        for c in range(8):
                            S.op("dve", lambda e: e.tensor_scalar(out=dg[:, c, :], in0=identf, scalar1=vfn(c), scalar2=None,
                                                                  op0=ALU.mult), reads=[cst_t, mT_t, gs_t], writes=[dg_t])
                        for c in range(8):
                            S.op("pe", lambda e: e.matmul(out=pB2[c // 4][:, (c % 4) * 128:(c % 4 + 1) * 128], lhsT=onesf,
                                                          rhs=dg[:, c, :], start=True, stop=True),
                                 reads=[dg_t, cst_t], writes=[pB2_t[c // 4]])
                        for hh in range(2):
                            S.op("act", lambda e: e.copy(out=dst[:, bb, hh * 512:(hh + 1) * 512], in_=pB2[hh][:]),
                                 reads=[pB2_t[hh]], writes=[GS_t])
                bfull = sb(p1, "p1bfull", [32, 3072], F32)
                bfb = sb(p1, "p1bfb", [32, 3072], BF16)
                bf_t = Tk()
                S.dma(bfull[:, 0:2048], ebgu_d[0], writes=[bf_t])
                S.dma(bfull[:, 2048:3072], ebd_d[0], writes=[bf_t])
                bfb_t = Tk()
                S.op("dve", lambda e: e.tensor_copy(out=bfb[:], in_=bfull[:]), reads=[bf_t], writes=[bfb_t])
                S.dma(bias_d[:, :], bfb[:], reads=[bfb_t])
                Wr = sb(p1, "Wr", [128, 8, 32], F32)
                Wr_t = Tk()
                S.dma(Wr[:], rw_d[0].rearrange("(kc p) n -> p kc n", p=128), writes=[Wr_t])
                rbs = sb(p1, "rbs", [1, 32], F32)
                rbs_t = Tk()
                S.dma(rbs[:], rb_d[:, :], writes=[rbs_t])
                xt = [sb(p1, f"p1x{i}", [128, D], F32) for i in range(2)]
                xt_t = [Tk(), Tk()]
                xn = sb(p1, "p1xn", [128, D], F32)
                xn_t = Tk()
                h2 = sb(p1, "p1h2", [128, D], F32)
                h2_t = Tk()
                h2bf = [sb(p1, f"p1h2b{i}", [128, D], BF16) for i in range(2)]
                h2bf_t = [Tk(), Tk()]
                h2f = sb(p1, "p1h2f", [128, 8, 128], F32)
                h2f_t = Tk()
                scr = sb(p1, "p1scr", [128, D], BF16)
                ssb = sb(p1, "p1ss", [128, 4], F32)
                tmp_t = Tk()
                lgt = sb(p1, "p1lg", [128, 32], F32)
                mk = sb(p1, "p1mk", [128, 32], F32)
                pos = sb(p1, "p1pos", [128, 32], F32)
                s32 = sb(p1, "p1s32", [128, 32], F32)
                m8 = sb(p1, "p1m8", [128, 8], F32)
                e4 = sb(p1, "p1e4", [128, 4], F32)
                sm = sb(p1, "p1sm", [128, 4], F32)
                rt_t = Tk()
                S.op("pool", lambda e: e.memset(base[:], 0.0), writes=[base_t])
                S.dma(xt[0][:], x1_d[0], reads=[x1_t[0]], writes=[xt_t[0]])
                for ti in range(NTI):
                    bb = ti // 32
                    if ti + 1 < NTI:
                        S.dma(xt[(ti + 1) % 2][:], x1_d[ti + 1], reads=[x1_t[ti + 1]], writes=[xt_t[(ti + 1) % 2]])
                    x_, x_t = xt[ti % 2], xt_t[ti % 2]
                    S.op("pool", lambda e: e.memset(ssb[:, 0:1], 0.0), writes=[tmp_t])
                    S.op("act", lambda e: e.activation(out=scr[:], in_=x_[:], func=AF.Square, accum_out=ssb[:, 0:1]),
                         reads=[x_t, tmp_t], writes=[tmp_t])
                    S.op("act", lambda e: e.activation(out=ssb[:, 1:2], in_=ssb[:, 0:1], func=AF.Sqrt, scale=1.0 / D, bias=EPS),
                         reads=[tmp_t], writes=[tmp_t])
                    S.op("dve", lambda e: e.reciprocal(out=ssb[:, 2:3], in_=ssb[:, 1:2]), reads=[tmp_t], writes=[tmp_t])
                    S.op("dve", lambda e: e.tensor_scalar(out=xn[:], in0=x_[:], scalar1=ssb[:, 2:3], scalar2=None, op0=ALU.mult),
                         reads=[x_t, tmp_t], writes=[xn_t])
                    S.op("dve", lambda e: e.tensor_tensor(out=xn[:], in0=xn[:], in1=GS2[:, bb, :], op=ALU.mult),
                         reads=[xn_t, GS_t], writes=[xn_t])
                    S.op("pool", lambda e: e.tensor_tensor(out=h2[:], in0=xn[:], in1=SH2[:, bb, :], op=ALU.add),
                         reads=[xn_t, GS_t], writes=[h2_t])
                    hb_, hb_t = h2bf[ti % 2], h2bf_t[ti % 2]
                    S.op("act", lambda e: e.copy(out=hb_[:], in_=h2[:]), reads=[h2_t], writes=[hb_t])
                    S.dma(h2_d[ti], hb_[:], reads=[hb_t], writes=[h2d_t[ti]])
                    for c in range(8):
                        S.op("pe", lambda e: e.transpose(out=pB2[c // 4][:, (c % 4) * 128:(c % 4 + 1) * 128],
                                                         in_=h2[:, c * 128:(c + 1) * 128], identity=identf),
                             reads=[h2_t, cst_t], writes=[pB2_t[c // 4]], signal=(c % 4 == 3))
                    S.op("dve", lambda e: e.tensor_copy(out=h2f[:, 0:4, :].rearrange("p a b -> p (a b)"), in_=pB2[0][:]),
                         reads=[pB2_t[0]], writes=[h2f_t])
                    S.op("act", lambda e: e.copy(out=h2f[:, 4:8, :].rearrange("p a b -> p (a b)"), in_=pB2[1][:]),
                         reads=[pB2_t[1]], writes=[h2f_t])
                    for kc in range(8):
                        S.op("pe", lambda e: e.matmul(out=pR[:, 0:32], lhsT=h2f[:, kc, :], rhs=Wr[:, kc, :], start=(kc == 0), stop=False),
                             reads=[h2f_t, Wr_t], writes=[pR_t], signal=False)
                    S.op("pe", lambda e: e.matmul(out=pR[:, 0:32], lhsT=cst[0:1, 1, :], rhs=rbs[:], start=False, stop=True),
                         reads=[cst_t, rbs_t], writes=[pR_t])
                    S.op("dve", lambda e: e.tensor_copy(out=lgt[:], in_=pR[:, 0:32]), reads=[pR_t], writes=[rt_t])
                    S.op("dve", lambda e: e.max(out=m8[:], in_=lgt[:]), reads=[rt_t], writes=[rt_t])
                    S.op("dve", lambda e: e.tensor_scalar(out=mk[:], in0=lgt[:], scalar1=m8[:, 3:4], scalar2=None, op0=ALU.is_ge),
                         reads=[rt_t], writes=[rt_t])
                    S.op("dve", lambda e: e.tensor_scalar(out=sm[:, 0:1], in0=m8[:, 0:1], scalar1=-1.0, scalar2=None, op0=ALU.mult),
                         reads=[rt_t], writes=[rt_t])
                    S.op("act", lambda e: e.activation(out=e4[:], in_=m8[:, 0:4], func=AF.Exp, bias=sm[:, 0:1], scale=1.0),
                         reads=[rt_t], writes=[rt_t])
                    S.op("dve", lambda e: e.reduce_sum(out=sm[:, 1:2], in_=e4[:], axis=mybir.AxisListType.X), reads=[rt_t], writes=[rt_t])
                    S.op("dve", lambda e: e.reciprocal(out=sm[:, 2:3], in_=sm[:, 1:2]), reads=[rt_t], writes=[rt_t])
                    S.op("dve", lambda e: e.tensor_scalar(out=g4[:, ti * 4:ti * 4 + 4], in0=e4[:], scalar1=sm[:, 2:3], scalar2=None, op0=ALU.mult),
                         reads=[rt_t], writes=[pk_t])
                    S.op("pe", lambda e: e.matmul(out=pR[:, 32:64], lhsT=cst[:, 9, :], rhs=mk[:], start=True, stop=True),
                         reads=[rt_t, cst_t], writes=[pR_t])
                    S.op("pe", lambda e: e.matmul(out=pR[:, 64:96], lhsT=onesf, rhs=mk[:], start=True, stop=True),
                         reads=[rt_t, cst_t], writes=[pR_t])
                    S.op("dve", lambda e: e.tensor_tensor(out=pos[:], in0=pR[:, 32:64], in1=base[:], op=ALU.add),
                         reads=[pR_t, base_t], writes=[rt_t])
                    for k in range(4):
                        S.op("dve", lambda e: e.scalar_tensor_tensor(out=s32[:], in0=lgt[:], scalar=m8[:, k:k + 1], in1=pos[:],
                                                                     op0=ALU.is_equal, op1=ALU.mult), reads=[rt_t], writes=[rt_t])
                        S.op("dve", lambda e: e.reduce_sum(out=posk[:, ti * 4 + k:ti * 4 + k + 1], in_=s32[:], axis=mybir.AxisListType.X),
                             reads=[rt_t], writes=[pk_t])
                        S.op("dve", lambda e: e.scalar_tensor_tensor(out=s32[:], in0=lgt[:], scalar=m8[:, k:k + 1], in1=iota32,
                                                                     op0=ALU.is_equal, op1=ALU.mult), reads=[rt_t, cst_t, pk_t], writes=[rt_t])
                        S.op("dve", lambda e: e.reduce_sum(out=ekk[:, ti * 4 + k:ti * 4 + k + 1], in_=s32[:], axis=mybir.AxisListType.X),
                             reads=[rt_t], writes=[pk_t])
                    S.op("dve", lambda e: e.tensor_tensor(out=base[:], in0=pR[:, 64:96], in1=base[:], op=ALU.add),
                         reads=[pR_t, base_t, rt_t], writes=[base_t])
                ci = sb(p1, "p2ci", [128, 32], I32)
                padded = sb(p1, "p2pad", [128, 32], F32)
                pst = sb(p1, "p2pst", [128, 32], F32)
                pend = sb(p1, "p2pend", [128, 32], F32)
                bst = sb(p1, "p2bst", [128, NBLK], F32)
                be = sb(p1, "p2be", [128, NBLK], F32)
                wf = sb(p1, "p2wf", [128, 8, NBLK], F32)
                df = sb(p1, "p2df", [128, NTI * 4], F32)
                p2_t = Tk()
                S.op("dve", lambda e: e.tensor_copy(out=ci[:], in_=base[:]), reads=[base_t], writes=[p2_t])
                S.op("dve", lambda e: e.tensor_scalar(out=ci[:], in0=ci[:], scalar1=511, scalar2=None, op0=ALU.add), reads=[p2_t], writes=[p2_t])
                S.op("dve", lambda e: e.tensor_scalar(out=ci[:], in0=ci[:], scalar1=-512, scalar2=None, op0=ALU.bitwise_and), reads=[p2_t], writes=[p2_t])
                S.op("dve", lambda e: e.tensor_copy(out=padded[:], in_=ci[:]), reads=[p2_t], writes=[p2_t])
                S.op("dve", lambda e: e.memset(pst[:], 0.0), reads=[p2_t], writes=[p2_t])
                for ee in range(1, 32):
                    S.op("dve", lambda e: e.tensor_tensor(out=pst[:, ee:ee + 1], in0=pst[:, ee - 1:ee], in1=padded[:, ee - 1:ee], op=ALU.add),
                         reads=[p2_t], writes=[p2_t])
                S.op("dve", lambda e: e.tensor_tensor(out=pend[:], in0=pst[:], in1=padded[:], op=ALU.add), reads=[p2_t], writes=[p2_t])
                S.op("dve", lambda e: e.tensor_scalar(out=bst[:], in0=cst[:, 10, 0:NBLK], scalar1=512.0, scalar2=None, op0=ALU.mult),
                     reads=[cst_t, p2_t], writes=[p2_t])
                S.op("dve", lambda e: e.memset(be[:], 0.0), reads=[p2_t], writes=[p2_t])
                for ee in range(32):
                    S.op("dve", lambda e: e.scalar_tensor_tensor(out=be[:], in0=bst[:], scalar=pend[:, ee:ee + 1], in1=be[:],
                                                                 op0=ALU.is_ge, op1=ALU.add), reads=[p2_t], writes=[p2_t])
                S.op("dve", lambda e: e.tensor_scalar(out=be[:], in0=be[:], scalar1=31.0, scalar2=None, op0=ALU.min), reads=[p2_t], writes=[p2_t])
                for kc in range(8):
                    S.op("dve", lambda e: e.tensor_scalar(out=wf[:, kc, :], in0=be[:], scalar1=1024.0, scalar2=cst[:, 11, kc:kc + 1],
                                                          op0=ALU.mult, op1=ALU.add), reads=[p2_t, cst_t], writes=[p2_t])
                S.op("dve", lambda e: e.tensor_copy(out=widx[:], in_=wf[:]), reads=[p2_t], writes=[widx_t])
                S.op("dve", lambda e: e.tensor_copy(out=bidx[:], in_=be[0:2, :]), reads=[p2_t], writes=[widx_t])
                for c in range(NTI * 4):
                    S.op("dve", lambda e: e.scalar_tensor_tensor(out=s32[:], in0=iota32, scalar=ekk[:, c:c + 1], in1=pst[:],
                                                                 op0=ALU.is_equal, op1=ALU.mult), reads=[p2_t, pk_t, cst_t, rt_t], writes=[rt_t])
                    S.op("dve", lambda e: e.reduce_sum(out=df[:, c:c + 1], in_=s32[:], axis=mybir.AxisListType.X), reads=[rt_t], writes=[p2_t])
                S.op("dve", lambda e: e.tensor_tensor(out=df[:], in0=df[:], in1=posk[:], op=ALU.add), reads=[p2_t, pk_t], writes=[p2_t])
                S.op("dve", lambda e: e.tensor_copy(out=desti[:], in_=df[:]), reads=[p2_t], writes=[desti_t])
                for ti in range(NTI):
                    hb_, hb_t = h2bf[ti % 2], h2bf_t[ti % 2]
                    S.dma(hb_[:], h2_d[ti], reads=[h2d_t[ti]], writes=[hb_t])
                    for k in range(4):
                        cidx = ti * 4 + k
                        S.dma_ind(lambda e: e.indirect_dma_start(
                            out=xs_d[:, :], out_offset=bass.IndirectOffsetOnAxis(ap=desti[:, cidx:cidx + 1], axis=0),
                            in_=hb_[:], in_offset=None),
                            reads=[hb_t, desti_t])
                S.barrier()

            with contextlib.ExitStack() as p4:
                egu2 = egu_d[0].rearrange("e k n -> (e k) n")
                edn2 = edn_d[0].rearrange("e k n -> (e k) n")
                Wgu = [sb(p4, f"Wgu{i}", [128, 8, 1024], BF16) for i in range(3)]
                Wgu_t = [Tk(), Tk(), Tk()]
                Wd = [sb(p4, f"Wd{i}", [128, 4, 1024], BF16) for i in range(2)]
                Wd_t = [Tk(), Tk()]
                stg = [sb(p4, f"p4stg{i}", [128, 2048], F32) for i in range(6)]
                stg_t = [Tk() for _ in range(6)]
                browb = [sb(p4, f"browb{i}", [2, 3072], BF16) for i in range(2)]
                browb_t = [Tk(), Tk()]
                xs = [sb(p4, f"p4xs{i}", [128, D], BF16) for i in range(4)]
                xs_t = [Tk() for _ in range(4)]
                xsT = sb(p4, "p4xsT", [128, 8, 512], BF16)
                xsT_t = Tk()
                aT = [sb(p4, f"aT{i}", [128, 4, 512], BF16) for i in range(2)]
                aT_t = [Tk(), Tk()]
                g1 = sb(p4, "p4g1", [128, 512], F32)
                u1 = sb(p4, "p4u1", [128, 512], F32)
                glu = sb(p4, "p4gl", [128, 512], F32)
                g1_t, u1_t, glu_t = Tk(), Tk(), Tk()
                ysb = sb(p4, "p4ys", [128, 4, D], F32)
                ysb_t = [[Tk(), Tk()] for _ in range(4)]
                pG = [ps(p4, f"p4G{i}", [128, 512]) for i in range(2)]
                pG_t = [Tk(), Tk()]
                pU = [ps(p4, f"p4U{i}", [128, 512]) for i in range(2)]
                pU_t = [Tk(), Tk()]
                pY = [ps(p4, f"p4Y{i}", [128, 512]) for i in range(2)]
                pY_t = [Tk(), Tk()]
                pX = [ps(p4, f"p4X{i}", [128, 2, 512], BF16) for i in range(2)]
                pX_t = [Tk(), Tk()]
                ISC = 1.0 / 1.702
                cnt = dict(sk=0, gk=0, yk=0)

                def w_load_gu_block(blk):
                    for kc in range(8):
                        si = cnt["sk"] % 6
                        cnt["sk"] += 1
                        S.dma_ind(lambda e: e.indirect_dma_start(
                            out=stg[si][:, :], out_offset=None, in_=egu2[:, :],
                            in_offset=bass.IndirectOffsetOnAxis(ap=widx[:, kc, blk:blk + 1], axis=0)),
                            reads=[widx_t], writes=[stg_t[si]])
                        for hf in range(2):
                            wi3 = (2 * blk + hf) % 3
                            S.op("act", lambda e: e.copy(out=Wgu[wi3][:, kc, :].rearrange("p (g n) -> p g n", g=2),
                                                         in_=stg[si][:].rearrange("p (g h n) -> p g h n", g=2, h=2)[:, :, hf, :]),
                                 reads=[stg_t[si]], writes=[Wgu_t[wi3]])

                def w_load_d(blk, hf, wi):
                    wd, wd_t = Wd[wi], Wd_t[wi]
                    for jq in range(2):
                        si = cnt["sk"] % 6
                        cnt["sk"] += 1
                        for i in range(2):
                            kc = hf * 4 + jq * 2 + i
                            S.dma_ind(lambda e: e.indirect_dma_start(
                                out=stg[si][:, i * 1024:(i + 1) * 1024], out_offset=None, in_=edn2[:, :],
                                in_offset=bass.IndirectOffsetOnAxis(ap=widx[:, kc, blk:blk + 1], axis=0)),
                                reads=[widx_t], writes=[stg_t[si]])
                        S.op("act", lambda e: e.mul(out=wd[:, jq * 2:(jq + 1) * 2, :], in_=stg[si][:].rearrange("p (a b) -> p a b", a=2), mul=ISC),
                             reads=[stg_t[si]], writes=[wd_t])

                def b_load(blk):
                    S.dma_ind(lambda e: e.indirect_dma_start(
                        out=browb[blk % 2][0:2, :], out_offset=None, in_=bias_d[:, :],
                        in_offset=bass.IndirectOffsetOnAxis(ap=bidx[0:2, blk:blk + 1], axis=0)),
                        reads=[widx_t], writes=[browb_t[blk % 2]])

                def x_dma(blk):
                    for tt in range(4):
                        r0 = blk * 512 + tt * 128
                        S.dma(xs[tt][:], xs_d[r0:r0 + 128, :], writes=[xs_t[tt]])

                def x_tr(blk):
                    for tt in range(4):
                        for kc in range(8):
                            S.op("pe", lambda e: e.transpose(out=pX[tt % 2][:, kc // 4, (kc % 4) * 128:(kc % 4 + 1) * 128],
                                                             in_=xs[tt][:, kc * 128:(kc + 1) * 128], identity=identb[:]),
                                 reads=[xs_t[tt], cb_t], writes=[pX_t[tt % 2]], signal=(kc == 7))
                        S.op("dve", lambda e: e.tensor_copy(out=xsT[:, 0:4, tt * 128:(tt + 1) * 128],
                                                            in_=pX[tt % 2][:, 0, :].rearrange("p (a b) -> p a b", a=4)),
                             reads=[pX_t[tt % 2]], writes=[xsT_t])
                        S.op("dve", lambda e: e.tensor_copy(out=xsT[:, 4:8, tt * 128:(tt + 1) * 128],
                                                            in_=pX[tt % 2][:, 1, :].rearrange("p (a b) -> p a b", a=4)),
                             reads=[pX_t[tt % 2]], writes=[xsT_t])

                def gu_unit(blk, hf, wi, au):
                    wg, wg_t = Wgu[(2 * blk + hf) % 3], Wgu_t[(2 * blk + hf) % 3]
                    bb_, bb_t = browb[blk % 2], browb_t[blk % 2]
                    a_, a_t = aT[au], aT_t[au]
                    for j in range(4):
                        i2 = cnt["gk"] % 2
                        cnt["gk"] += 1
                        fcol = hf * 512 + j * 128
                        for kc in range(8):
                            S.op("pe", lambda e: e.matmul(out=pG[i2][:], lhsT=wg[:, kc, j * 128:(j + 1) * 128], rhs=xsT[:, kc, :],
                                                          start=(kc == 0), stop=False), reads=[wg_t, xsT_t], writes=[pG_t[i2]], signal=False)
                        S.op("pe", lambda e: e.matmul(out=pG[i2][:], lhsT=bb_[0:1, fcol:fcol + 128], rhs=cbones[0:1, :], start=False, stop=True),
                             reads=[bb_t, cb_t], writes=[pG_t[i2]])
                        for kc in range(8):
                            S.op("pe", lambda e: e.matmul(out=pU[i2][:], lhsT=wg[:, kc, 512 + j * 128:512 + (j + 1) * 128], rhs=xsT[:, kc, :],
                                                          start=(kc == 0), stop=False), reads=[wg_t, xsT_t], writes=[pU_t[i2]], signal=False)
                        S.op("pe", lambda e: e.matmul(out=pU[i2][:], lhsT=bb_[0:1, 1024 + fcol:1024 + fcol + 128], rhs=cbones[0:1, :],
                                                      start=False, stop=True), reads=[bb_t, cb_t], writes=[pU_t[i2]])
                        S.op("dve", lambda e: e.tensor_scalar(out=g1[:], in0=pG[i2][:], scalar1=7.0, scalar2=None, op0=ALU.min),
                             reads=[pG_t[i2]], writes=[g1_t])
                        S.op("act", lambda e: e.activation(out=glu[:], in_=g1[:], func=AF.Silu, scale=1.702), reads=[g1_t], writes=[glu_t])
                        S.op("dve", lambda e: e.tensor_scalar(out=u1[:], in0=pU[i2][:], scalar1=1.0, scalar2=8.0, op0=ALU.add, op1=ALU.min),
                             reads=[pU_t[i2]], writes=[u1_t])
                        S.op("dve", lambda e: e.scalar_tensor_tensor(out=a_[:, j, :], in0=u1[:], scalar=-6.0, in1=glu[:],
                                                                     op0=ALU.max, op1=ALU.mult), reads=[u1_t, glu_t], writes=[a_t])

                def dn_unit(blk, hf, wi, au):
                    wd, wd_t = Wd[wi], Wd_t[wi]
                    bb_, bb_t = browb[blk % 2], browb_t[blk % 2]
                    a_, a_t = aT[au], aT_t[au]
                    for tt in range(4):
                        for cb in range(2):
                            yi = cnt["yk"] % 2
                            cnt["yk"] += 1
                            for j in range(4):
                                S.op("pe", lambda e: e.matmul(out=pY[yi][:], lhsT=a_[:, j, tt * 128:(tt + 1) * 128],
                                                              rhs=wd[:, j, cb * 512:(cb + 1) * 512], start=(j == 0), stop=(j == 3 and hf == 1)),
                                     reads=[a_t, wd_t], writes=[pY_t[yi]], signal=(j == 3 and hf == 1))
                            yv = ysb[:, tt, cb * 512:(cb + 1) * 512]
                            if hf == 0:
                                S.op("pe", lambda e: e.matmul(out=pY[yi][:], lhsT=cbones[0:1, 0:128], rhs=bb_[0:1, 2048 + cb * 512:2048 + (cb + 1) * 512],
                                                              start=False, stop=True), reads=[bb_t, cb_t], writes=[pY_t[yi]])
                                S.op("dve", lambda e: e.tensor_copy(out=yv, in_=pY[yi][:]), reads=[pY_t[yi]], writes=[ysb_t[tt][cb]])
                            else:
                                S.op("dve", lambda e: e.tensor_tensor(out=yv, in0=pY[yi][:], in1=yv, op=ALU.add),
                                     reads=[pY_t[yi], ysb_t[tt][cb]], writes=[ysb_t[tt][cb]])
                    if hf == 1:
                        S.dma(ys_d[blk * 512:(blk + 1) * 512, :].rearrange("(t p) c -> p t c", p=128), ysb[:],
                              reads=[ysb_t[tt][cb] for tt in range(4) for cb in range(2)])

                cbones = sb(p4, "cbones", [1, 512], BF16)
                S.op("dve", lambda e: e.memset(cbones[:], 1.0), reads=[cb_t], writes=[cb_t])
                units = [(blk, hf) for blk in range(NBLK) for hf in range(2)]
                NU = len(units)
                b_load(0)
                x_dma(0)
                w_load_gu_block(0)
                w_load_d(0, 0, 0)
                w_load_d(0, 1, 1)
                x_tr(0)
                gu_unit(0, 0, 0, 0)
                w_load_gu_block(1)
                b_load(1)
                x_dma(1)
                for ui, (blk, hf) in enumerate(units):
                    if ui + 1 < NU:
                        nb_, nh_ = units[ui + 1]
                        if nh_ == 0:
                            x_tr(nb_)
                        gu_unit(nb_, nh_, (ui + 1) % 2, (ui + 1) % 2)
                        if nh_ == 0 and nb_ + 1 < NBLK:
                            w_load_gu_block(nb_ + 1)
                        if nh_ == 0 and nb_ + 1 < NBLK:
                            b_load(nb_ + 1)
                            x_dma(nb_ + 1)
                    dn_unit(blk, hf, ui % 2, ui % 2)
                    if ui + 2 < NU:
                        b2, h2_ = units[ui + 2]
                        w_load_d(b2, h2_, (ui + 2) % 2)
                S.barrier()

            with contextlib.ExitStack() as p5:
                yk = [sb(p5, f"p5y{i}", [128, D], F32) for i in range(4)]
                yk_t = [Tk() for _ in range(4)]
                accm = sb(p5, "p5acc", [128, D], F32)
                acc_t = Tk()
                x_ = sb(p5, "p5x", [128, D], F32)
                x_t = Tk()
                scr = sb(p5, "p5scr", [128, D], BF16)
                ssb = sb(p5, "p5ss", [128, 4], F32)
                tmp_t = Tk()
                yo = sb(p5, "p5yo", [128, D], F32)
                yo_t = Tk()
                for ti in range(NTI):
                    bb = ti // 32
                    S.dma(x_[:], x1_d[ti], reads=[x1_t[ti]], writes=[x_t])
                    for k in range(4):
                        cidx = ti * 4 + k
                        S.dma_ind(lambda e: e.indirect_dma_start(
                            out=yk[k][:], out_offset=None, in_=ys_d[:, :],
                            in_offset=bass.IndirectOffsetOnAxis(ap=desti[:, cidx:cidx + 1], axis=0)),
                            reads=[desti_t], writes=[yk_t[k]])
                    S.op("dve", lambda e: e.tensor_scalar(out=accm[:], in0=yk[0][:], scalar1=g4[:, ti * 4:ti * 4 + 1], scalar2=None, op0=ALU.mult),
                         reads=[yk_t[0], pk_t], writes=[acc_t])
                    for k in range(1, 4):
                        S.op("dve", lambda e: e.scalar_tensor_tensor(out=accm[:], in0=yk[k][:], scalar=g4[:, ti * 4 + k:ti * 4 + k + 1], in1=accm[:],
                                                                     op0=ALU.mult, op1=ALU.add), reads=[yk_t[k], pk_t, acc_t], writes=[acc_t])
                    S.op("dve", lambda e: e.tensor_tensor(out=accm[:], in0=accm[:], in1=G2[:, bb, :], op=ALU.mult),
                         reads=[acc_t, G_t], writes=[acc_t])
                    S.op("dve", lambda e: e.tensor_tensor(out=accm[:], in0=accm[:], in1=x_[:], op=ALU.add), reads=[acc_t, x_t], writes=[acc_t])
                    S.op("dve", lambda e: e.memset(ssb[:, 0:1], 0.0), writes=[tmp_t])
                    S.op("act", lambda e: e.activation(out=scr[:], in_=accm[:], func=AF.Square, accum_out=ssb[:, 0:1]),
                         reads=[acc_t, tmp_t], writes=[tmp_t])
                    S.op("act", lambda e: e.activation(out=ssb[:, 1:2], in_=ssb[:, 0:1], func=AF.Sqrt, scale=1.0 / D, bias=EPS),
                         reads=[tmp_t], writes=[tmp_t])
                    S.op("dve", lambda e: e.reciprocal(out=ssb[:, 2:3], in_=ssb[:, 1:2]), reads=[tmp_t], writes=[tmp_t])
                    S.op("dve", lambda e: e.scalar_tensor_tensor(out=yo[:], in0=accm[:], scalar=ssb[:, 2:3], in1=FG[:], op0=ALU.mult, op1=ALU.mult),
                         reads=[acc_t, tmp_t, G_t], writes=[yo_t])
                    r0 = (ti % 32) * 128
                    S.dma(out_d[bb, r0:r0 + 128, :], yo[:], reads=[yo_t])
            S.final_wait()
        print("ops:", S.n_ops, "dmas:", S.dma_n, "sig:", S.cnt)
    return nc


_CACHE = {}


def make_in_maps(inputs, n_cores=8):
    RC, RS, MC, MS = rope_tables()
    cst, _ = const_tables()
    f = lambda a: np.ascontiguousarray(np.asarray(a, dtype=np.float32))
    shared = {k: f(inputs[k]) for k in ("norm1_g", "norm2_g", "ada_w", "ada_b", "w_in", "ret_decay_fwd", "ret_decay_bwd",
                                        "mla_q_norm_g", "mla_w_uq", "mla_kv_norm_g", "mla_w_ukv", "w_branch_ret",
                                        "w_branch_mla", "w_out", "router_w", "router_b", "exp_w_gu", "exp_b_gu",
                                        "exp_w_down", "exp_b_down", "final_norm_g")}
    shared.update(consts=cst, rope_rc=RC, rope_rs=RS, rope_mc=MC, rope_ms=MS)
    x, c, ctx, c_ctx = f(inputs["x"]), f(inputs["c"]), f(inputs["ctx"]), f(inputs["c_ctx"])
    maps = []
    for i in range(n_cores):
        m = dict(shared)
        m["x"] = x[i * NB:(i + 1) * NB]
        m["ctx"] = ctx[i * NB:(i + 1) * NB]
        m["cvec"] = np.ascontiguousarray(np.concatenate([c[i * NB:(i + 1) * NB], c_ctx[None, :]], axis=0))
        maps.append(m)
    return maps


def kernel(**inputs):
    if "nc" not in _CACHE:
        _CACHE["nc"] = build()
    nc = _CACHE["nc"]
    maps = make_in_maps(inputs)
    res = run_bass_kernel_spmd(nc, maps, core_ids=list(range(8)))
    return np.concatenate([r["out"] for r in res.results], axis=0).astype(np.float32)
```

```python
import contextlib
import numpy as np
import concourse.bass as bass
import concourse.mybir as mybir
from concourse.bass_utils import run_bass_kernel_spmd

F32 = mybir.dt.float32
BF16 = mybir.dt.bfloat16
AF = mybir.ActivationFunctionType
ALU = mybir.AluOpType

NB = 2
L = 4096
CT = 256
LT = L + CT
NT = LT // 128
D = 1024
EPS = 1e-6
NS_DMA = 16
ERA = 16000


class Tk:
    __slots__ = ("w", "r", "name")

    def __init__(self, name=""):
        self.w = []
        self.r = []
        self.name = name


class Sched:
    def __init__(self, nc, stack):
        self.nc = nc
        self.stack = stack
        self.engs = {"pe": nc.tensor, "act": nc.scalar, "dve": nc.vector, "pool": nc.gpsimd, "sp": nc.sync}
        self.sems = {}
        self.seq = {e: 0 for e in self.engs}
        self.sig = {e: [] for e in self.engs}
        self.cnt = {e: 0 for e in self.engs}
        self.waited = {e: {} for e in self.engs}
        self.waited_d = {e: {} for e in self.engs}
        self.ring = [stack.enter_context(nc.semaphore(f"dq{i}")) for i in range(NS_DMA)]
        self.dma_n = 0
        self.n_ops = 0

    def _sem(self, eng, era):
        k = (eng, era)
        if k not in self.sems:
            self.sems[k] = self.stack.enter_context(self.nc.semaphore(f"s_{eng}_{era}"))
        return self.sems[k]

    def _wait(self, eng, tk):
        e = self.engs[eng]
        if tk[0] == "d":
            _, ring, val = tk
            if self.waited_d[eng].get(ring, 0) >= val:
                return
            e.wait_ge(self.ring[ring], val)
            self.waited_d[eng][ring] = val
            return
        _, peng, seq = tk
        lst = self.sig[peng]
        lo, hi = 0, len(lst)
        while lo < hi:
            mid = (lo + hi) // 2
            if lst[mid][0] >= seq:
                hi = mid
            else:
                lo = mid + 1
        if lo >= len(lst):
            raise RuntimeError(f"no signalling op after seq {seq} on {peng}")
        count = lst[lo][1]
        if self.waited[eng].get(peng, 0) >= count:
            return
        era, val = (count - 1) // ERA, (count - 1) % ERA + 1
        e.wait_ge(self._sem(peng, era), val)
        self.waited[eng][peng] = count

    def op(self, eng, fn, reads=(), writes=(), signal=True):
        deps = []
        for t in reads:
            deps.extend(t.w)
        for t in writes:
            for tk in t.w:
                if tk[0] == "d" or tk[1] != eng:
                    deps.append(tk)
            for tk in t.r:
                if tk[0] == "d" or tk[1] != eng:
                    deps.append(tk)
        for tk in deps:
            self._wait(eng, tk)
        ins = fn(self.engs[eng])
        self.seq[eng] += 1
        seq = self.seq[eng]
        if signal:
            self.cnt[eng] += 1
            c = self.cnt[eng]
            era, val = (c - 1) // ERA, (c - 1) % ERA + 1
            ins.then_inc(self._sem(eng, era), 1)
            self.sig[eng].append((seq, c))
        tk = ("c", eng, seq)
        for t in reads:
            t.r = [x for x in t.r if not (x[0] == "c" and x[1] == eng)]
            t.r.append(tk)
        for t in writes:
            t.w = [tk]
            t.r = []
        self.n_ops += 1
        return ins

    def dma(self, out, in_, reads=(), writes=()):
        eng = "sp"
        deps = []
        for t in reads:
            deps.extend(t.w)
        for t in writes:
            deps.extend(t.w)
            deps.extend(t.r)
        n = self.dma_n
        ring = n % NS_DMA
        val = 16 * (n // NS_DMA + 1)
        if n >= NS_DMA:
            deps.append(("d", ring, val - 16))
        for tk in deps:
            self._wait(eng, tk)
        self.engs[eng].dma_start(out=out, in_=in_).then_inc(self.ring[ring], 16)
        self.dma_n += 1
        tk = ("d", ring, val)
        for t in reads:
            t.r.append(tk)
        for t in writes:
            t.w = [tk]
            t.r = []
        self.n_ops += 1
        return tk

    def dma_ind(self, fn, reads=(), writes=()):
        eng = "pool"
        deps = []
        for t in reads:
            deps.extend(t.w)
        for t in writes:
            deps.extend(t.w)
            deps.extend(t.r)
        n = self.dma_n
        ring = n % NS_DMA
        val = 16 * (n // NS_DMA + 1)
        if n >= NS_DMA:
            deps.append(("d", ring, val - 16))
        for tk in deps:
            self._wait(eng, tk)
        fn(self.engs[eng]).then_inc(self.ring[ring], 16)
        self.dma_n += 1
        tk = ("d", ring, val)
        for t in reads:
            t.r.append(tk)
        for t in writes:
            t.w = [tk]
            t.r = []
        self.n_ops += 1
        return tk

    def barrier(self):
        tks = []
        for e in self.engs:
            if self.sig[e]:
                tks.append(("c", e, self.sig[e][-1][0]))
        n = self.dma_n
        for k in range(max(0, n - NS_DMA), n):
            tks.append(("d", k % NS_DMA, 16 * (k // NS_DMA + 1)))
        for e in self.engs:
            for tk in tks:
                if tk[0] == "c" and tk[1] == e:
                    continue
                self._wait(e, tk)

    def final_wait(self):
        n = self.dma_n
        for k in range(max(0, n - NS_DMA), n):
            self._wait("sp", ("d", k % NS_DMA, 16 * (k // NS_DMA + 1)))


def rope_tables():
    pos = np.arange(L)
    rows = (pos // 64).astype(np.float32)
    cols = (pos % 64).astype(np.float32)

    def tab(dr):
        half = dr // 2
        hh = half // 2
        freqs = (10000.0 ** (-np.arange(hh, dtype=np.float32) / hh)).astype(np.float32)
        C = np.ones((LT, dr), np.float32)
        S = np.zeros((LT, dr), np.float32)
        for part, p in enumerate((rows, cols)):
            ang = (p[:, None] * freqs[None, :]).astype(np.float32)
            c, s = np.cos(ang).astype(np.float32), np.sin(ang).astype(np.float32)
            o = part * half
            C[CT:, o:o + hh] = c
            C[CT:, o + hh:o + half] = c
            S[CT:, o:o + hh] = -s
            S[CT:, o + hh:o + half] = s
        return C, S

    RC, RS = tab(256)
    MC, MS = tab(64)
    return RC, RS, np.ascontiguousarray(MC.T), np.ascontiguousarray(MS.T)


def const_tables():
    j = np.arange(128, dtype=np.float32)[:, None]
    i = np.arange(128, dtype=np.float32)[None, :]
    c = {}
    c["ident"] = np.eye(128, dtype=np.float32)
    c["ones"] = np.ones((128, 128), np.float32)
    c["d1"] = np.maximum(i - j, 0.0) + 0 * j
    c["mf"] = (i >= j).astype(np.float32)
    c["d2"] = np.maximum(j - i, 0.0)
    c["mb"] = (i < j).astype(np.float32)
    c["ip1"] = (i + 1.0) + 0 * j
    c["rev"] = (128.0 - i) + 0 * j
    col = np.zeros((128, 128), np.float32)
    col[:, 0] = 127.0 - np.arange(128)
    col[:, 1] = np.arange(128)
    col[:, 2] = 128.0
    c["col"] = col
    c["lt"] = (i > j).astype(np.float32)
    c["iota"] = i + 0 * j
    rowb = np.zeros((128, 128), np.float32)
    for kc in range(8):
        rowb[:, kc] = kc * 128 + np.arange(128)
    c["rowb"] = rowb
    names = ["ident", "ones", "d1", "mf", "d2", "mb", "ip1", "rev", "col", "lt", "iota", "rowb"]
    return np.stack([c[n].astype(np.float32) for n in names], axis=1), names


def build(dbg=()):
    nc = bass.Bass("TRN2", target_bir_lowering=False)
    try:
        nc.allow_low_precision("bf16 matmul operands with fp32 accumulation")
    except Exception:
        pass
    try:
        nc.allow_non_contiguous_dma("strided weight/activation tiles")
    except Exception:
        pass

    def din(name, shape, dt=F32):
        return nc.dram_tensor(name, list(shape), dt, kind="ExternalInput").ap()

    def dscr(name, shape, dt):
        kind = "ExternalOutput" if name in dbg else "Internal"
        return nc.dram_tensor(name, list(shape), dt, kind=kind).ap()

    x_d = din("x", [NB, L, D])
    ctx_d = din("ctx", [NB, CT, D])
    cv_d = din("cvec", [3, D])
    n1_d = din("norm1_g", [1, D])
    n2_d = din("norm2_g", [1, D])
    adaw_d = din("ada_w", [1, D, 6 * D])
    adab_d = din("ada_b", [1, 6 * D])
    win_d = din("w_in", [1, D, 8896])
    rdf_d = din("ret_decay_fwd", [1, 4])
    rdb_d = din("ret_decay_bwd", [1, 4])
    qg_d = din("mla_q_norm_g", [1, 384])
    wuq_d = din("mla_w_uq", [1, 384, 1536])
    kvg_d = din("mla_kv_norm_g", [1, 256])
    wukv_d = din("mla_w_ukv", [1, 256, 2048])
    wbr_d = din("w_branch_ret", [1, 2048, D])
    wbm_d = din("w_branch_mla", [1, D, D])
    wo_d = din("w_out", [1, D, D])
    rw_d = din("router_w", [1, D, 32])
    rb_d = din("router_b", [1, 32])
    egu_d = din("exp_w_gu", [1, 32, D, 2 * D])
    ebgu_d = din("exp_b_gu", [1, 32, 2 * D])
    edn_d = din("exp_w_down", [1, 32, D, D])
    ebd_d = din("exp_b_down", [1, 32, D])
    fg_d = din("final_norm_g", [D])
    cst_d = din("consts", [128, 12, 128])
    rc_d = din("rope_rc", [LT, 256])
    rs_d = din("rope_rs", [LT, 256])
    mc_d = din("rope_mc", [64, LT])
    ms_d = din("rope_ms", [64, LT])
    out_d = nc.dram_tensor("out", [NB, L, D], F32, kind="ExternalOutput").ap()

    hT_d = dscr("hT_s", [NT, 128, 8, 128], BF16)
    att_d = dscr("att_s", [8, 128, L], BF16)
    rT_d = dscr("rT_s", [4, 4, 128, L], BF16)
    of_d = dscr("of_s", [32, 128, 512], F32)
    x1_d = dscr("x1_s", [NB * 32, 128, D], F32)
    NROWS = NB * L * 4 + 32 * 512
    NBLK = NROWS // 512
    h2_d = dscr("h2_s", [NB * 32, 128, D], BF16)
    xs_d = dscr("xs_s", [NROWS, D], BF16)
    ys_d = dscr("ys_s", [NROWS, D], F32)
    bias_d = dscr("bias_s", [32, 3072], BF16)
    hT_t = [Tk() for _ in range(NT)]
    att_t = [Tk() for _ in range(8)]
    rT_t = [Tk() for _ in range(8)]
    of_t = [Tk() for _ in range(32)]
    x1_t = [Tk() for _ in range(NB * 32)]
    out_t = Tk()

    with contextlib.ExitStack() as gstack:
        S = Sched(nc, gstack)

        uid = [0]

        def sb(stack, name, shape, dt):
            uid[0] += 1
            return stack.enter_context(nc.sbuf_tensor(f"{name}_u{uid[0]}", list(shape), dt))

        def ps(stack, name, shape, dt=F32):
            uid[0] += 1
            return stack.enter_context(nc.psum_tensor(f"{name}_u{uid[0]}", list(shape), dt))

        cst = sb(gstack, "cst", [128, 12, 128], F32)
        cst_t = Tk()
        S.dma(cst[:], cst_d[:, :, :], writes=[cst_t])
        identf = cst[:, 0, :]
        onesf = cst[:, 1, :]
        identb = sb(gstack, "identb", [128, 128], BF16)
        onesb = sb(gstack, "onesb", [128, 128], BF16)
        cb_t = Tk()
        S.op("dve", lambda e: e.tensor_copy(out=identb[:], in_=identf), reads=[cst_t], writes=[cb_t])
        S.op("dve", lambda e: e.tensor_copy(out=onesb[:], in_=onesf), reads=[cst_t], writes=[cb_t])

        mT = sb(gstack, "mT", [128, 48, 3], F32)
        mT_t = Tk()
        gs1 = sb(gstack, "gs1", [128, 3, 8], F32)
        gs2 = sb(gstack, "gs2", [128, 3, 8], F32)
        gs_t = Tk()
        mix_stack = contextlib.ExitStack()
        G2 = sb(gstack, "G2", [128, NB, D], F32)
        FG = sb(gstack, "FG", [128, D], F32)
        G_t = Tk()
        gq = sb(gstack, "gq", [128, 8], F32)
        gq_t = Tk()
        KD = sb(gstack, "KD", [128, 4, 8], F32)
        G1 = sb(mix_stack, "G1", [128, NB, D], F32)
        MTf = sb(mix_stack, "MTf", [128, 4, 128], F32)
        MTb = sb(mix_stack, "MTb", [128, 4, 128], F32)
        QDf = sb(mix_stack, "QDf", [128, 4, 128], F32)
        QDb = sb(mix_stack, "QDb", [128, 4, 128], F32)
        dec_t = Tk()

        def featmajor_load(stack, pst, pst_t, dst_ap, src_rows_ap, nrows, tmpname):
            tmp = sb(stack, tmpname, [nrows, 128], F32)
            tt = Tk()
            S.dma(tmp[:], src_rows_ap, writes=[tt])
            S.op("pe", lambda e: e.transpose(out=pst[:, 0:nrows], in_=tmp[:], identity=cst[0:nrows, 0, 0:nrows]),
                 reads=[tt, cst_t], writes=[pst_t])
            return tmp

        with contextlib.ExitStack() as st:
            pA = ps(st, "pA0", [128, 512])
            pA_t = Tk()
            pB = ps(st, "pB0", [128, 2, 512])
            pB_t = Tk()
            cv = sb(st, "cv", [3, D], F32)
            cvs = sb(st, "cvs", [3, D], F32)
            cv_t = Tk()
            S.dma(cv[:], cv_d[:, :], writes=[cv_t])
            cvs_t = Tk()
            S.op("act", lambda e: e.activation(out=cvs[:], in_=cv[:], func=AF.Silu), reads=[cv_t], writes=[cvs_t])
            sT = sb(st, "sT", [128, 8, 3], F32)
            sT_t = Tk()
            for kc in range(8):
                S.op("pe", lambda e: e.transpose(out=pA[:, kc * 4:kc * 4 + 3], in_=cvs[:, kc * 128:(kc + 1) * 128],
                                                 identity=cst[0:3, 0, 0:3]), reads=[cvs_t, cst_t], writes=[pA_t])
            for kc in range(8):
                S.op("dve", lambda e: e.tensor_copy(out=sT[:, kc, :], in_=pA[:, kc * 4:kc * 4 + 3]),
                     reads=[pA_t], writes=[sT_t])
            abT = sb(st, "abT", [128, 48], F32)
            g12 = sb(st, "g12", [128, 16], F32)
            ab_t = Tk()
            featmajor_load(st, pA, pA_t, None, adab_d[0, :].rearrange("(r p) -> r p", p=128), 48, "t_ab")
            S.op("dve", lambda e: e.tensor_copy(out=abT[:], in_=pA[:, 0:48]), reads=[pA_t], writes=[ab_t])
            featmajor_load(st, pA, pA_t, None, n1_d[0, :].rearrange("(r p) -> r p", p=128), 8, "t_n1")
            S.op("dve", lambda e: e.tensor_copy(out=g12[:, 0:8], in_=pA[:, 0:8]), reads=[pA_t], writes=[ab_t])
            featmajor_load(st, pA, pA_t, None, n2_d[0, :].rearrange("(r p) -> r p", p=128), 8, "t_n2")
            S.op("dve", lambda e: e.tensor_copy(out=g12[:, 8:16], in_=pA[:, 0:8]), reads=[pA_t], writes=[ab_t])
            featmajor_load(st, pA, pA_t, None, qg_d[0, :].rearrange("(r p) -> r p", p=128), 3, "t_qg")
            S.op("dve", lambda e: e.tensor_copy(out=gq[:, 0:3], in_=pA[:, 0:3]), reads=[pA_t], writes=[gq_t])
            featmajor_load(st, pA, pA_t, None, kvg_d[0, :].rearrange("(r p) -> r p", p=128), 2, "t_kvg")
            S.op("dve", lambda e: e.tensor_copy(out=gq[:, 4:6], in_=pA[:, 0:2]), reads=[pA_t], writes=[gq_t])
            fgT = sb(st, "fgT", [128, 8], F32)
            fg_t = Tk()
            featmajor_load(st, pA, pA_t, None, fg_d.rearrange("(r p) -> r p", p=128), 8, "t_fg")
            S.op("dve", lambda e: e.tensor_copy(out=fgT[:], in_=pA[:, 0:8]), reads=[pA_t], writes=[fg_t])
            awv = adaw_d[0].rearrange("(kc p) n -> p kc n", p=128)
            aw = [sb(st, f"aw{i}", [128, 8, 512], F32) for i in range(2)]
            aw_t = [Tk(), Tk()]
            pm = ps(st, "pm", [128, 48, 4])
            pm_t = Tk()
            for blk in range(12):
                w = aw[blk % 2]
                wt = aw_t[blk % 2]
                S.dma(w[:], awv[:, :, blk * 512:(blk + 1) * 512], writes=[wt])
                for jj in range(4):
                    j = blk * 4 + jj
                    for kc in range(8):
                        S.op("pe", lambda e: e.matmul(out=pm[:, j, 0:3], lhsT=w[:, kc, jj * 128:(jj + 1) * 128],
                                                      rhs=sT[:, kc, :], start=(kc == 0), stop=(kc == 7)),
                             reads=[wt, sT_t], writes=[pm_t], signal=(kc == 7))
            for r in range(3):
                S.op("dve", lambda e: e.tensor_tensor(out=mT[:, :, r], in0=pm[:, :, r], in1=abT[:], op=ALU.add),
                     reads=[pm_t, ab_t], writes=[mT_t])
            for r in range(3):
                S.op("dve", lambda e: e.scalar_tensor_tensor(out=gs1[:, r, :], in0=mT[:, 8:16, r], scalar=1.0,
                                                             in1=g12[:, 0:8], op0=ALU.add, op1=ALU.mult),
                     reads=[mT_t, ab_t], writes=[gs_t])
                S.op("dve", lambda e: e.scalar_tensor_tensor(out=gs2[:, r, :], in0=mT[:, 32:40, r], scalar=1.0,
                                                             in1=g12[:, 8:16], op0=ALU.add, op1=ALU.mult),
                     reads=[mT_t, ab_t], writes=[gs_t])
            dg = sb(st, "dg", [128, 8, 128], F32)
            dg_t = Tk()

            def bcast_tile(dst_ap, vec_fn):
                for c in range(8):
                    S.op("dve", lambda e: e.tensor_scalar(out=dg[:, c, :], in0=identf, scalar1=vec_fn(c), scalar2=None,
                                                          op0=ALU.mult), reads=[cst_t, mT_t, fg_t], writes=[dg_t])
                for c in range(8):
                    S.op("pe", lambda e: e.matmul(out=pB[:, c // 4, (c % 4) * 128:(c % 4 + 1) * 128], lhsT=onesf,
                                                  rhs=dg[:, c, :], start=True, stop=True),
                         reads=[dg_t, cst_t], writes=[pB_t])
                S.op("act", lambda e: e.copy(out=dst_ap, in_=pB[:].rearrange("p a b -> p (a b)")),
                     reads=[pB_t], writes=[G_t])

            for b in range(NB):
                bcast_tile(G1[:, b, :], lambda c: mT[:, 16 + c, b:b + 1])
                bcast_tile(G2[:, b, :], lambda c: mT[:, 40 + c, b:b + 1])
            bcast_tile(FG[:], lambda c: fgT[:, c:c + 1])

            rd = sb(st, "rd", [1, 8], F32)
            rd_t = Tk()
            S.dma(rd[:, 0:4], rdf_d[:, :], writes=[rd_t])
            S.dma(rd[:, 4:8], rdb_d[:, :], writes=[rd_t])
            S.op("pe", lambda e: e.matmul(out=pA[:, 0:8], lhsT=cst[0:1, 1, :], rhs=rd[:], start=True, stop=True),
                 reads=[rd_t, cst_t], writes=[pA_t])
            lg = sb(st, "lg", [128, 8], F32)
            lg_t = Tk()
            S.op("act", lambda e: e.activation(out=lg[:], in_=pA[:, 0:8], func=AF.Exp, scale=-1.0),
                 reads=[pA_t], writes=[lg_t])
            S.op("act", lambda e: e.activation(out=lg[:], in_=lg[:], func=AF.Ln, bias=1.0, scale=1.0),
                 reads=[lg_t], writes=[lg_t])
            S.op("dve", lambda e: e.tensor_scalar(out=lg[:], in0=lg[:], scalar1=-1.0, scalar2=None, op0=ALU.mult),
                 reads=[lg_t], writes=[lg_t])
            tmpd = sb(st, "tmpd", [128, 128], F32)
            tmpd_t = Tk()
            for h in range(4):
                for (dst, dtab, mtab, col) in ((MTf, 2, 3, h), (MTb, 4, 5, 4 + h)):
                    S.op("act", lambda e: e.activation(out=tmpd[:], in_=cst[:, dtab, :], func=AF.Exp,
                                                       scale=lg[:, col:col + 1]),
                         reads=[cst_t, lg_t], writes=[tmpd_t])
                    S.op("dve", lambda e: e.tensor_tensor(out=dst[:, h, :], in0=tmpd[:], in1=cst[:, mtab, :],
                                                          op=ALU.mult), reads=[tmpd_t, cst_t], writes=[dec_t])
                S.op("act", lambda e: e.activation(out=QDf[:, h, :], in_=cst[:, 6, :], func=AF.Exp,
                                                   scale=lg[:, h:h + 1]), reads=[cst_t, lg_t], writes=[dec_t])
                S.op("act", lambda e: e.activation(out=QDb[:, h, :], in_=cst[:, 7, :], func=AF.Exp,
                                                   scale=lg[:, 4 + h:5 + h]), reads=[cst_t, lg_t], writes=[dec_t])
                S.op("act", lambda e: e.activation(out=KD[:, h, 0:1], in_=cst[:, 8, 0:1], func=AF.Exp,
                                                   scale=lg[:, h:h + 1]), reads=[cst_t, lg_t], writes=[dec_t])
                S.op("act", lambda e: e.activation(out=KD[:, h, 1:2], in_=cst[:, 8, 1:2], func=AF.Exp,
                                                   scale=lg[:, 4 + h:5 + h]), reads=[cst_t, lg_t], writes=[dec_t])
                S.op("act", lambda e: e.activation(out=KD[:, h, 2:3], in_=cst[:, 8, 2:3], func=AF.Exp,
                                                   scale=lg[:, h:h + 1]), reads=[cst_t, lg_t], writes=[dec_t])
                S.op("act", lambda e: e.activation(out=KD[:, h, 3:4], in_=cst[:, 8, 2:3], func=AF.Exp,
                                                   scale=lg[:, 4 + h:5 + h]), reads=[cst_t, lg_t], writes=[dec_t])
            S.barrier()

        def load_cast(stack_stage, dst, dst_t, src_ap, shape, stg, stg_t, eng, scale=None, dst_view=None):
            S.dma(stg, src_ap, writes=[stg_t])
            dv = dst if dst_view is None else dst_view
            if scale is None:
                S.op(eng, lambda e: e.tensor_copy(out=dv, in_=stg), reads=[stg_t], writes=[dst_t])
            else:
                S.op(eng, lambda e: e.tensor_scalar(out=dv, in0=stg, scalar1=scale, scalar2=None, op0=ALU.mult),
                     reads=[stg_t], writes=[dst_t])

        def norm_T(stack, pfx, src_tile, src_t, gs_ap, sh_ap, pT, pT_t, hTo, hTo_t, scr, ssb, tmp_t, xn, xn_t,
                   f32_out=None, f32_t=None):
            S.op("pool", lambda e: e.memset(ssb[:, 0:1], 0.0), writes=[tmp_t])
            S.op("act", lambda e: e.activation(out=scr[:], in_=src_tile, func=AF.Square, accum_out=ssb[:, 0:1]),
                 reads=[src_t, tmp_t], writes=[tmp_t])
            S.op("act", lambda e: e.activation(out=ssb[:, 1:2], in_=ssb[:, 0:1], func=AF.Sqrt, scale=1.0 / D, bias=EPS),
                 reads=[tmp_t], writes=[tmp_t])
            S.op("dve", lambda e: e.reciprocal(out=ssb[:, 2:3], in_=ssb[:, 1:2]), reads=[tmp_t], writes=[tmp_t])
            S.op("dve", lambda e: e.tensor_scalar(out=xn[:], in0=src_tile, scalar1=ssb[:, 2:3], scalar2=None,
                                                  op0=ALU.mult), reads=[src_t, tmp_t], writes=[xn_t])
            for c in range(8):
                S.op("pe", lambda e: e.transpose(out=pT[c // 4][:, (c % 4) * 128:(c % 4 + 1) * 128],
                                                 in_=xn[:, c * 128:(c + 1) * 128], identity=identf),
                     reads=[xn_t, cst_t], writes=[pT_t[c // 4]], signal=(c % 4 == 3))
            for c in range(8):
                src = pT[c // 4][:, (c % 4) * 128:(c % 4 + 1) * 128]
                if f32_out is not None:
                    S.op("dve", lambda e: e.tensor_scalar(out=f32_out[:, c, :], in0=src, scalar1=gs_ap[:, c:c + 1],
                                                          scalar2=sh_ap(c), op0=ALU.mult, op1=ALU.add),
                         reads=[pT_t[c // 4], gs_t, mT_t], writes=[f32_t])
                    S.op("pool", lambda e: e.tensor_copy(out=hTo[:, c, :], in_=f32_out[:, c, :]),
                         reads=[f32_t], writes=[hTo_t])
                else:
                    S.op("dve", lambda e: e.tensor_scalar(out=hTo[:, c, :], in0=src, scalar1=gs_ap[:, c:c + 1],
                                                          scalar2=sh_ap(c), op0=ALU.mult, op1=ALU.add),
                         reads=[pT_t[c // 4], gs_t, mT_t], writes=[hTo_t])

        winv = win_d[0].rearrange("(kc p) n -> p kc n", p=128)

        for b in range(NB):
            with contextlib.ExitStack() as st:
                xt = [sb(st, f"s0x{i}", [128, D], F32) for i in range(2)]
                xt_t = [Tk(), Tk()]
                xn = sb(st, "s0xn", [128, D], F32)
                xn_t = Tk()
                scr = sb(st, "s0scr", [128, D], BF16)
                ssb = sb(st, "s0ss", [128, 4], F32)
                tmp_t = Tk()
                hTo = [sb(st, f"s0h{i}", [128, 8, 128], BF16) for i in range(2)]
                hTo_t = [Tk(), Tk()]
                pT = [ps(st, f"s0p{i}", [128, 512]) for i in range(2)]
                pT_t = [Tk(), Tk()]

                def s0_load(t):
                    src = ctx_d[b, t * 128:(t + 1) * 128, :] if t < 2 else x_d[b, (t - 2) * 128:(t - 1) * 128, :]
                    S.dma(xt[t % 2][:], src, writes=[xt_t[t % 2]])

                s0_load(0)
                for t in range(NT):
                    if t + 1 < NT:
                        s0_load(t + 1)
                    r = 2 if t < 2 else b
                    norm_T(st, "s0", xt[t % 2][:], xt_t[t % 2], gs1[:, r, :], lambda c: mT[:, c, r:r + 1],
                           pT, pT_t, hTo[t % 2], hTo_t[t % 2], scr, ssb, tmp_t, xn, xn_t)
                    S.dma(hT_d[t], hTo[t % 2][:], reads=[hTo_t[t % 2]], writes=[hT_t[t]])
                S.barrier()

            with contextlib.ExitStack() as st:
                cqn = sb(st, "cqn", [128, 3, L], BF16)
                cqn_t = Tk()
                ckvn = sb(st, "ckvn", [128, 2, LT], BF16)
                ckvn_t = Tk()
                krT = sb(st, "krT", [64, LT], BF16)
                krT_t = Tk()
                with contextlib.ExitStack() as s1:
                    Wm = sb(s1, "Wm", [128, 8, 768], BF16)
                    Wm_t = Tk()
                    stg = sb(s1, "s1stg", [128, 8, 704], F32)
                    stg_t = Tk()
                    S.dma(stg[:], winv[:, :, 6144:6848], writes=[stg_t])
                    S.op("dve", lambda e: e.tensor_copy(out=Wm[:, :, 0:704], in_=stg[:]), reads=[stg_t], writes=[Wm_t])
                    for (dst, src) in ((704, 656), (720, 640), (736, 688), (752, 672)):
                        S.op("dve", lambda e: e.tensor_copy(out=Wm[:, :, dst:dst + 16], in_=stg[:, :, src:src + 16]),
                             reads=[stg_t], writes=[Wm_t])
                    hb = [sb(s1, f"s1h{i}", [128, 8, 512], BF16) for i in range(2)]
                    hb_t = [Tk(), Tk()]
                    tc_ = [sb(s1, f"s1tc{i}", [64, 2, 512], F32) for i in range(2)]
                    tc_t = [Tk(), Tk()]
                    pq = [ps(s1, f"s1pq{i}", [128, 512]) for i in range(3)]
                    pq_t = [Tk() for _ in range(3)]
                    pk = [ps(s1, f"s1pk{i}", [128, 512]) for i in range(2)]
                    pk_t = [Tk() for _ in range(2)]
                    pr = [ps(s1, f"s1pr{i}", [128, 512]) for i in range(2)]
                    pr_t = [Tk() for _ in range(2)]
                    pss = ps(s1, "s1pss", [128, 512])
                    pss_t = Tk()
                    xf = sb(s1, "s1xf", [128, 3, 512], F32)
                    xf_t = Tk()
                    sq = sb(s1, "s1sq", [128, 3, 512], F32)
                    sq_t = Tk()
                    rstd = sb(s1, "s1rstd", [128, 512], F32)
                    rstd_t = Tk()
                    r1 = sb(s1, "s1r1", [64, 512], F32)
                    r2 = sb(s1, "s1r2", [64, 512], F32)
                    r_t = Tk()

                    def blk_range(j):
                        return (0, 256) if j == 0 else (256 + (j - 1) * 512, 512)

                    def s1_load(j):
                        t0, n = blk_range(j)
                        for i in range(n // 128):
                            S.dma(hb[j % 2][:, :, i * 128:(i + 1) * 128], hT_d[t0 // 128 + i],
                                  reads=[hT_t[t0 // 128 + i]], writes=[hb_t[j % 2]])
                        S.dma(tc_[j % 2][:, 0, 0:n], mc_d[:, t0:t0 + n], writes=[tc_t[j % 2]])
                        S.dma(tc_[j % 2][:, 1, 0:n], ms_d[:, t0:t0 + n], writes=[tc_t[j % 2]])

                    def rms_T(pl, pl_t, nch, rank, gcol, dst, dst_t, dcol0, n):
                        for c in range(nch):
                            S.op("act", lambda e: e.copy(out=xf[:, c, 0:n], in_=pl[c][:, 0:n]),
                                 reads=[pl_t[c]], writes=[xf_t])
                            S.op("act", lambda e: e.activation(out=sq[:, c, 0:n], in_=pl[c][:, 0:n], func=AF.Square),
                                 reads=[pl_t[c]], writes=[sq_t])
                        for c in range(nch):
                            S.op("pe", lambda e: e.matmul(out=pss[:, 0:n], lhsT=onesf, rhs=sq[:, c, 0:n],
                                                          start=(c == 0), stop=(c == nch - 1)),
                                 reads=[sq_t, cst_t], writes=[pss_t], signal=(c == nch - 1))
                        S.op("act", lambda e: e.activation(out=rstd[:, 0:n], in_=pss[:, 0:n], func=AF.Sqrt,
                                                           scale=1.0 / rank, bias=EPS), reads=[pss_t], writes=[rstd_t])
                        S.op("dve", lambda e: e.reciprocal(out=rstd[:, 0:n], in_=rstd[:, 0:n]),
                             reads=[rstd_t], writes=[rstd_t])
                        for c in range(nch):
                            S.op("dve", lambda e: e.scalar_tensor_tensor(
                                out=dst[:, c, dcol0:dcol0 + n], in0=xf[:, c, 0:n], scalar=gq[:, gcol + c:gcol + c + 1],
                                in1=rstd[:, 0:n], op0=ALU.mult, op1=ALU.mult),
                                 reads=[xf_t, rstd_t, gq_t], writes=[dst_t])

                    s1_load(0)
                    for j in range(9):
                        if j + 1 < 9:
                            s1_load(j + 1)
                        t0, n = blk_range(j)
                        h_, h_t = hb[j % 2], hb_t[j % 2]
                        if j > 0:
                            for c in range(3):
                                for kc in range(8):
                                    S.op("pe", lambda e: e.matmul(out=pq[c][:, 0:n], lhsT=Wm[:, kc, c * 128:(c + 1) * 128],
                                                                  rhs=h_[:, kc, 0:n], start=(kc == 0), stop=(kc == 7)),
                                         reads=[Wm_t, h_t], writes=[pq_t[c]], signal=(kc == 7))
                        for c in range(2):
                            for kc in range(8):
                                S.op("pe", lambda e: e.matmul(out=pk[c][:, 0:n], lhsT=Wm[:, kc, 384 + c * 128:384 + (c + 1) * 128],
                                                              rhs=h_[:, kc, 0:n], start=(kc == 0), stop=(kc == 7)),
                                     reads=[Wm_t, h_t], writes=[pk_t[c]], signal=(kc == 7))
                        for c in range(2):
                            for kc in range(8):
                                S.op("pe", lambda e: e.matmul(out=pr[c][0:64, 0:n], lhsT=Wm[:, kc, 640 + c * 64:704 + c * 64],
                                                              rhs=h_[:, kc, 0:n], start=(kc == 0), stop=(kc == 7)),
                                     reads=[Wm_t, h_t], writes=[pr_t[c]], signal=(kc == 7))
                        if j > 0:
                            rms_T(pq, pq_t, 3, 384, 0, cqn, cqn_t, t0 - 256, n)
                        rms_T(pk, pk_t, 2, 256, 4, ckvn, ckvn_t, t0, n)
                        tcc, tcc_t = tc_[j % 2], tc_t[j % 2]
                        S.op("dve", lambda e: e.tensor_tensor(out=r1[:, 0:n], in0=pr[0][0:64, 0:n], in1=tcc[:, 0, 0:n],
                                                              op=ALU.mult), reads=[pr_t[0], tcc_t], writes=[r_t])
                        S.op("dve", lambda e: e.tensor_tensor(out=r2[:, 0:n], in0=pr[1][0:64, 0:n], in1=tcc[:, 1, 0:n],
                                                              op=ALU.mult), reads=[pr_t[1], tcc_t], writes=[r_t])
                        S.op("dve", lambda e: e.tensor_tensor(out=krT[:, t0:t0 + n], in0=r1[:, 0:n], in1=r2[:, 0:n],
                                                              op=ALU.add), reads=[r_t], writes=[krT_t])
                    S.barrier()

                with contextlib.ExitStack() as s2:
                    KT = sb(s2, "KT", [128, LT], BF16)
                    KT_t = Tk()
                    V = sb(s2, "V", [128, NT, 128], BF16)
                    V_t = Tk()
                    QT = sb(s2, "QT", [128, L], BF16)
                    QT_t = Tk()
                    QrT = sb(s2, "QrT", [64, L], BF16)
                    QrT_t = Tk()
                    Wq = sb(s2, "Wq", [128, 3, 256], BF16)
                    Wq_t = Tk()
                    Wkv = sb(s2, "Wkv", [128, 2, 256], BF16)
                    Wkv_t = Tk()
                    sq_ = sb(s2, "s2sq", [128, 3, 192], F32)
                    sq_t = Tk()
                    skv = sb(s2, "s2skv", [128, 2, 256], F32)
                    skv_t = Tk()
                    tq = [sb(s2, f"s2tq{i}", [64, 2, 512], F32) for i in range(2)]
                    tq_t = [Tk(), Tk()]
                    r1 = sb(s2, "s2r1", [64, 512], F32)
                    r2 = sb(s2, "s2r2", [64, 512], F32)
                    r_t = Tk()
                    PT = [sb(s2, f"PT{i}", [128, 512], BF16) for i in range(4)]
                    PT_t = [Tk() for _ in range(4)]
                    accA = [sb(s2, f"s2accA{i}", [128, 512], F32) for i in range(2)]
                    accB = [sb(s2, f"s2accB{i}", [128, 512], F32) for i in range(2)]
                    accA_t = [Tk(), Tk()]
                    accB_t = [Tk(), Tk()]
                    rs = sb(s2, "s2rs", [128, 512], F32)
                    rs_t = Tk()
                    ot = [sb(s2, f"s2ot{i}", [128, 512], BF16) for i in range(2)]
                    ot_t = [Tk(), Tk()]
                    pS = [ps(s2, f"pS{i}", [128, 512]) for i in range(3)]
                    pS_t = [Tk() for _ in range(3)]
                    pO = [ps(s2, f"pO{i}", [128, 512]) for i in range(2)]
                    pO_t = [Tk() for _ in range(2)]
                    pZ = [ps(s2, f"pZ{i}", [128, 512]) for i in range(2)]
                    pZ_t = [Tk() for _ in range(2)]
                    pX = ps(s2, "pX", [128, 512])
                    pX_t = Tk()
                    wuqv = wuq_d[0].rearrange("(kc p) n -> p kc n", p=128)
                    wukvv = wukv_d[0].rearrange("(kc p) n -> p kc n", p=128)
                    sc = 192.0 ** -0.5
                    cnt = 0
                    for h in range(8):
                        S.dma(sq_[:], wuqv[:, :, h * 192:(h + 1) * 192], writes=[sq_t])
                        S.op("pool", lambda e: e.tensor_copy(out=Wq[:, :, 0:192], in_=sq_[:]), reads=[sq_t], writes=[Wq_t])
                        for (dst, src) in ((192, 144), (208, 128), (224, 176), (240, 160)):
                            S.op("pool", lambda e: e.tensor_copy(out=Wq[:, :, dst:dst + 16], in_=sq_[:, :, src:src + 16]),
                                 reads=[sq_t], writes=[Wq_t])
                        S.dma(skv[:], wukvv[:, :, h * 256:(h + 1) * 256], writes=[skv_t])
                        S.op("pool", lambda e: e.tensor_copy(out=Wkv[:], in_=skv[:]), reads=[skv_t], writes=[Wkv_t])
                        for j in range(9):
                            t0, n = (0, 256) if j == 0 else (256 + (j - 1) * 512, 512)
                            for kc in range(2):
                                S.op("pe", lambda e: e.matmul(out=pX[:, 0:n], lhsT=Wkv[:, kc, 0:128], rhs=ckvn[:, kc, t0:t0 + n],
                                                              start=(kc == 0), stop=(kc == 1)),
                                     reads=[Wkv_t, ckvn_t], writes=[pX_t], signal=(kc == 1))
                            S.op("act" if j % 2 else "dve", (lambda e: e.copy(out=KT[:, t0:t0 + n], in_=pX[:, 0:n])) if j % 2
                                 else (lambda e: e.tensor_copy(out=KT[:, t0:t0 + n], in_=pX[:, 0:n])),
                                 reads=[pX_t], writes=[KT_t])
                        for g in range(9):
                            tiles = list(range(g * 4, min(g * 4 + 4, NT)))
                            for i, t in enumerate(tiles):
                                for kc in range(2):
                                    S.op("pe", lambda e: e.matmul(out=pX[:, i * 128:(i + 1) * 128],
                                                                  lhsT=ckvn[:, kc, t * 128:(t + 1) * 128], rhs=Wkv[:, kc, 128:256],
                                                                  start=(kc == 0), stop=(kc == 1)),
                                         reads=[Wkv_t, ckvn_t], writes=[pX_t], signal=(kc == 1 and i == len(tiles) - 1))
                            nn = len(tiles)
                            S.op("act" if g % 2 else "dve",
                                 (lambda e: e.copy(out=V[:, tiles[0]:tiles[0] + nn, :].rearrange("p a b -> p (a b)"),
                                                   in_=pX[:, 0:nn * 128])) if g % 2 else
                                 (lambda e: e.tensor_copy(out=V[:, tiles[0]:tiles[0] + nn, :].rearrange("p a b -> p (a b)"),
                                                          in_=pX[:, 0:nn * 128])),
                                 reads=[pX_t], writes=[V_t])
                        for j in range(8):
                            q0 = j * 512
                            S.dma(tq[j % 2][:, 0, :], mc_d[:, 256 + q0:256 + q0 + 512], writes=[tq_t[j % 2]])
                            S.dma(tq[j % 2][:, 1, :], ms_d[:, 256 + q0:256 + q0 + 512], writes=[tq_t[j % 2]])
                            for kc in range(3):
                                S.op("pe", lambda e: e.matmul(out=pX[:], lhsT=Wq[:, kc, 0:128], rhs=cqn[:, kc, q0:q0 + 512],
                                                              start=(kc == 0), stop=(kc == 2)),
                                     reads=[Wq_t, cqn_t], writes=[pX_t], signal=(kc == 2))
                            S.op("act", lambda e: e.copy(out=QT[:, q0:q0 + 512], in_=pX[:]), reads=[pX_t], writes=[QT_t])
                            for c in range(2):
                                for kc in range(3):
                                    S.op("pe", lambda e: e.matmul(out=pZ[c][0:64, :], lhsT=Wq[:, kc, 128 + c * 64:192 + c * 64],
                                                                  rhs=cqn[:, kc, q0:q0 + 512], start=(kc == 0), stop=(kc == 2)),
                                         reads=[Wq_t, cqn_t], writes=[pZ_t[c]], signal=(kc == 2))
                            tt_, tt_t = tq[j % 2], tq_t[j % 2]
                            S.op("dve", lambda e: e.tensor_tensor(out=r1[:], in0=pZ[0][0:64, :], in1=tt_[:, 0, :], op=ALU.mult),
                                 reads=[pZ_t[0], tt_t], writes=[r_t])
                            S.op("dve", lambda e: e.tensor_tensor(out=r2[:], in0=pZ[1][0:64, :], in1=tt_[:, 1, :], op=ALU.mult),
                                 reads=[pZ_t[1], tt_t], writes=[r_t])
                            S.op("dve", lambda e: e.tensor_tensor(out=QrT[:, q0:q0 + 512], in0=r1[:], in1=r2[:], op=ALU.add),
                                 reads=[r_t], writes=[QrT_t])
                        for qb in range(8):
                            q0 = qb * 512
                            po, po_t = pO[qb % 2], pO_t[qb % 2]
                            pz, pz_t = pZ[qb % 2], pZ_t[qb % 2]
                            def emit_st(kt, cn):
                                s_, s_t = pS[cn % 3], pS_t[cn % 3]
                                S.op("pe", lambda e: e.matmul(out=s_[:], lhsT=KT[:, kt * 128:(kt + 1) * 128], rhs=QT[:, q0:q0 + 512],
                                                              start=True, stop=False), reads=[KT_t, QT_t], writes=[s_t], signal=False)
                                S.op("pe", lambda e: e.matmul(out=s_[:], lhsT=krT[:, kt * 128:(kt + 1) * 128], rhs=QrT[:, q0:q0 + 512],
                                                              start=False, stop=True), reads=[krT_t, QrT_t], writes=[s_t])

                            emit_st(0, cnt)
                            for kt in range(NT):
                                s_, s_t = pS[cnt % 3], pS_t[cnt % 3]
                                p_, p_t = PT[cnt % 4], PT_t[cnt % 4]
                                if kt + 1 < NT:
                                    emit_st(kt + 1, cnt + 1)
                                cnt += 1
                                S.op("act", lambda e: e.activation(out=p_[:], in_=s_[:], func=AF.Exp, scale=sc),
                                     reads=[s_t], writes=[p_t])
                                S.op("pe", lambda e: e.matmul(out=po[:], lhsT=V[:, kt, :], rhs=p_[:], start=(kt == 0),
                                                              stop=(kt == NT - 1)), reads=[V_t, p_t], writes=[po_t])
                                aa, aa_t = accA[qb % 2], accA_t[qb % 2]
                                if kt == 0:
                                    S.op("dve", lambda e: e.tensor_copy(out=aa[:], in_=p_[:]), reads=[p_t], writes=[aa_t])
                                else:
                                    S.op("dve", lambda e: e.tensor_tensor(out=aa[:], in0=aa[:], in1=p_[:], op=ALU.add),
                                         reads=[p_t, aa_t], writes=[aa_t])
                            S.op("pe", lambda e: e.matmul(out=pz[:], lhsT=onesf, rhs=accA[qb % 2][:], start=True, stop=True),
                                 reads=[cst_t, accA_t[qb % 2]], writes=[pz_t])
                            S.op("dve", lambda e: e.reciprocal(out=rs[:], in_=pz[:]), reads=[pz_t], writes=[rs_t])
                            o_, o_t = ot[qb % 2], ot_t[qb % 2]
                            S.op("dve", lambda e: e.tensor_tensor(out=o_[:], in0=po[:], in1=rs[:], op=ALU.mult),
                                 reads=[po_t, rs_t], writes=[o_t])
                            S.dma(att_d[h, :, q0:q0 + 512], o_[:], reads=[o_t], writes=[att_t[qb]])
                    S.barrier()

            with contextlib.ExitStack() as st:
                Wqk = sb(st, "Wqk", [128, 8, 512], BF16)
                Wv = sb(st, "Wv", [128, 8, 512], BF16)
                Wg = sb(st, "Wg", [128, 8, 512], BF16)
                Wqk_t, Wv_t, Wg_t = Tk(), Tk(), Tk()
                stg = sb(st, "s3stg", [128, 8, 512], F32)
                stg_t = Tk()
                hb = [sb(st, f"s3h{i}", [128, 8, 128], BF16) for i in range(3)]
                hb_t = [Tk() for _ in range(3)]
                tb = [sb(st, f"s3tb{i}", [128, 2, 256], F32) for i in range(3)]
                tb_t = [Tk() for _ in range(3)]
                ofb = [sb(st, f"s3of{i}", [128, 512], F32) for i in range(3)]
                ofb_t = [Tk() for _ in range(3)]
                Sf = sb(st, "Sf", [128, 2, 512], F32)
                Sb_ = sb(st, "Sb", [128, 2, 512], BF16)
                Sf_t = Tk()
                Sb_t = Tk()
                t1 = sb(st, "s3t1", [128, 512], F32)
                t2 = sb(st, "s3t2", [128, 512], F32)
                t12_t = Tk()
                qk_all = sb(st, "s3qkall", [128, NT, 512], BF16)
                qka_t = [Tk() for _ in range(NT)]
                V_all = sb(st, "s3Vall", [128, NT, 512], BF16)
                Va_t = [Tk() for _ in range(NT)]
                Kdb = [sb(st, f"s3Kd{i}", [128, 256], BF16) for i in range(2)]
                Kdb_t = [Tk(), Tk()]
                KTt = sb(st, "s3KT", [128, 2, 128], BF16)
                QTt = sb(st, "s3QT", [128, 2, 128], BF16)
                QdT = sb(st, "s3QdT", [128, 2, 128], BF16)
                tr_t = Tk()
                STm = sb(st, "s3STm", [128, 128], BF16)
                STm_t = Tk()
                osb = sb(st, "s3o", [128, 512], F32)
                osb_t = Tk()
                sg = sb(st, "s3sg", [128, 512], F32)
                sg_t = Tk()
                scr = sb(st, "s3scr", [128, 512], BF16)
                ssb = sb(st, "s3ss", [128, 4], F32)
                ss_t = Tk()
                rr = sb(st, "s3r", [128, 512], BF16)
                rr_t = Tk()
                rTs = [sb(st, f"s3rT{i}", [128, 4, 128], BF16) for i in range(2)]
                rTs_t = [Tk(), Tk()]
                pAl = [ps(st, f"s3pA{i}", [128, 512]) for i in range(2)]
                pAl_t = [Tk(), Tk()]
                pBl = [ps(st, f"s3pB{i}", [128, 512]) for i in range(2)]
                pBl_t = [Tk(), Tk()]
                pC = ps(st, "s3pC", [128, 512])
                pD = ps(st, "s3pD", [128, 2, 512], BF16)
                pE = ps(st, "s3pE", [128, 512])
                pF = ps(st, "s3pF", [128, 512])
                pC_t, pD_t, pD2_t, pE_t, pF_t = (Tk() for _ in range(5))
                pH = [pC, pE]
                pH_t = [pC_t, pE_t]
                for h in range(4):
                    for (dst, c0, scl) in ((Wqk[:, :, 0:256], h * 256, None), (Wqk[:, :, 256:512], 1024 + h * 256, 0.0625)):
                        S.dma(stg[:, :, 0:256], winv[:, :, c0:c0 + 256], writes=[stg_t])
                        if scl is None:
                            S.op("act", lambda e: e.copy(out=dst, in_=stg[:, :, 0:256]), reads=[stg_t], writes=[Wqk_t])
                        else:
                            S.op("act", lambda e: e.mul(out=dst, in_=stg[:, :, 0:256], mul=scl), reads=[stg_t], writes=[Wqk_t])
                    for (dst, c0, wt_) in ((Wv, 2048 + h * 512, Wv_t), (Wg, 4096 + h * 512, Wg_t)):
                        S.dma(stg[:], winv[:, :, c0:c0 + 512], writes=[stg_t])
                        S.op("act", lambda e: e.copy(out=dst[:], in_=stg[:]), reads=[stg_t], writes=[wt_])
                    for di, dirn in enumerate(("f", "b")):
                        order = list(range(NT)) if dirn == "f" else [1, 0] + list(range(NT - 1, 1, -1))
                        MT = MTf if dirn == "f" else MTb
                        QD = QDf if dirn == "f" else QDb
                        kdc = KD[:, h, di:di + 1]
                        cdc = KD[:, h, 2 + di:3 + di]
                        S.op("pool", lambda e: e.memset(Sf[:], 0.0), writes=[Sf_t])
                        S.op("pool", lambda e: e.memset(Sb_[:], 0.0), writes=[Sb_t])

                        def s3_load(idx):
                            t = order[idx]
                            S.dma(hb[idx % 3][:], hT_d[t], reads=[hT_t[t]], writes=[hb_t[idx % 3]])
                            if dirn == "f":
                                S.dma(tb[idx % 3][:, 0, :], rc_d[t * 128:(t + 1) * 128, :], writes=[tb_t[idx % 3]])
                                S.dma(tb[idx % 3][:, 1, :], rs_d[t * 128:(t + 1) * 128, :], writes=[tb_t[idx % 3]])
                            if dirn == "b" and t >= 2:
                                S.dma(ofb[idx % 3][:], of_d[t - 2], reads=[of_t[t - 2]], writes=[ofb_t[idx % 3]])

                        def s3_proj(idx):
                            if dirn == "b":
                                return
                            h_, h_t = hb[idx % 3], hb_t[idx % 3]
                            pA, pA_t = pAl[idx % 2], pAl_t[idx % 2]
                            pB, pB_t = pBl[idx % 2], pBl_t[idx % 2]
                            for kc in range(8):
                                S.op("pe", lambda e: e.matmul(out=pA[:], lhsT=h_[:, kc, :], rhs=Wqk[:, kc, :], start=(kc == 0),
                                                              stop=(kc == 7)), reads=[h_t, Wqk_t], writes=[pA_t], signal=(kc == 7))
                            for kc in range(8):
                                S.op("pe", lambda e: e.matmul(out=pB[:], lhsT=h_[:, kc, :], rhs=Wv[:, kc, :], start=(kc == 0),
                                                              stop=(kc == 7)), reads=[h_t, Wv_t], writes=[pB_t], signal=(kc == 7))

                        def s3_A(idx):
                            t = order[idx]
                            tb_, tbt = tb[idx % 3], tb_t[idx % 3]
                            pA, pA_t = pAl[idx % 2], pAl_t[idx % 2]
                            pB, pB_t = pBl[idx % 2], pBl_t[idx % 2]
                            qk, qk_t = qk_all[:, t, :], qka_t[t]
                            Vb, Vb_t = V_all[:, t, :], Va_t[t]
                            Kd, Kd_t = Kdb[idx % 2], Kdb_t[idx % 2]
                            for half in (range(2) if dirn == "f" else ()):
                                o = half * 256
                                S.op("dve", lambda e: e.tensor_tensor(out=t1[:, o:o + 256], in0=pA[:, o:o + 256], in1=tb_[:, 0, :],
                                                                      op=ALU.mult), reads=[pA_t, tbt], writes=[t12_t])
                                for part in range(2):
                                    a = o + part * 128
                                    S.op("dve", lambda e: e.tensor_tensor(out=t2[:, a:a + 64], in0=pA[:, a + 64:a + 128],
                                                                          in1=tb_[:, 1, part * 128:part * 128 + 64], op=ALU.mult),
                                         reads=[pA_t, tbt], writes=[t12_t])
                                    S.op("dve", lambda e: e.tensor_tensor(out=t2[:, a + 64:a + 128], in0=pA[:, a:a + 64],
                                                                          in1=tb_[:, 1, part * 128 + 64:part * 128 + 128], op=ALU.mult),
                                         reads=[pA_t, tbt], writes=[t12_t])
                            if dirn == "f":
                                S.op("pool", lambda e: e.tensor_tensor(out=qk[:], in0=t1[:], in1=t2[:], op=ALU.add),
                                     reads=[t12_t], writes=[qk_t])
                                S.op("act", lambda e: e.copy(out=Vb[:], in_=pB[:]), reads=[pB_t], writes=[Vb_t])
                            S.op("pool", lambda e: e.tensor_scalar(out=Kd[:], in0=qk[:, 256:512], scalar1=kdc, scalar2=None, op0=ALU.mult),
                                 reads=[qk_t, dec_t], writes=[Kd_t])

                        s3_load(0)
                        s3_load(1)
                        s3_proj(0)
                        s3_A(0)
                        for idx in range(NT):
                            if idx + 2 < NT:
                                s3_load(idx + 2)
                            if idx + 1 < NT:
                                s3_proj(idx + 1)
                                s3_A(idx + 1)
                            t = order[idx]
                            lat = t >= 2
                            h_, h_t = hb[idx % 3], hb_t[idx % 3]
                            tb_, tbt = tb[idx % 3], tb_t[idx % 3]
                            pA, pA_t = pAl[idx % 2], pAl_t[idx % 2]
                            pB, pB_t = pBl[idx % 2], pBl_t[idx % 2]
                            qk, qk_t = qk_all[:, t, :], qka_t[t]
                            Vb, Vb_t = V_all[:, t, :], Va_t[t]
                            Kd, Kd_t = Kdb[idx % 2], Kdb_t[idx % 2]
                            if lat and dirn == "b":
                                for kc in range(8):
                                    S.op("pe", lambda e: e.matmul(out=pC[:], lhsT=h_[:, kc, :], rhs=Wg[:, kc, :], start=(kc == 0),
                                                                  stop=(kc == 7)), reads=[h_t, Wg_t], writes=[pC_t], signal=(kc == 7))
                                S.op("act", lambda e: e.activation(out=sg[:], in_=pC[:], func=AF.Silu), reads=[pC_t], writes=[sg_t])
                            if lat:
                                for c in range(4):
                                    S.op("pe", lambda e: e.transpose(out=pD[:, 0, c * 128:(c + 1) * 128], in_=qk[:, c * 128:(c + 1) * 128],
                                                                     identity=identb[:]), reads=[qk_t, cb_t], writes=[pD_t], signal=(c == 3))
                                S.op("act", lambda e: e.copy(out=KTt[:].rearrange("p a b -> p (a b)"), in_=pD[:, 0, 256:512]),
                                     reads=[pD_t], writes=[tr_t])
                                S.op("act", lambda e: e.copy(out=QTt[:].rearrange("p a b -> p (a b)"), in_=pD[:, 0, 0:256]),
                                     reads=[pD_t], writes=[tr_t])
                                for dc in range(2):
                                    S.op("dve", lambda e: e.tensor_tensor(out=QdT[:, dc, :], in0=pD[:, 0, dc * 128:(dc + 1) * 128],
                                                                          in1=QD[:, h, :], op=ALU.mult), reads=[pD_t, dec_t], writes=[tr_t])
                                for dc in range(2):
                                    S.op("pe", lambda e: e.matmul(out=pE[:, 0:128], lhsT=KTt[:, dc, :], rhs=QTt[:, dc, :], start=(dc == 0),
                                                                  stop=(dc == 1)), reads=[tr_t], writes=[pE_t], signal=(dc == 1))
                                S.op("dve", lambda e: e.tensor_tensor(out=STm[:], in0=pE[:, 0:128], in1=MT[:, h, :], op=ALU.mult),
                                     reads=[pE_t, dec_t], writes=[STm_t])
                                S.op("pe", lambda e: e.matmul(out=pF[:], lhsT=STm[:], rhs=Vb[:], start=True, stop=False),
                                     reads=[STm_t, Vb_t], writes=[pF_t], signal=False)
                                for dc in range(2):
                                    S.op("pe", lambda e: e.matmul(out=pF[:], lhsT=QdT[:, dc, :], rhs=Sb_[:, dc, :], start=False,
                                                                  stop=(dc == 1)), reads=[tr_t, Sb_t], writes=[pF_t], signal=(dc == 1))
                            for dc in range(2):
                                S.op("pe", lambda e: e.matmul(out=pH[dc][:], lhsT=Kd[:, dc * 128:(dc + 1) * 128], rhs=Vb[:], start=True,
                                                              stop=True), reads=[Kd_t, Vb_t], writes=[pH_t[dc]])
                            if lat:
                                if dirn == "f":
                                    S.op("act", lambda e: e.copy(out=osb[:], in_=pF[:]), reads=[pF_t], writes=[osb_t])
                                    S.dma(of_d[t - 2], osb[:], reads=[osb_t], writes=[of_t[t - 2]])
                                else:
                                    of_, of_tt = ofb[idx % 3], ofb_t[idx % 3]
                                    S.op("dve", lambda e: e.tensor_tensor(out=osb[:], in0=pF[:], in1=of_[:], op=ALU.add),
                                         reads=[pF_t, of_tt], writes=[osb_t])
                                    S.op("pool", lambda e: e.memset(ssb[:, 0:1], 0.0), writes=[ss_t])
                                    S.op("act", lambda e: e.activation(out=scr[:], in_=osb[:], func=AF.Square, accum_out=ssb[:, 0:1]),
                                         reads=[osb_t, ss_t], writes=[ss_t])
                                    S.op("act", lambda e: e.activation(out=ssb[:, 1:2], in_=ssb[:, 0:1], func=AF.Sqrt, scale=1.0 / 512,
                                                                       bias=EPS), reads=[ss_t], writes=[ss_t])
                                    S.op("dve", lambda e: e.reciprocal(out=ssb[:, 2:3], in_=ssb[:, 1:2]), reads=[ss_t], writes=[ss_t])
                                    S.op("dve", lambda e: e.scalar_tensor_tensor(out=rr[:], in0=osb[:], scalar=ssb[:, 2:3], in1=sg[:],
                                                                                 op0=ALU.mult, op1=ALU.mult),
                                         reads=[osb_t, ss_t, sg_t], writes=[rr_t])
                                    for c in range(4):
                                        S.op("pe", lambda e: e.transpose(out=pD[:, 1, c * 128:(c + 1) * 128], in_=rr[:, c * 128:(c + 1) * 128],
                                                                         identity=identb[:]), reads=[rr_t, cb_t], writes=[pD2_t], signal=(c == 3))
                                    rT_, rT_tt = rTs[idx % 2], rTs_t[idx % 2]
                                    S.op("act", lambda e: e.copy(out=rT_[:].rearrange("p a b -> p (a b)"), in_=pD[:, 1, :]),
                                         reads=[pD2_t], writes=[rT_tt])
                                    tk0 = (t - 2) * 128
                                    S.dma(rT_d[h, :, :, tk0:tk0 + 128].rearrange("c p t -> p c t"), rT_[:], reads=[rT_tt],
                                          writes=[rT_t[(t - 2) // 4]])
                            for dc in range(2):
                                S.op("dve", lambda e: e.scalar_tensor_tensor(out=Sb_[:, dc, :], in0=Sf[:, dc, :], scalar=cdc, in1=pH[dc][:],
                                                                             op0=ALU.mult, op1=ALU.add),
                                     reads=[Sf_t, pH_t[dc], dec_t], writes=[Sb_t])
                                S.op("dve", lambda e: e.scalar_tensor_tensor(out=Sf[:, dc, :], in0=Sf[:, dc, :], scalar=cdc, in1=pH[dc][:],
                                                                             op0=ALU.mult, op1=ALU.add),
                                     reads=[Sf_t, pH_t[dc], dec_t], writes=[Sf_t])
                S.barrier()

            with contextlib.ExitStack() as st:
                Wgr = sb(st, "Wgr", [128, 8, 1024], BF16)
                Wgm = sb(st, "Wgm", [128, 8, 1024], BF16)
                Wbr = sb(st, "Wbr", [128, 16, 1024], BF16)
                Wbm = sb(st, "Wbm", [128, 8, 1024], BF16)
                Wo = sb(st, "Wo", [128, 8, 1024], BF16)
                stg = [sb(st, f"s4stg{i}", [128, 8, 256], F32) for i in range(2)]
                stg_t = [Tk(), Tk()]
                wbrv = wbr_d[0].rearrange("(kc p) n -> p kc n", p=128)
                wbmv = wbm_d[0].rearrange("(kc p) n -> p kc n", p=128)
                wov = wo_d[0].rearrange("(kc p) n -> p kc n", p=128)
                k = 0
                jobs = []
                W4_t = [Tk() for _ in range(4)]
                Wo_t = Tk()
                for cb in range(4):
                    cs = slice(cb * 256, (cb + 1) * 256)
                    jobs.append((Wbr[:, 0:8, cs], wbrv[:, 0:8, cs], W4_t[cb]))
                    jobs.append((Wbr[:, 8:16, cs], wbrv[:, 8:16, cs], W4_t[cb]))
                    jobs.append((Wbm[:, :, cs], wbmv[:, :, cs], W4_t[cb]))
                    jobs.append((Wgr[:, :, cs], winv[:, :, 6848 + cb * 256:6848 + (cb + 1) * 256], W4_t[cb]))
                    jobs.append((Wgm[:, :, cs], winv[:, :, 7872 + cb * 256:7872 + (cb + 1) * 256], W4_t[cb]))
                for cb in range(4):
                    cs = slice(cb * 256, (cb + 1) * 256)
                    jobs.append((Wo[:, :, cs], wov[:, :, cs], Wo_t))
                for (dst, src, wt_) in jobs:
                    load_cast(st, dst, wt_, src, None, stg[k % 2][:], stg_t[k % 2], "pool" if k % 2 else "dve")
                    k += 1
                TK4 = 256
                hb = sb(st, "s4h", [128, 8, TK4], BF16)
                hb_t = Tk()
                rb = sb(st, "s4r", [128, 16, TK4], BF16)
                rb_t = Tk()
                ab = sb(st, "s4a", [128, 8, TK4], BF16)
                ab_t = Tk()
                mTb = sb(st, "s4m", [128, 8, TK4], BF16)
                mTb_t = Tk()
                s3_ = sb(st, "s4s3", [128, TK4], F32)
                s4_ = sb(st, "s4s4", [128, TK4], F32)
                sg_t = Tk()
                m1 = sb(st, "s4m1", [128, TK4], F32)
                m2 = sb(st, "s4m2", [128, TK4], F32)
                m_t = Tk()
                x_ = sb(st, "s4x", [128, D], F32)
                x_t = Tk()
                yt = sb(st, "s4y", [128, D], F32)
                yt_t = Tk()
                o_ = sb(st, "s4xo", [128, D], F32)
                o_t = Tk()
                P = [ps(st, f"s4p{i}", [128, 512]) for i in range(4)]
                P_t = [Tk() for _ in range(4)]
                PY = [ps(st, f"s4py{i}", [128, 512]) for i in range(2)]
                PY_t = [Tk() for _ in range(2)]
                for tbk in range(L // TK4):
                    q0 = tbk * TK4
                    for i in range(TK4 // 128):
                        tl = 2 + tbk * (TK4 // 128) + i
                        S.dma(hb[:, :, i * 128:(i + 1) * 128], hT_d[tl], reads=[hT_t[tl]], writes=[hb_t])
                    for hh in range(4):
                        S.dma(rb[:, hh * 4:(hh + 1) * 4, :], rT_d[hh, :, :, q0:q0 + TK4].rearrange("c p t -> p c t"),
                              reads=[rT_t[q0 // 512]], writes=[rb_t])
                    S.dma(ab[:], att_d[:, :, q0:q0 + TK4].rearrange("h p t -> p h t"), reads=[att_t[q0 // 512]], writes=[ab_t])
                    for fc in range(8):
                        fs = slice(fc * 128, (fc + 1) * 128)
                        for kc in range(16):
                            S.op("pe", lambda e: e.matmul(out=P[0][:, 0:TK4], lhsT=Wbr[:, kc, fs], rhs=rb[:, kc, :], start=(kc == 0), stop=(kc == 15)),
                                 reads=[W4_t[fc // 2], rb_t], writes=[P_t[0]], signal=(kc == 15))
                        for (pi, Wx, src, src_t) in ((1, Wbm, ab, ab_t), (2, Wgr, hb, hb_t), (3, Wgm, hb, hb_t)):
                            for kc in range(8):
                                S.op("pe", lambda e: e.matmul(out=P[pi][:, 0:TK4], lhsT=Wx[:, kc, fs], rhs=src[:, kc, :], start=(kc == 0),
                                                              stop=(kc == 7)), reads=[W4_t[fc // 2], src_t], writes=[P_t[pi]], signal=(kc == 7))
                        S.op("act", lambda e: e.activation(out=s3_[:], in_=P[2][:, 0:TK4], func=AF.Sigmoid), reads=[P_t[2]], writes=[sg_t])
                        S.op("act", lambda e: e.activation(out=s4_[:], in_=P[3][:, 0:TK4], func=AF.Sigmoid), reads=[P_t[3]], writes=[sg_t])
                        S.op("dve", lambda e: e.tensor_tensor(out=m1[:], in0=P[0][:, 0:TK4], in1=s3_[:], op=ALU.mult),
                             reads=[P_t[0], sg_t], writes=[m_t])
                        S.op("dve", lambda e: e.tensor_tensor(out=m2[:], in0=P[1][:, 0:TK4], in1=s4_[:], op=ALU.mult),
                             reads=[P_t[1], sg_t], writes=[m_t])
                        S.op("pool", lambda e: e.tensor_tensor(out=mTb[:, fc, :], in0=m1[:], in1=m2[:], op=ALU.add),
                             reads=[m_t], writes=[mTb_t])
                    for tt in range(TK4 // 128):
                        gt = tbk * (TK4 // 128) + tt
                        S.dma(x_[:], x_d[b, gt * 128:(gt + 1) * 128, :], writes=[x_t])
                        for cb in range(2):
                            for kc in range(8):
                                S.op("pe", lambda e: e.matmul(out=PY[cb][:], lhsT=mTb[:, kc, tt * 128:(tt + 1) * 128],
                                                              rhs=Wo[:, kc, cb * 512:(cb + 1) * 512], start=(kc == 0), stop=(kc == 7)),
                                     reads=[mTb_t, Wo_t], writes=[PY_t[cb]], signal=(kc == 7))
                            S.op("dve", lambda e: e.tensor_tensor(out=yt[:, cb * 512:(cb + 1) * 512], in0=PY[cb][:],
                                                                  in1=G1[:, b, cb * 512:(cb + 1) * 512], op=ALU.mult),
                                 reads=[PY_t[cb], G_t], writes=[yt_t])
                        S.op("pool", lambda e: e.tensor_tensor(out=o_[:], in0=yt[:], in1=x_[:], op=ALU.add),
                             reads=[yt_t, x_t], writes=[o_t])
                        S.dma(x1_d[b * 32 + gt], o_[:], reads=[o_t], writes=[x1_t[b * 32 + gt]])
                S.barrier()

        mix_stack.close()
        I32 = mybir.dt.int32
        NTI = NB * 32
        with contextlib.ExitStack() as st:
            posk = sb(st, "posk", [128, NTI * 4], F32)
            ekk = sb(st, "ekk", [128, NTI * 4], F32)
            g4 = sb(st, "g4", [128, NTI * 4], F32)
            pk_t = Tk()
            base = sb(st, "base", [128, 32], F32)
            base_t = Tk()
            desti = sb(st, "desti", [128, NTI * 4], I32)
            desti_t = Tk()
            widx = sb(st, "widx", [128, 8, NBLK], I32)
            bidx = sb(st, "bidx", [2, NBLK], I32)
            widx_t = Tk()
            iota32 = cst[:, 10, 0:32]
            h2d_t = [Tk() for _ in range(NTI)]
            with contextlib.ExitStack() as p1:
                GS2 = sb(p1, "GS2", [128, NB, D], F32)
                SH2 = sb(p1, "SH2", [128, NB, D], F32)
                GS_t = Tk()
                pB2 = [ps(p1, f"p1B{i}", [128, 512]) for i in range(2)]
                pB2_t = [Tk(), Tk()]
                pR = ps(p1, "p1R", [128, 512])
                pR_t = Tk()
                dg = sb(p1, "p1dg", [128, 8, 128], F32)
                dg_t = Tk()
                for bb in range(NB):
                    for (dst, vfn) in ((GS2, lambda c: gs2[:, bb, c:c + 1]), (SH2, lambda c: mT[:, 24 + c, bb:bb + 1])):
                        for c in range(8):
                            S.op("dve", lambda e: e.tensor_scalar(out=dg[:, c, :], in0=identf, scalar1=vfn(c), scalar2=None,
                                                                  op0=ALU.mult), reads=[cst_t, mT_t, gs_t], writes=[dg_t])
                        for c in range(8):
                            S.op("pe", lambda e: e.matmul(out=pB2[c // 4][:, (c % 4) * 128:(c % 4 + 1) * 128], lhsT=onesf,
                                                          rhs=dg[:, c, :], start=True, stop=True),
                                 reads=[dg_t, cst_t], writes=[pB2_t[c // 4]])
                        for hh in range(2):
                            S.op("act", lambda e: e.copy(out=dst[:, bb, hh * 512:(hh + 1) * 512], in_=pB2[hh][:]),
                                 reads=[pB2_t[hh]], writes=[GS_t])
                bfull = sb(p1, "p1bfull", [32, 3072], F32)
                bfb = sb(p1, "p1bfb", [32, 3072], BF16)
                bf_t = Tk()
                S.dma(bfull[:, 0:2048], ebgu_d[0], writes=[bf_t])
                S.dma(bfull[:, 2048:3072], ebd_d[0], writes=[bf_t])
                bfb_t = Tk()
                S.op("dve", lambda e: e.tensor_copy(out=bfb[:], in_=bfull[:]), reads=[bf_t], writes=[bfb_t])
                S.dma(bias_d[:, :], bfb[:], reads=[bfb_t])
                Wr = sb(p1, "Wr", [128, 8, 32], F32)
                Wr_t = Tk()
                S.dma(Wr[:], rw_d[0].rearrange("(kc p) n -> p kc n", p=128), writes=[Wr_t])
                rbs = sb(p1, "rbs", [1, 32], F32)
                rbs_t = Tk()
                S.dma(rbs[:], rb_d[:, :], writes=[rbs_t])
                xt = [sb(p1, f"p1x{i}", [128, D], F32) for i in range(2)]
                xt_t = [Tk(), Tk()]
                xn = sb(p1, "p1xn", [128, D], F32)
                xn_t = Tk()
                h2 = sb(p1, "p1h2", [128, D], F32)
                h2_t = Tk()
                h2bf = [sb(p1, f"p1h2b{i}", [128, D], BF16) for i in range(2)]
                h2bf_t = [Tk(), Tk()]
                h2f = sb(p1, "p1h2f", [128, 8, 128], F32)
                h2f_t = Tk()
                scr = sb(p1, "p1scr", [128, D], BF16)
                ssb = sb(p1, "p1ss", [128, 4], F32)
                tmp_t = Tk()
                lgt = sb(p1, "p1lg", [128, 32], F32)
                mk = sb(p1, "p1mk", [128, 32], F32)
                pos = sb(p1, "p1pos", [128, 32], F32)
                s32 = sb(p1, "p1s32", [128, 32], F32)
                m8 = sb(p1, "p1m8", [128, 8], F32)
                e4 = sb(p1, "p1e4", [128, 4], F32)
                sm = sb(p1, "p1sm", [128, 4], F32)
                rt_t = Tk()
                S.op("pool", lambda e: e.memset(base[:], 0.0), writes=[base_t])
                S.dma(xt[0][:], x1_d[0], reads=[x1_t[0]], writes=[xt_t[0]])
                for ti in range(NTI):
                    bb = ti // 32
                    if ti + 1 < NTI:
                        S.dma(xt[(ti + 1) % 2][:], x1_d[ti + 1], reads=[x1_t[ti + 1]], writes=[xt_t[(ti + 1) % 2]])
                    x_, x_t = xt[ti % 2], xt_t[ti % 2]
                    S.op("pool", lambda e: e.memset(ssb[:, 0:1], 0.0), writes=[tmp_t])
                    S.op("act", lambda e: e.activation(out=scr[:], in_=x_[:], func=AF.Square, accum_out=ssb[:, 0:1]),
                         reads=[x_t, tmp_t], writes=[tmp_t])
                    S.op("act", lambda e: e.activation(out=ssb[:, 1:2], in_=ssb[:, 0:1], func=AF.Sqrt, scale=1.0 / D, bias=EPS),
                         reads=[tmp_t], writes=[tmp_t])
                    S.op("dve", lambda e: e.reciprocal(out=ssb[:, 2:3], in_=ssb[:, 1:2]), reads=[tmp_t], writes=[tmp_t])
                    S.op("dve", lambda e: e.tensor_scalar(out=xn[:], in0=x_[:], scalar1=ssb[:, 2:3], scalar2=None, op0=ALU.mult),
                         reads=[x_t, tmp_t], writes=[xn_t])
                    S.op("dve", lambda e: e.tensor_tensor(out=xn[:], in0=xn[:], in1=GS2[:, bb, :], op=ALU.mult),
                         reads=[xn_t, GS_t], writes=[xn_t])
                    S.op("pool", lambda e: e.tensor_tensor(out=h2[:], in0=xn[:], in1=SH2[:, bb, :], op=ALU.add),
                         reads=[xn_t, GS_t], writes=[h2_t])
                    hb_, hb_t = h2bf[ti % 2], h2bf_t[ti % 2]
                    S.op("act", lambda e: e.copy(out=hb_[:], in_=h2[:]), reads=[h2_t], writes=[hb_t])
                    S.dma(h2_d[ti], hb_[:], reads=[hb_t], writes=[h2d_t[ti]])
                    for c in range(8):
                        S.op("pe", lambda e: e.transpose(out=pB2[c // 4][:, (c % 4) * 128:(c % 4 + 1) * 128],
                                                         in_=h2[:, c * 128:(c + 1) * 128], identity=identf),
                             reads=[h2_t, cst_t], writes=[pB2_t[c // 4]], signal=(c % 4 == 3))
                    S.op("dve", lambda e: e.tensor_copy(out=h2f[:, 0:4, :].rearrange("p a b -> p (a b)"), in_=pB2[0][:]),
                         reads=[pB2_t[0]], writes=[h2f_t])
                    S.op("act", lambda e: e.copy(out=h2f[:, 4:8, :].rearrange("p a b -> p (a b)"), in_=pB2[1][:]),
                         reads=[pB2_t[1]], writes=[h2f_t])
                    for kc in range(8):
                        S.op("pe", lambda e: e.matmul(out=pR[:, 0:32], lhsT=h2f[:, kc, :], rhs=Wr[:, kc, :], start=(kc == 0), stop=False),
                             reads=[h2f_t, Wr_t], writes=[pR_t], signal=False)
                    S.op("pe", lambda e: e.matmul(out=pR[:, 0:32], lhsT=cst[0:1, 1, :], rhs=rbs[:], start=False, stop=True),
                         reads=[cst_t, rbs_t], writes=[pR_t])
                    S.op("dve", lambda e: e.tensor_copy(out=lgt[:], in_=pR[:, 0:32]), reads=[pR_t], writes=[rt_t])
                    S.op("dve", lambda e: e.max(out=m8[:], in_=lgt[:]), reads=[rt_t], writes=[rt_t])
                    S.op("dve", lambda e: e.tensor_scalar(out=mk[:], in0=lgt[:], scalar1=m8[:, 3:4], scalar2=None, op0=ALU.is_ge),
                         reads=[rt_t], writes=[rt_t])
                    S.op("dve", lambda e: e.tensor_scalar(out=sm[:, 0:1], in0=m8[:, 0:1], scalar1=-1.0, scalar2=None, op0=ALU.mult),
                         reads=[rt_t], writes=[rt_t])
                    S.op("act", lambda e: e.activation(out=e4[:], in_=m8[:, 0:4], func=AF.Exp, bias=sm[:, 0:1], scale=1.0),
                         reads=[rt_t], writes=[rt_t])
                    S.op("dve", lambda e: e.reduce_sum(out=sm[:, 1:2], in_=e4[:], axis=mybir.AxisListType.X), reads=[rt_t], writes=[rt_t])
                    S.op("dve", lambda e: e.reciprocal(out=sm[:, 2:3], in_=sm[:, 1:2]), reads=[rt_t], writes=[rt_t])
                    S.op("dve", lambda e: e.tensor_scalar(out=g4[:, ti * 4:ti * 4 + 4], in0=e4[:], scalar1=sm[:, 2:3], scalar2=None, op0=ALU.mult),
                         reads=[rt_t], writes=[pk_t])
                    S.op("pe", lambda e: e.matmul(out=pR[:, 32:64], lhsT=cst[:, 9, :], rhs=mk[:], start=True, stop=True),
                         reads=[rt_t, cst_t], writes=[pR_t])
                    S.op("pe", lambda e: e.matmul(out=pR[:, 64:96], lhsT=onesf, rhs=mk[:], start=True, stop=True),
                         reads=[rt_t, cst_t], writes=[pR_t])
                    S.op("dve", lambda e: e.tensor_tensor(out=pos[:], in0=pR[:, 32:64], in1=base[:], op=ALU.add),
                         reads=[pR_t, base_t], writes=[rt_t])
                    for k in range(4):
                        S.op("dve", lambda e: e.scalar_tensor_tensor(out=s32[:], in0=lgt[:], scalar=m8[:, k:k + 1], in1=pos[:],
                                                                     op0=ALU.is_equal, op1=ALU.mult), reads=[rt_t], writes=[rt_t])
                        S.op("dve", lambda e: e.reduce_sum(out=posk[:, ti * 4 + k:ti * 4 + k + 1], in_=s32[:], axis=mybir.AxisListType.X),
                             reads=[rt_t], writes=[pk_t])
                        S.op("dve", lambda e: e.scalar_tensor_tensor(out=s32[:], in0=lgt[:], scalar=m8[:, k:k + 1], in1=iota32,
                                                                     op0=ALU.is_equal, op1=ALU.mult), reads=[rt_t, cst_t, pk_t], writes=[rt_t])
                        S.op("dve", lambda e: e.reduce_sum(out=ekk[:, ti * 4 + k:ti * 4 + k + 1], in_=s32[:], axis=mybir.AxisListType.X),
                             reads=[rt_t], writes=[pk_t])
                    S.op("dve", lambda e: e.tensor_tensor(out=base[:], in0=pR[:, 64:96], in1=base[:], op=ALU.add),
                         reads=[pR_t, base_t, rt_t], writes=[base_t])
                ci = sb(p1, "p2ci", [128, 32], I32)
                padded = sb(p1, "p2pad", [128, 32], F32)
                pst = sb(p1, "p2pst", [128, 32], F32)
                pend = sb(p1, "p2pend", [128, 32], F32)
                bst = sb(p1, "p2bst", [128, NBLK], F32)
                be = sb(p1, "p2be", [128, NBLK], F32)
                wf = sb(p1, "p2wf", [128, 8, NBLK], F32)
                df = sb(p1, "p2df", [128, NTI * 4], F32)
                p2_t = Tk()
                S.op("dve", lambda e: e.tensor_copy(out=ci[:], in_=base[:]), reads=[base_t], writes=[p2_t])
                S.op("dve", lambda e: e.tensor_scalar(out=ci[:], in0=ci[:], scalar1=511, scalar2=None, op0=ALU.add), reads=[p2_t], writes=[p2_t])
                S.op("dve", lambda e: e.tensor_scalar(out=ci[:], in0=ci[:], scalar1=-512, scalar2=None, op0=ALU.bitwise_and), reads=[p2_t], writes=[p2_t])
                S.op("dve", lambda e: e.tensor_copy(out=padded[:], in_=ci[:]), reads=[p2_t], writes=[p2_t])
                S.op("dve", lambda e: e.memset(pst[:], 0.0), reads=[p2_t], writes=[p2_t])
                for ee in range(1, 32):
                    S.op("dve", lambda e: e.tensor_tensor(out=pst[:, ee:ee + 1], in0=pst[:, ee - 1:ee], in1=padded[:, ee - 1:ee], op=ALU.add),
                         reads=[p2_t], writes=[p2_t])
                S.op("dve", lambda e: e.tensor_tensor(out=pend[:], in0=pst[:], in1=padded[:], op=ALU.add), reads=[p2_t], writes=[p2_t])
                S.op("dve", lambda e: e.tensor_scalar(out=bst[:], in0=cst[:, 10, 0:NBLK], scalar1=512.0, scalar2=None, op0=ALU.mult),
                     reads=[cst_t, p2_t], writes=[p2_t])
                S.op("dve", lambda e: e.memset(be[:], 0.0), reads=[p2_t], writes=[p2_t])
                for ee in range(32):
                    S.op("dve", lambda e: e.scalar_tensor_tensor(out=be[:], in0=bst[:], scalar=pend[:, ee:ee + 1], in1=be[:],
                                                                 op0=ALU.is_ge, op1=ALU.add), reads=[p2_t], writes=[p2_t])
                S.op("dve", lambda e: e.tensor_scalar(out=be[:], in0=be[:], scalar1=31.0, scalar2=None, op0=ALU.min), reads=[p2_t], writes=[p2_t])
                for kc in range(8):
                    S.op("dve", lambda e: e.tensor_scalar(out=wf[:, kc, :], in0=be[:], scalar1=1024.0, scalar2=cst[:, 11, kc:kc + 1],
                                                          op0=ALU.mult, op1=ALU.add), reads=[p2_t, cst_t], writes=[p2_t])
                S.op("dve", lambda e: e.tensor_copy(out=widx[:], in_=wf[:]), reads=[p2_t], writes=[widx_t])
                S.op("dve", lambda e: e.tensor_copy(out=bidx[:], in_=be[0:2, :]), reads=[p2_t], writes=[widx_t])
                for c in range(NTI * 4):
                    S.op("dve", lambda e: e.scalar_tensor_tensor(out=s32[:], in0=iota32, scalar=ekk[:, c:c + 1], in1=pst[:],
                                                                 op0=ALU.is_equal, op1=ALU.mult), reads=[p2_t, pk_t, cst_t, rt_t], writes=[rt_t])
                    S.op("dve", lambda e: e.reduce_sum(out=df[:, c:c + 1], in_=s32[:], axis=mybir.AxisListType.X), reads=[rt_t], writes=[p2_t])
                S.op("dve", lambda e: e.tensor_tensor(out=df[:], in0=df[:], in1=posk[:], op=ALU.add), reads=[p2_t, pk_t], writes=[p2_t])
                S.op("dve", lambda e: e.tensor_copy(out=desti[:], in_=df[:]), reads=[p2_t], writes=[desti_t])
                for ti in range(NTI):
                    hb_, hb_t = h2bf[ti % 2], h2bf_t[ti % 2]
                    S.dma(hb_[:], h2_d[ti], reads=[h2d_t[ti]], writes=[hb_t])
                    for k in range(4):
                        cidx = ti * 4 + k
                        S.dma_ind(lambda e: e.indirect_dma_start(
                            out=xs_d[:, :], out_offset=bass.IndirectOffsetOnAxis(ap=desti[:, cidx:cidx + 1], axis=0),
                            in_=hb_[:], in_offset=None),
                            reads=[hb_t, desti_t])
                S.barrier()

            with contextlib.ExitStack() as p4:
                egu2 = egu_d[0].rearrange("e k n -> (e k) n")
                edn2 = edn_d[0].rearrange("e k n -> (e k) n")
                Wgu = [sb(p4, f"Wgu{i}", [128, 8, 1024], BF16) for i in range(3)]
                Wgu_t = [Tk(), Tk(), Tk()]
                Wd = [sb(p4, f"Wd{i}", [128, 4, 1024], BF16) for i in range(2)]
                Wd_t = [Tk(), Tk()]
                stg = [sb(p4, f"p4stg{i}", [128, 2048], F32) for i in range(7)]
                stg_t = [Tk() for _ in range(7)]
                browb = [sb(p4, f"browb{i}", [2, 3072], BF16) for i in range(2)]
                browb_t = [Tk(), Tk()]
                xs = [sb(p4, f"p4xs{i}", [128, D], BF16) for i in range(4)]
                xs_t = [Tk() for _ in range(4)]
                xsT = sb(p4, "p4xsT", [128, 8, 512], BF16)
                xsT_t = Tk()
                aT = [sb(p4, f"aT{i}", [128, 4, 512], BF16) for i in range(2)]
                aT_t = [Tk(), Tk()]
                g1 = sb(p4, "p4g1", [128, 512], F32)
                u1 = sb(p4, "p4u1", [128, 512], F32)
                glu = sb(p4, "p4gl", [128, 512], F32)
                g1_t, u1_t, glu_t = Tk(), Tk(), Tk()
                ysb = sb(p4, "p4ys", [128, 4, D], F32)
                ysb_t = [[Tk(), Tk()] for _ in range(4)]
                pG = [ps(p4, f"p4G{i}", [128, 512]) for i in range(2)]
                pG_t = [Tk(), Tk()]
                pU = [ps(p4, f"p4U{i}", [128, 512]) for i in range(2)]
                pU_t = [Tk(), Tk()]
                pY = [ps(p4, f"p4Y{i}", [128, 512]) for i in range(2)]
                pY_t = [Tk(), Tk()]
                pX = [ps(p4, f"p4X{i}", [128, 2, 512], BF16) for i in range(2)]
                pX_t = [Tk(), Tk()]
                ISC = 1.0 / 1.702
                cnt = dict(sk=0, gk=0, yk=0)

                def w_load_gu_block(blk):
                    for kc in range(8):
                        si = cnt["sk"] % 7
                        cnt["sk"] += 1
                        S.dma_ind(lambda e: e.indirect_dma_start(
                            out=stg[si][:, :], out_offset=None, in_=egu2[:, :],
                            in_offset=bass.IndirectOffsetOnAxis(ap=widx[:, kc, blk:blk + 1], axis=0)),
                            reads=[widx_t], writes=[stg_t[si]])
                        for hf in range(2):
                            wi3 = (2 * blk + hf) % 3
                            S.op("act", lambda e: e.copy(out=Wgu[wi3][:, kc, :].rearrange("p (g n) -> p g n", g=2),
                                                         in_=stg[si][:].rearrange("p (g h n) -> p g h n", g=2, h=2)[:, :, hf, :]),
                                 reads=[stg_t[si]], writes=[Wgu_t[wi3]])

                def w_load_d(blk, hf, wi):
                    wd, wd_t = Wd[wi], Wd_t[wi]
                    for jq in range(2):
                        si = cnt["sk"] % 7
                        cnt["sk"] += 1
                        for i in range(2):
                            kc = hf * 4 + jq * 2 + i
                            S.dma_ind(lambda e: e.indirect_dma_start(
                                out=stg[si][:, i * 1024:(i + 1) * 1024], out_offset=None, in_=edn2[:, :],
                                in_offset=bass.IndirectOffsetOnAxis(ap=widx[:, kc, blk:blk + 1], axis=0)),
                                reads=[widx_t], writes=[stg_t[si]])
                        S.op("act", lambda e: e.mul(out=wd[:, jq * 2:(jq + 1) * 2, :], in_=stg[si][:].rearrange("p (a b) -> p a b", a=2), mul=ISC),
                             reads=[stg_t[si]], writes=[wd_t])

                def b_load(blk):
                    S.dma_ind(lambda e: e.indirect_dma_start(
                        out=browb[blk % 2][0:2, :], out_offset=None, in_=bias_d[:, :],
                        in_offset=bass.IndirectOffsetOnAxis(ap=bidx[0:2, blk:blk + 1], axis=0)),
                        reads=[widx_t], writes=[browb_t[blk % 2]])

                def x_dma(blk):
                    for tt in range(4):
                        r0 = blk * 512 + tt * 128
                        S.dma(xs[tt][:], xs_d[r0:r0 + 128, :], writes=[xs_t[tt]])

                def x_tr(blk):
                    for tt in range(4):
                        for kc in range(8):
                            S.op("pe", lambda e: e.transpose(out=pX[tt % 2][:, kc // 4, (kc % 4) * 128:(kc % 4 + 1) * 128],
                                                             in_=xs[tt][:, kc * 128:(kc + 1) * 128], identity=identb[:]),
                                 reads=[xs_t[tt], cb_t], writes=[pX_t[tt % 2]], signal=(kc == 7))
                        S.op("dve", lambda e: e.tensor_copy(out=xsT[:, 0:4, tt * 128:(tt + 1) * 128],
                                                            in_=pX[tt % 2][:, 0, :].rearrange("p (a b) -> p a b", a=4)),
                             reads=[pX_t[tt % 2]], writes=[xsT_t])
                        S.op("dve", lambda e: e.tensor_copy(out=xsT[:, 4:8, tt * 128:(tt + 1) * 128],
                                                            in_=pX[tt % 2][:, 1, :].rearrange("p (a b) -> p a b", a=4)),
                             reads=[pX_t[tt % 2]], writes=[xsT_t])

                def gu_unit(blk, hf, wi, au):
                    wg, wg_t = Wgu[(2 * blk + hf) % 3], Wgu_t[(2 * blk + hf) % 3]
                    bb_, bb_t = browb[blk % 2], browb_t[blk % 2]
                    a_, a_t = aT[au], aT_t[au]
                    for j in range(4):
                        i2 = cnt["gk"] % 2
                        cnt["gk"] += 1
                        fcol = hf * 512 + j * 128
                        for kc in range(8):
                            S.op("pe", lambda e: e.matmul(out=pG[i2][:], lhsT=wg[:, kc, j * 128:(j + 1) * 128], rhs=xsT[:, kc, :],
                                                          start=(kc == 0), stop=False), reads=[wg_t, xsT_t], writes=[pG_t[i2]], signal=False)
                        S.op("pe", lambda e: e.matmul(out=pG[i2][:], lhsT=bb_[0:1, fcol:fcol + 128], rhs=cbones[0:1, :], start=False, stop=True),
                             reads=[bb_t, cb_t], writes=[pG_t[i2]])
                        for kc in range(8):
                            S.op("pe", lambda e: e.matmul(out=pU[i2][:], lhsT=wg[:, kc, 512 + j * 128:512 + (j + 1) * 128], rhs=xsT[:, kc, :],
                                                          start=(kc == 0), stop=False), reads=[wg_t, xsT_t], writes=[pU_t[i2]], signal=False)
                        S.op("pe", lambda e: e.matmul(out=pU[i2][:], lhsT=bb_[0:1, 1024 + fcol:1024 + fcol + 128], rhs=cbones[0:1, :],
                                                      start=False, stop=True), reads=[bb_t, cb_t], writes=[pU_t[i2]])
                        S.op("dve", lambda e: e.tensor_scalar(out=g1[:], in0=pG[i2][:], scalar1=7.0, scalar2=None, op0=ALU.min),
                             reads=[pG_t[i2]], writes=[g1_t])
                        S.op("act", lambda e: e.activation(out=glu[:], in_=g1[:], func=AF.Silu, scale=1.702), reads=[g1_t], writes=[glu_t])
                        S.op("dve", lambda e: e.tensor_scalar(out=u1[:], in0=pU[i2][:], scalar1=1.0, scalar2=8.0, op0=ALU.add, op1=ALU.min),
                             reads=[pU_t[i2]], writes=[u1_t])
                        S.op("dve", lambda e: e.scalar_tensor_tensor(out=a_[:, j, :], in0=u1[:], scalar=-6.0, in1=glu[:],
                                                                     op0=ALU.max, op1=ALU.mult), reads=[u1_t, glu_t], writes=[a_t])

                def dn_unit(blk, hf, wi, au):
                    wd, wd_t = Wd[wi], Wd_t[wi]
                    bb_, bb_t = browb[blk % 2], browb_t[blk % 2]
                    a_, a_t = aT[au], aT_t[au]
                    for tt in range(4):
                        for cb in range(2):
                            yi = cnt["yk"] % 2
                            cnt["yk"] += 1
                            for j in range(4):
                                S.op("pe", lambda e: e.matmul(out=pY[yi][:], lhsT=a_[:, j, tt * 128:(tt + 1) * 128],
                                                              rhs=wd[:, j, cb * 512:(cb + 1) * 512], start=(j == 0), stop=(j == 3 and hf == 1)),
                                     reads=[a_t, wd_t], writes=[pY_t[yi]], signal=(j == 3 and hf == 1))
                            yv = ysb[:, tt, cb * 512:(cb + 1) * 512]
                            if hf == 0:
                                S.op("pe", lambda e: e.matmul(out=pY[yi][:], lhsT=cbones[0:1, 0:128], rhs=bb_[0:1, 2048 + cb * 512:2048 + (cb + 1) * 512],
                                                              start=False, stop=True), reads=[bb_t, cb_t], writes=[pY_t[yi]])
                                S.op("dve", lambda e: e.tensor_copy(out=yv, in_=pY[yi][:]), reads=[pY_t[yi]], writes=[ysb_t[tt][cb]])
                            else:
                                S.op("dve", lambda e: e.tensor_tensor(out=yv, in0=pY[yi][:], in1=yv, op=ALU.add),
                                     reads=[pY_t[yi], ysb_t[tt][cb]], writes=[ysb_t[tt][cb]])
                    if hf == 1:
                        S.dma(ys_d[blk * 512:(blk + 1) * 512, :].rearrange("(t p) c -> p t c", p=128), ysb[:],
                              reads=[ysb_t[tt][cb] for tt in range(4) for cb in range(2)])

                cbones = sb(p4, "cbones", [1, 512], BF16)
                S.op("dve", lambda e: e.memset(cbones[:], 1.0), reads=[cb_t], writes=[cb_t])
                units = [(blk, hf) for blk in range(NBLK) for hf in range(2)]
                NU = len(units)
                b_load(0)
                x_dma(0)
                w_load_gu_block(0)
                w_load_d(0, 0, 0)
                w_load_d(0, 1, 1)
                x_tr(0)
                gu_unit(0, 0, 0, 0)
                w_load_gu_block(1)
                b_load(1)
                x_dma(1)
                for ui, (blk, hf) in enumerate(units):
                    if ui + 1 < NU:
                        nb_, nh_ = units[ui + 1]
                        if nh_ == 0:
                            x_tr(nb_)
                        gu_unit(nb_, nh_, (ui + 1) % 2, (ui + 1) % 2)
                        if nh_ == 0 and nb_ + 1 < NBLK:
                            w_load_gu_block(nb_ + 1)
                        if nh_ == 0 and nb_ + 1 < NBLK:
                            b_load(nb_ + 1)
                            x_dma(nb_ + 1)
                    dn_unit(blk, hf, ui % 2, ui % 2)
                    if ui + 2 < NU:
                        b2, h2_ = units[ui + 2]
                        w_load_d(b2, h2_, (ui + 2) % 2)
                S.barrier()

            with contextlib.ExitStack() as p5:
                yk = [sb(p5, f"p5y{i}", [128, D], F32) for i in range(4)]
                yk_t = [Tk() for _ in range(4)]
                accm = sb(p5, "p5acc", [128, D], F32)
                acc_t = Tk()
                x_ = sb(p5, "p5x", [128, D], F32)
                x_t = Tk()
                scr = sb(p5, "p5scr", [128, D], BF16)
                ssb = sb(p5, "p5ss", [128, 4], F32)
                tmp_t = Tk()
                yo = sb(p5, "p5yo", [128, D], F32)
                yo_t = Tk()
                for ti in range(NTI):
                    bb = ti // 32
                    S.dma(x_[:], x1_d[ti], reads=[x1_t[ti]], writes=[x_t])
                    for k in range(4):
                        cidx = ti * 4 + k
                        S.dma_ind(lambda e: e.indirect_dma_start(
                            out=yk[k][:], out_offset=None, in_=ys_d[:, :],
                            in_offset=bass.IndirectOffsetOnAxis(ap=desti[:, cidx:cidx + 1], axis=0)),
                            reads=[desti_t], writes=[yk_t[k]])
                    S.op("dve", lambda e: e.tensor_scalar(out=accm[:], in0=yk[0][:], scalar1=g4[:, ti * 4:ti * 4 + 1], scalar2=None, op0=ALU.mult),
                         reads=[yk_t[0], pk_t], writes=[acc_t])
                    for k in range(1, 4):
                        S.op("dve", lambda e: e.scalar_tensor_tensor(out=accm[:], in0=yk[k][:], scalar=g4[:, ti * 4 + k:ti * 4 + k + 1], in1=accm[:],
                                                                     op0=ALU.mult, op1=ALU.add), reads=[yk_t[k], pk_t, acc_t], writes=[acc_t])
                    S.op("dve", lambda e: e.tensor_tensor(out=accm[:], in0=accm[:], in1=G2[:, bb, :], op=ALU.mult),
                         reads=[acc_t, G_t], writes=[acc_t])
                    S.op("dve", lambda e: e.tensor_tensor(out=accm[:], in0=accm[:], in1=x_[:], op=ALU.add), reads=[acc_t, x_t], writes=[acc_t])
                    S.op("dve", lambda e: e.memset(ssb[:, 0:1], 0.0), writes=[tmp_t])
                    S.op("act", lambda e: e.activation(out=scr[:], in_=accm[:], func=AF.Square, accum_out=ssb[:, 0:1]),
                         reads=[acc_t, tmp_t], writes=[tmp_t])
                    S.op("act", lambda e: e.activation(out=ssb[:, 1:2], in_=ssb[:, 0:1], func=AF.Sqrt, scale=1.0 / D, bias=EPS),
                         reads=[tmp_t], writes=[tmp_t])
                    S.op("dve", lambda e: e.reciprocal(out=ssb[:, 2:3], in_=ssb[:, 1:2]), reads=[tmp_t], writes=[tmp_t])
                    S.op("dve", lambda e: e.scalar_tensor_tensor(out=yo[:], in0=accm[:], scalar=ssb[:, 2:3], in1=FG[:], op0=ALU.mult, op1=ALU.mult),
                         reads=[acc_t, tmp_t, G_t], writes=[yo_t])
                    r0 = (ti % 32) * 128
                    S.dma(out_d[bb, r0:r0 + 128, :], yo[:], reads=[yo_t])
            S.final_wait()
        print("ops:", S.n_ops, "dmas:", S.dma_n, "sig:", S.cnt)
    return nc


_CACHE = {}


def make_in_maps(inputs, n_cores=8):
    RC, RS, MC, MS = rope_tables()
    cst, _ = const_tables()
    f = lambda a: np.ascontiguousarray(np.asarray(a, dtype=np.float32))
    shared = {k: f(inputs[k]) for k in ("norm1_g", "norm2_g", "ada_w", "ada_b", "w_in", "ret_decay_fwd", "ret_decay_bwd",
                                        "mla_q_norm_g", "mla_w_uq", "mla_kv_norm_g", "mla_w_ukv", "w_branch_ret",
                                        "w_branch_mla", "w_out", "router_w", "router_b", "exp_w_gu", "exp_b_gu",
                                        "exp_w_down", "exp_b_down", "final_norm_g")}
    shared.update(consts=cst, rope_rc=RC, rope_rs=RS, rope_mc=MC, rope_ms=MS)
    x, c, ctx, c_ctx = f(inputs["x"]), f(inputs["c"]), f(inputs["ctx"]), f(inputs["c_ctx"])
    maps = []
    for i in range(n_cores):
        m = dict(shared)
        m["x"] = x[i * NB:(i + 1) * NB]
        m["ctx"] = ctx[i * NB:(i + 1) * NB]
        m["cvec"] = np.ascontiguousarray(np.concatenate([c[i * NB:(i + 1) * NB], c_ctx[None, :]], axis=0))
        maps.append(m)
    return maps


def kernel(**inputs):
    if "nc" not in _CACHE:
        _CACHE["nc"] = build()
    nc = _CACHE["nc"]
    maps = make_in_maps(inputs)
    res = run_bass_kernel_spmd(nc, maps, core_ids=list(range(8)))
    return np.concatenate([r["out"] for r in res.results], axis=0).astype(np.float32)
```

```python
import contextlib
import numpy as np
import concourse.bass as bass
import concourse.mybir as mybir
from concourse.bass_utils import run_bass_kernel_spmd

F32 = mybir.dt.float32
BF16 = mybir.dt.bfloat16
AF = mybir.ActivationFunctionType
ALU = mybir.AluOpType

NB = 2
L = 4096
CT = 256
LT = L + CT
NT = LT // 128
D = 1024
EPS = 1e-6
NS_DMA = 16
ERA = 16000


class Tk:
    __slots__ = ("w", "r", "name")

    def __init__(self, name=""):
        self.w = []
        self.r = []
        self.name = name


class Sched:
    def __init__(self, nc, stack):
        self.nc = nc
        self.stack = stack
        self.engs = {"pe": nc.tensor, "act": nc.scalar, "dve": nc.vector, "pool": nc.gpsimd, "sp": nc.sync}
        self.sems = {}
        self.seq = {e: 0 for e in self.engs}
        self.sig = {e: [] for e in self.engs}
        self.cnt = {e: 0 for e in self.engs}
        self.waited = {e: {} for e in self.engs}
        self.waited_d = {e: {} for e in self.engs}
        self.ring = [stack.enter_context(nc.semaphore(f"dq{i}")) for i in range(NS_DMA)]
        self.dma_n = 0
        self.n_ops = 0

    def _sem(self, eng, era):
        k = (eng, era)
        if k not in self.sems:
            self.sems[k] = self.stack.enter_context(self.nc.semaphore(f"s_{eng}_{era}"))
        return self.sems[k]

    def _wait(self, eng, tk):
        e = self.engs[eng]
        if tk[0] == "d":
            _, ring, val = tk
            if self.waited_d[eng].get(ring, 0) >= val:
                return
            e.wait_ge(self.ring[ring], val)
            self.waited_d[eng][ring] = val
            return
        _, peng, seq = tk
        lst = self.sig[peng]
        lo, hi = 0, len(lst)
        while lo < hi:
            mid = (lo + hi) // 2
            if lst[mid][0] >= seq:
                hi = mid
            else:
                lo = mid + 1
        if lo >= len(lst):
            raise RuntimeError(f"no signalling op after seq {seq} on {peng}")
        count = lst[lo][1]
        if self.waited[eng].get(peng, 0) >= count:
            return
        era, val = (count - 1) // ERA, (count - 1) % ERA + 1
        e.wait_ge(self._sem(peng, era), val)
        self.waited[eng][peng] = count

    def op(self, eng, fn, reads=(), writes=(), signal=True):
        deps = []
        for t in reads:
            deps.extend(t.w)
        for t in writes:
            for tk in t.w:
                if tk[0] == "d" or tk[1] != eng:
                    deps.append(tk)
            for tk in t.r:
                if tk[0] == "d" or tk[1] != eng:
                    deps.append(tk)
        for tk in deps:
            self._wait(eng, tk)
        ins = fn(self.engs[eng])
        self.seq[eng] += 1
        seq = self.seq[eng]
        if signal:
            self.cnt[eng] += 1
            c = self.cnt[eng]
            era, val = (c - 1) // ERA, (c - 1) % ERA + 1
            ins.then_inc(self._sem(eng, era), 1)
            self.sig[eng].append((seq, c))
        tk = ("c", eng, seq)
        for t in reads:
            t.r = [x for x in t.r if not (x[0] == "c" and x[1] == eng)]
            t.r.append(tk)
        for t in writes:
            t.w = [tk]
            t.r = []
        self.n_ops += 1
        return ins

    def dma(self, out, in_, reads=(), writes=()):
        eng = "sp"
        deps = []
        for t in reads:
            deps.extend(t.w)
        for t in writes:
            deps.extend(t.w)
            deps.extend(t.r)
        n = self.dma_n
        ring = n % NS_DMA
        val = 16 * (n // NS_DMA + 1)
        if n >= NS_DMA:
            deps.append(("d", ring, val - 16))
        for tk in deps:
            self._wait(eng, tk)
        self.engs[eng].dma_start(out=out, in_=in_).then_inc(self.ring[ring], 16)
        self.dma_n += 1
        tk = ("d", ring, val)
        for t in reads:
            t.r.append(tk)
        for t in writes:
            t.w = [tk]
            t.r = []
        self.n_ops += 1
        return tk

    def dma_ind(self, fn, reads=(), writes=()):
        eng = "pool"
        deps = []
        for t in reads:
            deps.extend(t.w)
        for t in writes:
            deps.extend(t.w)
            deps.extend(t.r)
        n = self.dma_n
        ring = n % NS_DMA
        val = 16 * (n // NS_DMA + 1)
        if n >= NS_DMA:
            deps.append(("d", ring, val - 16))
        for tk in deps:
            self._wait(eng, tk)
        fn(self.engs[eng]).then_inc(self.ring[ring], 16)
        self.dma_n += 1
        tk = ("d", ring, val)
        for t in reads:
            t.r.append(tk)
        for t in writes:
            t.w = [tk]
            t.r = []
        self.n_ops += 1
        return tk

    def barrier(self):
        tks = []
        for e in self.engs:
            if self.sig[e]:
                tks.append(("c", e, self.sig[e][-1][0]))
        n = self.dma_n
        for k in range(max(0, n - NS_DMA), n):
            tks.append(("d", k % NS_DMA, 16 * (k // NS_DMA + 1)))
        for e in self.engs:
            for tk in tks:
                if tk[0] == "c" and tk[1] == e:
                    continue
                self._wait(e, tk)

    def final_wait(self):
        n = self.dma_n
        for k in range(max(0, n - NS_DMA), n):
            self._wait("sp", ("d", k % NS_DMA, 16 * (k // NS_DMA + 1)))


def rope_tables():
    pos = np.arange(L)
    rows = (pos // 64).astype(np.float32)
    cols = (pos % 64).astype(np.float32)

    def tab(dr):
        half = dr // 2
        hh = half // 2
        freqs = (10000.0 ** (-np.arange(hh, dtype=np.float32) / hh)).astype(np.float32)
        C = np.ones((LT, dr), np.float32)
        S = np.zeros((LT, dr), np.float32)
        for part, p in enumerate((rows, cols)):
            ang = (p[:, None] * freqs[None, :]).astype(np.float32)
            c, s = np.cos(ang).astype(np.float32), np.sin(ang).astype(np.float32)
            o = part * half
            C[CT:, o:o + hh] = c
            C[CT:, o + hh:o + half] = c
            S[CT:, o:o + hh] = -s
            S[CT:, o + hh:o + half] = s
        return C, S

    RC, RS = tab(256)
    MC, MS = tab(64)
    return RC, RS, np.ascontiguousarray(MC.T), np.ascontiguousarray(MS.T)


def const_tables():
    j = np.arange(128, dtype=np.float32)[:, None]
    i = np.arange(128, dtype=np.float32)[None, :]
    c = {}
    c["ident"] = np.eye(128, dtype=np.float32)
    c["ones"] = np.ones((128, 128), np.float32)
    c["d1"] = np.maximum(i - j, 0.0) + 0 * j
    c["mf"] = (i >= j).astype(np.float32)
    c["d2"] = np.maximum(j - i, 0.0)
    c["mb"] = (i < j).astype(np.float32)
    c["ip1"] = (i + 1.0) + 0 * j
    c["rev"] = (128.0 - i) + 0 * j
    col = np.zeros((128, 128), np.float32)
    col[:, 0] = 127.0 - np.arange(128)
    col[:, 1] = np.arange(128)
    col[:, 2] = 128.0
    c["col"] = col
    c["lt"] = (i > j).astype(np.float32)
    c["iota"] = i + 0 * j
    rowb = np.zeros((128, 128), np.float32)
    for kc in range(8):
        rowb[:, kc] = kc * 128 + np.arange(128)
    c["rowb"] = rowb
    names = ["ident", "ones", "d1", "mf", "d2", "mb", "ip1", "rev", "col", "lt", "iota", "rowb"]
    return np.stack([c[n].astype(np.float32) for n in names], axis=1), names


def build(dbg=()):
    nc = bass.Bass("TRN2", target_bir_lowering=False)
    try:
        nc.allow_low_precision("bf16 matmul operands with fp32 accumulation")
    except Exception:
        pass
    try:
        nc.allow_non_contiguous_dma("strided weight/activation tiles")
    except Exception:
        pass

    def din(name, shape, dt=F32):
        return nc.dram_tensor(name, list(shape), dt, kind="ExternalInput").ap()

    def dscr(name, shape, dt):
        kind = "ExternalOutput" if name in dbg else "Internal"
        return nc.dram_tensor(name, list(shape), dt, kind=kind).ap()

    x_d = din("x", [NB, L, D])
    ctx_d = din("ctx", [NB, CT, D])
    cv_d = din("cvec", [3, D])
    n1_d = din("norm1_g", [1, D])
    n2_d = din("norm2_g", [1, D])
    adaw_d = din("ada_w", [1, D, 6 * D])
    adab_d = din("ada_b", [1, 6 * D])
    win_d = din("w_in", [1, D, 8896])
    rdf_d = din("ret_decay_fwd", [1, 4])
    rdb_d = din("ret_decay_bwd", [1, 4])
    qg_d = din("mla_q_norm_g", [1, 384])
    wuq_d = din("mla_w_uq", [1, 384, 1536])
    kvg_d = din("mla_kv_norm_g", [1, 256])
    wukv_d = din("mla_w_ukv", [1, 256, 2048])
    wbr_d = din("w_branch_ret", [1, 2048, D])
    wbm_d = din("w_branch_mla", [1, D, D])
    wo_d = din("w_out", [1, D, D])
    rw_d = din("router_w", [1, D, 32])
    rb_d = din("router_b", [1, 32])
    egu_d = din("exp_w_gu", [1, 32, D, 2 * D])
    ebgu_d = din("exp_b_gu", [1, 32, 2 * D])
    edn_d = din("exp_w_down", [1, 32, D, D])
    ebd_d = din("exp_b_down", [1, 32, D])
    fg_d = din("final_norm_g", [D])
    cst_d = din("consts", [128, 12, 128])
    rc_d = din("rope_rc", [LT, 256])
    rs_d = din("rope_rs", [LT, 256])
    mc_d = din("rope_mc", [64, LT])
    ms_d = din("rope_ms", [64, LT])
    out_d = nc.dram_tensor("out", [NB, L, D], F32, kind="ExternalOutput").ap()

    hT_d = dscr("hT_s", [NT, 128, 8, 128], BF16)
    att_d = dscr("att_s", [8, 128, L], BF16)
    rT_d = dscr("rT_s", [4, 4, 128, L], BF16)
    of_d = dscr("of_s", [32, 128, 512], F32)
    x1_d = dscr("x1_s", [NB * 32, 128, D], F32)
    NROWS = NB * L * 4 + 32 * 512
    NBLK = NROWS // 512
    h2_d = dscr("h2_s", [NB * 32, 128, D], BF16)
    xs_d = dscr("xs_s", [NROWS, D], BF16)
    ys_d = dscr("ys_s", [NROWS, D], F32)
    bias_d = dscr("bias_s", [32, 3072], BF16)
    hT_t = [Tk() for _ in range(NT)]
    att_t = [Tk() for _ in range(8)]
    rT_t = [Tk() for _ in range(8)]
    of_t = [Tk() for _ in range(32)]
    x1_t = [Tk() for _ in range(NB * 32)]
    out_t = Tk()

    with contextlib.ExitStack() as gstack:
        S = Sched(nc, gstack)

        uid = [0]

        def sb(stack, name, shape, dt):
            uid[0] += 1
            return stack.enter_context(nc.sbuf_tensor(f"{name}_u{uid[0]}", list(shape), dt))

        def ps(stack, name, shape, dt=F32):
            uid[0] += 1
            return stack.enter_context(nc.psum_tensor(f"{name}_u{uid[0]}", list(shape), dt))

        cst = sb(gstack, "cst", [128, 12, 128], F32)
        cst_t = Tk()
        S.dma(cst[:], cst_d[:, :, :], writes=[cst_t])
        identf = cst[:, 0, :]
        onesf = cst[:, 1, :]
        identb = sb(gstack, "identb", [128, 128], BF16)
        onesb = sb(gstack, "onesb", [128, 128], BF16)
        cb_t = Tk()
        S.op("dve", lambda e: e.tensor_copy(out=identb[:], in_=identf), reads=[cst_t], writes=[cb_t])
        S.op("dve", lambda e: e.tensor_copy(out=onesb[:], in_=onesf), reads=[cst_t], writes=[cb_t])

        mT = sb(gstack, "mT", [128, 48, 3], F32)
        mT_t = Tk()
        gs1 = sb(gstack, "gs1", [128, 3, 8], F32)
        gs2 = sb(gstack, "gs2", [128, 3, 8], F32)
        gs_t = Tk()
        mix_stack = contextlib.ExitStack()
        G2 = sb(gstack, "G2", [128, NB, D], F32)
        FG = sb(gstack, "FG", [128, D], F32)
        G_t = Tk()
        gq = sb(gstack, "gq", [128, 8], F32)
        gq_t = Tk()
        KD = sb(gstack, "KD", [128, 4, 8], F32)
        G1 = sb(mix_stack, "G1", [128, NB, D], F32)
        MTf = sb(mix_stack, "MTf", [128, 4, 128], F32)
        MTb = sb(mix_stack, "MTb", [128, 4, 128], F32)
        QDf = sb(mix_stack, "QDf", [128, 4, 128], F32)
        QDb = sb(mix_stack, "QDb", [128, 4, 128], F32)
        dec_t = Tk()

        def featmajor_load(stack, pst, pst_t, dst_ap, src_rows_ap, nrows, tmpname):
            tmp = sb(stack, tmpname, [nrows, 128], F32)
            tt = Tk()
            S.dma(tmp[:], src_rows_ap, writes=[tt])
            S.op("pe", lambda e: e.transpose(out=pst[:, 0:nrows], in_=tmp[:], identity=cst[0:nrows, 0, 0:nrows]),
                 reads=[tt, cst_t], writes=[pst_t])
            return tmp

        with contextlib.ExitStack() as st:
            pA = ps(st, "pA0", [128, 512])
            pA_t = Tk()
            pB = ps(st, "pB0", [128, 2, 512])
            pB_t = Tk()
            cv = sb(st, "cv", [3, D], F32)
            cvs = sb(st, "cvs", [3, D], F32)
            cv_t = Tk()
            S.dma(cv[:], cv_d[:, :], writes=[cv_t])
            cvs_t = Tk()
            S.op("act", lambda e: e.activation(out=cvs[:], in_=cv[:], func=AF.Silu), reads=[cv_t], writes=[cvs_t])
            sT = sb(st, "sT", [128, 8, 3], F32)
            sT_t = Tk()
            for kc in range(8):
                S.op("pe", lambda e: e.transpose(out=pA[:, kc * 4:kc * 4 + 3], in_=cvs[:, kc * 128:(kc + 1) * 128],
                                                 identity=cst[0:3, 0, 0:3]), reads=[cvs_t, cst_t], writes=[pA_t])
            for kc in range(8):
                S.op("dve", lambda e: e.tensor_copy(out=sT[:, kc, :], in_=pA[:, kc * 4:kc * 4 + 3]),
                     reads=[pA_t], writes=[sT_t])
            abT = sb(st, "abT", [128, 48], F32)
            g12 = sb(st, "g12", [128, 16], F32)
            ab_t = Tk()
            featmajor_load(st, pA, pA_t, None, adab_d[0, :].rearrange("(r p) -> r p", p=128), 48, "t_ab")
            S.op("dve", lambda e: e.tensor_copy(out=abT[:], in_=pA[:, 0:48]), reads=[pA_t], writes=[ab_t])
            featmajor_load(st, pA, pA_t, None, n1_d[0, :].rearrange("(r p) -> r p", p=128), 8, "t_n1")
            S.op("dve", lambda e: e.tensor_copy(out=g12[:, 0:8], in_=pA[:, 0:8]), reads=[pA_t], writes=[ab_t])
            featmajor_load(st, pA, pA_t, None, n2_d[0, :].rearrange("(r p) -> r p", p=128), 8, "t_n2")
            S.op("dve", lambda e: e.tensor_copy(out=g12[:, 8:16], in_=pA[:, 0:8]), reads=[pA_t], writes=[ab_t])
            featmajor_load(st, pA, pA_t, None, qg_d[0, :].rearrange("(r p) -> r p", p=128), 3, "t_qg")
            S.op("dve", lambda e: e.tensor_copy(out=gq[:, 0:3], in_=pA[:, 0:3]), reads=[pA_t], writes=[gq_t])
            featmajor_load(st, pA, pA_t, None, kvg_d[0, :].rearrange("(r p) -> r p", p=128), 2, "t_kvg")
            S.op("dve", lambda e: e.tensor_copy(out=gq[:, 4:6], in_=pA[:, 0:2]), reads=[pA_t], writes=[gq_t])
            fgT = sb(st, "fgT", [128, 8], F32)
            fg_t = Tk()
            featmajor_load(st, pA, pA_t, None, fg_d.rearrange("(r p) -> r p", p=128), 8, "t_fg")
            S.op("dve", lambda e: e.tensor_copy(out=fgT[:], in_=pA[:, 0:8]), reads=[pA_t], writes=[fg_t])
            awv = adaw_d[0].rearrange("(kc p) n -> p kc n", p=128)
            aw = [sb(st, f"aw{i}", [128, 8, 512], F32) for i in range(2)]
            aw_t = [Tk(), Tk()]
            pm = ps(st, "pm", [128, 48, 4])
            pm_t = Tk()
            for blk in range(12):
                w = aw[blk % 2]
                wt = aw_t[blk % 2]
                S.dma(w[:], awv[:, :, blk * 512:(blk + 1) * 512], writes=[wt])
                for jj in range(4):
                    j = blk * 4 + jj
                    for kc in range(8):
                        S.op("pe", lambda e: e.matmul(out=pm[:, j, 0:3], lhsT=w[:, kc, jj * 128:(jj + 1) * 128],
                                                      rhs=sT[:, kc, :], start=(kc == 0), stop=(kc == 7)),
                             reads=[wt, sT_t], writes=[pm_t], signal=(kc == 7))
            for r in range(3):
                S.op("dve", lambda e: e.tensor_tensor(out=mT[:, :, r], in0=pm[:, :, r], in1=abT[:], op=ALU.add),
                     reads=[pm_t, ab_t], writes=[mT_t])
            for r in range(3):
                S.op("dve", lambda e: e.scalar_tensor_tensor(out=gs1[:, r, :], in0=mT[:, 8:16, r], scalar=1.0,
                                                             in1=g12[:, 0:8], op0=ALU.add, op1=ALU.mult),
                     reads=[mT_t, ab_t], writes=[gs_t])
                S.op("dve", lambda e: e.scalar_tensor_tensor(out=gs2[:, r, :], in0=mT[:, 32:40, r], scalar=1.0,
                                                             in1=g12[:, 8:16], op0=ALU.add, op1=ALU.mult),
                     reads=[mT_t, ab_t], writes=[gs_t])
            dg = sb(st, "dg", [128, 8, 128], F32)
            dg_t = Tk()

            def bcast_tile(dst_ap, vec_fn):
                for c in range(8):
                    S.op("dve", lambda e: e.tensor_scalar(out=dg[:, c, :], in0=identf, scalar1=vec_fn(c), scalar2=None,
                                                          op0=ALU.mult), reads=[cst_t, mT_t, fg_t], writes=[dg_t])
                for c in range(8):
                    S.op("pe", lambda e: e.matmul(out=pB[:, c // 4, (c % 4) * 128:(c % 4 + 1) * 128], lhsT=onesf,
                                                  rhs=dg[:, c, :], start=True, stop=True),
                         reads=[dg_t, cst_t], writes=[pB_t])
                S.op("act", lambda e: e.copy(out=dst_ap, in_=pB[:].rearrange("p a b -> p (a b)")),
                     reads=[pB_t], writes=[G_t])

            for b in range(NB):
                bcast_tile(G1[:, b, :], lambda c: mT[:, 16 + c, b:b + 1])
                bcast_tile(G2[:, b, :], lambda c: mT[:, 40 + c, b:b + 1])
            bcast_tile(FG[:], lambda c: fgT[:, c:c + 1])

            rd = sb(st, "rd", [1, 8], F32)
            rd_t = Tk()
            S.dma(rd[:, 0:4], rdf_d[:, :], writes=[rd_t])
            S.dma(rd[:, 4:8], rdb_d[:, :], writes=[rd_t])
            S.op("pe", lambda e: e.matmul(out=pA[:, 0:8], lhsT=cst[0:1, 1, :], rhs=rd[:], start=True, stop=True),
                 reads=[rd_t, cst_t], writes=[pA_t])
            lg = sb(st, "lg", [128, 8], F32)
            lg_t = Tk()
            S.op("act", lambda e: e.activation(out=lg[:], in_=pA[:, 0:8], func=AF.Exp, scale=-1.0),
                 reads=[pA_t], writes=[lg_t])
            S.op("act", lambda e: e.activation(out=lg[:], in_=lg[:], func=AF.Ln, bias=1.0, scale=1.0),
                 reads=[lg_t], writes=[lg_t])
            S.op("dve", lambda e: e.tensor_scalar(out=lg[:], in0=lg[:], scalar1=-1.0, scalar2=None, op0=ALU.mult),
                 reads=[lg_t], writes=[lg_t])
            tmpd = sb(st, "tmpd", [128, 128], F32)
            tmpd_t = Tk()
            for h in range(4):
                for (dst, dtab, mtab, col) in ((MTf, 2, 3, h), (MTb, 4, 5, 4 + h)):
                    S.op("act", lambda e: e.activation(out=tmpd[:], in_=cst[:, dtab, :], func=AF.Exp,
                                                       scale=lg[:, col:col + 1]),
                         reads=[cst_t, lg_t], writes=[tmpd_t])
                    S.op("dve", lambda e: e.tensor_tensor(out=dst[:, h, :], in0=tmpd[:], in1=cst[:, mtab, :],
                                                          op=ALU.mult), reads=[tmpd_t, cst_t], writes=[dec_t])
                S.op("act", lambda e: e.activation(out=QDf[:, h, :], in_=cst[:, 6, :], func=AF.Exp,
                                                   scale=lg[:, h:h + 1]), reads=[cst_t, lg_t], writes=[dec_t])
                S.op("act", lambda e: e.activation(out=QDb[:, h, :], in_=cst[:, 7, :], func=AF.Exp,
                                                   scale=lg[:, 4 + h:5 + h]), reads=[cst_t, lg_t], writes=[dec_t])
                S.op("act", lambda e: e.activation(out=KD[:, h, 0:1], in_=cst[:, 8, 0:1], func=AF.Exp,
                                                   scale=lg[:, h:h + 1]), reads=[cst_t, lg_t], writes=[dec_t])
                S.op("act", lambda e: e.activation(out=KD[:, h, 1:2], in_=cst[:, 8, 1:2], func=AF.Exp,
                                                   scale=lg[:, 4 + h:5 + h]), reads=[cst_t, lg_t], writes=[dec_t])
                S.op("act", lambda e: e.activation(out=KD[:, h, 2:3], in_=cst[:, 8, 2:3], func=AF.Exp,
                                                   scale=lg[:, h:h + 1]), reads=[cst_t, lg_t], writes=[dec_t])
                S.op("act", lambda e: e.activation(out=KD[:, h, 3:4], in_=cst[:, 8, 2:3], func=AF.Exp,
                                                   scale=lg[:, 4 + h:5 + h]), reads=[cst_t, lg_t], writes=[dec_t])
            S.barrier()

        def load_cast(stack_stage, dst, dst_t, src_ap, shape, stg, stg_t, eng, scale=None, dst_view=None):
            S.dma(stg, src_ap, writes=[stg_t])
            dv = dst if dst_view is None else dst_view
            if scale is None:
                S.op(eng, lambda e: e.tensor_copy(out=dv, in_=stg), reads=[stg_t], writes=[dst_t])
            else:
                S.op(eng, lambda e: e.tensor_scalar(out=dv, in0=stg, scalar1=scale, scalar2=None, op0=ALU.mult),
                     reads=[stg_t], writes=[dst_t])

        def norm_T(stack, pfx, src_tile, src_t, gs_ap, sh_ap, pT, pT_t, hTo, hTo_t, scr, ssb, tmp_t, xn, xn_t,
                   f32_out=None, f32_t=None):
            S.op("pool", lambda e: e.memset(ssb[:, 0:1], 0.0), writes=[tmp_t])
            S.op("act", lambda e: e.activation(out=scr[:], in_=src_tile, func=AF.Square, accum_out=ssb[:, 0:1]),
                 reads=[src_t, tmp_t], writes=[tmp_t])
            S.op("act", lambda e: e.activation(out=ssb[:, 1:2], in_=ssb[:, 0:1], func=AF.Sqrt, scale=1.0 / D, bias=EPS),
                 reads=[tmp_t], writes=[tmp_t])
            S.op("dve", lambda e: e.reciprocal(out=ssb[:, 2:3], in_=ssb[:, 1:2]), reads=[tmp_t], writes=[tmp_t])
            S.op("dve", lambda e: e.tensor_scalar(out=xn[:], in0=src_tile, scalar1=ssb[:, 2:3], scalar2=None,
                                                  op0=ALU.mult), reads=[src_t, tmp_t], writes=[xn_t])
            for c in range(8):
                S.op("pe", lambda e: e.transpose(out=pT[c // 4][:, (c % 4) * 128:(c % 4 + 1) * 128],
                                                 in_=xn[:, c * 128:(c + 1) * 128], identity=identf),
                     reads=[xn_t, cst_t], writes=[pT_t[c // 4]], signal=(c % 4 == 3))
            for c in range(8):
                src = pT[c // 4][:, (c % 4) * 128:(c % 4 + 1) * 128]
                if f32_out is not None:
                    S.op("dve", lambda e: e.tensor_scalar(out=f32_out[:, c, :], in0=src, scalar1=gs_ap[:, c:c + 1],
                                                          scalar2=sh_ap(c), op0=ALU.mult, op1=ALU.add),
                         reads=[pT_t[c // 4], gs_t, mT_t], writes=[f32_t])
                    S.op("pool", lambda e: e.tensor_copy(out=hTo[:, c, :], in_=f32_out[:, c, :]),
                         reads=[f32_t], writes=[hTo_t])
                else:
                    S.op("dve", lambda e: e.tensor_scalar(out=hTo[:, c, :], in0=src, scalar1=gs_ap[:, c:c + 1],
                                                          scalar2=sh_ap(c), op0=ALU.mult, op1=ALU.add),
                         reads=[pT_t[c // 4], gs_t, mT_t], writes=[hTo_t])

        winv = win_d[0].rearrange("(kc p) n -> p kc n", p=128)

        for b in range(NB):
            with contextlib.ExitStack() as st:
                xt = [sb(st, f"s0x{i}", [128, D], F32) for i in range(2)]
                xt_t = [Tk(), Tk()]
                xn = sb(st, "s0xn", [128, D], F32)
                xn_t = Tk()
                scr = sb(st, "s0scr", [128, D], BF16)
                ssb = sb(st, "s0ss", [128, 4], F32)
                tmp_t = Tk()
                hTo = [sb(st, f"s0h{i}", [128, 8, 128], BF16) for i in range(2)]
                hTo_t = [Tk(), Tk()]
                pT = [ps(st, f"s0p{i}", [128, 512]) for i in range(2)]
                pT_t = [Tk(), Tk()]

                def s0_load(t):
                    src = ctx_d[b, t * 128:(t + 1) * 128, :] if t < 2 else x_d[b, (t - 2) * 128:(t - 1) * 128, :]
                    S.dma(xt[t % 2][:], src, writes=[xt_t[t % 2]])

                s0_load(0)
                for t in range(NT):
                    if t + 1 < NT:
                        s0_load(t + 1)
                    r = 2 if t < 2 else b
                    norm_T(st, "s0", xt[t % 2][:], xt_t[t % 2], gs1[:, r, :], lambda c: mT[:, c, r:r + 1],
                           pT, pT_t, hTo[t % 2], hTo_t[t % 2], scr, ssb, tmp_t, xn, xn_t)
                    S.dma(hT_d[t], hTo[t % 2][:], reads=[hTo_t[t % 2]], writes=[hT_t[t]])
                S.barrier()

            with contextlib.ExitStack() as st:
                cqn = sb(st, "cqn", [128, 3, L], BF16)
                cqn_t = Tk()
                ckvn = sb(st, "ckvn", [128, 2, LT], BF16)
                ckvn_t = Tk()
                krT = sb(st, "krT", [64, LT], BF16)
                krT_t = Tk()
                with contextlib.ExitStack() as s1:
                    Wm = sb(s1, "Wm", [128, 8, 768], BF16)
                    Wm_t = Tk()
                    stg = sb(s1, "s1stg", [128, 8, 704], F32)
                    stg_t = Tk()
                    S.dma(stg[:], winv[:, :, 6144:6848], writes=[stg_t])
                    S.op("dve", lambda e: e.tensor_copy(out=Wm[:, :, 0:704], in_=stg[:]), reads=[stg_t], writes=[Wm_t])
                    for (dst, src) in ((704, 656), (720, 640), (736, 688), (752, 672)):
                        S.op("dve", lambda e: e.tensor_copy(out=Wm[:, :, dst:dst + 16], in_=stg[:, :, src:src + 16]),
                             reads=[stg_t], writes=[Wm_t])
                    hb = [sb(s1, f"s1h{i}", [128, 8, 512], BF16) for i in range(2)]
                    hb_t = [Tk(), Tk()]
                    tc_ = [sb(s1, f"s1tc{i}", [64, 2, 512], F32) for i in range(2)]
                    tc_t = [Tk(), Tk()]
                    pq = [ps(s1, f"s1pq{i}", [128, 512]) for i in range(3)]
                    pq_t = [Tk() for _ in range(3)]
                    pk = [ps(s1, f"s1pk{i}", [128, 512]) for i in range(2)]
                    pk_t = [Tk() for _ in range(2)]
                    pr = [ps(s1, f"s1pr{i}", [128, 512]) for i in range(2)]
                    pr_t = [Tk() for _ in range(2)]
                    pss = ps(s1, "s1pss", [128, 512])
                    pss_t = Tk()
                    xf = sb(s1, "s1xf", [128, 3, 512], F32)
                    xf_t = Tk()
                    sq = sb(s1, "s1sq", [128, 3, 512], F32)
                    sq_t = Tk()
                    rstd = sb(s1, "s1rstd", [128, 512], F32)
                    rstd_t = Tk()
                    r1 = sb(s1, "s1r1", [64, 512], F32)
                    r2 = sb(s1, "s1r2", [64, 512], F32)
                    r_t = Tk()

                    def blk_range(j):
                        return (0, 256) if j == 0 else (256 + (j - 1) * 512, 512)

                    def s1_load(j):
                        t0, n = blk_range(j)
                        for i in range(n // 128):
                            S.dma(hb[j % 2][:, :, i * 128:(i + 1) * 128], hT_d[t0 // 128 + i],
                                  reads=[hT_t[t0 // 128 + i]], writes=[hb_t[j % 2]])
                        S.dma(tc_[j % 2][:, 0, 0:n], mc_d[:, t0:t0 + n], writes=[tc_t[j % 2]])
                        S.dma(tc_[j % 2][:, 1, 0:n], ms_d[:, t0:t0 + n], writes=[tc_t[j % 2]])

                    def rms_T(pl, pl_t, nch, rank, gcol, dst, dst_t, dcol0, n):
                        for c in range(nch):
                            S.op("act", lambda e: e.copy(out=xf[:, c, 0:n], in_=pl[c][:, 0:n]),
                                 reads=[pl_t[c]], writes=[xf_t])
                            S.op("act", lambda e: e.activation(out=sq[:, c, 0:n], in_=pl[c][:, 0:n], func=AF.Square),
                                 reads=[pl_t[c]], writes=[sq_t])
                        for c in range(nch):
                            S.op("pe", lambda e: e.matmul(out=pss[:, 0:n], lhsT=onesf, rhs=sq[:, c, 0:n],
                                                          start=(c == 0), stop=(c == nch - 1)),
                                 reads=[sq_t, cst_t], writes=[pss_t], signal=(c == nch - 1))
                        S.op("act", lambda e: e.activation(out=rstd[:, 0:n], in_=pss[:, 0:n], func=AF.Sqrt,
                                                           scale=1.0 / rank, bias=EPS), reads=[pss_t], writes=[rstd_t])
                        S.op("dve", lambda e: e.reciprocal(out=rstd[:, 0:n], in_=rstd[:, 0:n]),
                             reads=[rstd_t], writes=[rstd_t])
                        for c in range(nch):
                            S.op("dve", lambda e: e.scalar_tensor_tensor(
                                out=dst[:, c, dcol0:dcol0 + n], in0=xf[:, c, 0:n], scalar=gq[:, gcol + c:gcol + c + 1],
                                in1=rstd[:, 0:n], op0=ALU.mult, op1=ALU.mult),
                                 reads=[xf_t, rstd_t, gq_t], writes=[dst_t])

                    s1_load(0)
                    for j in range(9):
                        if j + 1 < 9:
                            s1_load(j + 1)
                        t0, n = blk_range(j)
                        h_, h_t = hb[j % 2], hb_t[j % 2]
                        if j > 0:
                            for c in range(3):
                                for kc in range(8):
                                    S.op("pe", lambda e: e.matmul(out=pq[c][:, 0:n], lhsT=Wm[:, kc, c * 128:(c + 1) * 128],
                                                                  rhs=h_[:, kc, 0:n], start=(kc == 0), stop=(kc == 7)),
                                         reads=[Wm_t, h_t], writes=[pq_t[c]], signal=(kc == 7))
                        for c in range(2):
                            for kc in range(8):
                                S.op("pe", lambda e: e.matmul(out=pk[c][:, 0:n], lhsT=Wm[:, kc, 384 + c * 128:384 + (c + 1) * 128],
                                                              rhs=h_[:, kc, 0:n], start=(kc == 0), stop=(kc == 7)),
                                     reads=[Wm_t, h_t], writes=[pk_t[c]], signal=(kc == 7))
                        for c in range(2):
                            for kc in range(8):
                                S.op("pe", lambda e: e.matmul(out=pr[c][0:64, 0:n], lhsT=Wm[:, kc, 640 + c * 64:704 + c * 64],
                                                              rhs=h_[:, kc, 0:n], start=(kc == 0), stop=(kc == 7)),
                                     reads=[Wm_t, h_t], writes=[pr_t[c]], signal=(kc == 7))
                        if j > 0:
                            rms_T(pq, pq_t, 3, 384, 0, cqn, cqn_t, t0 - 256, n)
                        rms_T(pk, pk_t, 2, 256, 4, ckvn, ckvn_t, t0, n)
                        tcc, tcc_t = tc_[j % 2], tc_t[j % 2]
                        S.op("dve", lambda e: e.tensor_tensor(out=r1[:, 0:n], in0=pr[0][0:64, 0:n], in1=tcc[:, 0, 0:n],
                                                              op=ALU.mult), reads=[pr_t[0], tcc_t], writes=[r_t])
                        S.op("dve", lambda e: e.tensor_tensor(out=r2[:, 0:n], in0=pr[1][0:64, 0:n], in1=tcc[:, 1, 0:n],
                                                              op=ALU.mult), reads=[pr_t[1], tcc_t], writes=[r_t])
                        S.op("dve", lambda e: e.tensor_tensor(out=krT[:, t0:t0 + n], in0=r1[:, 0:n], in1=r2[:, 0:n],
                                                              op=ALU.add), reads=[r_t], writes=[krT_t])
                    S.barrier()

                with contextlib.ExitStack() as s2:
                    KT = sb(s2, "KT", [128, LT], BF16)
                    KT_t = Tk()
                    V = sb(s2, "V", [128, NT, 128], BF16)
                    V_t = Tk()
                    QT = sb(s2, "QT", [128, L], BF16)
                    QT_t = Tk()
                    QrT = sb(s2, "QrT", [64, L], BF16)
                    QrT_t = Tk()
                    Wq = sb(s2, "Wq", [128, 3, 256], BF16)
                    Wq_t = Tk()
                    Wkv = sb(s2, "Wkv", [128, 2, 256], BF16)
                    Wkv_t = Tk()
                    sq_ = sb(s2, "s2sq", [128, 3, 192], F32)
                    sq_t = Tk()
                    skv = sb(s2, "s2skv", [128, 2, 256], F32)
                    skv_t = Tk()
                    tq = [sb(s2, f"s2tq{i}", [64, 2, 512], F32) for i in range(2)]
                    tq_t = [Tk(), Tk()]
                    r1 = sb(s2, "s2r1", [64, 512], F32)
                    r2 = sb(s2, "s2r2", [64, 512], F32)
                    r_t = Tk()
                    PT = [sb(s2, f"PT{i}", [128, 512], BF16) for i in range(4)]
                    PT_t = [Tk() for _ in range(4)]
                    accA = [sb(s2, f"s2accA{i}", [128, 512], F32) for i in range(2)]
                    accB = [sb(s2, f"s2accB{i}", [128, 512], F32) for i in range(2)]
                    accA_t = [Tk(), Tk()]
                    accB_t = [Tk(), Tk()]
                    rs = sb(s2, "s2rs", [128, 512], F32)
                    rs_t = Tk()
                    ot = [sb(s2, f"s2ot{i}", [128, 512], BF16) for i in range(2)]
                    ot_t = [Tk(), Tk()]
                    pS = [ps(s2, f"pS{i}", [128, 512]) for i in range(3)]
                    pS_t = [Tk() for _ in range(3)]
                    pO = [ps(s2, f"pO{i}", [128, 512]) for i in range(2)]
                    pO_t = [Tk() for _ in range(2)]
                    pZ = [ps(s2, f"pZ{i}", [128, 512]) for i in range(2)]
                    pZ_t = [Tk() for _ in range(2)]
                    pX = ps(s2, "pX", [128, 512])
                    pX_t = Tk()
                    wuqv = wuq_d[0].rearrange("(kc p) n -> p kc n", p=128)
                    wukvv = wukv_d[0].rearrange("(kc p) n -> p kc n", p=128)
                    sc = 192.0 ** -0.5
                    cnt = 0
                    for h in range(8):
                        S.dma(sq_[:], wuqv[:, :, h * 192:(h + 1) * 192], writes=[sq_t])
                        S.op("pool", lambda e: e.tensor_copy(out=Wq[:, :, 0:192], in_=sq_[:]), reads=[sq_t], writes=[Wq_t])
                        for (dst, src) in ((192, 144), (208, 128), (224, 176), (240, 160)):
                            S.op("pool", lambda e: e.tensor_copy(out=Wq[:, :, dst:dst + 16], in_=sq_[:, :, src:src + 16]),
                                 reads=[sq_t], writes=[Wq_t])
                        S.dma(skv[:], wukvv[:, :, h * 256:(h + 1) * 256], writes=[skv_t])
                        S.op("pool", lambda e: e.tensor_copy(out=Wkv[:], in_=skv[:]), reads=[skv_t], writes=[Wkv_t])
                        for j in range(9):
                            t0, n = (0, 256) if j == 0 else (256 + (j - 1) * 512, 512)
                            for kc in range(2):
                                S.op("pe", lambda e: e.matmul(out=pX[:, 0:n], lhsT=Wkv[:, kc, 0:128], rhs=ckvn[:, kc, t0:t0 + n],
                                                              start=(kc == 0), stop=(kc == 1)),
                                     reads=[Wkv_t, ckvn_t], writes=[pX_t], signal=(kc == 1))
                            S.op("act" if j % 2 else "dve", (lambda e: e.copy(out=KT[:, t0:t0 + n], in_=pX[:, 0:n])) if j % 2
                                 else (lambda e: e.tensor_copy(out=KT[:, t0:t0 + n], in_=pX[:, 0:n])),
                                 reads=[pX_t], writes=[KT_t])
                        for g in range(9):
                            tiles = list(range(g * 4, min(g * 4 + 4, NT)))
                            for i, t in enumerate(tiles):
                                for kc in range(2):
                                    S.op("pe", lambda e: e.matmul(out=pX[:, i * 128:(i + 1) * 128],
                                                                  lhsT=ckvn[:, kc, t * 128:(t + 1) * 128], rhs=Wkv[:, kc, 128:256],
                                                                  start=(kc == 0), stop=(kc == 1)),
                                         reads=[Wkv_t, ckvn_t], writes=[pX_t], signal=(kc == 1 and i == len(tiles) - 1))
                            nn = len(tiles)
                            S.op("act" if g % 2 else "dve",
                                 (lambda e: e.copy(out=V[:, tiles[0]:tiles[0] + nn, :].rearrange("p a b -> p (a b)"),
                                                   in_=pX[:, 0:nn * 128])) if g % 2 else
                                 (lambda e: e.tensor_copy(out=V[:, tiles[0]:tiles[0] + nn, :].rearrange("p a b -> p (a b)"),
                                                          in_=pX[:, 0:nn * 128])),
                                 reads=[pX_t], writes=[V_t])
                        for j in range(8):
                            q0 = j * 512
                            S.dma(tq[j % 2][:, 0, :], mc_d[:, 256 + q0:256 + q0 + 512], writes=[tq_t[j % 2]])
                            S.dma(tq[j % 2][:, 1, :], ms_d[:, 256 + q0:256 + q0 + 512], writes=[tq_t[j % 2]])
                            for kc in range(3):
                                S.op("pe", lambda e: e.matmul(out=pX[:], lhsT=Wq[:, kc, 0:128], rhs=cqn[:, kc, q0:q0 + 512],
                                                              start=(kc == 0), stop=(kc == 2)),
                                     reads=[Wq_t, cqn_t], writes=[pX_t], signal=(kc == 2))
                            S.op("act", lambda e: e.copy(out=QT[:, q0:q0 + 512], in_=pX[:]), reads=[pX_t], writes=[QT_t])
                            for c in range(2):
                                for kc in range(3):
                                    S.op("pe", lambda e: e.matmul(out=pZ[c][0:64, :], lhsT=Wq[:, kc, 128 + c * 64:192 + c * 64],
                                                                  rhs=cqn[:, kc, q0:q0 + 512], start=(kc == 0), stop=(kc == 2)),
                                         reads=[Wq_t, cqn_t], writes=[pZ_t[c]], signal=(kc == 2))
                            tt_, tt_t = tq[j % 2], tq_t[j % 2]
                            S.op("dve", lambda e: e.tensor_tensor(out=r1[:], in0=pZ[0][0:64, :], in1=tt_[:, 0, :], op=ALU.mult),
                                 reads=[pZ_t[0], tt_t], writes=[r_t])
                            S.op("dve", lambda e: e.tensor_tensor(out=r2[:], in0=pZ[1][0:64, :], in1=tt_[:, 1, :], op=ALU.mult),
                                 reads=[pZ_t[1], tt_t], writes=[r_t])
                            S.op("dve", lambda e: e.tensor_tensor(out=QrT[:, q0:q0 + 512], in0=r1[:], in1=r2[:], op=ALU.add),
                                 reads=[r_t], writes=[QrT_t])
                        for qb in range(8):
                            q0 = qb * 512
                            po, po_t = pO[qb % 2], pO_t[qb % 2]
                            pz, pz_t = pZ[qb % 2], pZ_t[qb % 2]
                            def emit_st(kt, cn):
                                s_, s_t = pS[cn % 3], pS_t[cn % 3]
                                S.op("pe", lambda e: e.matmul(out=s_[:], lhsT=KT[:, kt * 128:(kt + 1) * 128], rhs=QT[:, q0:q0 + 512],
                                                              start=True, stop=False), reads=[KT_t, QT_t], writes=[s_t], signal=False)
                                S.op("pe", lambda e: e.matmul(out=s_[:], lhsT=krT[:, kt * 128:(kt + 1) * 128], rhs=QrT[:, q0:q0 + 512],
                                                              start=False, stop=True), reads=[krT_t, QrT_t], writes=[s_t])

                            emit_st(0, cnt)
                            for kt in range(NT):
                                s_, s_t = pS[cnt % 3], pS_t[cnt % 3]
                                p_, p_t = PT[cnt % 4], PT_t[cnt % 4]
                                if kt + 1 < NT:
                                    emit_st(kt + 1, cnt + 1)
                                cnt += 1
                                S.op("act", lambda e: e.activation(out=p_[:], in_=s_[:], func=AF.Exp, scale=sc),
                                     reads=[s_t], writes=[p_t])
                                S.op("pe", lambda e: e.matmul(out=po[:], lhsT=V[:, kt, :], rhs=p_[:], start=(kt == 0),
                                                              stop=(kt == NT - 1)), reads=[V_t, p_t], writes=[po_t])
                                aa, aa_t = accA[qb % 2], accA_t[qb % 2]
                                if kt == 0:
                                    S.op("dve", lambda e: e.tensor_copy(out=aa[:], in_=p_[:]), reads=[p_t], writes=[aa_t])
                                else:
                                    S.op("dve", lambda e: e.tensor_tensor(out=aa[:], in0=aa[:], in1=p_[:], op=ALU.add),
                                         reads=[p_t, aa_t], writes=[aa_t])
                            S.op("pe", lambda e: e.matmul(out=pz[:], lhsT=onesf, rhs=accA[qb % 2][:], start=True, stop=True),
                                 reads=[cst_t, accA_t[qb % 2]], writes=[pz_t])
                            S.op("dve", lambda e: e.reciprocal(out=rs[:], in_=pz[:]), reads=[pz_t], writes=[rs_t])
                            o_, o_t = ot[qb % 2], ot_t[qb % 2]
                            S.op("dve", lambda e: e.tensor_tensor(out=o_[:], in0=po[:], in1=rs[:], op=ALU.mult),
                                 reads=[po_t, rs_t], writes=[o_t])
                            S.dma(att_d[h, :, q0:q0 + 512], o_[:], reads=[o_t], writes=[att_t[qb]])
                    S.barrier()

            with contextlib.ExitStack() as st:
                Wqk = sb(st, "Wqk", [128, 8, 512], BF16)
                Wv = sb(st, "Wv", [128, 8, 512], BF16)
                Wg = sb(st, "Wg", [128, 8, 512], BF16)
                Wqk_t, Wv_t, Wg_t = Tk(), Tk(), Tk()
                stg = sb(st, "s3stg", [128, 8, 512], F32)
                stg_t = Tk()
                hb = [sb(st, f"s3h{i}", [128, 8, 128], BF16) for i in range(3)]
                hb_t = [Tk() for _ in range(3)]
                tb = [sb(st, f"s3tb{i}", [128, 2, 256], F32) for i in range(3)]
                tb_t = [Tk() for _ in range(3)]
                ofb = [sb(st, f"s3of{i}", [128, 512], F32) for i in range(3)]
                ofb_t = [Tk() for _ in range(3)]
                Sf = sb(st, "Sf", [128, 2, 512], F32)
                Sb_ = sb(st, "Sb", [128, 2, 512], BF16)
                Sf_t = Tk()
                Sb_t = Tk()
                t1 = sb(st, "s3t1", [128, 512], F32)
                t2 = sb(st, "s3t2", [128, 512], F32)
                t12_t = Tk()
                qk_all = sb(st, "s3qkall", [128, NT, 512], BF16)
                qka_t = [Tk() for _ in range(NT)]
                V_all = sb(st, "s3Vall", [128, NT, 512], BF16)
                Va_t = [Tk() for _ in range(NT)]
                Kdb = [sb(st, f"s3Kd{i}", [128, 256], BF16) for i in range(2)]
                Kdb_t = [Tk(), Tk()]
                KTt = sb(st, "s3KT", [128, 2, 128], BF16)
                QTt = sb(st, "s3QT", [128, 2, 128], BF16)
                QdT = sb(st, "s3QdT", [128, 2, 128], BF16)
                tr_t = Tk()
                STm = sb(st, "s3STm", [128, 128], BF16)
                STm_t = Tk()
                osb = sb(st, "s3o", [128, 512], F32)
                osb_t = Tk()
                sg = sb(st, "s3sg", [128, 512], F32)
                sg_t = Tk()
                scr = sb(st, "s3scr", [128, 512], BF16)
                ssb = sb(st, "s3ss", [128, 4], F32)
                ss_t = Tk()
                rr = sb(st, "s3r", [128, 512], BF16)
                rr_t = Tk()
                rTs = [sb(st, f"s3rT{i}", [128, 4, 128], BF16) for i in range(2)]
                rTs_t = [Tk(), Tk()]
                pAl = [ps(st, f"s3pA{i}", [128, 512]) for i in range(2)]
                pAl_t = [Tk(), Tk()]
                pBl = [ps(st, f"s3pB{i}", [128, 512]) for i in range(2)]
                pBl_t = [Tk(), Tk()]
                pC = ps(st, "s3pC", [128, 512])
                pD = ps(st, "s3pD", [128, 2, 512], BF16)
                pE = ps(st, "s3pE", [128, 512])
                pF = ps(st, "s3pF", [128, 512])
                pC_t, pD_t, pD2_t, pE_t, pF_t = (Tk() for _ in range(5))
                pH = [pC, pE]
                pH_t = [pC_t, pE_t]
                for h in range(4):
                    for (dst, c0, scl) in ((Wqk[:, :, 0:256], h * 256, None), (Wqk[:, :, 256:512], 1024 + h * 256, 0.0625)):
                        S.dma(stg[:, :, 0:256], winv[:, :, c0:c0 + 256], writes=[stg_t])
                        if scl is None:
                            S.op("act", lambda e: e.copy(out=dst, in_=stg[:, :, 0:256]), reads=[stg_t], writes=[Wqk_t])
                        else:
                            S.op("act", lambda e: e.mul(out=dst, in_=stg[:, :, 0:256], mul=scl), reads=[stg_t], writes=[Wqk_t])
                    for (dst, c0, wt_) in ((Wv, 2048 + h * 512, Wv_t), (Wg, 4096 + h * 512, Wg_t)):
                        S.dma(stg[:], winv[:, :, c0:c0 + 512], writes=[stg_t])
                        S.op("act", lambda e: e.copy(out=dst[:], in_=stg[:]), reads=[stg_t], writes=[wt_])
                    for di, dirn in enumerate(("f", "b")):
                        order = list(range(NT)) if dirn == "f" else [1, 0] + list(range(NT - 1, 1, -1))
                        MT = MTf if dirn == "f" else MTb
                        QD = QDf if dirn == "f" else QDb
                        kdc = KD[:, h, di:di + 1]
                        cdc = KD[:, h, 2 + di:3 + di]
                        S.op("pool", lambda e: e.memset(Sf[:], 0.0), writes=[Sf_t])
                        S.op("pool", lambda e: e.memset(Sb_[:], 0.0), writes=[Sb_t])

                        def s3_load(idx):
                            t = order[idx]
                            S.dma(hb[idx % 3][:], hT_d[t], reads=[hT_t[t]], writes=[hb_t[idx % 3]])
                            if dirn == "f":
                                S.dma(tb[idx % 3][:, 0, :], rc_d[t * 128:(t + 1) * 128, :], writes=[tb_t[idx % 3]])
                                S.dma(tb[idx % 3][:, 1, :], rs_d[t * 128:(t + 1) * 128, :], writes=[tb_t[idx % 3]])
                            if dirn == "b" and t >= 2:
                                S.dma(ofb[idx % 3][:], of_d[t - 2], reads=[of_t[t - 2]], writes=[ofb_t[idx % 3]])

                        def s3_proj(idx):
                            if dirn == "b":
                                return
                            h_, h_t = hb[idx % 3], hb_t[idx % 3]
                            pA, pA_t = pAl[idx % 2], pAl_t[idx % 2]
                            pB, pB_t = pBl[idx % 2], pBl_t[idx % 2]
                            for kc in range(8):
                                S.op("pe", lambda e: e.matmul(out=pA[:], lhsT=h_[:, kc, :], rhs=Wqk[:, kc, :], start=(kc == 0),
                                                              stop=(kc == 7)), reads=[h_t, Wqk_t], writes=[pA_t], signal=(kc == 7))
                            for kc in range(8):
                                S.op("pe", lambda e: e.matmul(out=pB[:], lhsT=h_[:, kc, :], rhs=Wv[:, kc, :], start=(kc == 0),
                                                              stop=(kc == 7)), reads=[h_t, Wv_t], writes=[pB_t], signal=(kc == 7))

                        def s3_A(idx):
                            t = order[idx]
                            tb_, tbt = tb[idx % 3], tb_t[idx % 3]
                            pA, pA_t = pAl[idx % 2], pAl_t[idx % 2]
                            pB, pB_t = pBl[idx % 2], pBl_t[idx % 2]
                            qk, qk_t = qk_all[:, t, :], qka_t[t]
                            Vb, Vb_t = V_all[:, t, :], Va_t[t]
                            Kd, Kd_t = Kdb[idx % 2], Kdb_t[idx % 2]
                            for half in (range(2) if dirn == "f" else ()):
                                o = half * 256
                                S.op("dve", lambda e: e.tensor_tensor(out=t1[:, o:o + 256], in0=pA[:, o:o + 256], in1=tb_[:, 0, :],
                                                                      op=ALU.mult), reads=[pA_t, tbt], writes=[t12_t])
                                for part in range(2):
                                    a = o + part * 128
                                    S.op("dve", lambda e: e.tensor_tensor(out=t2[:, a:a + 64], in0=pA[:, a + 64:a + 128],
                                                                          in1=tb_[:, 1, part * 128:part * 128 + 64], op=ALU.mult),
                                         reads=[pA_t, tbt], writes=[t12_t])
                                    S.op("dve", lambda e: e.tensor_tensor(out=t2[:, a + 64:a + 128], in0=pA[:, a:a + 64],
                                                                          in1=tb_[:, 1, part * 128 + 64:part * 128 + 128], op=ALU.mult),
                                         reads=[pA_t, tbt], writes=[t12_t])
                            if dirn == "f":
                                S.op("pool", lambda e: e.tensor_tensor(out=qk[:], in0=t1[:], in1=t2[:], op=ALU.add),
                                     reads=[t12_t], writes=[qk_t])
                                S.op("act", lambda e: e.copy(out=Vb[:], in_=pB[:]), reads=[pB_t], writes=[Vb_t])
                            S.op("pool", lambda e: e.tensor_scalar(out=Kd[:], in0=qk[:, 256:512], scalar1=kdc, scalar2=None, op0=ALU.mult),
                                 reads=[qk_t, dec_t], writes=[Kd_t])

                        s3_load(0)
                        s3_load(1)
                        s3_proj(0)
                        s3_A(0)
                        for idx in range(NT):
                            if idx + 2 < NT:
                                s3_load(idx + 2)
                            if idx + 1 < NT:
                                s3_proj(idx + 1)
                                s3_A(idx + 1)
                            t = order[idx]
                            lat = t >= 2
                            h_, h_t = hb[idx % 3], hb_t[idx % 3]
                            tb_, tbt = tb[idx % 3], tb_t[idx % 3]
                            pA, pA_t = pAl[idx % 2], pAl_t[idx % 2]
                            pB, pB_t = pBl[idx % 2], pBl_t[idx % 2]
                            qk, qk_t = qk_all[:, t, :], qka_t[t]
                            Vb, Vb_t = V_all[:, t, :], Va_t[t]
                            Kd, Kd_t = Kdb[idx % 2], Kdb_t[idx % 2]
                            if lat and dirn == "b":
                                for kc in range(8):
                                    S.op("pe", lambda e: e.matmul(out=pC[:], lhsT=h_[:, kc, :], rhs=Wg[:, kc, :], start=(kc == 0),
                                                                  stop=(kc == 7)), reads=[h_t, Wg_t], writes=[pC_t], signal=(kc == 7))
                                S.op("act", lambda e: e.activation(out=sg[:], in_=pC[:], func=AF.Silu), reads=[pC_t], writes=[sg_t])
                            if lat:
                                for c in range(4):
                                    S.op("pe", lambda e: e.transpose(out=pD[:, 0, c * 128:(c + 1) * 128], in_=qk[:, c * 128:(c + 1) * 128],
                                                                     identity=identb[:]), reads=[qk_t, cb_t], writes=[pD_t], signal=(c == 3))
                                S.op("act", lambda e: e.copy(out=KTt[:].rearrange("p a b -> p (a b)"), in_=pD[:, 0, 256:512]),
                                     reads=[pD_t], writes=[tr_t])
                                S.op("act", lambda e: e.copy(out=QTt[:].rearrange("p a b -> p (a b)"), in_=pD[:, 0, 0:256]),
                                     reads=[pD_t], writes=[tr_t])
                                for dc in range(2):
                                    S.op("dve", lambda e: e.tensor_tensor(out=QdT[:, dc, :], in0=pD[:, 0, dc * 128:(dc + 1) * 128],
                                                                          in1=QD[:, h, :], op=ALU.mult), reads=[pD_t, dec_t], writes=[tr_t])
                                for dc in range(2):
                                    S.op("pe", lambda e: e.matmul(out=pE[:, 0:128], lhsT=KTt[:, dc, :], rhs=QTt[:, dc, :], start=(dc == 0),
                                                                  stop=(dc == 1)), reads=[tr_t], writes=[pE_t], signal=(dc == 1))
                                S.op("dve", lambda e: e.tensor_tensor(out=STm[:], in0=pE[:, 0:128], in1=MT[:, h, :], op=ALU.mult),
                                     reads=[pE_t, dec_t], writes=[STm_t])
                                S.op("pe", lambda e: e.matmul(out=pF[:], lhsT=STm[:], rhs=Vb[:], start=True, stop=False),
                                     reads=[STm_t, Vb_t], writes=[pF_t], signal=False)
                                for dc in range(2):
                                    S.op("pe", lambda e: e.matmul(out=pF[:], lhsT=QdT[:, dc, :], rhs=Sb_[:, dc, :], start=False,
                                                                  stop=(dc == 1)), reads=[tr_t, Sb_t], writes=[pF_t], signal=(dc == 1))
                            for dc in range(2):
                                S.op("pe", lambda e: e.matmul(out=pH[dc][:], lhsT=Kd[:, dc * 128:(dc + 1) * 128], rhs=Vb[:], start=True,
                                                              stop=True), reads=[Kd_t, Vb_t], writes=[pH_t[dc]])
                            if lat:
                                if dirn == "f":
                                    S.op("act", lambda e: e.copy(out=osb[:], in_=pF[:]), reads=[pF_t], writes=[osb_t])
                                    S.dma(of_d[t - 2], osb[:], reads=[osb_t], writes=[of_t[t - 2]])
                                else:
                                    of_, of_tt = ofb[idx % 3], ofb_t[idx % 3]
                                    S.op("dve", lambda e: e.tensor_tensor(out=osb[:], in0=pF[:], in1=of_[:], op=ALU.add),
                                         reads=[pF_t, of_tt], writes=[osb_t])
                                    S.op("pool", lambda e: e.memset(ssb[:, 0:1], 0.0), writes=[ss_t])
                                    S.op("act", lambda e: e.activation(out=scr[:], in_=osb[:], func=AF.Square, accum_out=ssb[:, 0:1]),
                                         reads=[osb_t, ss_t], writes=[ss_t])
                                    S.op("act", lambda e: e.activation(out=ssb[:, 1:2], in_=ssb[:, 0:1], func=AF.Sqrt, scale=1.0 / 512,
                                                                       bias=EPS), reads=[ss_t], writes=[ss_t])
                                    S.op("dve", lambda e: e.reciprocal(out=ssb[:, 2:3], in_=ssb[:, 1:2]), reads=[ss_t], writes=[ss_t])
                                    S.op("dve", lambda e: e.scalar_tensor_tensor(out=rr[:], in0=osb[:], scalar=ssb[:, 2:3], in1=sg[:],
                                                                                 op0=ALU.mult, op1=ALU.mult),
                                         reads=[osb_t, ss_t, sg_t], writes=[rr_t])
                                    for c in range(4):
                                        S.op("pe", lambda e: e.transpose(out=pD[:, 1, c * 128:(c + 1) * 128], in_=rr[:, c * 128:(c + 1) * 128],
                                                                         identity=identb[:]), reads=[rr_t, cb_t], writes=[pD2_t], signal=(c == 3))
                                    rT_, rT_tt = rTs[idx % 2], rTs_t[idx % 2]
                                    S.op("act", lambda e: e.copy(out=rT_[:].rearrange("p a b -> p (a b)"), in_=pD[:, 1, :]),
                                         reads=[pD2_t], writes=[rT_tt])
                                    tk0 = (t - 2) * 128
                                    S.dma(rT_d[h, :, :, tk0:tk0 + 128].rearrange("c p t -> p c t"), rT_[:], reads=[rT_tt],
                                          writes=[rT_t[(t - 2) // 4]])
                            for dc in range(2):
                                S.op("dve", lambda e: e.scalar_tensor_tensor(out=Sb_[:, dc, :], in0=Sf[:, dc, :], scalar=cdc, in1=pH[dc][:],
                                                                             op0=ALU.mult, op1=ALU.add),
                                     reads=[Sf_t, pH_t[dc], dec_t], writes=[Sb_t])
                                S.op("dve", lambda e: e.scalar_tensor_tensor(out=Sf[:, dc, :], in0=Sf[:, dc, :], scalar=cdc, in1=pH[dc][:],
                                                                             op0=ALU.mult, op1=ALU.add),
                                     reads=[Sf_t, pH_t[dc], dec_t], writes=[Sf_t])
                S.barrier()

            with contextlib.ExitStack() as st:
                Wgr = sb(st, "Wgr", [128, 8, 1024], BF16)
                Wgm = sb(st, "Wgm", [128, 8, 1024], BF16)
                Wbr = sb(st, "Wbr", [128, 16, 1024], BF16)
                Wbm = sb(st, "Wbm", [128, 8, 1024], BF16)
                Wo = sb(st, "Wo", [128, 8, 1024], BF16)
                stg = [sb(st, f"s4stg{i}", [128, 8, 256], F32) for i in range(2)]
                stg_t = [Tk(), Tk()]
                wbrv = wbr_d[0].rearrange("(kc p) n -> p kc n", p=128)
                wbmv = wbm_d[0].rearrange("(kc p) n -> p kc n", p=128)
                wov = wo_d[0].rearrange("(kc p) n -> p kc n", p=128)
                k = 0
                jobs = []
                W4_t = [Tk() for _ in range(4)]
                Wo_t = Tk()
                for cb in range(4):
                    cs = slice(cb * 256, (cb + 1) * 256)
                    jobs.append((Wbr[:, 0:8, cs], wbrv[:, 0:8, cs], W4_t[cb]))
                    jobs.append((Wbr[:, 8:16, cs], wbrv[:, 8:16, cs], W4_t[cb]))
                    jobs.append((Wbm[:, :, cs], wbmv[:, :, cs], W4_t[cb]))
                    jobs.append((Wgr[:, :, cs], winv[:, :, 6848 + cb * 256:6848 + (cb + 1) * 256], W4_t[cb]))
                    jobs.append((Wgm[:, :, cs], winv[:, :, 7872 + cb * 256:7872 + (cb + 1) * 256], W4_t[cb]))
                for cb in range(4):
                    cs = slice(cb * 256, (cb + 1) * 256)
                    jobs.append((Wo[:, :, cs], wov[:, :, cs], Wo_t))
                for (dst, src, wt_) in jobs:
                    load_cast(st, dst, wt_, src, None, stg[k % 2][:], stg_t[k % 2], "pool" if k % 2 else "dve")
                    k += 1
                TK4 = 256
                hb = sb(st, "s4h", [128, 8, TK4], BF16)
                hb_t = Tk()
                rb = sb(st, "s4r", [128, 16, TK4], BF16)
                rb_t = Tk()
                ab = sb(st, "s4a", [128, 8, TK4], BF16)
                ab_t = Tk()
                mTb = sb(st, "s4m", [128, 8, TK4], BF16)
                mTb_t = Tk()
                s3_ = sb(st, "s4s3", [128, TK4], F32)
                s4_ = sb(st, "s4s4", [128, TK4], F32)
                sg_t = Tk()
                m1 = sb(st, "s4m1", [128, TK4], F32)
                m2 = sb(st, "s4m2", [128, TK4], F32)
                m_t = Tk()
                x_ = sb(st, "s4x", [128, D], F32)
                x_t = Tk()
                yt = sb(st, "s4y", [128, D], F32)
                yt_t = Tk()
                o_ = sb(st, "s4xo", [128, D], F32)
                o_t = Tk()
                P = [ps(st, f"s4p{i}", [128, 512]) for i in range(4)]
                P_t = [Tk() for _ in range(4)]
                PY = [ps(st, f"s4py{i}", [128, 512]) for i in range(2)]
                PY_t = [Tk() for _ in range(2)]
                for tbk in range(L // TK4):
                    q0 = tbk * TK4
                    for i in range(TK4 // 128):
                        tl = 2 + tbk * (TK4 // 128) + i
                        S.dma(hb[:, :, i * 128:(i + 1) * 128], hT_d[tl], reads=[hT_t[tl]], writes=[hb_t])
                    for hh in range(4):
                        S.dma(rb[:, hh * 4:(hh + 1) * 4, :], rT_d[hh, :, :, q0:q0 + TK4].rearrange("c p t -> p c t"),
                              reads=[rT_t[q0 // 512]], writes=[rb_t])
                    S.dma(ab[:], att_d[:, :, q0:q0 + TK4].rearrange("h p t -> p h t"), reads=[att_t[q0 // 512]], writes=[ab_t])
                    for fc in range(8):
                        fs = slice(fc * 128, (fc + 1) * 128)
                        for kc in range(16):
                            S.op("pe", lambda e: e.matmul(out=P[0][:, 0:TK4], lhsT=Wbr[:, kc, fs], rhs=rb[:, kc, :], start=(kc == 0), stop=(kc == 15)),
                                 reads=[W4_t[fc // 2], rb_t], writes=[P_t[0]], signal=(kc == 15))
                        for (pi, Wx, src, src_t) in ((1, Wbm, ab, ab_t), (2, Wgr, hb, hb_t), (3, Wgm, hb, hb_t)):
                            for kc in range(8):
                                S.op("pe", lambda e: e.matmul(out=P[pi][:, 0:TK4], lhsT=Wx[:, kc, fs], rhs=src[:, kc, :], start=(kc == 0),
                                                              stop=(kc == 7)), reads=[W4_t[fc // 2], src_t], writes=[P_t[pi]], signal=(kc == 7))
                        S.op("act", lambda e: e.activation(out=s3_[:], in_=P[2][:, 0:TK4], func=AF.Sigmoid), reads=[P_t[2]], writes=[sg_t])
                        S.op("act", lambda e: e.activation(out=s4_[:], in_=P[3][:, 0:TK4], func=AF.Sigmoid), reads=[P_t[3]], writes=[sg_t])
                        S.op("dve", lambda e: e.tensor_tensor(out=m1[:], in0=P[0][:, 0:TK4], in1=s3_[:], op=ALU.mult),
                             reads=[P_t[0], sg_t], writes=[m_t])
                        S.op("dve", lambda e: e.tensor_tensor(out=m2[:], in0=P[1][:, 0:TK4], in1=s4_[:], op=ALU.mult),
                             reads=[P_t[1], sg_t], writes=[m_t])
                        S.op("pool", lambda e: e.tensor_tensor(out=mTb[:, fc, :], in0=m1[:], in1=m2[:], op=ALU.add),
                             reads=[m_t], writes=[mTb_t])
                    for tt in range(TK4 // 128):
                        gt = tbk * (TK4 // 128) + tt
                        S.dma(x_[:], x_d[b, gt * 128:(gt + 1) * 128, :], writes=[x_t])
                        for cb in range(2):
                            for kc in range(8):
                                S.op("pe", lambda e: e.matmul(out=PY[cb][:], lhsT=mTb[:, kc, tt * 128:(tt + 1) * 128],
                                                              rhs=Wo[:, kc, cb * 512:(cb + 1) * 512], start=(kc == 0), stop=(kc == 7)),
                                     reads=[mTb_t, Wo_t], writes=[PY_t[cb]], signal=(kc == 7))
                            S.op("dve", lambda e: e.tensor_tensor(out=yt[:, cb * 512:(cb + 1) * 512], in0=PY[cb][:],
                                                                  in1=G1[:, b, cb * 512:(cb + 1) * 512], op=ALU.mult),
                                 reads=[PY_t[cb], G_t], writes=[yt_t])
                        S.op("pool", lambda e: e.tensor_tensor(out=o_[:], in0=yt[:], in1=x_[:], op=ALU.add),
                             reads=[yt_t, x_t], writes=[o_t])
                        S.dma(x1_d[b * 32 + gt], o_[:], reads=[o_t], writes=[x1_t[b * 32 + gt]])
                S.barrier()

        mix_stack.close()
        I32 = mybir.dt.int32
        NTI = NB * 32
        with contextlib.ExitStack() as st:
            posk = sb(st, "posk", [128, NTI * 4], F32)
            ekk = sb(st, "ekk", [128, NTI * 4], F32)
            g4 = sb(st, "g4", [128, NTI * 4], F32)
            pk_t = Tk()
            base = sb(st, "base", [128, 32], F32)
            base_t = Tk()
            desti = sb(st, "desti", [128, NTI * 4], I32)
            desti_t = Tk()
            widx = sb(st, "widx", [128, 8, NBLK], I32)
            bidx = sb(st, "bidx", [2, NBLK], I32)
            widx_t = Tk()
            iota32 = cst[:, 10, 0:32]
            h2d_t = [Tk() for _ in range(NTI)]
            with contextlib.ExitStack() as p1:
                GS2 = sb(p1, "GS2", [128, NB, D], F32)
                SH2 = sb(p1, "SH2", [128, NB, D], F32)
                GS_t = Tk()
                pB2 = [ps(p1, f"p1B{i}", [128, 512]) for i in range(2)]
                pB2_t = [Tk(), Tk()]
                pR = ps(p1, "p1R", [128, 512])
                pR_t = Tk()
                dg = sb(p1, "p1dg", [128, 8, 128], F32)
                dg_t = Tk()
                for bb in range(NB):
                    for (dst, vfn) in ((GS2, lambda c: gs2[:, bb, c:c + 1]), (SH2, lambda c: mT[:, 24 + c, bb:bb + 1])):
                        for c in range(8):
                            S.op("dve", lambda e: e.tensor_scalar(out=dg[:, c, :], in0=identf, scalar1=vfn(c), scalar2=None,
                                                                  op0=ALU.mult), reads=[cst_t, mT_t, gs_t], writes=[dg_t])
                        for c in range(8):
                            S.op("pe", lambda e: e.matmul(out=pB2[c // 4][:, (c % 4) * 128:(c % 4 + 1) * 128], lhsT=onesf,
                                                          rhs=dg[:, c, :], start=True, stop=True),
                                 reads=[dg_t, cst_t], writes=[pB2_t[c // 4]])
                        for hh in range(2):
                            S.op("act", lambda e: e.copy(out=dst[:, bb, hh * 512:(hh + 1) * 512], in_=pB2[hh][:]),
                                 reads=[pB2_t[hh]], writes=[GS_t])
                bfull = sb(p1, "p1bfull", [32, 3072], F32)
                bfb = sb(p1, "p1bfb", [32, 3072], BF16)
                bf_t = Tk()
                S.dma(bfull[:, 0:2048], ebgu_d[0], writes=[bf_t])
                S.dma(bfull[:, 2048:3072], ebd_d[0], writes=[bf_t])
                bfb_t = Tk()
                S.op("dve", lambda e: e.tensor_copy(out=bfb[:], in_=bfull[:]), reads=[bf_t], writes=[bfb_t])
                S.dma(bias_d[:, :], bfb[:], reads=[bfb_t])
                Wr = sb(p1, "Wr", [128, 8, 32], F32)
                Wr_t = Tk()
                S.dma(Wr[:], rw_d[0].rearrange("(kc p) n -> p kc n", p=128), writes=[Wr_t])
                rbs = sb(p1, "rbs", [1, 32], F32)
                rbs_t = Tk()
                S.dma(rbs[:], rb_d[:, :], writes=[rbs_t])
                xt = [sb(p1, f"p1x{i}", [128, D], F32) for i in range(2)]
                xt_t = [Tk(), Tk()]
                xn = sb(p1, "p1xn", [128, D], F32)
                xn_t = Tk()
                h2 = sb(p1, "p1h2", [128, D], F32)
                h2_t = Tk()
                h2bf = [sb(p1, f"p1h2b{i}", [128, D], BF16) for i in range(2)]
                h2bf_t = [Tk(), Tk()]
                h2f = sb(p1, "p1h2f", [128, 8, 128], F32)
                h2f_t = Tk()
                scr = sb(p1, "p1scr", [128, D], BF16)
                ssb = sb(p1, "p1ss", [128, 4], F32)
                tmp_t = Tk()
                lgt = sb(p1, "p1lg", [128, 32], F32)
                mk = sb(p1, "p1mk", [128, 32], F32)
                pos = sb(p1, "p1pos", [128, 32], F32)
                s32 = sb(p1, "p1s32", [128, 32], F32)
                m8 = sb(p1, "p1m8", [128, 8], F32)
                e4 = sb(p1, "p1e4", [128, 4], F32)
                sm = sb(p1, "p1sm", [128, 4], F32)
                rt_t = Tk()
                S.op("pool", lambda e: e.memset(base[:], 0.0), writes=[base_t])
                S.dma(xt[0][:], x1_d[0], reads=[x1_t[0]], writes=[xt_t[0]])
                for ti in range(NTI):
                    bb = ti // 32
                    if ti + 1 < NTI:
                        S.dma(xt[(ti + 1) % 2][:], x1_d[ti + 1], reads=[x1_t[ti + 1]], writes=[xt_t[(ti + 1) % 2]])
                    x_, x_t = xt[ti % 2], xt_t[ti % 2]
                    S.op("pool", lambda e: e.memset(ssb[:, 0:1], 0.0), writes=[tmp_t])
                    S.op("act", lambda e: e.activation(out=scr[:], in_=x_[:], func=AF.Square, accum_out=ssb[:, 0:1]),
                         reads=[x_t, tmp_t], writes=[tmp_t])
                    S.op("act", lambda e: e.activation(out=ssb[:, 1:2], in_=ssb[:, 0:1], func=AF.Sqrt, scale=1.0 / D, bias=EPS),
                         reads=[tmp_t], writes=[tmp_t])
                    S.op("dve", lambda e: e.reciprocal(out=ssb[:, 2:3], in_=ssb[:, 1:2]), reads=[tmp_t], writes=[tmp_t])
                    S.op("dve", lambda e: e.tensor_scalar(out=xn[:], in0=x_[:], scalar1=ssb[:, 2:3], scalar2=None, op0=ALU.mult),
                         reads=[x_t, tmp_t], writes=[xn_t])
                    S.op("dve", lambda e: e.tensor_tensor(out=xn[:], in0=xn[:], in1=GS2[:, bb, :], op=ALU.mult),
                         reads=[xn_t, GS_t], writes=[xn_t])
                    S.op("pool", lambda e: e.tensor_tensor(out=h2[:], in0=xn[:], in1=SH2[:, bb, :], op=ALU.add),
                         reads=[xn_t, GS_t], writes=[h2_t])
                    hb_, hb_t = h2bf[ti % 2], h2bf_t[ti % 2]
                    S.op("act", lambda e: e.copy(out=hb_[:], in_=h2[:]), reads=[h2_t], writes=[hb_t])
                    S.dma(h2_d[ti], hb_[:], reads=[hb_t], writes=[h2d_t[ti]])
                    for c in range(8):
                        S.op("pe", lambda e: e.transpose(out=pB2[c // 4][:, (c % 4) * 128:(c % 4 + 1) * 128],
                                                         in_=h2[:, c * 128:(c + 1) * 128], identity=identf),
                             reads=[h2_t, cst_t], writes=[pB2_t[c // 4]], signal=(c % 4 == 3))
                    S.op("dve", lambda e: e.tensor_copy(out=h2f[:, 0:4, :].rearrange("p a b -> p (a b)"), in_=pB2[0][:]),
                         reads=[pB2_t[0]], writes=[h2f_t])
                    S.op("act", lambda e: e.copy(out=h2f[:, 4:8, :].rearrange("p a b -> p (a b)"), in_=pB2[1][:]),
                         reads=[pB2_t[1]], writes=[h2f_t])
                    for kc in range(8):
                        S.op("pe", lambda e: e.matmul(out=pR[:, 0:32], lhsT=h2f[:, kc, :], rhs=Wr[:, kc, :], start=(kc == 0), stop=False),
                             reads=[h2f_t, Wr_t], writes=[pR_t], signal=False)
                    S.op("pe", lambda e: e.matmul(out=pR[:, 0:32], lhsT=cst[0:1, 1, :], rhs=rbs[:], start=False, stop=True),
                         reads=[cst_t, rbs_t], writes=[pR_t])
                    S.op("dve", lambda e: e.tensor_copy(out=lgt[:], in_=pR[:, 0:32]), reads=[pR_t], writes=[rt_t])
                    S.op("dve", lambda e: e.max(out=m8[:], in_=lgt[:]), reads=[rt_t], writes=[rt_t])
                    S.op("dve", lambda e: e.tensor_scalar(out=mk[:], in0=lgt[:], scalar1=m8[:, 3:4], scalar2=None, op0=ALU.is_ge),
                         reads=[rt_t], writes=[rt_t])
                    S.op("dve", lambda e: e.tensor_scalar(out=sm[:, 0:1], in0=m8[:, 0:1], scalar1=-1.0, scalar2=None, op0=ALU.mult),
                         reads=[rt_t], writes=[rt_t])
                    S.op("act", lambda e: e.activation(out=e4[:], in_=m8[:, 0:4], func=AF.Exp, bias=sm[:, 0:1], scale=1.0),
                         reads=[rt_t], writes=[rt_t])
                    S.op("dve", lambda e: e.reduce_sum(out=sm[:, 1:2], in_=e4[:], axis=mybir.AxisListType.X), reads=[rt_t], writes=[rt_t])
                    S.op("dve", lambda e: e.reciprocal(out=sm[:, 2:3], in_=sm[:, 1:2]), reads=[rt_t], writes=[rt_t])
                    S.op("dve", lambda e: e.tensor_scalar(out=g4[:, ti * 4:ti * 4 + 4], in0=e4[:], scalar1=sm[:, 2:3], scalar2=None, op0=ALU.mult),
                         reads=[rt_t], writes=[pk_t])
                    S.op("pe", lambda e: e.matmul(out=pR[:, 32:64], lhsT=cst[:, 9, :], rhs=mk[:], start=True, stop=True),
                         reads=[rt_t, cst_t], writes=[pR_t])
                    S.op("pe", lambda e: e.matmul(out=pR[:, 64:96], lhsT=onesf, rhs=mk[:], start=True, stop=True),
                         reads=[rt_t, cst_t], writes=[pR_t])
                    S.op("dve", lambda e: e.tensor_tensor(out=pos[:], in0=pR[:, 32:64], in1=base[:], op=ALU.add),
                         reads=[pR_t, base_t], writes=[rt_t])
                    for k in range(4):
                        S.op("dve", lambda e: e.scalar_tensor_tensor(out=s32[:], in0=lgt[:], scalar=m8[:, k:k + 1], in1=pos[:],
                                                                     op0=ALU.is_equal, op1=ALU.mult), reads=[rt_t], writes=[rt_t])
                        S.op("dve", lambda e: e.reduce_sum(out=posk[:, ti * 4 + k:ti * 4 + k + 1], in_=s32[:], axis=mybir.AxisListType.X),
                             reads=[rt_t], writes=[pk_t])
                        S.op("dve", lambda e: e.scalar_tensor_tensor(out=s32[:], in0=lgt[:], scalar=m8[:, k:k + 1], in1=iota32,
                                                                     op0=ALU.is_equal, op1=ALU.mult), reads=[rt_t, cst_t, pk_t], writes=[rt_t])
                        S.op("dve", lambda e: e.reduce_sum(out=ekk[:, ti * 4 + k:ti * 4 + k + 1], in_=s32[:], axis=mybir.AxisListType.X),
                             reads=[rt_t], writes=[pk_t])
                    S.op("dve", lambda e: e.tensor_tensor(out=base[:], in0=pR[:, 64:96], in1=base[:], op=ALU.add),
                         reads=[pR_t, base_t, rt_t], writes=[base_t])
                ci = sb(p1, "p2ci", [128, 32], I32)
                padded = sb(p1, "p2pad", [128, 32], F32)
                pst = sb(p1, "p2pst", [128, 32], F32)
                pend = sb(p1, "p2pend", [128, 32], F32)
                bst = sb(p1, "p2bst", [128, NBLK], F32)
                be = sb(p1, "p2be", [128, NBLK], F32)
                wf = sb(p1, "p2wf", [128, 8, NBLK], F32)
                df = sb(p1, "p2df", [128, NTI * 4], F32)
                p2_t = Tk()
                S.op("dve", lambda e: e.tensor_copy(out=ci[:], in_=base[:]), reads=[base_t], writes=[p2_t])
                S.op("dve", lambda e: e.tensor_scalar(out=ci[:], in0=ci[:], scalar1=511, scalar2=None, op0=ALU.add), reads=[p2_t], writes=[p2_t])
                S.op("dve", lambda e: e.tensor_scalar(out=ci[:], in0=ci[:], scalar1=-512, scalar2=None, op0=ALU.bitwise_and), reads=[p2_t], writes=[p2_t])
                S.op("dve", lambda e: e.tensor_copy(out=padded[:], in_=ci[:]), reads=[p2_t], writes=[p2_t])
                S.op("dve", lambda e: e.memset(pst[:], 0.0), reads=[p2_t], writes=[p2_t])
                for ee in range(1, 32):
                    S.op("dve", lambda e: e.tensor_tensor(out=pst[:, ee:ee + 1], in0=pst[:, ee - 1:ee], in1=padded[:, ee - 1:ee], op=ALU.add),
                         reads=[p2_t], writes=[p2_t])
                S.op("dve", lambda e: e.tensor_tensor(out=pend[:], in0=pst[:], in1=padded[:], op=ALU.add), reads=[p2_t], writes=[p2_t])
                S.op("dve", lambda e: e.tensor_scalar(out=bst[:], in0=cst[:, 10, 0:NBLK], scalar1=512.0, scalar2=None, op0=ALU.mult),
                     reads=[cst_t, p2_t], writes=[p2_t])
                S.op("dve", lambda e: e.memset(be[:], 0.0), reads=[p2_t], writes=[p2_t])
                for ee in range(32):
                    S.op("dve", lambda e: e.scalar_tensor_tensor(out=be[:], in0=bst[:], scalar=pend[:, ee:ee + 1], in1=be[:],
                                                                 op0=ALU.is_ge, op1=ALU.add), reads=[p2_t], writes=[p2_t])
                S.op("dve", lambda e: e.tensor_scalar(out=be[:], in0=be[:], scalar1=31.0, scalar2=None, op0=ALU.min), reads=[p2_t], writes=[p2_t])
                for kc in range(8):
                    S.op("dve", lambda e: e.tensor_scalar(out=wf[:, kc, :], in0=be[:], scalar1=1024.0, scalar2=cst[:, 11, kc:kc + 1],
                                                          op0=ALU.mult, op1=ALU.add), reads=[p2_t, cst_t], writes=[p2_t])
                S.op("dve", lambda e: e.tensor_copy(out=widx[:], in_=wf[:]), reads=[p2_t], writes=[widx_t])
                S.op("dve", lambda e: e.tensor_copy(out=bidx[:], in_=be[0:2, :]), reads=[p2_t], writes=[widx_t])
                for c in range(NTI * 4):
                    S.op("dve", lambda e: e.scalar_tensor_tensor(out=s32[:], in0=iota32, scalar=ekk[:, c:c + 1], in1=pst[:],
                                                                 op0=ALU.is_equal, op1=ALU.mult), reads=[p2_t, pk_t, cst_t, rt_t], writes=[rt_t])
                    S.op("dve", lambda e: e.reduce_sum(out=df[:, c:c + 1], in_=s32[:], axis=mybir.AxisListType.X), reads=[rt_t], writes=[p2_t])
                S.op("dve", lambda e: e.tensor_tensor(out=df[:], in0=df[:], in1=posk[:], op=ALU.add), reads=[p2_t, pk_t], writes=[p2_t])
                S.op("dve", lambda e: e.tensor_copy(out=desti[:], in_=df[:]), reads=[p2_t], writes=[desti_t])
                for ti in range(NTI):
                    hb_, hb_t = h2bf[ti % 2], h2bf_t[ti % 2]
                    S.dma(hb_[:], h2_d[ti], reads=[h2d_t[ti]], writes=[hb_t])
                    for k in range(4):
                        cidx = ti * 4 + k
                        S.dma_ind(lambda e: e.indirect_dma_start(
                            out=xs_d[:, :], out_offset=bass.IndirectOffsetOnAxis(ap=desti[:, cidx:cidx + 1], axis=0),
                            in_=hb_[:], in_offset=None),
                            reads=[hb_t, desti_t])
                S.barrier()

            with contextlib.ExitStack() as p4:
                egu2 = egu_d[0].rearrange("e k n -> (e k) n")
                edn2 = edn_d[0].rearrange("e k n -> (e k) n")
                Wgu = [sb(p4, f"Wgu{i}", [128, 8, 1024], BF16) for i in range(3)]
                Wgu_t = [Tk(), Tk(), Tk()]
                Wd = [sb(p4, f"Wd{i}", [128, 4, 1024], BF16) for i in range(2)]
                Wd_t = [Tk(), Tk()]
                stg = [sb(p4, f"p4stg{i}", [128, 2048], F32) for i in range(6)]
                stg_t = [Tk() for _ in range(6)]
                browb = [sb(p4, f"browb{i}", [2, 3072], BF16) for i in range(2)]
                browb_t = [Tk(), Tk()]
                xs = [sb(p4, f"p4xs{i}", [128, D], BF16) for i in range(4)]
                xs_t = [Tk() for _ in range(4)]
                xsT = sb(p4, "p4xsT", [128, 8, 512], BF16)
                xsT_t = Tk()
                aT = [sb(p4, f"aT{i}", [128, 4, 512], BF16) for i in range(2)]
                aT_t = [Tk(), Tk()]
                g1 = sb(p4, "p4g1", [128, 512], F32)
                u1 = sb(p4, "p4u1", [128, 512], F32)
                glu = sb(p4, "p4gl", [128, 512], F32)
                g1_t, u1_t, glu_t = Tk(), Tk(), Tk()
                ysb = sb(p4, "p4ys", [128, 4, D], F32)
                ysb_t = [[Tk(), Tk()] for _ in range(4)]
                pG = [ps(p4, f"p4G{i}", [128, 512]) for i in range(2)]
                pG_t = [Tk(), Tk()]
                pU = [ps(p4, f"p4U{i}", [128, 512]) for i in range(2)]
                pU_t = [Tk(), Tk()]
                pY = [ps(p4, f"p4Y{i}", [128, 512]) for i in range(2)]
                pY_t = [Tk(), Tk()]
                pX = [ps(p4, f"p4X{i}", [128, 2, 512], BF16) for i in range(2)]
                pX_t = [Tk(), Tk()]
                ISC = 1.0 / 1.702
                cnt = dict(sk=0, gk=0, yk=0)

                def w_load_gu_block(blk):
                    for kc in range(8):
                        si = cnt["sk"] % 6
                        cnt["sk"] += 1
                        S.dma_ind(lambda e: e.indirect_dma_start(
                            out=stg[si][:, :], out_offset=None, in_=egu2[:, :],
                            in_offset=bass.IndirectOffsetOnAxis(ap=widx[:, kc, blk:blk + 1], axis=0)),
                            reads=[widx_t], writes=[stg_t[si]])
                        for hf in range(2):
                            wi3 = (2 * blk + hf) % 3
                            S.op("act", lambda e: e.copy(out=Wgu[wi3][:, kc, :].rearrange("p (g n) -> p g n", g=2),
                                                         in_=stg[si][:].rearrange("p (g h n) -> p g h n", g=2, h=2)[:, :, hf, :]),
                                 reads=[stg_t[si]], writes=[Wgu_t[wi3]])

                def w_load_d(blk, hf, wi):
                    wd, wd_t = Wd[wi], Wd_t[wi]
                    for jq in range(2):
                        si = cnt["sk"] % 6
                        cnt["sk"] += 1
                        for i in range(2):
                            kc = hf * 4 + jq * 2 + i
                            S.dma_ind(lambda e: e.indirect_dma_start(
                                out=stg[si][:, i * 1024:(i + 1) * 1024], out_offset=None, in_=edn2[:, :],
                                in_offset=bass.IndirectOffsetOnAxis(ap=widx[:, kc, blk:blk + 1], axis=0)),
                                reads=[widx_t], writes=[stg_t[si]])
                        S.op("act", lambda e: e.mul(out=wd[:, jq * 2:(jq + 1) * 2, :], in_=stg[si][:].rearrange("p (a b) -> p a b", a=2), mul=ISC),
                             reads=[stg_t[si]], writes=[wd_t])

                def b_load(blk):
                    S.dma_ind(lambda e: e.indirect_dma_start(
                        out=browb[blk % 2][0:2, :], out_offset=None, in_=bias_d[:, :],
                        in_offset=bass.IndirectOffsetOnAxis(ap=bidx[0:2, blk:blk + 1], axis=0)),
                        reads=[widx_t], writes=[browb_t[blk % 2]])

                def x_dma(blk):
                    for tt in range(4):
                        r0 = blk * 512 + tt * 128
                        S.dma(xs[tt][:], xs_d[r0:r0 + 128, :], writes=[xs_t[tt]])

                def x_tr(blk):
                    for tt in range(4):
                        for kc in range(8):
                            S.op("pe", lambda e: e.transpose(out=pX[tt % 2][:, kc // 4, (kc % 4) * 128:(kc % 4 + 1) * 128],
                                                             in_=xs[tt][:, kc * 128:(kc + 1) * 128], identity=identb[:]),
                                 reads=[xs_t[tt], cb_t], writes=[pX_t[tt % 2]], signal=(kc == 7))
                        S.op("dve", lambda e: e.tensor_copy(out=xsT[:, 0:4, tt * 128:(tt + 1) * 128],
                                                            in_=pX[tt % 2][:, 0, :].rearrange("p (a b) -> p a b", a=4)),
                             reads=[pX_t[tt % 2]], writes=[xsT_t])
                        S.op("dve", lambda e: e.tensor_copy(out=xsT[:, 4:8, tt * 128:(tt + 1) * 128],
                                                            in_=pX[tt % 2][:, 1, :].rearrange("p (a b) -> p a b", a=4)),
                             reads=[pX_t[tt % 2]], writes=[xsT_t])

                def gu_unit(blk, hf, wi, au):
                    wg, wg_t = Wgu[(2 * blk + hf) % 3], Wgu_t[(2 * blk + hf) % 3]
                    bb_, bb_t = browb[blk % 2], browb_t[blk % 2]
                    a_, a_t = aT[au], aT_t[au]
                    for j in range(4):
                        i2 = cnt["gk"] % 2
                        cnt["gk"] += 1
                        fcol = hf * 512 + j * 128
                        for kc in range(8):
                            S.op("pe", lambda e: e.matmul(out=pG[i2][:], lhsT=wg[:, kc, j * 128:(j + 1) * 128], rhs=xsT[:, kc, :],
                                                          start=(kc == 0), stop=False), reads=[wg_t, xsT_t], writes=[pG_t[i2]], signal=False)
                        S.op("pe", lambda e: e.matmul(out=pG[i2][:], lhsT=bb_[0:1, fcol:fcol + 128], rhs=cbones[0:1, :], start=False, stop=True),
                             reads=[bb_t, cb_t], writes=[pG_t[i2]])
                        for kc in range(8):
                            S.op("pe", lambda e: e.matmul(out=pU[i2][:], lhsT=wg[:, kc, 512 + j * 128:512 + (j + 1) * 128], rhs=xsT[:, kc, :],
                                                          start=(kc == 0), stop=False), reads=[wg_t, xsT_t], writes=[pU_t[i2]], signal=False)
                        S.op("pe", lambda e: e.matmul(out=pU[i2][:], lhsT=bb_[0:1, 1024 + fcol:1024 + fcol + 128], rhs=cbones[0:1, :],
                                                      start=False, stop=True), reads=[bb_t, cb_t], writes=[pU_t[i2]])
                        S.op("dve", lambda e: e.tensor_scalar(out=g1[:], in0=pG[i2][:], scalar1=7.0, scalar2=None, op0=ALU.min),
                             reads=[pG_t[i2]], writes=[g1_t])
                        S.op("act", lambda e: e.activation(out=glu[:], in_=g1[:], func=AF.Silu, scale=1.702), reads=[g1_t], writes=[glu_t])
                        S.op("dve", lambda e: e.tensor_scalar(out=u1[:], in0=pU[i2][:], scalar1=1.0, scalar2=8.0, op0=ALU.add, op1=ALU.min),
                             reads=[pU_t[i2]], writes=[u1_t])
                        S.op("dve", lambda e: e.scalar_tensor_tensor(out=a_[:, j, :], in0=u1[:], scalar=-6.0, in1=glu[:],
                                                                     op0=ALU.max, op1=ALU.mult), reads=[u1_t, glu_t], writes=[a_t])

                def dn_unit(blk, hf, wi, au):
                    wd, wd_t = Wd[wi], Wd_t[wi]
                    bb_, bb_t = browb[blk % 2], browb_t[blk % 2]
                    a_, a_t = aT[au], aT_t[au]
                    for tt in range(4):
                        for cb in range(2):
                            yi = cnt["yk"] % 2
                            cnt["yk"] += 1
                            for j in range(4):
                                S.op("pe", lambda e: e.matmul(out=pY[yi][:], lhsT=a_[:, j, tt * 128:(tt + 1) * 128],
                                                              rhs=wd[:, j, cb * 512:(cb + 1) * 512], start=(j == 0), stop=(j == 3 and hf == 1)),
                                     reads=[a_t, wd_t], writes=[pY_t[yi]], signal=(j == 3 and hf == 1))
                            yv = ysb[:, tt, cb * 512:(cb + 1) * 512]
                            if hf == 0:
                                S.op("pe", lambda e: e.matmul(out=pY[yi][:], lhsT=cbones[0:1, 0:128], rhs=bb_[0:1, 2048 + cb * 512:2048 + (cb + 1) * 512],
                                                              start=False, stop=True), reads=[bb_t, cb_t], writes=[pY_t[yi]])
                                S.op("dve", lambda e: e.tensor_copy(out=yv, in_=pY[yi][:]), reads=[pY_t[yi]], writes=[ysb_t[tt][cb]])
                            else:
                                S.op("dve", lambda e: e.tensor_tensor(out=yv, in0=pY[yi][:], in1=yv, op=ALU.add),
                                     reads=[pY_t[yi], ysb_t[tt][cb]], writes=[ysb_t[tt][cb]])
                    if hf == 1:
                        S.dma(ys_d[blk * 512:(blk + 1) * 512, :].rearrange("(t p) c -> p t c", p=128), ysb[:],
                              reads=[ysb_t[tt][cb] for tt in range(4) for cb in range(2)])

                cbones = sb(p4, "cbones", [1, 512], BF16)
                S.op("dve", lambda e: e.memset(cbones[:], 1.0), reads=[cb_t], writes=[cb_t])
                units = [(blk, hf) for blk in range(NBLK) for hf in range(2)]
                NU = len(units)
                b_load(0)
                x_dma(0)
                w_load_gu_block(0)
                w_load_d(0, 0, 0)
                w_load_d(0, 1, 1)
                x_tr(0)
                gu_unit(0, 0, 0, 0)
                w_load_gu_block(1)
                b_load(1)
                x_dma(1)
                for ui, (blk, hf) in enumerate(units):
                    if ui + 1 < NU:
                        nb_, nh_ = units[ui + 1]
                        if nh_ == 0:
                            x_tr(nb_)
                        gu_unit(nb_, nh_, (ui + 1) % 2, (ui + 1) % 2)
                        if nh_ == 0 and nb_ + 1 < NBLK:
                            w_load_gu_block(nb_ + 1)
                        if nh_ == 0 and nb_ + 1 < NBLK:
                            b_load(nb_ + 1)
                            x_dma(nb_ + 1)
                    dn_unit(blk, hf, ui % 2, ui % 2)
                    if ui + 2 < NU:
                        b2, h2_ = units[ui + 2]
                        w_load_d(b2, h2_, (ui + 2) % 2)
                S.barrier()

            with contextlib.ExitStack() as p5:
                yk8 = [sb(p5, f"p5y{i}", [128, D], F32) for i in range(8)]
                yk8_t = [Tk() for _ in range(8)]
                accm = sb(p5, "p5acc", [128, D], F32)
                acc_t = Tk()
                x_ = sb(p5, "p5x", [128, D], F32)
                x_t = Tk()
                scr = sb(p5, "p5scr", [128, D], BF16)
                ssb = sb(p5, "p5ss", [128, 4], F32)
                tmp_t = Tk()
                yo = sb(p5, "p5yo", [128, D], F32)
                yo_t = Tk()
                for ti in range(NTI):
                    bb = ti // 32
                    yk = yk8[(ti % 2) * 4:(ti % 2) * 4 + 4]
                    yk_t = yk8_t[(ti % 2) * 4:(ti % 2) * 4 + 4]
                    S.dma(x_[:], x1_d[ti], reads=[x1_t[ti]], writes=[x_t])
                    for k in range(4):
                        cidx = ti * 4 + k
                        S.dma_ind(lambda e: e.indirect_dma_start(
                            out=yk[k][:], out_offset=None, in_=ys_d[:, :],
                            in_offset=bass.IndirectOffsetOnAxis(ap=desti[:, cidx:cidx + 1], axis=0)),
                            reads=[desti_t], writes=[yk_t[k]])
                    S.op("dve", lambda e: e.tensor_scalar(out=accm[:], in0=yk[0][:], scalar1=g4[:, ti * 4:ti * 4 + 1], scalar2=None, op0=ALU.mult),
                         reads=[yk_t[0], pk_t], writes=[acc_t])
                    for k in range(1, 4):
                        S.op("dve", lambda e: e.scalar_tensor_tensor(out=accm[:], in0=yk[k][:], scalar=g4[:, ti * 4 + k:ti * 4 + k + 1], in1=accm[:],
                                                                     op0=ALU.mult, op1=ALU.add), reads=[yk_t[k], pk_t, acc_t], writes=[acc_t])
                    S.op("dve", lambda e: e.tensor_tensor(out=accm[:], in0=accm[:], in1=G2[:, bb, :], op=ALU.mult),
                         reads=[acc_t, G_t], writes=[acc_t])
                    S.op("dve", lambda e: e.tensor_tensor(out=accm[:], in0=accm[:], in1=x_[:], op=ALU.add), reads=[acc_t, x_t], writes=[acc_t])
                    S.op("dve", lambda e: e.memset(ssb[:, 0:1], 0.0), writes=[tmp_t])
                    S.op("act", lambda e: e.activation(out=scr[:], in_=accm[:], func=AF.Square, accum_out=ssb[:, 0:1]),
                         reads=[acc_t, tmp_t], writes=[tmp_t])
                    S.op("act", lambda e: e.activation(out=ssb[:, 1:2], in_=ssb[:, 0:1], func=AF.Sqrt, scale=1.0 / D, bias=EPS),
                         reads=[tmp_t], writes=[tmp_t])
                    S.op("dve", lambda e: e.reciprocal(out=ssb[:, 2:3], in_=ssb[:, 1:2]), reads=[tmp_t], writes=[tmp_t])
                    S.op("dve", lambda e: e.scalar_tensor_tensor(out=yo[:], in0=accm[:], scalar=ssb[:, 2:3], in1=FG[:], op0=ALU.mult, op1=ALU.mult),
                         reads=[acc_t, tmp_t, G_t], writes=[yo_t])
                    r0 = (ti % 32) * 128
                    S.dma(out_d[bb, r0:r0 + 128, :], yo[:], reads=[yo_t])
            S.final_wait()
        print("ops:", S.n_ops, "dmas:", S.dma_n, "sig:", S.cnt)
    return nc


_CACHE = {}


def make_in_maps(inputs, n_cores=8):
    RC, RS, MC, MS = rope_tables()
    cst, _ = const_tables()
    f = lambda a: np.ascontiguousarray(np.asarray(a, dtype=np.float32))
    shared = {k: f(inputs[k]) for k in ("norm1_g", "norm2_g", "ada_w", "ada_b", "w_in", "ret_decay_fwd", "ret_decay_bwd",
                                        "mla_q_norm_g", "mla_w_uq", "mla_kv_norm_g", "mla_w_ukv", "w_branch_ret",
                                        "w_branch_mla", "w_out", "router_w", "router_b", "exp_w_gu", "exp_b_gu",
                                        "exp_w_down", "exp_b_down", "final_norm_g")}
    shared.update(consts=cst, rope_rc=RC, rope_rs=RS, rope_mc=MC, rope_ms=MS)
    x, c, ctx, c_ctx = f(inputs["x"]), f(inputs["c"]), f(inputs["ctx"]), f(inputs["c_ctx"])
    maps = []
    for i in range(n_cores):
        m = dict(shared)
        m["x"] = x[i * NB:(i + 1) * NB]
        m["ctx"] = ctx[i * NB:(i + 1) * NB]
        m["cvec"] = np.ascontiguousarray(np.concatenate([c[i * NB:(i + 1) * NB], c_ctx[None, :]], axis=0))
        maps.append(m)
    return maps


def kernel(**inputs):
    if "nc" not in _CACHE:
        _CACHE["nc"] = build()
    nc = _CACHE["nc"]
    maps = make_in_maps(inputs)
    res = run_bass_kernel_spmd(nc, maps, core_ids=list(range(8)))
    return np.concatenate([r["out"] for r in res.results], axis=0).astype(np.float32)
```
